# Optimizing a Trainium2 kernel written in Bass

```python
import jax, jax.numpy as jnp
from jax import lax
import numpy as np

D_MODEL = 1024
BATCH = 8
SEQ = 4096
DEPTH = 2

N_A_LAYERS = DEPTH // 2
N_B_LAYERS = DEPTH - N_A_LAYERS
PLE_DIM = 256
EPS = 1e-6

ML_HEADS = 8
ML_DV = D_MODEL // ML_HEADS
ML_DQK = ML_DV // 2
ML_CHUNK = 64
GATE_CAP = 15.0
ML_SPLITS = (ML_HEADS * ML_DQK, ML_HEADS * ML_DQK, ML_HEADS * ML_DV, ML_HEADS * ML_DV, ML_HEADS, ML_HEADS)
ML_IN = sum(ML_SPLITS)

MB_HEADS = 16
MB_KV_HEADS = 4
MB_HD = D_MODEL // MB_HEADS
MB_GROUP = MB_HEADS // MB_KV_HEADS
MB_BLOCK = 256
MB_TOPK = 3
MB_QCHUNK = 16
ROPE_THETA = 500000.0
ROPE_DIM = MB_HD // 4

FFN_HIDDEN = -(-8 * D_MODEL // (3 * 256)) * 256

kernel_name = "yoco_mlstm_moba_hybrid"


def rmsnorm(x, g):
    xf = x.astype(jnp.float32)
    y = xf * lax.rsqrt(jnp.mean(xf * xf, axis=-1, keepdims=True) + EPS)
    return (y * g.astype(jnp.float32)).astype(x.dtype)


def rope_partial(x):
    s_len = x.shape[2]
    half = ROPE_DIM // 2
    inv = ROPE_THETA ** (-jnp.arange(0, ROPE_DIM, 2, dtype=jnp.float32) / ROPE_DIM)
    ang = jnp.arange(s_len, dtype=jnp.float32)[:, None] * inv[None, :]
    cos, sin = jnp.cos(ang), jnp.sin(ang)
    xf = x.astype(jnp.float32)
    x1, x2 = xf[..., :half], xf[..., half:ROPE_DIM]
    out = jnp.concatenate([x1 * cos - x2 * sin, x2 * cos + x1 * sin, xf[..., ROPE_DIM:]], axis=-1)
    return out.astype(x.dtype)


def swiglu(h, w_gate_up, w_down):
    g, u = jnp.split(h @ w_gate_up, 2, axis=-1)
    return (jax.nn.silu(g) * u) @ w_down


def mlstm_chunkwise(q, k, v, logi, logf):
    b, nh, s_len, dqk = q.shape
    dv = v.shape[-1]
    L = ML_CHUNK
    nc = s_len // L
    qc = q.reshape(b, nh, nc, L, dqk) * (dqk ** -0.5)
    kc = k.reshape(b, nh, nc, L, dqk)
    vc = v.reshape(b, nh, nc, L, dv)
    li = logi.reshape(b, nh, nc, L)
    g = jnp.cumsum(logf.reshape(b, nh, nc, L), axis=-1)
    g_last = g[..., -1]
    causal = jnp.tril(jnp.ones((L, L), dtype=bool))
    dmat = jnp.where(causal, g[..., :, None] - g[..., None, :] + li[..., None, :], -jnp.inf)
    m_intra = jnp.max(dmat, axis=-1)
    a = g_last[..., None] - g + li
    m_loc = jnp.max(a, axis=-1)
    wloc = jnp.exp(a - m_loc[..., None])
    c_loc = jnp.einsum('bhcsv,bhcsk->bhcvk', vc * wloc[..., None], kc)
    n_loc = jnp.einsum('bhcs,bhcsk->bhck', wloc, kc)

    def step(carry, inp):
        c_st, n_st, m_st = carry
        cl, nl, ml, gl = inp
        m_new = jnp.maximum(gl + m_st, ml)
        sp = jnp.exp(gl + m_st - m_new)
        sl = jnp.exp(ml - m_new)
        c_new = sp[..., None, None] * c_st + sl[..., None, None] * cl
        n_new = sp[..., None] * n_st + sl[..., None] * nl
        return (c_new, n_new, m_new), (c_st, n_st, m_st)

    init = (jnp.zeros((b, nh, dv, dqk), jnp.float32), jnp.zeros((b, nh, dqk), jnp.float32),
            jnp.zeros((b, nh), jnp.float32))
    xs = (jnp.moveaxis(c_loc, 2, 0), jnp.moveaxis(n_loc, 2, 0), jnp.moveaxis(m_loc, 2, 0), jnp.moveaxis(g_last, 2, 0))
    _, (c_prev, n_prev, m_prev) = lax.scan(step, init, xs)
    c_prev = jnp.moveaxis(c_prev, 0, 2)
    n_prev = jnp.moveaxis(n_prev, 0, 2)
    m_prev = jnp.moveaxis(m_prev, 0, 2)

    m_inter = g + m_prev[..., None]
    m_comb = jnp.maximum(m_inter, m_intra)
    w_inter = jnp.exp(m_inter - m_comb)
    s = jnp.einsum('bhcjk,bhcsk->bhcjs', qc, kc) * jnp.exp(dmat - m_comb[..., None])
    num = w_inter[..., None] * jnp.einsum('bhcjk,bhcvk->bhcjv', qc, c_prev) + jnp.einsum('bhcjs,bhcsv->bhcjv', s, vc)
    den = w_inter * jnp.einsum('bhcjk,bhck->bhcj', qc, n_prev) + jnp.sum(s, axis=-1)
    hcell = num / jnp.maximum(jnp.abs(den), jnp.exp(-m_comb))[..., None]
    return hcell.reshape(b, nh, s_len, dv)


def mlstm_mixer(hn, w_in, b_gate, mh_gain, w_out):
    b, s_len, _ = hn.shape
    idx = np.cumsum(ML_SPLITS)[:-1].tolist()
    q, k, v, o, gi, gf = jnp.split(hn @ w_in, idx, axis=-1)
    heads = lambda t, d: t.reshape(b, s_len, ML_HEADS, d).transpose(0, 2, 1, 3).astype(jnp.float32)
    q, k, v = heads(q, ML_DQK), heads(k, ML_DQK), heads(v, ML_DV)
    cap = lambda t: GATE_CAP * jnp.tanh(t / GATE_CAP)
    b_i, b_f = b_gate[:ML_HEADS], b_gate[ML_HEADS:]
    logi = cap(gi.astype(jnp.float32) + b_i).transpose(0, 2, 1)
    logf = jax.nn.log_sigmoid(cap(gf.astype(jnp.float32) + b_f)).transpose(0, 2, 1)
    hcell = mlstm_chunkwise(q, k, v, logi, logf)
    hcell = rmsnorm(hcell.transpose(0, 2, 1, 3), mh_gain).reshape(b, s_len, ML_HEADS * ML_DV)
    return ((jax.nn.sigmoid(o.astype(jnp.float32)) * hcell).astype(hn.dtype)) @ w_out


def shared_kv(h, kv_norm, w_kv, k_norm):
    b, s_len, _ = h.shape
    k, v = jnp.split(rmsnorm(h, kv_norm) @ w_kv, 2, axis=-1)
    k = k.reshape(b, s_len, MB_KV_HEADS, MB_HD).transpose(0, 2, 1, 3)
    v = v.reshape(b, s_len, MB_KV_HEADS, MB_HD).transpose(0, 2, 1, 3)
    k = rope_partial(rmsnorm(k, k_norm))
    nb = -(-s_len // MB_BLOCK)
    pad = ((0, 0), (0, 0), (0, nb * MB_BLOCK - s_len), (0, 0))
    k_blocks = jnp.pad(k, pad).reshape(b, MB_KV_HEADS, nb, MB_BLOCK, MB_HD)
    v_blocks = jnp.pad(v, pad).reshape(b, MB_KV_HEADS, nb, MB_BLOCK, MB_HD)
    k_mean = jnp.mean(k_blocks.astype(jnp.float32), axis=3)
    return k_blocks, v_blocks, k_mean


def moba_mixer(hn, w_q, q_norm, w_o, k_blocks, v_blocks, k_mean):
    b, s_len, _ = hn.shape
    nb = k_blocks.shape[2]
    scale = MB_HD ** -0.5
    q = (hn @ w_q).reshape(b, s_len, MB_HEADS, MB_HD).transpose(0, 2, 1, 3)
    q = rope_partial(rmsnorm(q, q_norm))
    gate = jnp.einsum('bkgsd,bknd->bkgsn', q.reshape(b, MB_KV_HEADS, MB_GROUP, s_len, MB_HD).astype(jnp.float32),
                      k_mean).reshape(b, MB_HEADS, s_len, nb)
    qblk = jnp.arange(s_len) // MB_BLOCK
    past = jnp.arange(nb)[None, :] < qblk[:, None]
    gate = jnp.where(past, gate, -jnp.inf)
    if nb < MB_TOPK:
        gate = jnp.concatenate([gate, jnp.full((b, MB_HEADS, s_len, MB_TOPK - nb), -jnp.inf, gate.dtype)], axis=-1)
    sel = jnp.minimum(lax.top_k(gate, MB_TOPK)[1], nb - 1)
    bi = jnp.arange(b)[:, None, None, None]
    gi = (jnp.arange(MB_HEADS) // MB_GROUP)[None, :, None, None]

    def chunk(c):
        t0 = c * MB_QCHUNK
        qc = lax.dynamic_slice_in_dim(q, t0, MB_QCHUNK, axis=2)
        selc = lax.dynamic_slice_in_dim(sel, t0, MB_QCHUNK, axis=2)
        tpos = t0 + jnp.arange(MB_QCHUNK)
        ob = t0 // MB_BLOCK
        k_sel = k_blocks[bi, gi, selc]
        v_sel = v_blocks[bi, gi, selc]
        s_sel = jnp.einsum('bhqd,bhqjud->bhqju', qc, k_sel).astype(jnp.float32) * scale
        valid = jnp.arange(MB_TOPK)[None, :] < (tpos // MB_BLOCK)[:, None]
        s_sel = jnp.where(valid[None, None, :, :, None], s_sel, -jnp.inf).reshape(b, MB_HEADS, MB_QCHUNK, MB_TOPK * MB_BLOCK)
        k_own = lax.dynamic_index_in_dim(k_blocks, ob, axis=2, keepdims=False)
        v_own = lax.dynamic_index_in_dim(v_blocks, ob, axis=2, keepdims=False)
        qg = qc.reshape(b, MB_KV_HEADS, MB_GROUP, MB_QCHUNK, MB_HD)
        s_own = jnp.einsum('bkgqd,bkud->bkgqu', qg, k_own).astype(jnp.float32).reshape(b, MB_HEADS, MB_QCHUNK, MB_BLOCK) * scale
        kpos = ob * MB_BLOCK + jnp.arange(MB_BLOCK)
        s_own = jnp.where(kpos[None, :] <= tpos[:, None], s_own, -jnp.inf)
        prob = jax.nn.softmax(jnp.concatenate([s_sel, s_own], axis=-1), axis=-1)
        p_sel = prob[..., :MB_TOPK * MB_BLOCK].reshape(b, MB_HEADS, MB_QCHUNK, MB_TOPK, MB_BLOCK).astype(v_sel.dtype)
        p_own = prob[..., MB_TOPK * MB_BLOCK:].reshape(b, MB_KV_HEADS, MB_GROUP, MB_QCHUNK, MB_BLOCK).astype(v_own.dtype)
        o_sel = jnp.einsum('bhqju,bhqjud->bhqd', p_sel, v_sel)
        o_own = jnp.einsum('bkgqu,bkud->bkgqd', p_own, v_own).reshape(b, MB_HEADS, MB_QCHUNK, MB_HD)
        return o_sel + o_own

    out = lax.map(chunk, jnp.arange(s_len // MB_QCHUNK))
    out = jnp.transpose(out, (1, 0, 3, 2, 4)).reshape(b, s_len, MB_HEADS * MB_HD)
    return out @ w_o


def setup_inputs(seed: int = 0) -> dict:
    key = jax.random.key(seed)
    ks = jax.random.split(key, 24)
    f32 = jnp.float32
    nrm = lambda k, shape, fan: jax.random.normal(k, shape, f32) * (fan ** -0.5)
    gain = lambda k, shape: 1.0 + 0.05 * jax.random.normal(k, shape, f32)
    b_gate = jnp.concatenate([
        0.1 * jax.random.normal(ks[4], (N_A_LAYERS, ML_HEADS), f32),
        jnp.linspace(3.0, 6.0, ML_HEADS, dtype=f32)[None, :] + 0.1 * jax.random.normal(ks[5], (N_A_LAYERS, ML_HEADS), f32)], axis=-1)
    return {
        "x": jax.random.normal(ks[0], (BATCH, SEQ, D_MODEL), f32),
        "p": jax.random.normal(ks[1], (DEPTH, BATCH, SEQ, PLE_DIM), f32),
        "norm_mix": gain(ks[2], (DEPTH, D_MODEL)),
        "a_w_in": nrm(ks[3], (N_A_LAYERS, D_MODEL, ML_IN), D_MODEL),
        "a_b_gate": b_gate,
        "a_mh_gain": gain(ks[6], (N_A_LAYERS, ML_DV)),
        "a_w_out": nrm(ks[7], (N_A_LAYERS, ML_HEADS * ML_DV, D_MODEL), ML_HEADS * ML_DV),
        "kv_norm": gain(ks[8], (D_MODEL,)),
        "w_kv": nrm(ks[9], (D_MODEL, 2 * MB_KV_HEADS * MB_HD), D_MODEL),
        "k_norm": gain(ks[10], (MB_HD,)),
        "b_w_q": nrm(ks[11], (N_B_LAYERS, D_MODEL, MB_HEADS * MB_HD), D_MODEL),
        "b_q_norm": gain(ks[12], (N_B_LAYERS, MB_HD)),
        "b_w_o": nrm(ks[13], (N_B_LAYERS, MB_HEADS * MB_HD, D_MODEL), MB_HEADS * MB_HD),
        "norm_ffn": gain(ks[14], (DEPTH, D_MODEL)),
        "w_gate_up": nrm(ks[15], (DEPTH, D_MODEL, 2 * FFN_HIDDEN), D_MODEL),
        "w_down": nrm(ks[16], (DEPTH, FFN_HIDDEN, D_MODEL), FFN_HIDDEN),
        "norm_ple": gain(ks[17], (DEPTH, D_MODEL)),
        "w_ple_gate": nrm(ks[18], (DEPTH, D_MODEL, D_MODEL), D_MODEL),
        "w_ple_up": nrm(ks[19], (DEPTH, PLE_DIM, D_MODEL), PLE_DIM),
    }


def reference(x, p, norm_mix, a_w_in, a_b_gate, a_mh_gain, a_w_out, kv_norm, w_kv, k_norm,
              b_w_q, b_q_norm, b_w_o, norm_ffn, w_gate_up, w_down, norm_ple, w_ple_gate, w_ple_up):
    h = x
    shared = None
    for i in range(DEPTH):
        hn = rmsnorm(h, norm_mix[i])
        if i < N_A_LAYERS:
            h = h + mlstm_mixer(hn, a_w_in[i], a_b_gate[i], a_mh_gain[i], a_w_out[i])
        else:
            if shared is None:
                shared = shared_kv(h, kv_norm, w_kv, k_norm)
            j = i - N_A_LAYERS
            h = h + moba_mixer(hn, b_w_q[j], b_q_norm[j], b_w_o[j], *shared)
        h = h + swiglu(rmsnorm(h, norm_ffn[i]), w_gate_up[i], w_down[i])
        gate = jax.nn.sigmoid(rmsnorm(h, norm_ple[i]) @ w_ple_gate[i])
        h = h + (p[i].astype(h.dtype) @ w_ple_up[i]) * gate
    return h
```

```python
import numpy as np
import concourse.bass as bass
import concourse.mybir as mybir
from concourse.bass_utils import run_bass_kernel_spmd
from contextlib import ExitStack

F32 = mybir.dt.float32
BF16 = mybir.dt.bfloat16
ALU = mybir.AluOpType
AF = mybir.ActivationFunctionType
AX = mybir.AxisListType

ENGS = ["tensor", "vector", "scalar", "gpsimd", "sync"]
NDMASEM = 12
SAME_ENGINE_SYNC = True

NT = 4096
NB = 512
NBLK = NT // NB
EPS = 1e-6
NEG = -30000.0


class Res:
    __slots__ = ("name", "w", "r", "gd")

    def __init__(self, name=""):
        self.name = name
        self.w = []
        self.r = []
        self.gd = []


class Op:
    __slots__ = ("eng", "fn", "waits", "pos", "sig", "isdma", "semi", "semk", "K", "sigidx")


class Sched:
    G = {"nc": None}

    @staticmethod
    def setup(nc):
        if Sched.G.get("nc") is nc:
            return
        es = ExitStack()
        G = {"nc": nc, "es": es, "sig": {e: 0 for e in ENGS}, "dma": {e: 0 for e in ENGS}}
        G["esem"] = {e: es.enter_context(nc.semaphore("sem_e_%s" % e)) for e in ENGS}
        G["dsem"] = {(e, i): es.enter_context(nc.semaphore("sem_d_%s_%d" % (e, i))) for e in ("sync", "gpsimd") for i in range(NDMASEM)}
        Sched.G = G

    def __init__(self, nc):
        Sched.setup(nc)
        self.nc = nc
        self.ops = {e: [] for e in ENGS}
        self.Kcur = {e: {} for e in ENGS}
        self.nops = 0

    limit = None

    def _record(self, eng, fn, reads, writes, isdma, join=False):
        if Sched.limit is not None and self.nops >= Sched.limit:
            return None
        o = Op()
        o.eng = eng
        o.fn = fn
        o.isdma = isdma
        o.sig = False
        o.pos = len(self.ops[eng])
        deps = {}
        for r in reads:
            for x in r.w:
                deps[id(x)] = x
        for w in writes:
            if join and not w.r:
                for x in w.gd:
                    deps[id(x)] = x
            else:
                for x in w.w:
                    deps[id(x)] = x
                for x in w.r:
                    deps[id(x)] = x
        K = self.Kcur[eng]
        newK = None
        waits = []
        if isdma:
            i = Sched.G["dma"][eng]
            Sched.G["dma"][eng] += 1
            o.semi = i % NDMASEM
            o.semk = i // NDMASEM + 1
            if o.semk > 1:
                key = ("d", eng, o.semi)
                if K.get(key, 0) < o.semk - 1:
                    waits.append(("dmaslot", eng, o.semi, o.semk - 1))
                    newK = dict(K)
                    newK[key] = o.semk - 1
        best = {}
        dl = []
        for y in deps.values():
            if y is o:
                continue
            if y.isdma:
                dl.append(y)
            elif y.eng not in best or best[y.eng].pos < y.pos:
                best[y.eng] = y
        for y in dl + list(best.values()):
            cur = K if newK is None else newK
            if y.isdma:
                key = ("d", y.eng, y.semi)
                if cur.get(key, 0) >= y.semk:
                    continue
                waits.append(("dma", y))
            else:
                if y.eng == eng and (eng == "tensor" or not SAME_ENGINE_SYNC) and not isdma:
                    continue
                if cur.get(y.eng, -1) >= y.pos:
                    continue
                y.sig = True
                waits.append(("eng", y))
            if newK is None:
                newK = dict(K)
            for k, v in y.K.items():
                if newK.get(k, -1) < v:
                    newK[k] = v
            if y.isdma:
                key = ("d", y.eng, y.semi)
                newK[key] = max(newK.get(key, 0), y.semk)
            else:
                if newK.get(y.eng, -1) < y.pos:
                    newK[y.eng] = y.pos
        if newK is not None:
            self.Kcur[eng] = newK
            K = newK
        o.K = K
        o.waits = waits
        for r in reads:
            r.r.append(o)
        for w in writes:
            if join and not w.r:
                w.w.append(o)
            else:
                w.gd = w.w + w.r
                w.w = [o]
                w.r = []
        self.ops[eng].append(o)
        self.nops += 1
        return o

    def op(self, eng, fn, reads=(), writes=(), join=False):
        return self._record(eng, fn, reads, writes, False, join)

    def dma(self, queue, out, in_, reads=(), writes=(), join=False, nonc=False, **kw):
        nc = self.nc
        if nonc:
            def f(e):
                with nc.allow_non_contiguous_dma(reason="small strided parameter load"):
                    return e.dma_start(out=out, in_=in_, **kw)
        else:
            def f(e):
                return e.dma_start(out=out, in_=in_, **kw)
        return self._record(queue, f, reads, writes, True, join)

    def emit(self):
        nc = self.nc
        G = Sched.G
        for e in ENGS:
            n = G["sig"][e]
            for o in self.ops[e]:
                if o.sig:
                    n += 1
                o.sigidx = n
            G["sig"][e] = n
        esem, dsem = G["esem"], G["dsem"]
        with nc.Block() as block:
            def stream(ename):
                def body(eng):
                    for o in self.ops[ename]:
                        for w in o.waits:
                            if w[0] == "dmaslot":
                                eng.wait_ge(dsem[(w[1], w[2])], 16 * w[3])
                            elif w[0] == "dma":
                                y = w[1]
                                eng.wait_ge(dsem[(y.eng, y.semi)], 16 * y.semk)
                            else:
                                y = w[1]
                                eng.wait_ge(esem[y.eng], y.sigidx)
                        ins = o.fn(eng)
                        if o.isdma:
                            ins.then_inc(dsem[(o.eng, o.semi)], 16)
                        elif o.sig:
                            ins.then_inc(esem[o.eng], 1)
                return body

            for e in ENGS:
                if self.ops[e]:
                    getattr(block, e)(stream(e))


class Phase:
    def __init__(self, nc, name):
        self.nc = nc
        self.name = name
        self.es = ExitStack()
        self.S = Sched(nc)
        self.n = 0
        self.Rdram = Res("dram_out")

    def sb(self, shape, dt, name=None):
        self.n += 1
        t = self.es.enter_context(self.nc.sbuf_tensor("%s_s%d" % (self.name, self.n), list(shape), dt))
        return t

    def ps(self, shape, dt, name=None):
        self.n += 1
        t = self.es.enter_context(self.nc.psum_tensor("%s_p%d" % (self.name, self.n), list(shape), dt))
        return t

    def finish(self):
        S = self.S
        S.op("sync", lambda e: e.nop(), reads=[self.Rdram])
        S.emit()
        self.es.close()

    def consts(self, C):
        S = self.S
        self.ident_f = self.sb([128, 128], F32)
        self.ident_b = self.sb([128, 128], BF16)
        self.ones_b = self.sb([128, 128], BF16)
        self.eps_t = self.sb([128, 1], F32)
        self.one_t = self.sb([128, 1], F32)
        self.Rc = Res("consts")
        S.dma("sync", self.ident_f[:], C["c_ident"], writes=[self.Rc], join=True)
        S.op("vector", lambda e: e.tensor_copy(out=self.ident_b[:], in_=self.ident_f[:]), reads=[self.Rc], writes=[self.Rc])
        S.op("vector", lambda e: e.memset(self.ones_b[:], 1.0), writes=[self.Rc], join=True)
        S.op("vector", lambda e: e.memset(self.eps_t[:], EPS), writes=[self.Rc], join=True)
        S.op("vector", lambda e: e.memset(self.one_t[:], 1.0), writes=[self.Rc], join=True)

    def load_vec_fm(self, ap1024, nk=8):
        t = self.sb([128, nk], F32)
        r = Res()
        self.S.dma("sync", t[:], ap1024.rearrange("(k p) -> p k", p=128), writes=[r], nonc=True)
        return t, r

    def load_bcast(self, ap_flat, n):
        t = self.sb([128, n], F32)
        r = Res()
        self.S.dma("sync", t[:], ap_flat.partition_broadcast(128), writes=[r])
        return t, r

    def load_w(self, src, K, N, rows=128, col_chunk=2048, queue="gpsimd"):
        t = self.sb([rows, K, N], BF16)
        rs = [Res() for _ in range(K)]
        nch = -(-N // col_chunk)
        cw = -(-N // nch)
        for k in range(K):
            c0 = 0
            while c0 < N:
                c1 = min(N, c0 + cw)
                self.S.dma(queue, t[:, k, c0:c1], src[k * rows:(k + 1) * rows, c0:c1], writes=[rs[k]], join=True)
                c0 = c1
        return t, rs

    def norm_setup(self):
        self.rstd = self.sb([128, NB], F32)
        self.Rrstd = Res()
        self.lnv = self.rstd
        self.Rlnv = self.Rrstd

    def fm_norm(self, hT, RhT, g, Rg, hn, Rhn, pss, Rpss):
        S = self.S
        sq, Rsq = hn, Rhn
        for k in range(8):
            S.op("scalar", lambda e, k=k: e.activation(out=sq[:, k, :], in_=hT[:, k, :], func=AF.Square),
                 reads=[RhT[k]], writes=[Rsq[k]])
        for k in range(8):
            S.op("tensor", lambda e, k=k: e.matmul(pss[:], lhsT=self.ones_b[:], rhs=sq[:, k, :], start=(k == 0), stop=(k == 7)),
                 reads=[Rsq[k], self.Rc], writes=[Rpss], join=(k > 0))
        S.op("scalar", lambda e: e.activation(out=self.lnv[:], in_=pss[:], func=AF.Ln, scale=1.0 / 1024.0, bias=self.eps_t[:, 0:1]),
             reads=[Rpss, self.Rc], writes=[self.Rlnv])
        S.op("scalar", lambda e: e.activation(out=self.rstd[:], in_=self.lnv[:], func=AF.Exp, scale=-0.5),
             reads=[self.Rlnv], writes=[self.Rrstd])
        for k in range(8):
            S.op("vector", lambda e, k=k: e.scalar_tensor_tensor(out=hn[:, k, :], in0=hT[:, k, :], scalar=g[:, k:k + 1], op0=ALU.mult,
                                                                 in1=self.rstd[:], op1=ALU.mult),
                 reads=[RhT[k], self.Rrstd, Rg], writes=[Rhn[k]])


def phase_ffn(nc, C, hin, hout, wgu_ap, wd_ap, g_ap, name):
    P = Phase(nc, name)
    S = P.S
    P.consts(C)
    g, Rg = P.load_vec_fm(g_ap)
    wgu, Rwgu = P.load_w(wgu_ap, 8, 5632, col_chunk=1408)
    wd, Rwd = P.load_w(wd_ap, 22, 1024)
    P.norm_setup()
    hT = P.sb([128, 8, NB], F32)
    RhT = [Res() for _ in range(8)]
    hn = P.sb([128, 8, NB], BF16)
    Rhn = [Res() for _ in range(8)]
    act = P.sb([128, 22, NB], BF16)
    Ract = [Res() for _ in range(22)]
    sg = [P.sb([128, NB], F32) for _ in range(2)]
    Rsg = [Res() for _ in range(2)]
    pss = P.ps([128, NB], F32)
    Rpss = Res()
    pg = [P.ps([128, NB], F32) for _ in range(2)]
    Rpg = [Res() for _ in range(2)]
    pu = [P.ps([128, NB], F32) for _ in range(2)]
    Rpu = [Res() for _ in range(2)]
    po = [P.ps([128, NB], F32) for _ in range(2)]
    Rpo = [Res() for _ in range(2)]
    for blk in range(NBLK):
        t0 = blk * NB
        for k in range(8):
            S.dma("sync", hT[:, k, :], hin[k, :, t0:t0 + NB], writes=[RhT[k]])
        P.fm_norm(hT, RhT, g, Rg, hn, Rhn, pss, Rpss)
        for c in range(22):
            b = c % 2
            for k in range(8):
                S.op("tensor", lambda e, k=k, c=c, b=b: e.matmul(pg[b][:], lhsT=wgu[:, k, c * 128:(c + 1) * 128], rhs=hn[:, k, :],
                                                                  start=(k == 0), stop=(k == 7)),
                     reads=[Rwgu[k], Rhn[k]], writes=[Rpg[b]], join=(k > 0))
            for k in range(8):
                S.op("tensor", lambda e, k=k, c=c, b=b: e.matmul(pu[b][:], lhsT=wgu[:, k, 2816 + c * 128:2816 + (c + 1) * 128], rhs=hn[:, k, :],
                                                                  start=(k == 0), stop=(k == 7)),
                     reads=[Rwgu[k], Rhn[k]], writes=[Rpu[b]], join=(k > 0))
            S.op("scalar", lambda e, b=b: e.activation(out=sg[b][:], in_=pg[b][:], func=AF.Silu), reads=[Rpg[b]], writes=[Rsg[b]])
            S.op("vector", lambda e, b=b, c=c: e.tensor_tensor(out=act[:, c, :], in0=pu[b][:], in1=sg[b][:], op=ALU.mult),
                 reads=[Rpu[b], Rsg[b]], writes=[Ract[c]])
        for d in range(8):
            b = d % 2
            for c in range(22):
                S.op("tensor", lambda e, c=c, d=d, b=b: e.matmul(po[b][:], lhsT=wd[:, c, d * 128:(d + 1) * 128], rhs=act[:, c, :],
                                                                  start=(c == 0), stop=(c == 21)),
                     reads=[Rwd[c], Ract[c]], writes=[Rpo[b]], join=(c > 0))
            S.op("vector", lambda e, d=d, b=b: e.tensor_tensor(out=hT[:, d, :], in0=po[b][:], in1=hT[:, d, :], op=ALU.add),
                 reads=[Rpo[b], RhT[d]], writes=[RhT[d]])
            S.dma("sync", hout[d, :, t0:t0 + NB], hT[:, d, :], reads=[RhT[d]], writes=[P.Rdram], join=True)
    P.finish()


def make_consts():
    c = {}
    c["c_ident"] = np.eye(128, dtype=np.float32)
    s = np.arange(128)
    c["c_tri"] = (s[:, None] <= s[None, :]).astype(np.float32)
    c["c_cbias"] = np.where(s[:, None] <= s[None, :], 0.0, NEG).astype(np.float32)
    inv = 500000.0 ** (-np.arange(0, 16, 2, dtype=np.float64) / 16.0)
    ang = np.arange(NT, dtype=np.float64)[:, None] * inv[None, :]
    c["c_rope"] = np.concatenate([np.cos(ang), np.sin(ang)], axis=1).astype(np.float32)
    u = np.arange(NT)
    c["c_onehot"] = (u[None, :] // 256 == np.arange(16)[:, None]).astype(np.float32)
    c["c_tribias4"] = np.tile(c["c_cbias"], (1, 4)).astype(np.float32)
    b = np.arange(16)
    c["c_past"] = np.where(b[None, :] < b[:, None], 0.0, NEG).astype(np.float32).reshape(256)
    c["c_own"] = (b[None, :] == b[:, None]).astype(np.float32).reshape(256)
    return c


class PLE:
    def __init__(self, P, C, p_ap, g_ap, wpg_ap, wpu_ap, banks):
        self.P = P
        S = P.S
        self.p_ap = p_ap
        self.g, self.Rg = P.load_vec_fm(g_ap)
        self.wpg, self.Rwpg = P.load_w(wpg_ap, 8, 1024)
        self.wpu, self.Rwpu = P.load_w(wpu_ap, 2, 1024)
        self.ptm = P.sb([128, 4, 256], F32)
        self.Rptm = Res()
        self.pT = P.sb([128, 2, NB], BF16)
        self.RpT = [Res(), Res()]
        self.sgate = P.sb([128, NB], F32)
        self.Rsgate = Res()
        self.tmp = P.sb([128, NB], F32)
        self.Rtmp = Res()
        self.banks = banks

    def emit(self, blk, hT, RhT, hn, Rhn, pss, Rpss):
        P = self.P
        S = P.S
        t0 = blk * NB
        (pa, Rpa), (pb, Rpb), (pc, Rpc) = self.banks[:3]
        P.fm_norm(hT, RhT, self.g, self.Rg, hn, Rhn, pss, Rpss)
        S.dma("sync", self.ptm[:], self.p_ap[t0:t0 + NB, :].rearrange("(s p) d -> p s d", p=128), writes=[self.Rptm])
        for kk in range(2):
            for s in range(4):
                S.op("tensor", lambda e, kk=kk, s=s: e.transpose(out=pc[:, s * 128:(s + 1) * 128], in_=self.ptm[:, s, kk * 128:(kk + 1) * 128],
                                                                 identity=P.ident_f[:]),
                     reads=[self.Rptm, P.Rc], writes=[Rpc], join=(s > 0))
            S.op("scalar", lambda e, kk=kk: e.copy(out=self.pT[:, kk, :], in_=pc[:]), reads=[Rpc], writes=[self.RpT[kk]])
        for d in range(8):
            for k in range(8):
                S.op("tensor", lambda e, k=k, d=d: e.matmul(pa[:], lhsT=self.wpg[:, k, d * 128:(d + 1) * 128], rhs=hn[:, k, :],
                                                             start=(k == 0), stop=(k == 7)),
                     reads=[self.Rwpg[k], Rhn[k]], writes=[Rpa], join=(k > 0))
            S.op("scalar", lambda e: e.activation(out=self.sgate[:], in_=pa[:], func=AF.Sigmoid), reads=[Rpa], writes=[self.Rsgate])
            for kk in range(2):
                S.op("tensor", lambda e, kk=kk, d=d: e.matmul(pb[:], lhsT=self.wpu[:, kk, d * 128:(d + 1) * 128], rhs=self.pT[:, kk, :],
                                                               start=(kk == 0), stop=(kk == 1)),
                     reads=[self.Rwpu[kk], self.RpT[kk]], writes=[Rpb], join=(kk > 0))
            S.op("vector", lambda e: e.tensor_tensor(out=self.tmp[:], in0=pb[:], in1=self.sgate[:], op=ALU.mult),
                 reads=[Rpb, self.Rsgate], writes=[self.Rtmp])
            S.op("gpsimd", lambda e, d=d: e.tensor_tensor(out=hT[:, d, :], in0=hT[:, d, :], in1=self.tmp[:], op=ALU.add),
                 reads=[self.Rtmp, RhT[d]], writes=[RhT[d]])


def phase_ple_out(nc, C, hin, out_ap, p_ap, g_ap, wpg_ap, wpu_ap, name):
    P = Phase(nc, name)
    S = P.S
    P.consts(C)
    P.norm_setup()
    banks = [(P.ps([128, NB], F32), Res()) for _ in range(5)]
    pss, Rpss = P.ps([128, NB], F32), Res()
    ple = PLE(P, C, p_ap, g_ap, wpg_ap, wpu_ap, banks)
    hT = P.sb([128, 8, NB], F32)
    RhT = [Res() for _ in range(8)]
    hn = P.sb([128, 8, NB], BF16)
    Rhn = [Res() for _ in range(8)]
    otm = P.sb([128, 4, 1024], F32)
    Rotm = [Res() for _ in range(4)]
    for blk in range(NBLK):
        t0 = blk * NB
        for k in range(8):
            S.dma("sync", hT[:, k, :], hin[k, :, t0:t0 + NB], writes=[RhT[k]])
        ple.emit(blk, hT, RhT, hn, Rhn, pss, Rpss)
        for s in range(4):
            for kq in range(2):
                pt, Rpt = banks[3 + kq]
                for k4 in range(4):
                    k = kq * 4 + k4
                    S.op("tensor", lambda e, k=k, k4=k4, s=s, pt=pt: e.transpose(out=pt[:, k4 * 128:(k4 + 1) * 128], in_=hT[:, k, s * 128:(s + 1) * 128],
                                                                                 identity=P.ident_f[:]),
                         reads=[RhT[k], P.Rc], writes=[Rpt], join=(k4 > 0))
                eng = "scalar" if kq == 0 else "vector"
                if eng == "scalar":
                    S.op("scalar", lambda e, s=s, kq=kq, pt=pt: e.copy(out=otm[:, s, kq * 512:(kq + 1) * 512], in_=pt[:]),
                         reads=[Rpt], writes=[Rotm[s]], join=(kq > 0))
                else:
                    S.op("vector", lambda e, s=s, kq=kq, pt=pt: e.tensor_copy(out=otm[:, s, kq * 512:(kq + 1) * 512], in_=pt[:]),
                         reads=[Rpt], writes=[Rotm[s]], join=(kq > 0))
            S.dma("sync", out_ap[t0 + s * 128:t0 + (s + 1) * 128, :], otm[:, s, :], reads=[Rotm[s]], writes=[P.Rdram], join=True)
    P.finish()


def phase_mlstm(nc, C, x_ap, hout, W, name, nblk=NBLK):
    P = Phase(nc, name)
    S = P.S
    P.consts(C)
    P.norm_setup()
    tri_f = P.sb([128, 128], F32)
    cbias = P.sb([128, 128], F32)
    ones_f = P.sb([128, 128], F32)
    S.dma("sync", tri_f[:], C["c_tri"], writes=[P.Rc], join=True)
    S.dma("sync", cbias[:], C["c_cbias"], writes=[P.Rc], join=True)
    S.op("vector", lambda e: e.memset(ones_f[:], 1.0), writes=[P.Rc], join=True)
    g, Rg = P.load_vec_fm(W["norm_mix0"])
    bgate, Rbgate = P.load_bcast(W["a_b_gate"], 16)
    mhg, Rmhg = P.load_bcast(W["a_mh_gain"], 128)
    win, Rwin = P.load_w(W["a_w_in"], 8, 3088, col_chunk=1544)
    wout, Rwout = P.load_w(W["a_w_out"], 8, 1024)

    xtm = P.sb([128, 4, 1024], F32); Rxtm = [Res() for _ in range(4)]
    hT = P.sb([128, 8, NB], F32); RhT = [Res() for _ in range(8)]
    hn = P.sb([128, 8, NB], BF16); Rhn = [Res() for _ in range(8)]
    qkT = P.sb([128, 8, NB], BF16); Rqk = [Res() for _ in range(8)]
    ktm = P.sb([128, 4, 512], BF16); Rktm = [Res() for _ in range(4)]
    vtm = P.sb([128, 4, 1024], BF16); Rvtm = [Res() for _ in range(4)]
    og = P.sb([128, 4, 1024], BF16); Rog = [Res() for _ in range(4)]
    sgt = P.sb([128, 512], F32); Rsgt = Res()
    gsb = P.sb([128, 4, 16], F32); Rgsb = Res()
    th = P.sb([128, 4, 16], F32); Rth = Res()
    ef = P.sb([128, 4, 8], F32); Ref = Res()
    spf = P.sb([128, 4, 8], F32); Rspf = Res()
    li = P.sb([128, 4, 8], F32); Rli = Res()
    lf = P.sb([128, 4, 8], F32); Rlf = Res()
    g_sb = P.sb([128, 8], F32); Rg_sb = Res()
    bb = P.sb([128, 8], F32); Rbb = Res()
    eg = P.sb([128, 8], F32); Reg = Res()
    wlp = P.sb([128, 8], F32); Rwlp = Res()
    wl = P.sb([128, 8], F32); Rwl = Res()
    egl = P.sb([128, 4], F32); Regl = Res()
    Gd = P.sb([128, 8, 128], F32); RGd = Res()
    arg = P.sb([128, 8, 128], F32); Rarg = Res()
    DT = P.sb([128, 8, 128], F32); RDT = Res()
    PT = P.sb([128, 8, 128], BF16); RPT = Res()
    kw = P.sb([128, 8, 64], BF16); Rkw = Res()
    numXs = P.sb([128, 8, 128], F32); RnumXs = Res()
    num = P.sb([128, 8, 128], F32); Rnum = Res()
    sqn = P.sb([128, 8, 128], F32); Rsqn = Res()
    sm = {n: (P.sb([128, 8], F32), Res()) for n in ["dxs", "den", "dd", "rec", "ssn", "t1", "t2", "lnt", "rs", "coef"]}
    y0 = P.sb([128, 8, 128], F32); Ry0 = Res()
    ytm = P.sb([128, 1024], BF16); Rytm = Res()
    yT = P.sb([128, 8, NB], BF16); RyT = [Res() for _ in range(4)]
    Cst = P.sb([128, 4, 128], F32); nst = P.sb([128, 4], F32); RC = Res()
    Cbf = P.sb([128, 4, 2, 128], BF16); nbf = P.sb([128, 4, 2], BF16); RCbf = Res()
    qbd = P.sb([128, 4, 2, NB], BF16); Rqbd = [Res() for _ in range(4)]
    nt1 = P.sb([128, 4], F32); Rnt1 = Res()

    pS = P.ps([128, 512], F32)
    RpS = Res()
    Rgcs = Rglast = RdenI = RdenX = Rdn = Rpgate = RpS
    pR = [P.ps([128, 512], F32) for _ in range(2)]; RpR = [Res(), Res()]
    pG = P.ps([128, 1024], F32); RpG = Res()
    pT2 = P.ps([128, 1024], F32); RpT2 = Res()
    pY = P.ps([128, 1024], BF16); RpY = Res()
    rot = [0]

    def nextbank():
        rot[0] ^= 1
        return pR[rot[0]], RpR[rot[0]]

    for t in (Cst, nst):
        S.op("vector", lambda e, t=t: e.memset(t[:], 0.0), writes=[RC], join=True)
    for t in (Cbf, nbf):
        S.op("vector", lambda e, t=t: e.memset(t[:], 0.0), writes=[RCbf], join=True)
    for c in range(4):
        S.op("gpsimd", lambda e, c=c: e.memset(qbd[:, c, :, :], 0.0), writes=[Rqbd[c]])

    for blk in range(nblk):
        t0 = blk * NB
        for s in range(4):
            S.dma("sync", xtm[:, s, :], x_ap[t0 + s * 128:t0 + (s + 1) * 128, :], writes=[Rxtm[s]])
        for k in range(8):
            pb, Rpb = nextbank()
            for s in range(4):
                S.op("tensor", lambda e, k=k, s=s, pb=pb: e.transpose(out=pb[:, s * 128:(s + 1) * 128], in_=xtm[:, s, k * 128:(k + 1) * 128],
                                                                       identity=P.ident_f[:]),
                     reads=[Rxtm[s], P.Rc], writes=[Rpb], join=(s > 0))
            S.op("scalar", lambda e, k=k, pb=pb: e.copy(out=hT[:, k, :], in_=pb[:]), reads=[Rpb], writes=[RhT[k]])
        pb, Rpb = nextbank()
        P.fm_norm(hT, RhT, g, Rg, hn, Rhn, pb, Rpb)
        for c in range(8):
            pb, Rpb = nextbank()
            for k in range(8):
                S.op("tensor", lambda e, k=k, c=c, pb=pb: e.matmul(pb[:], lhsT=win[:, k, c * 128:(c + 1) * 128], rhs=hn[:, k, :],
                                                                    start=(k == 0), stop=(k == 7)),
                     reads=[Rwin[k], Rhn[k]], writes=[Rpb], join=(k > 0))
            sc = 0.125 if c < 4 else 1.0
            S.op("scalar", lambda e, c=c, pb=pb, sc=sc: e.activation(out=qkT[:, c, :], in_=pb[:], func=AF.Copy, scale=sc),
                 reads=[Rpb], writes=[Rqk[c]])
            if c < 4:
                S.op("gpsimd", lambda e, c=c: e.tensor_copy(out=qbd[0:64, c, 0, :], in_=qkT[0:64, c, :]), reads=[Rqk[c]], writes=[Rqbd[c]])
                S.op("gpsimd", lambda e, c=c: e.tensor_copy(out=qbd[64:128, c, 1, :], in_=qkT[64:128, c, :]), reads=[Rqk[c]], writes=[Rqbd[c]], join=True)
        for s in range(4):
            ts = slice(s * 128, (s + 1) * 128)
            pb, Rpb = nextbank()
            for k in range(8):
                S.op("tensor", lambda e, k=k, ts=ts, pb=pb: e.matmul(pb[:], lhsT=hn[:, k, ts], rhs=win[:, k, 512:1024], start=(k == 0), stop=(k == 7)),
                     reads=[Rwin[k], Rhn[k]], writes=[Rpb], join=(k > 0))
            S.op("scalar", lambda e, s=s, pb=pb: e.copy(out=ktm[:, s, :], in_=pb[:]), reads=[Rpb], writes=[Rktm[s]])
            for half in range(2):
                pb, Rpb = nextbank()
                c0 = 1024 + half * 512
                for k in range(8):
                    S.op("tensor", lambda e, k=k, ts=ts, pb=pb, c0=c0: e.matmul(pb[:], lhsT=hn[:, k, ts], rhs=win[:, k, c0:c0 + 512],
                                                                                 start=(k == 0), stop=(k == 7)),
                         reads=[Rwin[k], Rhn[k]], writes=[Rpb], join=(k > 0))
                S.op("vector", lambda e, s=s, half=half, pb=pb: e.tensor_copy(out=vtm[:, s, half * 512:(half + 1) * 512], in_=pb[:]),
                     reads=[Rpb], writes=[Rvtm[s]], join=(half > 0))
            for half in range(2):
                pb, Rpb = nextbank()
                c0 = 2048 + half * 512
                for k in range(8):
                    S.op("tensor", lambda e, k=k, ts=ts, pb=pb, c0=c0: e.matmul(pb[:], lhsT=hn[:, k, ts], rhs=win[:, k, c0:c0 + 512],
                                                                                 start=(k == 0), stop=(k == 7)),
                         reads=[Rwin[k], Rhn[k]], writes=[Rpb], join=(k > 0))
                S.op("scalar", lambda e, pb=pb: e.activation(out=sgt[:], in_=pb[:], func=AF.Sigmoid), reads=[Rpb], writes=[Rsgt])
                S.op("gpsimd", lambda e, s=s, half=half: e.tensor_tensor(
                    out=og[:, s, half * 512:(half + 1) * 512].rearrange("p (h v) -> p h v", h=4),
                    in0=sgt[:].rearrange("p (h v) -> p h v", h=4),
                    in1=mhg[:].unsqueeze(1).broadcast_to([128, 4, 128]), op=ALU.mult),
                     reads=[Rsgt, Rmhg], writes=[Rog[s]], join=(half > 0))
            for k in range(8):
                S.op("tensor", lambda e, k=k, ts=ts, s=s: e.matmul(pS[:, 64 + s * 16:64 + (s + 1) * 16], lhsT=hn[:, k, ts], rhs=win[:, k, 3072:3088],
                                                                    start=(k == 0), stop=(k == 7)),
                     reads=[Rwin[k], Rhn[k]], writes=[Rpgate], join=(k > 0))
            S.op("vector", lambda e, s=s: e.tensor_tensor(out=gsb[:, s, :], in0=pS[:, 64 + s * 16:64 + (s + 1) * 16], in1=bgate[:], op=ALU.add),
                 reads=[Rpgate, Rbgate], writes=[Rgsb], join=(s > 0))
        S.op("scalar", lambda e: e.activation(out=th[:], in_=gsb[:], func=AF.Tanh, scale=1.0 / 15.0), reads=[Rgsb], writes=[Rth])
        S.op("vector", lambda e: e.tensor_scalar(out=li[:], in0=th[:, :, 0:8], scalar1=15.0, scalar2=None, op0=ALU.mult), reads=[Rth], writes=[Rli])
        S.op("scalar", lambda e: e.activation(out=ef[:], in_=th[:, :, 8:16], func=AF.Exp, scale=-15.0), reads=[Rth], writes=[Ref])
        S.op("scalar", lambda e: e.activation(out=spf[:], in_=ef[:], func=AF.Ln, bias=P.one_t[:, 0:1]), reads=[Ref, P.Rc], writes=[Rspf])
        S.op("vector", lambda e: e.tensor_scalar(out=lf[:], in0=spf[:], scalar1=-1.0, scalar2=None, op0=ALU.mult), reads=[Rspf], writes=[Rlf])

        for s in range(4):
            ts = slice(s * 128, (s + 1) * 128)
            S.op("tensor", lambda e, s=s: e.matmul(pS[:, 0:8], lhsT=tri_f[:], rhs=lf[:, s, :], start=True, stop=True),
                 reads=[Rlf, P.Rc], writes=[Rgcs])
            S.op("tensor", lambda e, s=s: e.matmul(pS[:, 8:16], lhsT=ones_f[:], rhs=lf[:, s, :], start=True, stop=True),
                 reads=[Rlf, P.Rc], writes=[Rglast])
            S.op("vector", lambda e: e.tensor_copy(out=g_sb[:], in_=pS[:, 0:8]), reads=[Rgcs], writes=[Rg_sb])
            S.op("vector", lambda e, s=s: e.tensor_tensor(out=bb[:], in0=li[:, s, :], in1=g_sb[:], op=ALU.subtract), reads=[Rli, Rg_sb], writes=[Rbb])
            S.op("scalar", lambda e: e.activation(out=eg[:], in_=g_sb[:], func=AF.Exp), reads=[Rg_sb], writes=[Reg])
            S.op("vector", lambda e: e.tensor_tensor(out=wlp[:], in0=pS[:, 8:16], in1=bb[:], op=ALU.add), reads=[Rglast, Rbb], writes=[Rwlp])
            S.op("scalar", lambda e: e.activation(out=wl[:], in_=wlp[:], func=AF.Exp), reads=[Rwlp], writes=[Rwl])
            S.op("scalar", lambda e: e.activation(out=egl[0:64, :], in_=pS[0:64, 8:16:2], func=AF.Exp), reads=[Rglast], writes=[Regl])
            S.op("scalar", lambda e: e.activation(out=egl[64:128, :], in_=pS[64:128, 9:16:2], func=AF.Exp), reads=[Rglast], writes=[Regl], join=True)
            S.op("vector", lambda e: e.tensor_tensor(out=Gd[:], in0=g_sb[:].unsqueeze(2).broadcast_to([128, 8, 128]),
                                                      in1=P.ident_f[:].unsqueeze(1).broadcast_to([128, 8, 128]), op=ALU.mult),
                 reads=[Rg_sb, P.Rc], writes=[RGd])
            for half in range(2):
                S.op("tensor", lambda e, half=half: e.matmul(pG[:, half * 512:(half + 1) * 512], lhsT=ones_f[:],
                                                               rhs=Gd[:, half * 4:(half + 1) * 4, :].rearrange("p h j -> p (h j)"),
                                                               start=True, stop=True),
                     reads=[RGd, P.Rc], writes=[RpG], join=(half > 0))
            for h in range(8):
                S.op("vector", lambda e, h=h: e.scalar_tensor_tensor(out=arg[:, h, :], in0=pG[:, h * 128:(h + 1) * 128], scalar=bb[:, h:h + 1], op0=ALU.add,
                                                                      in1=cbias[:], op1=ALU.add),
                     reads=[RpG, Rbb, P.Rc], writes=[Rarg], join=(h > 0))
            S.op("scalar", lambda e: e.activation(out=DT[:], in_=arg[:], func=AF.Exp), reads=[Rarg], writes=[RDT])
            for c in range(4):
                S.op("tensor", lambda e, c=c, ts=ts: e.matmul(pT2[:, c * 256:(c + 1) * 256], lhsT=qkT[:, 4 + c, ts], rhs=qbd[:, c, :, ts],
                                                               start=True, stop=True),
                     reads=[Rqbd[c], Rqk[4 + c]], writes=[RpT2], join=(c > 0))
            S.op("vector", lambda e: e.tensor_tensor(out=PT[:].rearrange("p h j -> p (h j)"), in0=pT2[:], in1=DT[:].rearrange("p h j -> p (h j)"), op=ALU.mult),
                 reads=[RpT2, RDT], writes=[RPT])
            for h in range(8):
                S.op("tensor", lambda e, h=h, s=s: e.matmul(pG[:, h * 128:(h + 1) * 128], lhsT=PT[:, h, :], rhs=vtm[:, s, h * 128:(h + 1) * 128],
                                                             start=True, stop=True),
                     reads=[RPT, Rvtm[s]], writes=[RpG], join=(h > 0))
            for h in range(8):
                S.op("tensor", lambda e, h=h: e.matmul(pS[:, 16 + h:17 + h], lhsT=PT[:, h, :], rhs=P.ones_b[:, 0:1], start=True, stop=True),
                     reads=[RPT, P.Rc], writes=[RdenI], join=(h > 0))
            for c in range(4):
                S.op("tensor", lambda e, c=c, ts=ts: e.matmul(pT2[:, c * 256:(c + 1) * 256], lhsT=qkT[:, c, ts], rhs=Cbf[:, c, :, :],
                                                               start=True, stop=True),
                     reads=[Rqk[c], RCbf], writes=[RpT2], join=(c > 0))
            for c in range(4):
                S.op("tensor", lambda e, c=c, ts=ts: e.matmul(pS[:, 24 + 2 * c:26 + 2 * c], lhsT=qkT[:, c, ts], rhs=nbf[:, c, :],
                                                               start=True, stop=True),
                     reads=[Rqk[c], RCbf], writes=[RdenX], join=(c > 0))
            S.op("vector", lambda e, s=s: e.tensor_tensor(out=kw[:], in0=ktm[:, s, :].rearrange("p (h d) -> p h d", h=8),
                                                           in1=wl[:].unsqueeze(2).broadcast_to([128, 8, 64]), op=ALU.mult),
                 reads=[Rktm[s], Rwl], writes=[Rkw])
            pd, Rpd = nextbank()
            for h in range(8):
                c, ph = h // 2, h % 2
                prt = slice(ph * 64, (ph + 1) * 64)
                S.op("tensor", lambda e, h=h, c=c, prt=prt, s=s, pd=pd: e.matmul(pd[prt, c * 128:(c + 1) * 128], lhsT=kw[:, h, :], rhs=vtm[:, s, h * 128:(h + 1) * 128],
                                                                                  start=True, stop=True),
                     reads=[Rkw, Rvtm[s]], writes=[Rpd], join=(h > 0))
            for h in range(8):
                c, ph = h // 2, h % 2
                prt = slice(ph * 64, (ph + 1) * 64)
                S.op("tensor", lambda e, h=h, c=c, prt=prt: e.matmul(pS[prt, 32 + c:33 + c], lhsT=kw[:, h, :], rhs=P.ones_b[:, 0:1], start=True, stop=True),
                     reads=[Rkw, P.Rc], writes=[Rdn], join=(h > 0))
            for c in range(4):
                S.op("vector", lambda e, c=c, pd=pd: e.scalar_tensor_tensor(out=Cst[:, c, :], in0=Cst[:, c, :], scalar=egl[:, c:c + 1], op0=ALU.mult,
                                                                            in1=pd[:, c * 128:(c + 1) * 128], op1=ALU.add),
                     reads=[Rpd, Regl, RC], writes=[RC])
            S.op("vector", lambda e: e.tensor_tensor(out=nt1[:], in0=nst[:], in1=egl[:], op=ALU.mult), reads=[RC, Regl], writes=[Rnt1])
            S.op("vector", lambda e: e.tensor_tensor(out=nst[:], in0=pS[:, 32:36], in1=nt1[:], op=ALU.add), reads=[Rdn, Rnt1], writes=[RC])
            S.op("vector", lambda e: e.tensor_tensor(out=numXs[:], in0=pT2[:].rearrange("p (h v) -> p h v", h=8),
                                                      in1=eg[:].unsqueeze(2).broadcast_to([128, 8, 128]), op=ALU.mult),
                 reads=[RpT2, Reg], writes=[RnumXs])
            S.op("vector", lambda e: e.tensor_tensor(out=num[:].rearrange("p h v -> p (h v)"), in0=pG[:], in1=numXs[:].rearrange("p h v -> p (h v)"), op=ALU.add),
                 reads=[RpG, RnumXs], writes=[Rnum])
            S.op("gpsimd", lambda e: e.tensor_copy(out=Cbf[0:64, :, 0, :], in_=Cst[0:64, :, :]), reads=[RC], writes=[RCbf])
            S.op("gpsimd", lambda e: e.tensor_copy(out=Cbf[64:128, :, 1, :], in_=Cst[64:128, :, :]), reads=[RC], writes=[RCbf], join=True)
            S.op("gpsimd", lambda e: e.tensor_copy(out=nbf[0:64, :, 0], in_=nst[0:64, :]), reads=[RC], writes=[RCbf], join=True)
            S.op("gpsimd", lambda e: e.tensor_copy(out=nbf[64:128, :, 1], in_=nst[64:128, :]), reads=[RC], writes=[RCbf], join=True)
            T = lambda n: sm[n][0]
            R_ = lambda n: sm[n][1]
            S.op("vector", lambda e: e.tensor_tensor(out=T("dxs")[:], in0=pS[:, 24:32], in1=eg[:], op=ALU.mult), reads=[RdenX, Reg], writes=[R_("dxs")])
            S.op("vector", lambda e: e.tensor_tensor(out=T("den")[:], in0=pS[:, 16:24], in1=T("dxs")[:], op=ALU.add), reads=[RdenI, R_("dxs")], writes=[R_("den")])
            S.op("vector", lambda e: e.scalar_tensor_tensor(out=T("t1")[:], in0=T("den")[:], scalar=-1.0, op0=ALU.mult, in1=T("den")[:], op1=ALU.max),
                 reads=[R_("den")], writes=[R_("t1")])
            S.op("vector", lambda e: e.tensor_scalar(out=T("dd")[:], in0=T("t1")[:], scalar1=1.0, scalar2=None, op0=ALU.max), reads=[R_("t1")], writes=[R_("dd")])
            S.op("vector", lambda e: e.reciprocal(out=T("rec")[:], in_=T("dd")[:]), reads=[R_("dd")], writes=[R_("rec")])
            S.op("gpsimd", lambda e: e.tensor_tensor(out=sqn[:], in0=num[:], in1=num[:], op=ALU.mult), reads=[Rnum], writes=[Rsqn])
            S.op("vector", lambda e: e.tensor_reduce(out=T("ssn")[:], in_=sqn[:], axis=AX.X, op=ALU.add), reads=[Rsqn], writes=[R_("ssn")])
            S.op("vector", lambda e: e.tensor_tensor(out=T("t1")[:], in0=T("rec")[:], in1=T("rec")[:], op=ALU.mult), reads=[R_("rec")], writes=[R_("t1")])
            S.op("vector", lambda e: e.tensor_tensor(out=T("t2")[:], in0=T("t1")[:], in1=T("ssn")[:], op=ALU.mult), reads=[R_("t1"), R_("ssn")], writes=[R_("t2")])
            S.op("scalar", lambda e: e.activation(out=T("lnt")[:], in_=T("t2")[:], func=AF.Ln, scale=1.0 / 128.0, bias=P.eps_t[:, 0:1]),
                 reads=[R_("t2"), P.Rc], writes=[R_("lnt")])
            S.op("scalar", lambda e: e.activation(out=T("rs")[:], in_=T("lnt")[:], func=AF.Exp, scale=-0.5), reads=[R_("lnt")], writes=[R_("rs")])
            S.op("vector", lambda e: e.tensor_tensor(out=T("coef")[:], in0=T("rec")[:], in1=T("rs")[:], op=ALU.mult), reads=[R_("rec"), R_("rs")], writes=[R_("coef")])
            S.op("vector", lambda e: e.tensor_tensor(out=y0[:], in0=num[:], in1=T("coef")[:].unsqueeze(2).broadcast_to([128, 8, 128]), op=ALU.mult),
                 reads=[Rnum, R_("coef")], writes=[Ry0])
            S.op("gpsimd", lambda e, s=s: e.tensor_tensor(out=ytm[:], in0=y0[:].rearrange("p h v -> p (h v)"), in1=og[:, s, :], op=ALU.mult),
                 reads=[Ry0, Rog[s]], writes=[Rytm])
            for h in range(8):
                S.op("tensor", lambda e, h=h: e.transpose(out=pY[:, h * 128:(h + 1) * 128], in_=ytm[:, h * 128:(h + 1) * 128], identity=P.ident_b[:]),
                     reads=[Rytm, P.Rc], writes=[RpY], join=(h > 0))
            S.op("scalar", lambda e, ts=ts: e.copy(out=yT[:, :, ts], in_=pY[:].rearrange("p (h j) -> p h j", h=8)), reads=[RpY], writes=[RyT[s]])
        for d in range(8):
            pb, Rpb = nextbank()
            for h in range(8):
                S.op("tensor", lambda e, h=h, d=d, pb=pb: e.matmul(pb[:], lhsT=wout[:, h, d * 128:(d + 1) * 128], rhs=yT[:, h, :], start=(h == 0), stop=(h == 7)),
                     reads=[Rwout[h]] + RyT, writes=[Rpb], join=(h > 0))
            S.op("vector", lambda e, d=d, pb=pb: e.tensor_tensor(out=hT[:, d, :], in0=pb[:], in1=hT[:, d, :], op=ALU.add), reads=[Rpb, RhT[d]], writes=[RhT[d]])
            S.dma("sync", hout[d, :, t0:t0 + NB], hT[:, d, :], reads=[RhT[d]], writes=[P.Rdram], join=True)
    P.finish()


def phase_moba(nc, C, hin, hout, W, name, nblk=NBLK, dbg=None):
    P = Phase(nc, name)
    S = P.S
    P.consts(C)
    P.norm_setup()
    c256 = P.sb([128, 1], F32)
    S.op("vector", lambda e: e.memset(c256[:], 1.0 / 256.0), writes=[P.Rc], join=True)
    tri4 = P.sb([128, 512], BF16)
    S.dma("gpsimd", tri4[:], C["c_tribias4"], writes=[P.Rc], join=True)
    ropet = P.sb([128, 32, 16], F32)
    S.dma("sync", ropet[:], C["c_rope"].rearrange("(i p) c -> p i c", p=128), writes=[P.Rc], join=True)
    pastb, Rpastb = P.load_bcast(C["c_past"], 256)
    ownb, Rownb = P.load_bcast(C["c_own"], 256)
    g_kv, Rg_kv = P.load_vec_fm(W["kv_norm"])
    g_mix, Rg_mix = P.load_vec_fm(W["norm_mix1"])
    knorm, Rknorm = P.load_bcast(W["k_norm"], 64)
    qnorm, Rqnorm = P.load_bcast(W["b_q_norm"], 64)
    wkv, Rwkv = P.load_w(W["w_kv"], 8, 512)
    wq, Rwq = P.load_w(W["b_w_q"], 8, 1024)
    wo, Rwo = P.load_w(W["b_w_o"], 8, 1024)

    pR = [P.ps([128, 512], F32) for _ in range(2)]; RpR = [Res(), Res()]
    pMf = P.ps([128, 512], F32); RpMf = Res()
    pMb = P.ps([128, 1024], BF16); RpMb = Res()
    psc = [P.ps([128, 512], F32) for _ in range(2)]; Rpsc = [Res(), Res()]
    pop = [P.ps([128, 512], F32) for _ in range(2)]; Rpop = [Res(), Res()]
    rot = [0]

    def nextbank():
        rot[0] ^= 1
        return pR[rot[0]], RpR[rot[0]]

    ple = PLE(P, C, W["p0"], W["norm_ple0"], W["w_ple_gate0"], W["w_ple_up0"], [(pR[0], RpR[0]), (pR[1], RpR[1]), (pMf, RpMf)])

    hT = P.sb([128, 8, NB], F32); RhT = [Res() for _ in range(8)]
    hn = P.sb([128, 8, NB], BF16); Rhn = [Res() for _ in range(8)]
    KT = P.sb([80, 4, NT], BF16); RKT = [Res() for _ in range(32)]
    Vaug = P.sb([128, 4, 32, 128], BF16); RV = [Res() for _ in range(32)]
    kmT = P.sb([80, 4, 16], BF16); RkmT = Res()
    kms = P.sb([64, 4, 2], F32); Rkms = Res()
    ksb = P.sb([128, 4, 64], F32); Rksb = Res()
    sqk = P.sb([128, 4, 64], F32); Rsqk = Res()
    kbf = P.sb([128, 4, 64], BF16); Rkbf = Res()
    qsb = P.sb([128, 16, 64], F32); Rqsb = Res()
    sqq = P.sb([128, 16, 64], F32); Rsqq = Res()
    qa = P.sb([128, 16, 80], BF16); Rqa = Res()
    QT0 = P.sb([80, 16, 128], BF16); RQT0 = Res()
    QTa = P.sb([80, 16, 128], BF16); RQTa = Res()
    gm = P.sb([128, 16, 16], F32); Rgm = Res()
    mx8 = P.sb([128, 16, 8], F32); Rmx8 = Res()
    vis = P.sb([128, 16, 16], F32); Rvis = Res()
    sk = {n: (P.sb([128, 16], F32), Res()) for n in ["ss", "ln", "r"]}
    rt = {n: (P.sb([128, 16, 8], F32), Res()) for n in ["t1", "t2", "t3", "t4"]}
    PTb = [P.sb([128, 512], BF16) for _ in range(2)]; RPTb = [Res() for _ in range(2)]
    rec = P.sb([128, 512], F32); Rrec = Res()
    OTb = P.sb([128, 8, NB], BF16); ROTb = [Res() for _ in range(4)]

    for kvh in range(4):
        for c0 in range(0, NT, 1024):
            S.dma("gpsimd", KT[64:80, kvh, c0:c0 + 1024], C["c_onehot"][:, c0:c0 + 1024], writes=[P.Rc], join=True)
    S.op("vector", lambda e: e.memset(Vaug[:].rearrange("p a b c -> p (a b c)"), 1.0), writes=[P.Rc], join=True)
    S.op("gpsimd", lambda e: e.memset(kmT[:], 0.0), writes=[RkmT])
    S.op("gpsimd", lambda e: e.memset(QT0[:], 0.0), writes=[RQT0])

    def head_norm_rope(x, Rx, sq_, Rsq_, nh, gbc, Rgbc, it):
        (ss, Rss), (ln, Rln), (r, Rr) = sk["ss"], sk["ln"], sk["r"]
        S.op("gpsimd", lambda e: e.tensor_tensor(out=sq_[:], in0=x[:], in1=x[:], op=ALU.mult), reads=[Rx], writes=[Rsq_])
        S.op("vector", lambda e: e.tensor_reduce(out=ss[:, 0:nh], in_=sq_[:], axis=AX.X, op=ALU.add), reads=[Rsq_], writes=[Rss])
        S.op("scalar", lambda e: e.activation(out=ln[:, 0:nh], in_=ss[:, 0:nh], func=AF.Ln, scale=1.0 / 64.0, bias=P.eps_t[:, 0:1]),
             reads=[Rss, P.Rc], writes=[Rln])
        S.op("scalar", lambda e: e.activation(out=r[:, 0:nh], in_=ln[:, 0:nh], func=AF.Exp, scale=-0.5), reads=[Rln], writes=[Rr])
        S.op("vector", lambda e: e.tensor_tensor(out=x[:], in0=x[:], in1=r[:, 0:nh].unsqueeze(2).broadcast_to([128, nh, 64]), op=ALU.mult),
             reads=[Rx, Rr], writes=[Rx])
        S.op("vector", lambda e: e.tensor_tensor(out=x[:], in0=x[:], in1=gbc[:].unsqueeze(1).broadcast_to([128, nh, 64]), op=ALU.mult),
             reads=[Rx, Rgbc], writes=[Rx])
        cs = ropet[:, it, 0:8].unsqueeze(1).broadcast_to([128, nh, 8])
        sn = ropet[:, it, 8:16].unsqueeze(1).broadcast_to([128, nh, 8])
        x1 = x[:, :, 0:8]
        x2 = x[:, :, 8:16]
        tt = {k: v[0][:, 0:nh, :] for k, v in rt.items()}
        Rt = {k: v[1] for k, v in rt.items()}
        S.op("vector", lambda e: e.tensor_tensor(out=tt["t1"], in0=x1, in1=cs, op=ALU.mult), reads=[Rx, P.Rc], writes=[Rt["t1"]])
        S.op("vector", lambda e: e.tensor_tensor(out=tt["t2"], in0=x2, in1=sn, op=ALU.mult), reads=[Rx, P.Rc], writes=[Rt["t2"]])
        S.op("vector", lambda e: e.tensor_tensor(out=tt["t3"], in0=x2, in1=cs, op=ALU.mult), reads=[Rx, P.Rc], writes=[Rt["t3"]])
        S.op("vector", lambda e: e.tensor_tensor(out=tt["t4"], in0=x1, in1=sn, op=ALU.mult), reads=[Rx, P.Rc], writes=[Rt["t4"]])
        S.op("vector", lambda e: e.tensor_tensor(out=x1, in0=tt["t1"], in1=tt["t2"], op=ALU.subtract), reads=[Rt["t1"], Rt["t2"], Rx], writes=[Rx])
        S.op("vector", lambda e: e.tensor_tensor(out=x2, in0=tt["t3"], in1=tt["t4"], op=ALU.add), reads=[Rt["t3"], Rt["t4"], Rx], writes=[Rx])

    nsc = [0]
    for blk in range(nblk):
        t0 = blk * NB
        for k in range(8):
            S.dma("sync", hT[:, k, :], hin[k, :, t0:t0 + NB], writes=[RhT[k]])
        pb, Rpb = nextbank()
        ple.emit(blk, hT, RhT, hn, Rhn, psc[0], Rpsc[0])
        P.fm_norm(hT, RhT, g_kv, Rg_kv, hn, Rhn, psc[0], Rpsc[0])
        for s in range(4):
            it = blk * 4 + s
            ts = slice(s * 128, (s + 1) * 128)
            pb, Rpb = nextbank()
            for k in range(8):
                S.op("tensor", lambda e, k=k, ts=ts, pb=pb: e.matmul(pb[:], lhsT=hn[:, k, ts], rhs=wkv[:, k, :], start=(k == 0), stop=(k == 7)),
                     reads=[Rwkv[k], Rhn[k]], writes=[Rpb], join=(k > 0))
            S.op("scalar", lambda e, pb=pb: e.copy(out=ksb[:].rearrange("p h d -> p (h d)"), in_=pb[:, 0:256]), reads=[Rpb], writes=[Rksb])
            S.op("scalar", lambda e, pb=pb, it=it: e.copy(out=Vaug[:, :, it, 0:64], in_=pb[:, 256:512].rearrange("p (h d) -> p h d", h=4)),
                 reads=[Rpb, P.Rc], writes=[RV[it]])
            head_norm_rope(ksb, Rksb, sqk, Rsqk, 4, knorm, Rknorm, it)
            S.op("gpsimd", lambda e: e.tensor_copy(out=kbf[:], in_=ksb[:]), reads=[Rksb], writes=[Rkbf])
            for kvh in range(4):
                S.op("tensor", lambda e, kvh=kvh: e.transpose(out=pMb[0:64, kvh * 128:(kvh + 1) * 128], in_=kbf[:, kvh, :], identity=P.ident_b[:]),
                     reads=[Rkbf, P.Rc], writes=[RpMb], join=(kvh > 0))
            S.op("scalar", lambda e, it=it: e.copy(out=KT[0:64, :, it * 128:(it + 1) * 128], in_=pMb[0:64, 0:512].rearrange("p (h t) -> p h t", h=4)),
                 reads=[RpMb, P.Rc], writes=[RKT[it]])
            for kvh in range(4):
                S.op("tensor", lambda e, kvh=kvh: e.matmul(pMf[0:64, kvh:kvh + 1], lhsT=ksb[:, kvh, :], rhs=c256[:, 0:1], start=True, stop=True),
                     reads=[Rksb, P.Rc], writes=[RpMf], join=(kvh > 0))
            S.op("vector", lambda e, s=s: e.tensor_copy(out=kms[:, :, s % 2], in_=pMf[0:64, 0:4]), reads=[RpMf], writes=[Rkms], join=(s % 2 == 1))
            if s % 2 == 1:
                n = it // 2
                S.op("vector", lambda e, n=n: e.tensor_tensor(out=kmT[0:64, :, n], in0=kms[:, :, 0], in1=kms[:, :, 1], op=ALU.add),
                     reads=[Rkms], writes=[RkmT])
        P.fm_norm(hT, RhT, g_mix, Rg_mix, hn, Rhn, psc[0], Rpsc[0])
        for s in range(4):
            it = blk * 4 + s
            b = it // 2
            ts = slice(s * 128, (s + 1) * 128)
            for half in range(2):
                pb, Rpb = nextbank()
                for k in range(8):
                    S.op("tensor", lambda e, k=k, ts=ts, pb=pb, half=half: e.matmul(pb[:], lhsT=hn[:, k, ts], rhs=wq[:, k, half * 512:(half + 1) * 512],
                                                                                     start=(k == 0), stop=(k == 7)),
                         reads=[Rwq[k], Rhn[k]], writes=[Rpb], join=(k > 0))
                S.op("scalar", lambda e, pb=pb, half=half: e.copy(out=qsb[:, half * 8:(half + 1) * 8, :].rearrange("p h d -> p (h d)"), in_=pb[:]),
                     reads=[Rpb], writes=[Rqsb], join=(half > 0))
            head_norm_rope(qsb, Rqsb, sqq, Rsqq, 16, qnorm, Rqnorm, it)
            S.op("gpsimd", lambda e: e.tensor_copy(out=qa[:, :, 0:64], in_=qsb[:]), reads=[Rqsb], writes=[Rqa])
            for r_ in range(2):
                for j in range(8):
                    h = r_ * 8 + j
                    S.op("tensor", lambda e, h=h, j=j: e.transpose(out=pMb[0:64, j * 128:(j + 1) * 128], in_=qa[:, h, 0:64], identity=P.ident_b[:]),
                         reads=[Rqa, P.Rc], writes=[RpMb], join=(j > 0))
                S.op("scalar", lambda e, r_=r_: e.copy(out=QT0[0:64, r_ * 8:(r_ + 1) * 8, :], in_=pMb[0:64, :].rearrange("p (h t) -> p h t", h=8)),
                     reads=[RpMb], writes=[RQT0], join=(r_ > 0))
            for h in range(16):
                S.op("tensor", lambda e, h=h: e.matmul(pMf[:, h * 16:(h + 1) * 16], lhsT=QT0[:, h, :], rhs=kmT[:, h // 4, :], start=True, stop=True),
                     reads=[RQT0, RkmT], writes=[RpMf], join=(h > 0))
            S.op("vector", lambda e, b=b: e.tensor_tensor(out=gm[:], in0=pMf[:, 0:256].rearrange("p (h n) -> p h n", h=16),
                                                           in1=pastb[:, b * 16:(b + 1) * 16].unsqueeze(1).broadcast_to([128, 16, 16]), op=ALU.add),
                 reads=[RpMf, Rpastb], writes=[Rgm])
            for h in range(16):
                S.op("vector", lambda e, h=h: e.max(out=mx8[:, h, :], in_=gm[:, h, :]), reads=[Rgm], writes=[Rmx8], join=(h > 0))
            S.op("vector", lambda e: e.tensor_tensor(out=vis[:], in0=gm[:], in1=mx8[:, :, 2:3].broadcast_to([128, 16, 16]), op=ALU.is_ge),
                 reads=[Rgm, Rmx8], writes=[Rvis])
            S.op("vector", lambda e, b=b: e.tensor_tensor(out=vis[:], in0=vis[:], in1=ownb[:, b * 16:(b + 1) * 16].unsqueeze(1).broadcast_to([128, 16, 16]), op=ALU.max),
                 reads=[Rvis, Rownb], writes=[Rvis])
            S.op("vector", lambda e: e.tensor_scalar(out=qa[:, :, 64:80], in0=vis[:], scalar1=-NEG, scalar2=NEG, op0=ALU.mult, op1=ALU.add),
                 reads=[Rvis, Rqa], writes=[Rqa])
            for r_ in range(2):
                for j in range(8):
                    h = r_ * 8 + j
                    S.op("tensor", lambda e, h=h, j=j: e.transpose(out=pMb[0:80, j * 128:(j + 1) * 128], in_=qa[:, h, :], identity=P.ident_b[:]),
                         reads=[Rqa, P.Rc], writes=[RpMb], join=(j > 0))
                S.op("scalar", lambda e, r_=r_: e.copy(out=QTa[:, r_ * 8:(r_ + 1) * 8, :], in_=pMb[0:80, :].rearrange("p (h t) -> p h t", h=8)),
                     reads=[RpMb], writes=[RQTa], join=(r_ > 0))
            for kvh in range(4):
                po, Rpo = pop[kvh % 2], Rpop[kvh % 2]
                qg = QTa[:, 4 * kvh:4 * kvh + 4, :].rearrange("p h t -> p (h t)")
                for j in range(it + 1):
                    n = nsc[0]
                    nsc[0] += 1
                    ps_, Rps_ = psc[n % 2], Rpsc[n % 2]
                    pt_, Rpt_ = PTb[n % 2], RPTb[n % 2]
                    diag = (j == it)
                    S.op("tensor", lambda e, kvh=kvh, j=j, ps_=ps_, qg=qg, diag=diag: e.matmul(ps_[:], lhsT=KT[:, kvh, j * 128:(j + 1) * 128], rhs=qg,
                                                                                              start=True, stop=(not diag)),
                         reads=[RKT[j], RQTa, P.Rc], writes=[Rps_])
                    if diag:
                        S.op("tensor", lambda e, ps_=ps_: e.matmul(ps_[:], lhsT=P.ident_b[:], rhs=tri4[:], start=False, stop=True),
                             reads=[P.Rc], writes=[Rps_], join=True)
                    S.op("scalar", lambda e, ps_=ps_, pt_=pt_: e.activation(out=pt_[:], in_=ps_[:], func=AF.Exp, scale=0.125),
                         reads=[Rps_], writes=[Rpt_])
                    S.op("tensor", lambda e, kvh=kvh, j=j, pt_=pt_, po=po, it=it: e.matmul(po[:], lhsT=Vaug[:, kvh, j, :], rhs=pt_[:], start=(j == 0), stop=(j == it)),
                         reads=[RV[j], Rpt_, P.Rc], writes=[Rpo], join=(j > 0))
                S.op("vector", lambda e, po=po: e.reciprocal(out=rec[64:128, :], in_=po[64:128, :]), reads=[Rpo], writes=[Rrec])
                for gq in range(4):
                    hh = 4 * kvh + gq
                    pr, ph = hh // 2, hh % 2
                    S.op("vector", lambda e, po=po, gq=gq, pr=pr, ph=ph, ts=ts: e.tensor_tensor(
                        out=OTb[ph * 64:(ph + 1) * 64, pr, ts], in0=po[0:64, gq * 128:(gq + 1) * 128], in1=rec[64:128, gq * 128:(gq + 1) * 128], op=ALU.mult),
                         reads=[Rpo, Rrec], writes=[ROTb[s]], join=True)
        for d in range(8):
            pb, Rpb = nextbank()
            for pr in range(8):
                S.op("tensor", lambda e, pr=pr, d=d, pb=pb: e.matmul(pb[:], lhsT=wo[:, pr, d * 128:(d + 1) * 128], rhs=OTb[:, pr, :], start=(pr == 0), stop=(pr == 7)),
                     reads=[Rwo[pr]] + ROTb, writes=[Rpb], join=(pr > 0))
            S.op("vector", lambda e, d=d, pb=pb: e.tensor_tensor(out=hT[:, d, :], in0=pb[:], in1=hT[:, d, :], op=ALU.add), reads=[Rpb, RhT[d]], writes=[RhT[d]])
            S.dma("sync", hout[d, :, t0:t0 + NB], hT[:, d, :], reads=[RhT[d]], writes=[P.Rdram], join=True)
    P.finish()


WSHAPES = {
    "norm_mix0": [1024], "norm_mix1": [1024], "a_w_in": [1024, 3088], "a_b_gate": [16], "a_mh_gain": [128], "a_w_out": [1024, 1024],
    "kv_norm": [1024], "w_kv": [1024, 512], "k_norm": [64], "b_w_q": [1024, 1024], "b_q_norm": [64], "b_w_o": [1024, 1024],
    "norm_ffn0": [1024], "norm_ffn1": [1024], "w_gate_up0": [1024, 5632], "w_gate_up1": [1024, 5632], "w_down0": [2816, 1024], "w_down1": [2816, 1024],
    "norm_ple0": [1024], "norm_ple1": [1024], "w_ple_gate0": [1024, 1024], "w_ple_gate1": [1024, 1024], "w_ple_up0": [256, 1024], "w_ple_up1": [256, 1024],
    "p0": [NT, 256], "p1": [NT, 256],
}


def build_program():
    nc = bass.Bass("TRN2", target_bir_lowering=False)
    Cn = make_consts()
    C = {k: nc.dram_tensor(k, list(v.shape), F32, kind="ExternalInput").ap() for k, v in Cn.items()}
    x = nc.dram_tensor("x", [NT, 1024], F32, kind="ExternalInput").ap()
    out = nc.dram_tensor("out", [NT, 1024], F32, kind="ExternalOutput").ap()
    W = {k: nc.dram_tensor(k, v, F32, kind="ExternalInput").ap() for k, v in WSHAPES.items()}
    hs = [nc.dram_tensor("h_scr%d" % i, [8, 128, NT], F32, kind="Internal").ap() for i in range(4)]
    phase_mlstm(nc, C, x, hs[0], W, "ml")
    phase_ffn(nc, C, hs[0], hs[1], W["w_gate_up0"], W["w_down0"], W["norm_ffn0"], "f0")
    phase_moba(nc, C, hs[1], hs[2], W, "mb")
    phase_ffn(nc, C, hs[2], hs[3], W["w_gate_up1"], W["w_down1"], W["norm_ffn1"], "f1")
    phase_ple_out(nc, C, hs[3], out, W["p1"], W["norm_ple1"], W["w_ple_gate1"], W["w_ple_up1"], "po")
    return nc, Cn


def make_in_maps(inputs, cores):
    f = lambda a: np.ascontiguousarray(np.asarray(a, dtype=np.float32))
    I = {k: np.asarray(v) for k, v in inputs.items()}
    shared = {
        "norm_mix0": f(I["norm_mix"][0]), "norm_mix1": f(I["norm_mix"][1]), "a_w_in": f(I["a_w_in"][0]), "a_b_gate": f(I["a_b_gate"][0]),
        "a_mh_gain": f(I["a_mh_gain"][0]), "a_w_out": f(I["a_w_out"][0]), "kv_norm": f(I["kv_norm"]), "w_kv": f(I["w_kv"]), "k_norm": f(I["k_norm"]),
        "b_w_q": f(I["b_w_q"][0]), "b_q_norm": f(I["b_q_norm"][0]), "b_w_o": f(I["b_w_o"][0]),
        "norm_ffn0": f(I["norm_ffn"][0]), "norm_ffn1": f(I["norm_ffn"][1]), "w_gate_up0": f(I["w_gate_up"][0]), "w_gate_up1": f(I["w_gate_up"][1]),
        "w_down0": f(I["w_down"][0]), "w_down1": f(I["w_down"][1]), "norm_ple0": f(I["norm_ple"][0]), "norm_ple1": f(I["norm_ple"][1]),
        "w_ple_gate0": f(I["w_ple_gate"][0]), "w_ple_gate1": f(I["w_ple_gate"][1]), "w_ple_up0": f(I["w_ple_up"][0]), "w_ple_up1": f(I["w_ple_up"][1]),
    }
    maps = []
    for b in cores:
        m = dict(shared)
        m["x"] = f(I["x"][b])
        m["p0"] = f(I["p"][0, b])
        m["p1"] = f(I["p"][1, b])
        maps.append(m)
    return maps


def kernel(**inputs):
    nc, Cn = build_program()
    maps = make_in_maps(inputs, list(range(8)))
    for m in maps:
        m.update(Cn)
    res = run_bass_kernel_spmd(nc, maps, core_ids=list(range(8)))
    return np.stack([np.asarray(r["out"], dtype=np.float32) for r in res.results], axis=0)
```

```python
import numpy as np
import concourse.bass as bass
import concourse.mybir as mybir
from concourse.bass_utils import run_bass_kernel_spmd
from contextlib import ExitStack

F32 = mybir.dt.float32
BF16 = mybir.dt.bfloat16
ALU = mybir.AluOpType
AF = mybir.ActivationFunctionType
AX = mybir.AxisListType

ENGS = ["tensor", "vector", "scalar", "gpsimd", "sync"]
NDMASEM = 12
SAME_ENGINE_SYNC = True

NT = 4096
NB = 512
NBLK = NT // NB
EPS = 1e-6
NEG = -30000.0


class Res:
    __slots__ = ("name", "w", "r", "gd")

    def __init__(self, name=""):
        self.name = name
        self.w = []
        self.r = []
        self.gd = []


class Op:
    __slots__ = ("eng", "fn", "waits", "pos", "sig", "isdma", "semi", "semk", "K", "sigidx")


class Sched:
    G = {"nc": None}

    @staticmethod
    def setup(nc):
        if Sched.G.get("nc") is nc:
            return
        es = ExitStack()
        G = {"nc": nc, "es": es, "sig": {e: 0 for e in ENGS}, "dma": {e: 0 for e in ENGS}}
        G["esem"] = {e: es.enter_context(nc.semaphore("sem_e_%s" % e)) for e in ENGS}
        G["dsem"] = {(e, i): es.enter_context(nc.semaphore("sem_d_%s_%d" % (e, i))) for e in ("sync", "gpsimd") for i in range(NDMASEM)}
        Sched.G = G

    def __init__(self, nc):
        Sched.setup(nc)
        self.nc = nc
        self.ops = {e: [] for e in ENGS}
        self.Kcur = {e: {} for e in ENGS}
        self.nops = 0

    limit = None

    def _record(self, eng, fn, reads, writes, isdma, join=False):
        if Sched.limit is not None and self.nops >= Sched.limit:
            return None
        o = Op()
        o.eng = eng
        o.fn = fn
        o.isdma = isdma
        o.sig = False
        o.pos = len(self.ops[eng])
        deps = {}
        for r in reads:
            for x in r.w:
                deps[id(x)] = x
        for w in writes:
            if join and not w.r:
                for x in w.gd:
                    deps[id(x)] = x
            else:
                for x in w.w:
                    deps[id(x)] = x
                for x in w.r:
                    deps[id(x)] = x
        K = self.Kcur[eng]
        newK = None
        waits = []
        if isdma:
            i = Sched.G["dma"][eng]
            Sched.G["dma"][eng] += 1
            o.semi = i % NDMASEM
            o.semk = i // NDMASEM + 1
            if o.semk > 1:
                key = ("d", eng, o.semi)
                if K.get(key, 0) < o.semk - 1:
                    waits.append(("dmaslot", eng, o.semi, o.semk - 1))
                    newK = dict(K)
                    newK[key] = o.semk - 1
        best = {}
        dl = []
        for y in deps.values():
            if y is o:
                continue
            if y.isdma:
                dl.append(y)
            elif y.eng not in best or best[y.eng].pos < y.pos:
                best[y.eng] = y
        for y in dl + list(best.values()):
            cur = K if newK is None else newK
            if y.isdma:
                key = ("d", y.eng, y.semi)
                if cur.get(key, 0) >= y.semk:
                    continue
                waits.append(("dma", y))
            else:
                if y.eng == eng and (eng == "tensor" or not SAME_ENGINE_SYNC) and not isdma:
                    continue
                if cur.get(y.eng, -1) >= y.pos:
                    continue
                y.sig = True
                waits.append(("eng", y))
            if newK is None:
                newK = dict(K)
            for k, v in y.K.items():
                if newK.get(k, -1) < v:
                    newK[k] = v
            if y.isdma:
                key = ("d", y.eng, y.semi)
                newK[key] = max(newK.get(key, 0), y.semk)
            else:
                if newK.get(y.eng, -1) < y.pos:
                    newK[y.eng] = y.pos
        if newK is not None:
            self.Kcur[eng] = newK
            K = newK
        o.K = K
        o.waits = waits
        for r in reads:
            r.r.append(o)
        for w in writes:
            if join and not w.r:
                w.w.append(o)
            else:
                w.gd = w.w + w.r
                w.w = [o]
                w.r = []
        self.ops[eng].append(o)
        self.nops += 1
        return o

    def op(self, eng, fn, reads=(), writes=(), join=False):
        return self._record(eng, fn, reads, writes, False, join)

    def dma(self, queue, out, in_, reads=(), writes=(), join=False, nonc=False, **kw):
        nc = self.nc
        if nonc:
            def f(e):
                with nc.allow_non_contiguous_dma(reason="small strided parameter load"):
                    return e.dma_start(out=out, in_=in_, **kw)
        else:
            def f(e):
                return e.dma_start(out=out, in_=in_, **kw)
        return self._record(queue, f, reads, writes, True, join)

    def emit(self):
        nc = self.nc
        G = Sched.G
        for e in ENGS:
            n = G["sig"][e]
            for o in self.ops[e]:
                if o.sig:
                    n += 1
                o.sigidx = n
            G["sig"][e] = n
        esem, dsem = G["esem"], G["dsem"]
        with nc.Block() as block:
            def stream(ename):
                def body(eng):
                    for o in self.ops[ename]:
                        for w in o.waits:
                            if w[0] == "dmaslot":
                                eng.wait_ge(dsem[(w[1], w[2])], 16 * w[3])
                            elif w[0] == "dma":
                                y = w[1]
                                eng.wait_ge(dsem[(y.eng, y.semi)], 16 * y.semk)
                            else:
                                y = w[1]
                                eng.wait_ge(esem[y.eng], y.sigidx)
                        ins = o.fn(eng)
                        if o.isdma:
                            ins.then_inc(dsem[(o.eng, o.semi)], 16)
                        elif o.sig:
                            ins.then_inc(esem[o.eng], 1)
                return body

            for e in ENGS:
                if self.ops[e]:
                    getattr(block, e)(stream(e))


class Phase:
    def __init__(self, nc, name):
        self.nc = nc
        self.name = name
        self.es = ExitStack()
        self.S = Sched(nc)
        self.n = 0
        self.Rdram = Res("dram_out")

    def sb(self, shape, dt, name=None):
        self.n += 1
        t = self.es.enter_context(self.nc.sbuf_tensor("%s_s%d" % (self.name, self.n), list(shape), dt))
        return t

    def ps(self, shape, dt, name=None):
        self.n += 1
        t = self.es.enter_context(self.nc.psum_tensor("%s_p%d" % (self.name, self.n), list(shape), dt))
        return t

    def finish(self):
        S = self.S
        S.op("sync", lambda e: e.nop(), reads=[self.Rdram])
        S.emit()
        self.es.close()

    def consts(self, C):
        S = self.S
        self.ident_f = self.sb([128, 128], F32)
        self.ident_b = self.sb([128, 128], BF16)
        self.ones_b = self.sb([128, 128], BF16)
        self.eps_t = self.sb([128, 1], F32)
        self.one_t = self.sb([128, 1], F32)
        self.Rc = Res("consts")
        S.dma("sync", self.ident_f[:], C["c_ident"], writes=[self.Rc], join=True)
        S.op("vector", lambda e: e.tensor_copy(out=self.ident_b[:], in_=self.ident_f[:]), reads=[self.Rc], writes=[self.Rc])
        S.op("vector", lambda e: e.memset(self.ones_b[:], 1.0), writes=[self.Rc], join=True)
        S.op("vector", lambda e: e.memset(self.eps_t[:], EPS), writes=[self.Rc], join=True)
        S.op("vector", lambda e: e.memset(self.one_t[:], 1.0), writes=[self.Rc], join=True)

    def load_vec_fm(self, ap1024, nk=8):
        t = self.sb([128, nk], F32)
        r = Res()
        self.S.dma("sync", t[:], ap1024.rearrange("(k p) -> p k", p=128), writes=[r], nonc=True)
        return t, r

    def load_bcast(self, ap_flat, n):
        t = self.sb([128, n], F32)
        r = Res()
        self.S.dma("sync", t[:], ap_flat.partition_broadcast(128), writes=[r])
        return t, r

    def load_w(self, src, K, N, rows=128, col_chunk=2048, queue="gpsimd"):
        t = self.sb([rows, K, N], BF16)
        rs = [Res() for _ in range(K)]
        nch = -(-N // col_chunk)
        cw = -(-N // nch)
        for k in range(K):
            c0 = 0
            while c0 < N:
                c1 = min(N, c0 + cw)
                self.S.dma(queue, t[:, k, c0:c1], src[k * rows:(k + 1) * rows, c0:c1], writes=[rs[k]], join=True)
                c0 = c1
        return t, rs

    def norm_setup(self):
        self.rstd = self.sb([128, NB], F32)
        self.Rrstd = Res()
        self.lnv = self.rstd
        self.Rlnv = self.Rrstd

    def fm_norm(self, hT, RhT, g, Rg, hn, Rhn, pss, Rpss):
        S = self.S
        sq, Rsq = hn, Rhn
        for k in range(8):
            S.op("scalar", lambda e, k=k: e.activation(out=sq[:, k, :], in_=hT[:, k, :], func=AF.Square),
                 reads=[RhT[k]], writes=[Rsq[k]])
        for k in range(8):
            S.op("tensor", lambda e, k=k: e.matmul(pss[:], lhsT=self.ones_b[:], rhs=sq[:, k, :], start=(k == 0), stop=(k == 7)),
                 reads=[Rsq[k], self.Rc], writes=[Rpss], join=(k > 0))
        S.op("scalar", lambda e: e.activation(out=self.lnv[:], in_=pss[:], func=AF.Ln, scale=1.0 / 1024.0, bias=self.eps_t[:, 0:1]),
             reads=[Rpss, self.Rc], writes=[self.Rlnv])
        S.op("scalar", lambda e: e.activation(out=self.rstd[:], in_=self.lnv[:], func=AF.Exp, scale=-0.5),
             reads=[self.Rlnv], writes=[self.Rrstd])
        for k in range(8):
            S.op("vector", lambda e, k=k: e.scalar_tensor_tensor(out=hn[:, k, :], in0=hT[:, k, :], scalar=g[:, k:k + 1], op0=ALU.mult,
                                                                 in1=self.rstd[:], op1=ALU.mult),
                 reads=[RhT[k], self.Rrstd, Rg], writes=[Rhn[k]])


def phase_ffn(nc, C, hin, hout, wgu_ap, wd_ap, g_ap, name):
    P = Phase(nc, name)
    S = P.S
    P.consts(C)
    g, Rg = P.load_vec_fm(g_ap)
    wgu, Rwgu = P.load_w(wgu_ap, 8, 5632, col_chunk=1408)
    wd, Rwd = P.load_w(wd_ap, 22, 1024)
    P.norm_setup()
    hT = P.sb([128, 8, NB], F32)
    RhT = [Res() for _ in range(8)]
    hn = P.sb([128, 8, NB], BF16)
    Rhn = [Res() for _ in range(8)]
    act = P.sb([128, 22, NB], BF16)
    Ract = [Res() for _ in range(22)]
    sg = [P.sb([128, NB], F32) for _ in range(2)]
    Rsg = [Res() for _ in range(2)]
    pss = P.ps([128, NB], F32)
    Rpss = Res()
    pg = [P.ps([128, NB], F32) for _ in range(2)]
    Rpg = [Res() for _ in range(2)]
    pu = [P.ps([128, NB], F32) for _ in range(2)]
    Rpu = [Res() for _ in range(2)]
    po = [P.ps([128, NB], F32) for _ in range(2)]
    Rpo = [Res() for _ in range(2)]
    for blk in range(NBLK):
        t0 = blk * NB
        for k in range(8):
            S.dma("sync", hT[:, k, :], hin[k, :, t0:t0 + NB], writes=[RhT[k]])
        P.fm_norm(hT, RhT, g, Rg, hn, Rhn, pss, Rpss)
        for c in range(22):
            b = c % 2
            for k in range(8):
                S.op("tensor", lambda e, k=k, c=c, b=b: e.matmul(pg[b][:], lhsT=wgu[:, k, c * 128:(c + 1) * 128], rhs=hn[:, k, :],
                                                                  start=(k == 0), stop=(k == 7)),
                     reads=[Rwgu[k], Rhn[k]], writes=[Rpg[b]], join=(k > 0))
            for k in range(8):
                S.op("tensor", lambda e, k=k, c=c, b=b: e.matmul(pu[b][:], lhsT=wgu[:, k, 2816 + c * 128:2816 + (c + 1) * 128], rhs=hn[:, k, :],
                                                                  start=(k == 0), stop=(k == 7)),
                     reads=[Rwgu[k], Rhn[k]], writes=[Rpu[b]], join=(k > 0))
            S.op("scalar", lambda e, b=b: e.activation(out=sg[b][:], in_=pg[b][:], func=AF.Silu), reads=[Rpg[b]], writes=[Rsg[b]])
            S.op("vector", lambda e, b=b, c=c: e.tensor_tensor(out=act[:, c, :], in0=pu[b][:], in1=sg[b][:], op=ALU.mult),
                 reads=[Rpu[b], Rsg[b]], writes=[Ract[c]])
        for d in range(8):
            b = d % 2
            for c in range(22):
                S.op("tensor", lambda e, c=c, d=d, b=b: e.matmul(po[b][:], lhsT=wd[:, c, d * 128:(d + 1) * 128], rhs=act[:, c, :],
                                                                  start=(c == 0), stop=(c == 21)),
                     reads=[Rwd[c], Ract[c]], writes=[Rpo[b]], join=(c > 0))
            S.op("vector", lambda e, d=d, b=b: e.tensor_tensor(out=hT[:, d, :], in0=po[b][:], in1=hT[:, d, :], op=ALU.add),
                 reads=[Rpo[b], RhT[d]], writes=[RhT[d]])
            S.dma("sync", hout[d, :, t0:t0 + NB], hT[:, d, :], reads=[RhT[d]], writes=[P.Rdram], join=True)
    P.finish()


def make_consts():
    c = {}
    c["c_ident"] = np.eye(128, dtype=np.float32)
    s = np.arange(128)
    c["c_tri"] = (s[:, None] <= s[None, :]).astype(np.float32)
    c["c_cbias"] = np.where(s[:, None] <= s[None, :], 0.0, NEG).astype(np.float32)
    inv = 500000.0 ** (-np.arange(0, 16, 2, dtype=np.float64) / 16.0)
    ang = np.arange(NT, dtype=np.float64)[:, None] * inv[None, :]
    c["c_rope"] = np.concatenate([np.cos(ang), np.sin(ang)], axis=1).astype(np.float32)
    u = np.arange(NT)
    c["c_onehot"] = (u[None, :] // 256 == np.arange(16)[:, None]).astype(np.float32)
    c["c_tribias4"] = np.tile(c["c_cbias"], (1, 4)).astype(np.float32)
    b = np.arange(16)
    c["c_past"] = np.where(b[None, :] < b[:, None], 0.0, NEG).astype(np.float32).reshape(256)
    c["c_own"] = (b[None, :] == b[:, None]).astype(np.float32).reshape(256)
    return c


class PLE:
    def __init__(self, P, C, p_ap, g_ap, wpg_ap, wpu_ap, banks):
        self.P = P
        S = P.S
        self.p_ap = p_ap
        self.g, self.Rg = P.load_vec_fm(g_ap)
        self.wpg, self.Rwpg = P.load_w(wpg_ap, 8, 1024)
        self.wpu, self.Rwpu = P.load_w(wpu_ap, 2, 1024)
        self.ptm = P.sb([128, 4, 256], F32)
        self.Rptm = Res()
        self.pT = P.sb([128, 2, NB], BF16)
        self.RpT = [Res(), Res()]
        self.sgate = P.sb([128, NB], F32)
        self.Rsgate = Res()
        self.tmp = P.sb([128, NB], F32)
        self.Rtmp = Res()
        self.banks = banks

    def emit(self, blk, hT, RhT, hn, Rhn, pss, Rpss):
        P = self.P
        S = P.S
        t0 = blk * NB
        (pa, Rpa), (pb, Rpb), (pc, Rpc) = self.banks[:3]
        P.fm_norm(hT, RhT, self.g, self.Rg, hn, Rhn, pss, Rpss)
        S.dma("sync", self.ptm[:], self.p_ap[t0:t0 + NB, :].rearrange("(s p) d -> p s d", p=128), writes=[self.Rptm])
        for kk in range(2):
            for s in range(4):
                S.op("tensor", lambda e, kk=kk, s=s: e.transpose(out=pc[:, s * 128:(s + 1) * 128], in_=self.ptm[:, s, kk * 128:(kk + 1) * 128],
                                                                 identity=P.ident_f[:]),
                     reads=[self.Rptm, P.Rc], writes=[Rpc], join=(s > 0))
            S.op("scalar", lambda e, kk=kk: e.copy(out=self.pT[:, kk, :], in_=pc[:]), reads=[Rpc], writes=[self.RpT[kk]])
        for d in range(8):
            for k in range(8):
                S.op("tensor", lambda e, k=k, d=d: e.matmul(pa[:], lhsT=self.wpg[:, k, d * 128:(d + 1) * 128], rhs=hn[:, k, :],
                                                             start=(k == 0), stop=(k == 7)),
                     reads=[self.Rwpg[k], Rhn[k]], writes=[Rpa], join=(k > 0))
            S.op("scalar", lambda e: e.activation(out=self.sgate[:], in_=pa[:], func=AF.Sigmoid), reads=[Rpa], writes=[self.Rsgate])
            for kk in range(2):
                S.op("tensor", lambda e, kk=kk, d=d: e.matmul(pb[:], lhsT=self.wpu[:, kk, d * 128:(d + 1) * 128], rhs=self.pT[:, kk, :],
                                                               start=(kk == 0), stop=(kk == 1)),
                     reads=[self.Rwpu[kk], self.RpT[kk]], writes=[Rpb], join=(kk > 0))
            S.op("vector", lambda e: e.tensor_tensor(out=self.tmp[:], in0=pb[:], in1=self.sgate[:], op=ALU.mult),
                 reads=[Rpb, self.Rsgate], writes=[self.Rtmp])
            S.op("gpsimd", lambda e, d=d: e.tensor_tensor(out=hT[:, d, :], in0=hT[:, d, :], in1=self.tmp[:], op=ALU.add),
                 reads=[self.Rtmp, RhT[d]], writes=[RhT[d]])


def phase_ple_out(nc, C, hin, out_ap, p_ap, g_ap, wpg_ap, wpu_ap, name):
    P = Phase(nc, name)
    S = P.S
    P.consts(C)
    P.norm_setup()
    banks = [(P.ps([128, NB], F32), Res()) for _ in range(5)]
    pss, Rpss = P.ps([128, NB], F32), Res()
    ple = PLE(P, C, p_ap, g_ap, wpg_ap, wpu_ap, banks)
    hT = P.sb([128, 8, NB], F32)
    RhT = [Res() for _ in range(8)]
    hn = P.sb([128, 8, NB], BF16)
    Rhn = [Res() for _ in range(8)]
    otm = P.sb([128, 4, 1024], F32)
    Rotm = [Res() for _ in range(4)]
    for blk in range(NBLK):
        t0 = blk * NB
        for k in range(8):
            S.dma("sync", hT[:, k, :], hin[k, :, t0:t0 + NB], writes=[RhT[k]])
        ple.emit(blk, hT, RhT, hn, Rhn, pss, Rpss)
        for s in range(4):
            for kq in range(2):
                pt, Rpt = banks[3 + kq]
                for k4 in range(4):
                    k = kq * 4 + k4
                    S.op("tensor", lambda e, k=k, k4=k4, s=s, pt=pt: e.transpose(out=pt[:, k4 * 128:(k4 + 1) * 128], in_=hT[:, k, s * 128:(s + 1) * 128],
                                                                                 identity=P.ident_f[:]),
                         reads=[RhT[k], P.Rc], writes=[Rpt], join=(k4 > 0))
                eng = "scalar" if kq == 0 else "vector"
                if eng == "scalar":
                    S.op("scalar", lambda e, s=s, kq=kq, pt=pt: e.copy(out=otm[:, s, kq * 512:(kq + 1) * 512], in_=pt[:]),
                         reads=[Rpt], writes=[Rotm[s]], join=(kq > 0))
                else:
                    S.op("vector", lambda e, s=s, kq=kq, pt=pt: e.tensor_copy(out=otm[:, s, kq * 512:(kq + 1) * 512], in_=pt[:]),
                         reads=[Rpt], writes=[Rotm[s]], join=(kq > 0))
            S.dma("sync", out_ap[t0 + s * 128:t0 + (s + 1) * 128, :], otm[:, s, :], reads=[Rotm[s]], writes=[P.Rdram], join=True)
    P.finish()


def phase_mlstm(nc, C, x_ap, hout, W, name, nblk=NBLK):
    P = Phase(nc, name)
    S = P.S
    P.consts(C)
    P.norm_setup()
    tri_f = P.sb([128, 128], F32)
    cbias = P.sb([128, 128], F32)
    ones_f = P.sb([128, 128], F32)
    S.dma("sync", tri_f[:], C["c_tri"], writes=[P.Rc], join=True)
    S.dma("sync", cbias[:], C["c_cbias"], writes=[P.Rc], join=True)
    S.op("vector", lambda e: e.memset(ones_f[:], 1.0), writes=[P.Rc], join=True)
    g, Rg = P.load_vec_fm(W["norm_mix0"])
    bgate, Rbgate = P.load_bcast(W["a_b_gate"], 16)
    mhg, Rmhg = P.load_bcast(W["a_mh_gain"], 128)
    win, Rwin = P.load_w(W["a_w_in"], 8, 3088, col_chunk=1544)
    wout, Rwout = P.load_w(W["a_w_out"], 8, 1024)

    xtm = P.sb([128, 4, 1024], F32); Rxtm = [Res() for _ in range(4)]
    hT = P.sb([128, 8, NB], F32); RhT = [Res() for _ in range(8)]
    hn = P.sb([128, 8, NB], BF16); Rhn = [Res() for _ in range(8)]
    qkT = P.sb([128, 8, NB], BF16); Rqk = [Res() for _ in range(8)]
    ktm = P.sb([128, 4, 512], BF16); Rktm = [Res() for _ in range(4)]
    vtm = P.sb([128, 4, 1024], BF16); Rvtm = [Res() for _ in range(4)]
    og = P.sb([128, 4, 1024], BF16); Rog = [Res() for _ in range(4)]
    sgt = P.sb([128, 512], F32); Rsgt = Res()
    gsb = P.sb([128, 4, 16], F32); Rgsb = Res()
    th = P.sb([128, 4, 16], F32); Rth = Res()
    ef = P.sb([128, 4, 8], F32); Ref = Res()
    spf = P.sb([128, 4, 8], F32); Rspf = Res()
    li = P.sb([128, 4, 8], F32); Rli = Res()
    lf = P.sb([128, 4, 8], F32); Rlf = Res()
    g_sb = P.sb([128, 8], F32); Rg_sb = Res()
    bb = P.sb([128, 8], F32); Rbb = Res()
    eg = P.sb([128, 8], F32); Reg = Res()
    wlp = P.sb([128, 8], F32); Rwlp = Res()
    wl = P.sb([128, 8], F32); Rwl = Res()
    egl = P.sb([128, 4], F32); Regl = Res()
    Gd = P.sb([128, 8, 128], F32); RGd = Res()
    arg = P.sb([128, 8, 128], F32); Rarg = Res()
    DT = P.sb([128, 8, 128], F32); RDT = Res()
    PT = P.sb([128, 8, 128], BF16); RPT = Res()
    kw = P.sb([128, 8, 64], BF16); Rkw = Res()
    numXs = P.sb([128, 8, 128], F32); RnumXs = Res()
    num = P.sb([128, 8, 128], F32); Rnum = Res()
    sqn = P.sb([128, 8, 128], F32); Rsqn = Res()
    sm = {n: (P.sb([128, 8], F32), Res()) for n in ["dxs", "den", "dd", "rec", "ssn", "t1", "t2", "lnt", "rs", "coef"]}
    y0 = P.sb([128, 8, 128], F32); Ry0 = Res()
    ytm = P.sb([128, 1024], BF16); Rytm = Res()
    yT = P.sb([128, 8, NB], BF16); RyT = [Res() for _ in range(4)]
    Cst = P.sb([128, 4, 128], F32); nst = P.sb([128, 4], F32); RC = Res()
    Cbf = P.sb([128, 4, 2, 128], BF16); nbf = P.sb([128, 4, 2], BF16); RCbf = Res()
    qbd = P.sb([128, 4, 2, NB], BF16); Rqbd = [Res() for _ in range(4)]
    nt1 = P.sb([128, 4], F32); Rnt1 = Res()

    pS = P.ps([128, 512], F32)
    RpS = Res()
    Rgcs = Rglast = RdenI = RdenX = Rdn = Rpgate = RpS
    pR = [P.ps([128, 512], F32) for _ in range(2)]; RpR = [Res(), Res()]
    pG = P.ps([128, 1024], F32); RpG = Res()
    pT2 = P.ps([128, 1024], F32); RpT2 = Res()
    pY = P.ps([128, 1024], BF16); RpY = Res()
    rot = [0]

    def nextbank():
        rot[0] ^= 1
        return pR[rot[0]], RpR[rot[0]]

    for t in (Cst, nst):
        S.op("vector", lambda e, t=t: e.memset(t[:], 0.0), writes=[RC], join=True)
    for t in (Cbf, nbf):
        S.op("vector", lambda e, t=t: e.memset(t[:], 0.0), writes=[RCbf], join=True)
    for c in range(4):
        S.op("gpsimd", lambda e, c=c: e.memset(qbd[:, c, :, :], 0.0), writes=[Rqbd[c]])

    for blk in range(nblk):
        t0 = blk * NB
        for s in range(4):
            S.dma("sync", xtm[:, s, :], x_ap[t0 + s * 128:t0 + (s + 1) * 128, :], writes=[Rxtm[s]])
        for k in range(8):
            pb, Rpb = nextbank()
            for s in range(4):
                S.op("tensor", lambda e, k=k, s=s, pb=pb: e.transpose(out=pb[:, s * 128:(s + 1) * 128], in_=xtm[:, s, k * 128:(k + 1) * 128],
                                                                       identity=P.ident_f[:]),
                     reads=[Rxtm[s], P.Rc], writes=[Rpb], join=(s > 0))
            S.op("scalar", lambda e, k=k, pb=pb: e.copy(out=hT[:, k, :], in_=pb[:]), reads=[Rpb], writes=[RhT[k]])
        pb, Rpb = nextbank()
        P.fm_norm(hT, RhT, g, Rg, hn, Rhn, pb, Rpb)
        for c in range(8):
            pb, Rpb = nextbank()
            for k in range(8):
                S.op("tensor", lambda e, k=k, c=c, pb=pb: e.matmul(pb[:], lhsT=win[:, k, c * 128:(c + 1) * 128], rhs=hn[:, k, :],
                                                                    start=(k == 0), stop=(k == 7)),
                     reads=[Rwin[k], Rhn[k]], writes=[Rpb], join=(k > 0))
            sc = 0.125 if c < 4 else 1.0
            S.op("scalar", lambda e, c=c, pb=pb, sc=sc: e.activation(out=qkT[:, c, :], in_=pb[:], func=AF.Copy, scale=sc),
                 reads=[Rpb], writes=[Rqk[c]])
            if c < 4:
                S.op("gpsimd", lambda e, c=c: e.tensor_copy(out=qbd[0:64, c, 0, :], in_=qkT[0:64, c, :]), reads=[Rqk[c]], writes=[Rqbd[c]])
                S.op("gpsimd", lambda e, c=c: e.tensor_copy(out=qbd[64:128, c, 1, :], in_=qkT[64:128, c, :]), reads=[Rqk[c]], writes=[Rqbd[c]], join=True)
        for s in range(4):
            ts = slice(s * 128, (s + 1) * 128)
            pb, Rpb = nextbank()
            for k in range(8):
                S.op("tensor", lambda e, k=k, ts=ts, pb=pb: e.matmul(pb[:], lhsT=hn[:, k, ts], rhs=win[:, k, 512:1024], start=(k == 0), stop=(k == 7)),
                     reads=[Rwin[k], Rhn[k]], writes=[Rpb], join=(k > 0))
            S.op("scalar", lambda e, s=s, pb=pb: e.copy(out=ktm[:, s, :], in_=pb[:]), reads=[Rpb], writes=[Rktm[s]])
            for half in range(2):
                pb, Rpb = nextbank()
                c0 = 1024 + half * 512
                for k in range(8):
                    S.op("tensor", lambda e, k=k, ts=ts, pb=pb, c0=c0: e.matmul(pb[:], lhsT=hn[:, k, ts], rhs=win[:, k, c0:c0 + 512],
                                                                                 start=(k == 0), stop=(k == 7)),
                         reads=[Rwin[k], Rhn[k]], writes=[Rpb], join=(k > 0))
                S.op("vector", lambda e, s=s, half=half, pb=pb: e.tensor_copy(out=vtm[:, s, half * 512:(half + 1) * 512], in_=pb[:]),
                     reads=[Rpb], writes=[Rvtm[s]], join=(half > 0))
            for half in range(2):
                pb, Rpb = nextbank()
                c0 = 2048 + half * 512
                for k in range(8):
                    S.op("tensor", lambda e, k=k, ts=ts, pb=pb, c0=c0: e.matmul(pb[:], lhsT=hn[:, k, ts], rhs=win[:, k, c0:c0 + 512],
                                                                                 start=(k == 0), stop=(k == 7)),
                         reads=[Rwin[k], Rhn[k]], writes=[Rpb], join=(k > 0))
                S.op("scalar", lambda e, pb=pb: e.activation(out=sgt[:], in_=pb[:], func=AF.Sigmoid), reads=[Rpb], writes=[Rsgt])
                S.op("gpsimd", lambda e, s=s, half=half: e.tensor_tensor(
                    out=og[:, s, half * 512:(half + 1) * 512].rearrange("p (h v) -> p h v", h=4),
                    in0=sgt[:].rearrange("p (h v) -> p h v", h=4),
                    in1=mhg[:].unsqueeze(1).broadcast_to([128, 4, 128]), op=ALU.mult),
                     reads=[Rsgt, Rmhg], writes=[Rog[s]], join=(half > 0))
            for k in range(8):
                S.op("tensor", lambda e, k=k, ts=ts, s=s: e.matmul(pS[:, 64 + s * 16:64 + (s + 1) * 16], lhsT=hn[:, k, ts], rhs=win[:, k, 3072:3088],
                                                                    start=(k == 0), stop=(k == 7)),
                     reads=[Rwin[k], Rhn[k]], writes=[Rpgate], join=(k > 0))
            S.op("vector", lambda e, s=s: e.tensor_tensor(out=gsb[:, s, :], in0=pS[:, 64 + s * 16:64 + (s + 1) * 16], in1=bgate[:], op=ALU.add),
                 reads=[Rpgate, Rbgate], writes=[Rgsb], join=(s > 0))
        S.op("scalar", lambda e: e.activation(out=th[:], in_=gsb[:], func=AF.Tanh, scale=1.0 / 15.0), reads=[Rgsb], writes=[Rth])
        S.op("vector", lambda e: e.tensor_scalar(out=li[:], in0=th[:, :, 0:8], scalar1=15.0, scalar2=None, op0=ALU.mult), reads=[Rth], writes=[Rli])
        S.op("scalar", lambda e: e.activation(out=ef[:], in_=th[:, :, 8:16], func=AF.Exp, scale=-15.0), reads=[Rth], writes=[Ref])
        S.op("scalar", lambda e: e.activation(out=spf[:], in_=ef[:], func=AF.Ln, bias=P.one_t[:, 0:1]), reads=[Ref, P.Rc], writes=[Rspf])
        S.op("vector", lambda e: e.tensor_scalar(out=lf[:], in0=spf[:], scalar1=-1.0, scalar2=None, op0=ALU.mult), reads=[Rspf], writes=[Rlf])

        for s in range(4):
            ts = slice(s * 128, (s + 1) * 128)
            S.op("tensor", lambda e, s=s: e.matmul(pS[:, 0:8], lhsT=tri_f[:], rhs=lf[:, s, :], start=True, stop=True),
                 reads=[Rlf, P.Rc], writes=[Rgcs])
            S.op("tensor", lambda e, s=s: e.matmul(pS[:, 8:16], lhsT=ones_f[:], rhs=lf[:, s, :], start=True, stop=True),
                 reads=[Rlf, P.Rc], writes=[Rglast])
            S.op("vector", lambda e: e.tensor_copy(out=g_sb[:], in_=pS[:, 0:8]), reads=[Rgcs], writes=[Rg_sb])
            S.op("vector", lambda e, s=s: e.tensor_tensor(out=bb[:], in0=li[:, s, :], in1=g_sb[:], op=ALU.subtract), reads=[Rli, Rg_sb], writes=[Rbb])
            S.op("scalar", lambda e: e.activation(out=eg[:], in_=g_sb[:], func=AF.Exp), reads=[Rg_sb], writes=[Reg])
            S.op("vector", lambda e: e.tensor_tensor(out=wlp[:], in0=pS[:, 8:16], in1=bb[:], op=ALU.add), reads=[Rglast, Rbb], writes=[Rwlp])
            S.op("scalar", lambda e: e.activation(out=wl[:], in_=wlp[:], func=AF.Exp), reads=[Rwlp], writes=[Rwl])
            S.op("scalar", lambda e: e.activation(out=egl[0:64, :], in_=pS[0:64, 8:16:2], func=AF.Exp), reads=[Rglast], writes=[Regl])
            S.op("scalar", lambda e: e.activation(out=egl[64:128, :], in_=pS[64:128, 9:16:2], func=AF.Exp), reads=[Rglast], writes=[Regl], join=True)
            S.op("vector", lambda e: e.tensor_tensor(out=Gd[:], in0=g_sb[:].unsqueeze(2).broadcast_to([128, 8, 128]),
                                                      in1=P.ident_f[:].unsqueeze(1).broadcast_to([128, 8, 128]), op=ALU.mult),
                 reads=[Rg_sb, P.Rc], writes=[RGd])
            for half in range(2):
                S.op("tensor", lambda e, half=half: e.matmul(pG[:, half * 512:(half + 1) * 512], lhsT=ones_f[:],
                                                               rhs=Gd[:, half * 4:(half + 1) * 4, :].rearrange("p h j -> p (h j)"),
                                                               start=True, stop=True),
                     reads=[RGd, P.Rc], writes=[RpG], join=(half > 0))
            for h in range(8):
                S.op("vector", lambda e, h=h: e.scalar_tensor_tensor(out=arg[:, h, :], in0=pG[:, h * 128:(h + 1) * 128], scalar=bb[:, h:h + 1], op0=ALU.add,
                                                                      in1=cbias[:], op1=ALU.add),
                     reads=[RpG, Rbb, P.Rc], writes=[Rarg], join=(h > 0))
            S.op("scalar", lambda e: e.activation(out=DT[:], in_=arg[:], func=AF.Exp), reads=[Rarg], writes=[RDT])
            for c in range(4):
                S.op("tensor", lambda e, c=c, ts=ts: e.matmul(pT2[:, c * 256:(c + 1) * 256], lhsT=qkT[:, 4 + c, ts], rhs=qbd[:, c, :, ts],
                                                               start=True, stop=True),
                     reads=[Rqbd[c], Rqk[4 + c]], writes=[RpT2], join=(c > 0))
            S.op("vector", lambda e: e.tensor_tensor(out=PT[:].rearrange("p h j -> p (h j)"), in0=pT2[:], in1=DT[:].rearrange("p h j -> p (h j)"), op=ALU.mult),
                 reads=[RpT2, RDT], writes=[RPT])
            for h in range(8):
                S.op("tensor", lambda e, h=h, s=s: e.matmul(pG[:, h * 128:(h + 1) * 128], lhsT=PT[:, h, :], rhs=vtm[:, s, h * 128:(h + 1) * 128],
                                                             start=True, stop=True),
                     reads=[RPT, Rvtm[s]], writes=[RpG], join=(h > 0))
            for h in range(8):
                S.op("tensor", lambda e, h=h: e.matmul(pS[:, 16 + h:17 + h], lhsT=PT[:, h, :], rhs=P.ones_b[:, 0:1], start=True, stop=True),
                     reads=[RPT, P.Rc], writes=[RdenI], join=(h > 0))
            for c in range(4):
                S.op("tensor", lambda e, c=c, ts=ts: e.matmul(pT2[:, c * 256:(c + 1) * 256], lhsT=qkT[:, c, ts], rhs=Cbf[:, c, :, :],
                                                               start=True, stop=True),
                     reads=[Rqk[c], RCbf], writes=[RpT2], join=(c > 0))
            for c in range(4):
                S.op("tensor", lambda e, c=c, ts=ts: e.matmul(pS[:, 24 + 2 * c:26 + 2 * c], lhsT=qkT[:, c, ts], rhs=nbf[:, c, :],
                                                               start=True, stop=True),
                     reads=[Rqk[c], RCbf], writes=[RdenX], join=(c > 0))
            S.op("vector", lambda e, s=s: e.tensor_tensor(out=kw[:], in0=ktm[:, s, :].rearrange("p (h d) -> p h d", h=8),
                                                           in1=wl[:].unsqueeze(2).broadcast_to([128, 8, 64]), op=ALU.mult),
                 reads=[Rktm[s], Rwl], writes=[Rkw])
            pd, Rpd = nextbank()
            for h in range(8):
                c, ph = h // 2, h % 2
                prt = slice(ph * 64, (ph + 1) * 64)
                S.op("tensor", lambda e, h=h, c=c, prt=prt, s=s, pd=pd: e.matmul(pd[prt, c * 128:(c + 1) * 128], lhsT=kw[:, h, :], rhs=vtm[:, s, h * 128:(h + 1) * 128],
                                                                                  start=True, stop=True),
                     reads=[Rkw, Rvtm[s]], writes=[Rpd], join=(h > 0))
            for h in range(8):
                c, ph = h // 2, h % 2
                prt = slice(ph * 64, (ph + 1) * 64)
                S.op("tensor", lambda e, h=h, c=c, prt=prt: e.matmul(pS[prt, 32 + c:33 + c], lhsT=kw[:, h, :], rhs=P.ones_b[:, 0:1], start=True, stop=True),
                     reads=[Rkw, P.Rc], writes=[Rdn], join=(h > 0))
            for c in range(4):
                S.op("vector", lambda e, c=c, pd=pd: e.scalar_tensor_tensor(out=Cst[:, c, :], in0=Cst[:, c, :], scalar=egl[:, c:c + 1], op0=ALU.mult,
                                                                            in1=pd[:, c * 128:(c + 1) * 128], op1=ALU.add),
                     reads=[Rpd, Regl, RC], writes=[RC])
            S.op("vector", lambda e: e.tensor_tensor(out=nt1[:], in0=nst[:], in1=egl[:], op=ALU.mult), reads=[RC, Regl], writes=[Rnt1])
            S.op("vector", lambda e: e.tensor_tensor(out=nst[:], in0=pS[:, 32:36], in1=nt1[:], op=ALU.add), reads=[Rdn, Rnt1], writes=[RC])
            S.op("vector", lambda e: e.tensor_tensor(out=numXs[:], in0=pT2[:].rearrange("p (h v) -> p h v", h=8),
                                                      in1=eg[:].unsqueeze(2).broadcast_to([128, 8, 128]), op=ALU.mult),
                 reads=[RpT2, Reg], writes=[RnumXs])
            S.op("vector", lambda e: e.tensor_tensor(out=num[:].rearrange("p h v -> p (h v)"), in0=pG[:], in1=numXs[:].rearrange("p h v -> p (h v)"), op=ALU.add),
                 reads=[RpG, RnumXs], writes=[Rnum])
            S.op("gpsimd", lambda e: e.tensor_copy(out=Cbf[0:64, :, 0, :], in_=Cst[0:64, :, :]), reads=[RC], writes=[RCbf])
            S.op("gpsimd", lambda e: e.tensor_copy(out=Cbf[64:128, :, 1, :], in_=Cst[64:128, :, :]), reads=[RC], writes=[RCbf], join=True)
            S.op("gpsimd", lambda e: e.tensor_copy(out=nbf[0:64, :, 0], in_=nst[0:64, :]), reads=[RC], writes=[RCbf], join=True)
            S.op("gpsimd", lambda e: e.tensor_copy(out=nbf[64:128, :, 1], in_=nst[64:128, :]), reads=[RC], writes=[RCbf], join=True)
            T = lambda n: sm[n][0]
            R_ = lambda n: sm[n][1]
            S.op("vector", lambda e: e.tensor_tensor(out=T("dxs")[:], in0=pS[:, 24:32], in1=eg[:], op=ALU.mult), reads=[RdenX, Reg], writes=[R_("dxs")])
            S.op("vector", lambda e: e.tensor_tensor(out=T("den")[:], in0=pS[:, 16:24], in1=T("dxs")[:], op=ALU.add), reads=[RdenI, R_("dxs")], writes=[R_("den")])
            S.op("vector", lambda e: e.scalar_tensor_tensor(out=T("t1")[:], in0=T("den")[:], scalar=-1.0, op0=ALU.mult, in1=T("den")[:], op1=ALU.max),
                 reads=[R_("den")], writes=[R_("t1")])
            S.op("vector", lambda e: e.tensor_scalar(out=T("dd")[:], in0=T("t1")[:], scalar1=1.0, scalar2=None, op0=ALU.max), reads=[R_("t1")], writes=[R_("dd")])
            S.op("vector", lambda e: e.reciprocal(out=T("rec")[:], in_=T("dd")[:]), reads=[R_("dd")], writes=[R_("rec")])
            S.op("gpsimd", lambda e: e.tensor_tensor(out=sqn[:], in0=num[:], in1=num[:], op=ALU.mult), reads=[Rnum], writes=[Rsqn])
            S.op("vector", lambda e: e.tensor_reduce(out=T("ssn")[:], in_=sqn[:], axis=AX.X, op=ALU.add), reads=[Rsqn], writes=[R_("ssn")])
            S.op("vector", lambda e: e.tensor_tensor(out=T("t1")[:], in0=T("rec")[:], in1=T("rec")[:], op=ALU.mult), reads=[R_("rec")], writes=[R_("t1")])
            S.op("vector", lambda e: e.tensor_tensor(out=T("t2")[:], in0=T("t1")[:], in1=T("ssn")[:], op=ALU.mult), reads=[R_("t1"), R_("ssn")], writes=[R_("t2")])
            S.op("scalar", lambda e: e.activation(out=T("lnt")[:], in_=T("t2")[:], func=AF.Ln, scale=1.0 / 128.0, bias=P.eps_t[:, 0:1]),
                 reads=[R_("t2"), P.Rc], writes=[R_("lnt")])
            S.op("scalar", lambda e: e.activation(out=T("rs")[:], in_=T("lnt")[:], func=AF.Exp, scale=-0.5), reads=[R_("lnt")], writes=[R_("rs")])
            S.op("vector", lambda e: e.tensor_tensor(out=T("coef")[:], in0=T("rec")[:], in1=T("rs")[:], op=ALU.mult), reads=[R_("rec"), R_("rs")], writes=[R_("coef")])
            S.op("vector", lambda e: e.tensor_tensor(out=y0[:], in0=num[:], in1=T("coef")[:].unsqueeze(2).broadcast_to([128, 8, 128]), op=ALU.mult),
                 reads=[Rnum, R_("coef")], writes=[Ry0])
            S.op("gpsimd", lambda e, s=s: e.tensor_tensor(out=ytm[:], in0=y0[:].rearrange("p h v -> p (h v)"), in1=og[:, s, :], op=ALU.mult),
                 reads=[Ry0, Rog[s]], writes=[Rytm])
            for h in range(8):
                S.op("tensor", lambda e, h=h: e.transpose(out=pY[:, h * 128:(h + 1) * 128], in_=ytm[:, h * 128:(h + 1) * 128], identity=P.ident_b[:]),
                     reads=[Rytm, P.Rc], writes=[RpY], join=(h > 0))
            S.op("scalar", lambda e, ts=ts: e.copy(out=yT[:, :, ts], in_=pY[:].rearrange("p (h j) -> p h j", h=8)), reads=[RpY], writes=[RyT[s]])
        for d in range(8):
            pb, Rpb = nextbank()
            for h in range(8):
                S.op("tensor", lambda e, h=h, d=d, pb=pb: e.matmul(pb[:], lhsT=wout[:, h, d * 128:(d + 1) * 128], rhs=yT[:, h, :], start=(h == 0), stop=(h == 7)),
                     reads=[Rwout[h]] + RyT, writes=[Rpb], join=(h > 0))
            S.op("vector", lambda e, d=d, pb=pb: e.tensor_tensor(out=hT[:, d, :], in0=pb[:], in1=hT[:, d, :], op=ALU.add), reads=[Rpb, RhT[d]], writes=[RhT[d]])
            S.dma("sync", hout[d, :, t0:t0 + NB], hT[:, d, :], reads=[RhT[d]], writes=[P.Rdram], join=True)
    P.finish()


def phase_moba(nc, C, hin, hout, W, name, nblk=NBLK, dbg=None):
    G = 2
    P = Phase(nc, name)
    S = P.S
    P.consts(C)
    P.norm_setup()
    c256 = P.sb([128, 1], F32)
    S.op("vector", lambda e: e.memset(c256[:], 1.0 / 256.0), writes=[P.Rc], join=True)
    tri4 = P.sb([128, 512], BF16)
    S.dma("gpsimd", tri4[:], C["c_tribias4"], writes=[P.Rc], join=True)
    ropet = P.sb([128, 32, 16], F32)
    S.dma("sync", ropet[:], C["c_rope"].rearrange("(i p) c -> p i c", p=128), writes=[P.Rc], join=True)
    pastb, Rpastb = P.load_bcast(C["c_past"], 256)
    ownb, Rownb = P.load_bcast(C["c_own"], 256)
    g_kv, Rg_kv = P.load_vec_fm(W["kv_norm"])
    g_mix, Rg_mix = P.load_vec_fm(W["norm_mix1"])
    knorm, Rknorm = P.load_bcast(W["k_norm"], 64)
    qnorm, Rqnorm = P.load_bcast(W["b_q_norm"], 64)
    wkv, Rwkv = P.load_w(W["w_kv"], 8, 512)
    wq, Rwq = P.load_w(W["b_w_q"], 8, 1024)
    wo, Rwo = P.load_w(W["b_w_o"], 8, 1024)

    pR = [P.ps([128, 512], F32) for _ in range(2)]; RpR = [Res(), Res()]
    psc = [P.ps([128, G * 512], F32) for _ in range(2)]; Rpsc = [Res(), Res()]
    pop = [P.ps([128, 512], F32) for _ in range(2)]; Rpop = [Res(), Res()]
    rot = [0]

    def nextbank():
        rot[0] ^= 1
        return pR[rot[0]], RpR[rot[0]]

    ple = PLE(P, C, W["p0"], W["norm_ple0"], W["w_ple_gate0"], W["w_ple_up0"], [(pR[0], RpR[0]), (pR[1], RpR[1]), (pR[0], RpR[0])])

    hT = P.sb([128, 8, NB], F32); RhT = [Res() for _ in range(8)]
    hn = P.sb([128, 8, NB], BF16); Rhn = [Res() for _ in range(8)]
    KT = P.sb([80, 4, NT], BF16); RKT = [Res() for _ in range(32)]
    Vaug = P.sb([128, 4, 32, 128], BF16); RV = [Res() for _ in range(32)]
    kmT = P.sb([80, 4, 16], BF16); RkmT = Res()
    kms = P.sb([64, 4, 2], F32); Rkms = Res()
    ksb = P.sb([128, 4, 64], F32); Rksb = Res()
    sqk = P.sb([128, 4, 64], F32); Rsqk = Res()
    kbf = P.sb([128, 4, 64], BF16); Rkbf = Res()
    qsb = P.sb([128, 16, 64], F32); Rqsb = Res()
    sqq = P.sb([128, 16, 64], F32); Rsqq = Res()
    qa = P.sb([128, 16, 80], BF16); Rqa = Res()
    QTa = [P.sb([80, 16, 128], BF16) for _ in range(2)]; RQTa = [Res(), Res()]
    gm = P.sb([128, 16, 16], F32); Rgm = Res()
    mx8 = P.sb([128, 16, 8], F32); Rmx8 = Res()
    vis = P.sb([128, 16, 16], F32); Rvis = Res()
    skq = {n: (P.sb([128, 16], F32), Res()) for n in ["ss", "ln", "r"]}
    skk = {n: (P.sb([128, 16], F32), Res()) for n in ["ss", "ln", "r"]}
    rtq = {n: (P.sb([128, 16, 8], F32), Res()) for n in ["t1", "t2", "t3", "t4"]}
    PTb = [P.sb([128, G * 512], BF16) for _ in range(2)]; RPTb = [Res() for _ in range(2)]
    rec = P.sb([128, 512], F32); Rrec = Res()
    OTb = P.sb([128, 8, NB], BF16); ROTb = [Res() for _ in range(4)]

    for kvh in range(4):
        for c0 in range(0, NT, 1024):
            S.dma("gpsimd", KT[64:80, kvh, c0:c0 + 1024], C["c_onehot"][:, c0:c0 + 1024], writes=[P.Rc], join=True)
    S.op("vector", lambda e: e.memset(Vaug[:].rearrange("p a b c -> p (a b c)"), 1.0), writes=[P.Rc], join=True)
    S.op("gpsimd", lambda e: e.memset(kmT[:], 0.0), writes=[RkmT])
    for i in range(2):
        S.op("gpsimd", lambda e, i=i: e.memset(QTa[i][:], 0.0), writes=[RQTa[i]])

    def head_norm_rope(x, Rx, sq_, Rsq_, nh, gbc, Rgbc, it, sk, rt):
        (ss, Rss), (ln, Rln), (r, Rr) = sk["ss"], sk["ln"], sk["r"]
        S.op("gpsimd", lambda e: e.tensor_tensor(out=sq_[:], in0=x[:], in1=x[:], op=ALU.mult), reads=[Rx], writes=[Rsq_])
        S.op("vector", lambda e: e.tensor_reduce(out=ss[:, 0:nh], in_=sq_[:], axis=AX.X, op=ALU.add), reads=[Rsq_], writes=[Rss])
        yield
        S.op("scalar", lambda e: e.activation(out=ln[:, 0:nh], in_=ss[:, 0:nh], func=AF.Ln, scale=1.0 / 64.0, bias=P.eps_t[:, 0:1]),
             reads=[Rss, P.Rc], writes=[Rln])
        S.op("scalar", lambda e: e.activation(out=r[:, 0:nh], in_=ln[:, 0:nh], func=AF.Exp, scale=-0.5), reads=[Rln], writes=[Rr])
        yield
        S.op("vector", lambda e: e.tensor_tensor(out=x[:], in0=x[:], in1=r[:, 0:nh].unsqueeze(2).broadcast_to([128, nh, 64]), op=ALU.mult),
             reads=[Rx, Rr], writes=[Rx])
        S.op("vector", lambda e: e.tensor_tensor(out=x[:], in0=x[:], in1=gbc[:].unsqueeze(1).broadcast_to([128, nh, 64]), op=ALU.mult),
             reads=[Rx, Rgbc], writes=[Rx])
        yield
        cs = ropet[:, it, 0:8].unsqueeze(1).broadcast_to([128, nh, 8])
        sn = ropet[:, it, 8:16].unsqueeze(1).broadcast_to([128, nh, 8])
        x1 = x[:, :, 0:8]
        x2 = x[:, :, 8:16]
        tt = {k: v[0][:, 0:nh, :] for k, v in rt.items()}
        Rt = {k: v[1] for k, v in rt.items()}
        S.op("vector", lambda e: e.tensor_tensor(out=tt["t1"], in0=x1, in1=cs, op=ALU.mult), reads=[Rx, P.Rc], writes=[Rt["t1"]])
        S.op("vector", lambda e: e.tensor_tensor(out=tt["t2"], in0=x2, in1=sn, op=ALU.mult), reads=[Rx, P.Rc], writes=[Rt["t2"]])
        S.op("vector", lambda e: e.tensor_tensor(out=tt["t3"], in0=x2, in1=cs, op=ALU.mult), reads=[Rx, P.Rc], writes=[Rt["t3"]])
        S.op("vector", lambda e: e.tensor_tensor(out=tt["t4"], in0=x1, in1=sn, op=ALU.mult), reads=[Rx, P.Rc], writes=[Rt["t4"]])
        yield
        S.op("vector", lambda e: e.tensor_tensor(out=x1, in0=tt["t1"], in1=tt["t2"], op=ALU.subtract), reads=[Rt["t1"], Rt["t2"], Rx], writes=[Rx])
        S.op("vector", lambda e: e.tensor_tensor(out=x2, in0=tt["t3"], in1=tt["t4"], op=ALU.add), reads=[Rt["t3"], Rt["t4"], Rx], writes=[Rx])
        yield

    def kv_path(blk, s):
        it = blk * 4 + s
        ts = slice(s * 128, (s + 1) * 128)
        pb, Rpb = nextbank()
        for k in range(8):
            S.op("tensor", lambda e, k=k, ts=ts, pb=pb: e.matmul(pb[:], lhsT=hn[:, k, ts], rhs=wkv[:, k, :], start=(k == 0), stop=(k == 7)),
                 reads=[Rwkv[k], Rhn[k]], writes=[Rpb], join=(k > 0))
        S.op("scalar", lambda e, pb=pb: e.copy(out=ksb[:].rearrange("p h d -> p (h d)"), in_=pb[:, 0:256]), reads=[Rpb], writes=[Rksb])
        S.op("scalar", lambda e, pb=pb, it=it: e.copy(out=Vaug[:, :, it, 0:64], in_=pb[:, 256:512].rearrange("p (h d) -> p h d", h=4)),
             reads=[Rpb, P.Rc], writes=[RV[it]])
        for _ in head_norm_rope(ksb, Rksb, sqk, Rsqk, 4, knorm, Rknorm, it, skk, rtq):
            pass
        S.op("gpsimd", lambda e: e.tensor_copy(out=kbf[:], in_=ksb[:]), reads=[Rksb], writes=[Rkbf])
        pb, Rpb = nextbank()
        pbb = pb[:].bitcast(BF16)
        for kvh in range(4):
            S.op("tensor", lambda e, kvh=kvh, pbb=pbb: e.transpose(out=pbb[0:64, kvh * 128:(kvh + 1) * 128], in_=kbf[:, kvh, :], identity=P.ident_b[:]),
                 reads=[Rkbf, P.Rc], writes=[Rpb], join=(kvh > 0))
        S.op("scalar", lambda e, it=it, pbb=pbb: e.copy(out=KT[0:64, :, it * 128:(it + 1) * 128], in_=pbb[0:64, 0:512].rearrange("p (h t) -> p h t", h=4)),
             reads=[Rpb, P.Rc], writes=[RKT[it]])
        pb, Rpb = nextbank()
        for kvh in range(4):
            S.op("tensor", lambda e, kvh=kvh, pb=pb: e.matmul(pb[0:64, kvh:kvh + 1], lhsT=ksb[:, kvh, :], rhs=c256[:, 0:1], start=True, stop=True),
                 reads=[Rksb, P.Rc], writes=[Rpb], join=(kvh > 0))
        S.op("vector", lambda e, s=s, pb=pb: e.tensor_copy(out=kms[:, :, s % 2], in_=pb[0:64, 0:4]), reads=[Rpb], writes=[Rkms], join=(s % 2 == 1))
        if s % 2 == 1:
            n = it // 2
            S.op("vector", lambda e, n=n: e.tensor_tensor(out=kmT[0:64, :, n], in0=kms[:, :, 0], in1=kms[:, :, 1], op=ALU.add),
                 reads=[Rkms], writes=[RkmT])

    def q_path(blk, s):
        it = blk * 4 + s
        b = it // 2
        ts = slice(s * 128, (s + 1) * 128)
        QT, RQT = QTa[it % 2], RQTa[it % 2]
        for half in range(2):
            pb, Rpb = nextbank()
            for k in range(8):
                S.op("tensor", lambda e, k=k, ts=ts, pb=pb, half=half: e.matmul(pb[:], lhsT=hn[:, k, ts], rhs=wq[:, k, half * 512:(half + 1) * 512],
                                                                                 start=(k == 0), stop=(k == 7)),
                     reads=[Rwq[k], Rhn[k]], writes=[Rpb], join=(k > 0))
                if k % 4 == 3:
                    yield
            S.op("scalar", lambda e, pb=pb, half=half: e.copy(out=qsb[:, half * 8:(half + 1) * 8, :].rearrange("p h d -> p (h d)"), in_=pb[:]),
                 reads=[Rpb], writes=[Rqsb], join=(half > 0))
            yield
        for _ in head_norm_rope(qsb, Rqsb, sqq, Rsqq, 16, qnorm, Rqnorm, it, skq, rtq):
            yield
        S.op("gpsimd", lambda e: e.tensor_copy(out=qa[:, :, 0:64], in_=qsb[:]), reads=[Rqsb], writes=[Rqa])
        yield
        for r_ in range(2):
            pb, Rpb = nextbank()
            pbb = pb[:].bitcast(BF16)
            for j in range(8):
                h = r_ * 8 + j
                S.op("tensor", lambda e, h=h, j=j, pbb=pbb: e.transpose(out=pbb[0:64, j * 128:(j + 1) * 128], in_=qa[:, h, 0:64], identity=P.ident_b[:]),
                     reads=[Rqa, P.Rc], writes=[Rpb], join=(j > 0))
                if j % 4 == 3:
                    yield
            S.op("scalar", lambda e, r_=r_, pbb=pbb: e.copy(out=QT[0:64, r_ * 8:(r_ + 1) * 8, :], in_=pbb[0:64, :].rearrange("p (h t) -> p h t", h=8)),
                 reads=[Rpb], writes=[RQT], join=(r_ > 0))
            yield
        pb, Rpb = nextbank()
        for h in range(16):
            S.op("tensor", lambda e, h=h, pb=pb: e.matmul(pb[:, h * 16:(h + 1) * 16], lhsT=QT[:, h, :], rhs=kmT[:, h // 4, :], start=True, stop=True),
                 reads=[RQT, RkmT], writes=[Rpb], join=(h > 0))
            if h % 4 == 3:
                yield
        S.op("vector", lambda e, b=b, pb=pb: e.tensor_tensor(out=gm[:], in0=pb[:, 0:256].rearrange("p (h n) -> p h n", h=16),
                                                              in1=pastb[:, b * 16:(b + 1) * 16].unsqueeze(1).broadcast_to([128, 16, 16]), op=ALU.add),
             reads=[Rpb, Rpastb], writes=[Rgm])
        yield
        for h in range(16):
            S.op("vector", lambda e, h=h: e.max(out=mx8[:, h, :], in_=gm[:, h, :]), reads=[Rgm], writes=[Rmx8], join=(h > 0))
            if h % 4 == 3:
                yield
        S.op("vector", lambda e: e.tensor_tensor(out=vis[:], in0=gm[:], in1=mx8[:, :, 2:3].broadcast_to([128, 16, 16]), op=ALU.is_ge),
             reads=[Rgm, Rmx8], writes=[Rvis])
        S.op("vector", lambda e, b=b: e.tensor_tensor(out=vis[:], in0=vis[:], in1=ownb[:, b * 16:(b + 1) * 16].unsqueeze(1).broadcast_to([128, 16, 16]), op=ALU.max),
             reads=[Rvis, Rownb], writes=[Rvis])
        S.op("vector", lambda e: e.tensor_scalar(out=qa[:, :, 64:80], in0=vis[:], scalar1=-NEG, scalar2=NEG, op0=ALU.mult, op1=ALU.add),
             reads=[Rvis, Rqa], writes=[Rqa])
        yield
        for r_ in range(2):
            pb, Rpb = nextbank()
            pbb = pb[:].bitcast(BF16)
            for j in range(8):
                h = r_ * 8 + j
                S.op("tensor", lambda e, h=h, j=j, pbb=pbb: e.transpose(out=pbb[0:80, j * 128:(j + 1) * 128], in_=qa[:, h, :], identity=P.ident_b[:]),
                     reads=[Rqa, P.Rc], writes=[Rpb], join=(j > 0))
                if j % 4 == 3:
                    yield
            S.op("scalar", lambda e, r_=r_, pbb=pbb: e.copy(out=QT[:, r_ * 8:(r_ + 1) * 8, :], in_=pbb[0:80, :].rearrange("p (h t) -> p h t", h=8)),
                 reads=[Rpb], writes=[RQT], join=(r_ > 0))
            yield

    nsc = [0]

    def attention(blk, s):
        it = blk * 4 + s
        ts = slice(s * 128, (s + 1) * 128)
        QT, RQT = QTa[it % 2], RQTa[it % 2]
        L = []
        for kvh in range(4):
            for j0 in range(0, it + 1, G):
                L.append((kvh, j0, min(it + 1, j0 + G)))
        bufs = {}

        def qk(n):
            kvh, j0, j1 = L[n]
            m = nsc[0]
            nsc[0] += 1
            bufs[n] = m % 2
            ps_, Rps_ = psc[m % 2], Rpsc[m % 2]
            qg = QT[:, 4 * kvh:4 * kvh + 4, :].rearrange("p h t -> p (h t)")
            for j in range(j0, j1):
                o = (j - j0) * 512
                diag = (j == it)
                S.op("tensor", lambda e, kvh=kvh, j=j, ps_=ps_, qg=qg, diag=diag, o=o: e.matmul(ps_[:, o:o + 512], lhsT=KT[:, kvh, j * 128:(j + 1) * 128], rhs=qg,
                                                                                               start=True, stop=(not diag)),
                     reads=[RKT[j], RQT, P.Rc], writes=[Rps_], join=(j > j0))
                if diag:
                    S.op("tensor", lambda e, ps_=ps_, o=o: e.matmul(ps_[:, o:o + 512], lhsT=P.ident_b[:], rhs=tri4[:], start=False, stop=True),
                         reads=[P.Rc], writes=[Rps_], join=True)

        def ex(n):
            kvh, j0, j1 = L[n]
            bi = bufs[n]
            w = (j1 - j0) * 512
            S.op("scalar", lambda e, bi=bi, w=w: e.activation(out=PTb[bi][:, 0:w], in_=psc[bi][:, 0:w], func=AF.Exp, scale=0.125),
                 reads=[Rpsc[bi]], writes=[RPTb[bi]])

        def pv(n):
            kvh, j0, j1 = L[n]
            bi = bufs[n]
            po, Rpo = pop[kvh % 2], Rpop[kvh % 2]
            for j in range(j0, j1):
                o = (j - j0) * 512
                S.op("tensor", lambda e, kvh=kvh, j=j, bi=bi, po=po, o=o, it=it: e.matmul(po[:], lhsT=Vaug[:, kvh, j, :], rhs=PTb[bi][:, o:o + 512],
                                                                                         start=(j == 0), stop=(j == it)),
                     reads=[RV[j], RPTb[bi], P.Rc], writes=[Rpo], join=(j > 0))

        def epi(kvh):
            po, Rpo = pop[kvh % 2], Rpop[kvh % 2]
            S.op("vector", lambda e, po=po: e.reciprocal(out=rec[64:128, :], in_=po[64:128, :]), reads=[Rpo], writes=[Rrec])
            for gq in range(4):
                hh = 4 * kvh + gq
                pr, ph = hh // 2, hh % 2
                S.op("vector", lambda e, po=po, gq=gq, pr=pr, ph=ph, ts=ts: e.tensor_tensor(
                    out=OTb[ph * 64:(ph + 1) * 64, pr, ts], in0=po[0:64, gq * 128:(gq + 1) * 128], in1=rec[64:128, gq * 128:(gq + 1) * 128], op=ALU.mult),
                     reads=[Rpo, Rrec], writes=[ROTb[s]], join=True)

        qk(0)
        for n in range(len(L)):
            if n + 1 < len(L):
                qk(n + 1)
            ex(n)
            pv(n)
            if n + 1 == len(L) or L[n + 1][0] != L[n][0]:
                epi(L[n][0])
            yield

    def drive(main, side, side_len):
        steps = list(range(0))
        mains = main
        if side is None:
            for _ in mains:
                pass
            return
        done = [False]

        def adv(k):
            for _ in range(k):
                if done[0]:
                    return
                try:
                    next(side)
                except StopIteration:
                    done[0] = True
        nmain = side_len[0]
        per = max(1, -(-side_len[1] // max(1, nmain)))
        for _ in mains:
            adv(per)
        while not done[0]:
            adv(8)

    for blk in range(nblk):
        t0 = blk * NB
        for k in range(8):
            S.dma("sync", hT[:, k, :], hin[k, :, t0:t0 + NB], writes=[RhT[k]])
        pb, Rpb = nextbank()
        ple.emit(blk, hT, RhT, hn, Rhn, pb, Rpb)
        pb, Rpb = nextbank()
        P.fm_norm(hT, RhT, g_kv, Rg_kv, hn, Rhn, pb, Rpb)
        for s in range(4):
            kv_path(blk, s)
        pb, Rpb = nextbank()
        P.fm_norm(hT, RhT, g_mix, Rg_mix, hn, Rhn, pb, Rpb)
        for _ in q_path(blk, 0):
            pass
        for s in range(4):
            it = blk * 4 + s
            nsteps = 4 * (-(-(it + 1) // G))
            side = q_path(blk, s + 1) if s < 3 else None
            drive(attention(blk, s), side, (nsteps, 60))
        for d in range(8):
            pb, Rpb = nextbank()
            for pr in range(8):
                S.op("tensor", lambda e, pr=pr, d=d, pb=pb: e.matmul(pb[:], lhsT=wo[:, pr, d * 128:(d + 1) * 128], rhs=OTb[:, pr, :], start=(pr == 0), stop=(pr == 7)),
                     reads=[Rwo[pr]] + ROTb, writes=[Rpb], join=(pr > 0))
            S.op("vector", lambda e, d=d, pb=pb: e.tensor_tensor(out=hT[:, d, :], in0=pb[:], in1=hT[:, d, :], op=ALU.add), reads=[Rpb, RhT[d]], writes=[RhT[d]])
            S.dma("sync", hout[d, :, t0:t0 + NB], hT[:, d, :], reads=[RhT[d]], writes=[P.Rdram], join=True)
    P.finish()


WSHAPES = {
    "norm_mix0": [1024], "norm_mix1": [1024], "a_w_in": [1024, 3088], "a_b_gate": [16], "a_mh_gain": [128], "a_w_out": [1024, 1024],
    "kv_norm": [1024], "w_kv": [1024, 512], "k_norm": [64], "b_w_q": [1024, 1024], "b_q_norm": [64], "b_w_o": [1024, 1024],
    "norm_ffn0": [1024], "norm_ffn1": [1024], "w_gate_up0": [1024, 5632], "w_gate_up1": [1024, 5632], "w_down0": [2816, 1024], "w_down1": [2816, 1024],
    "norm_ple0": [1024], "norm_ple1": [1024], "w_ple_gate0": [1024, 1024], "w_ple_gate1": [1024, 1024], "w_ple_up0": [256, 1024], "w_ple_up1": [256, 1024],
    "p0": [NT, 256], "p1": [NT, 256],
}


def build_program():
    nc = bass.Bass("TRN2", target_bir_lowering=False)
    Cn = make_consts()
    C = {k: nc.dram_tensor(k, list(v.shape), F32, kind="ExternalInput").ap() for k, v in Cn.items()}
    x = nc.dram_tensor("x", [NT, 1024], F32, kind="ExternalInput").ap()
    out = nc.dram_tensor("out", [NT, 1024], F32, kind="ExternalOutput").ap()
    W = {k: nc.dram_tensor(k, v, F32, kind="ExternalInput").ap() for k, v in WSHAPES.items()}
    hs = [nc.dram_tensor("h_scr%d" % i, [8, 128, NT], F32, kind="Internal").ap() for i in range(4)]
    phase_mlstm(nc, C, x, hs[0], W, "ml")
    phase_ffn(nc, C, hs[0], hs[1], W["w_gate_up0"], W["w_down0"], W["norm_ffn0"], "f0")
    phase_moba(nc, C, hs[1], hs[2], W, "mb")
    phase_ffn(nc, C, hs[2], hs[3], W["w_gate_up1"], W["w_down1"], W["norm_ffn1"], "f1")
    phase_ple_out(nc, C, hs[3], out, W["p1"], W["norm_ple1"], W["w_ple_gate1"], W["w_ple_up1"], "po")
    return nc, Cn


def make_in_maps(inputs, cores):
    f = lambda a: np.ascontiguousarray(np.asarray(a, dtype=np.float32))
    I = {k: np.asarray(v) for k, v in inputs.items()}
    shared = {
        "norm_mix0": f(I["norm_mix"][0]), "norm_mix1": f(I["norm_mix"][1]), "a_w_in": f(I["a_w_in"][0]), "a_b_gate": f(I["a_b_gate"][0]),
        "a_mh_gain": f(I["a_mh_gain"][0]), "a_w_out": f(I["a_w_out"][0]), "kv_norm": f(I["kv_norm"]), "w_kv": f(I["w_kv"]), "k_norm": f(I["k_norm"]),
        "b_w_q": f(I["b_w_q"][0]), "b_q_norm": f(I["b_q_norm"][0]), "b_w_o": f(I["b_w_o"][0]),
        "norm_ffn0": f(I["norm_ffn"][0]), "norm_ffn1": f(I["norm_ffn"][1]), "w_gate_up0": f(I["w_gate_up"][0]), "w_gate_up1": f(I["w_gate_up"][1]),
        "w_down0": f(I["w_down"][0]), "w_down1": f(I["w_down"][1]), "norm_ple0": f(I["norm_ple"][0]), "norm_ple1": f(I["norm_ple"][1]),
        "w_ple_gate0": f(I["w_ple_gate"][0]), "w_ple_gate1": f(I["w_ple_gate"][1]), "w_ple_up0": f(I["w_ple_up"][0]), "w_ple_up1": f(I["w_ple_up"][1]),
    }
    maps = []
    for b in cores:
        m = dict(shared)
        m["x"] = f(I["x"][b])
        m["p0"] = f(I["p"][0, b])
        m["p1"] = f(I["p"][1, b])
        maps.append(m)
    return maps


def kernel(**inputs):
    nc, Cn = build_program()
    maps = make_in_maps(inputs, list(range(8)))
    for m in maps:
        m.update(Cn)
    res = run_bass_kernel_spmd(nc, maps, core_ids=list(range(8)))
    return np.stack([np.asarray(r["out"], dtype=np.float32) for r in res.results], axis=0)
```

```python
import numpy as np
import concourse.bass as bass
import concourse.mybir as mybir
from concourse.bass_utils import run_bass_kernel_spmd
from contextlib import ExitStack

F32 = mybir.dt.float32
BF16 = mybir.dt.bfloat16
ALU = mybir.AluOpType
AF = mybir.ActivationFunctionType
AX = mybir.AxisListType

ENGS = ["tensor", "vector", "scalar", "gpsimd", "sync"]
NDMASEM = 12
SAME_ENGINE_SYNC = True

NT = 4096
NB = 512
NBLK = NT // NB
EPS = 1e-6
NEG = -30000.0


class Res:
    __slots__ = ("name", "w", "r", "gd")

    def __init__(self, name=""):
        self.name = name
        self.w = []
        self.r = []
        self.gd = []


class Op:
    __slots__ = ("eng", "fn", "waits", "pos", "sig", "isdma", "semi", "semk", "K", "sigidx")


class Sched:
    G = {"nc": None}

    @staticmethod
    def setup(nc):
        if Sched.G.get("nc") is nc:
            return
        es = ExitStack()
        G = {"nc": nc, "es": es, "sig": {e: 0 for e in ENGS}, "dma": {e: 0 for e in ENGS}}
        G["esem"] = {e: es.enter_context(nc.semaphore("sem_e_%s" % e)) for e in ENGS}
        G["dsem"] = {(e, i): es.enter_context(nc.semaphore("sem_d_%s_%d" % (e, i))) for e in ("sync", "gpsimd") for i in range(NDMASEM)}
        Sched.G = G

    def __init__(self, nc):
        Sched.setup(nc)
        self.nc = nc
        self.ops = {e: [] for e in ENGS}
        self.Kcur = {e: {} for e in ENGS}
        self.nops = 0

    limit = None

    def _record(self, eng, fn, reads, writes, isdma, join=False):
        if Sched.limit is not None and self.nops >= Sched.limit:
            return None
        o = Op()
        o.eng = eng
        o.fn = fn
        o.isdma = isdma
        o.sig = False
        o.pos = len(self.ops[eng])
        deps = {}
        for r in reads:
            for x in r.w:
                deps[id(x)] = x
        for w in writes:
            if join and not w.r:
                for x in w.gd:
                    deps[id(x)] = x
            else:
                for x in w.w:
                    deps[id(x)] = x
                for x in w.r:
                    deps[id(x)] = x
        K = self.Kcur[eng]
        newK = None
        waits = []
        if isdma:
            i = Sched.G["dma"][eng]
            Sched.G["dma"][eng] += 1
            o.semi = i % NDMASEM
            o.semk = i // NDMASEM + 1
            if o.semk > 1:
                key = ("d", eng, o.semi)
                if K.get(key, 0) < o.semk - 1:
                    waits.append(("dmaslot", eng, o.semi, o.semk - 1))
                    newK = dict(K)
                    newK[key] = o.semk - 1
        best = {}
        dl = []
        for y in deps.values():
            if y is o:
                continue
            if y.isdma:
                dl.append(y)
            elif y.eng not in best or best[y.eng].pos < y.pos:
                best[y.eng] = y
        for y in dl + list(best.values()):
            cur = K if newK is None else newK
            if y.isdma:
                key = ("d", y.eng, y.semi)
                if cur.get(key, 0) >= y.semk:
                    continue
                waits.append(("dma", y))
            else:
                if y.eng == eng and (eng == "tensor" or not SAME_ENGINE_SYNC) and not isdma:
                    continue
                if cur.get(y.eng, -1) >= y.pos:
                    continue
                y.sig = True
                waits.append(("eng", y))
            if newK is None:
                newK = dict(K)
            for k, v in y.K.items():
                if newK.get(k, -1) < v:
                    newK[k] = v
            if y.isdma:
                key = ("d", y.eng, y.semi)
                newK[key] = max(newK.get(key, 0), y.semk)
            else:
                if newK.get(y.eng, -1) < y.pos:
                    newK[y.eng] = y.pos
        if newK is not None:
            self.Kcur[eng] = newK
            K = newK
        o.K = K
        o.waits = waits
        for r in reads:
            r.r.append(o)
        for w in writes:
            if join and not w.r:
                w.w.append(o)
            else:
                w.gd = w.w + w.r
                w.w = [o]
                w.r = []
        self.ops[eng].append(o)
        self.nops += 1
        return o

    def op(self, eng, fn, reads=(), writes=(), join=False):
        return self._record(eng, fn, reads, writes, False, join)

    def dma(self, queue, out, in_, reads=(), writes=(), join=False, nonc=False, **kw):
        nc = self.nc
        if nonc:
            def f(e):
                with nc.allow_non_contiguous_dma(reason="small strided parameter load"):
                    return e.dma_start(out=out, in_=in_, **kw)
        else:
            def f(e):
                return e.dma_start(out=out, in_=in_, **kw)
        return self._record(queue, f, reads, writes, True, join)

    def emit(self):
        nc = self.nc
        G = Sched.G
        for e in ENGS:
            n = G["sig"][e]
            for o in self.ops[e]:
                if o.sig:
                    n += 1
                o.sigidx = n
            G["sig"][e] = n
        esem, dsem = G["esem"], G["dsem"]
        with nc.Block() as block:
            def stream(ename):
                def body(eng):
                    for o in self.ops[ename]:
                        for w in o.waits:
                            if w[0] == "dmaslot":
                                eng.wait_ge(dsem[(w[1], w[2])], 16 * w[3])
                            elif w[0] == "dma":
                                y = w[1]
                                eng.wait_ge(dsem[(y.eng, y.semi)], 16 * y.semk)
                            else:
                                y = w[1]
                                eng.wait_ge(esem[y.eng], y.sigidx)
                        ins = o.fn(eng)
                        if o.isdma:
                            ins.then_inc(dsem[(o.eng, o.semi)], 16)
                        elif o.sig:
                            ins.then_inc(esem[o.eng], 1)
                return body

            for e in ENGS:
                if self.ops[e]:
                    getattr(block, e)(stream(e))


class Proxy:
    def __init__(self, real):
        self.real = real
        self.tgt = real

    def op(self, *a, **k):
        return self.tgt.op(*a, **k)

    def dma(self, *a, **k):
        return self.tgt.dma(*a, **k)


class Mux:
    def __init__(self, real, chunks):
        self.real = real
        self.chunks = chunks
        self.last = None

    def _maybe(self, eng):
        if self.last == "tensor" and eng != "tensor" and self.chunks:
            for kind, a, k in self.chunks.pop(0):
                getattr(self.real, kind)(*a, **k)
        self.last = eng

    def op(self, eng, *a, **k):
        self._maybe(eng)
        return self.real.op(eng, *a, **k)

    def dma(self, eng, *a, **k):
        self._maybe(eng)
        return self.real.dma(eng, *a, **k)

    def flush(self):
        while self.chunks:
            for kind, a, k in self.chunks.pop(0):
                getattr(self.real, kind)(*a, **k)


def mm_chunks(q):
    runs = []
    for item in q:
        is_mm = (item[0] == "op" and item[1][0] == "tensor")
        if runs and runs[-1][0] == is_mm:
            runs[-1][1].append(item)
        else:
            runs.append((is_mm, [item]))
    chunks = []
    cur = []
    for is_mm, items in runs:
        cur.extend(items)
        if is_mm:
            chunks.append(cur)
            cur = []
    if cur:
        chunks.append(cur)
    return chunks


class Deferred:
    def __init__(self):
        self.q = []

    def op(self, *a, **k):
        self.q.append(("op", a, k))

    def dma(self, *a, **k):
        self.q.append(("dma", a, k))


def interleave(main_gen, real, q):
    steps = list(main_gen) if False else None
    i = 0
    n = getattr(main_gen, "nsteps", None)
    return i


class Phase:
    def __init__(self, nc, name):
        self.nc = nc
        self.name = name
        self.es = ExitStack()
        self.S = Sched(nc)
        self.n = 0
        self.Rdram = Res("dram_out")

    def sb(self, shape, dt, name=None):
        self.n += 1
        t = self.es.enter_context(self.nc.sbuf_tensor("%s_s%d" % (self.name, self.n), list(shape), dt))
        return t

    def ps(self, shape, dt, name=None):
        self.n += 1
        t = self.es.enter_context(self.nc.psum_tensor("%s_p%d" % (self.name, self.n), list(shape), dt))
        return t

    def finish(self):
        S = self.S
        S.op("sync", lambda e: e.nop(), reads=[self.Rdram])
        S.emit()
        self.es.close()

    def consts(self, C):
        S = self.S
        self.ident_f = self.sb([128, 128], F32)
        self.ident_b = self.sb([128, 128], BF16)
        self.ones_b = self.sb([128, 128], BF16)
        self.eps_t = self.sb([128, 1], F32)
        self.one_t = self.sb([128, 1], F32)
        self.Rc = Res("consts")
        S.dma("sync", self.ident_f[:], C["c_ident"], writes=[self.Rc], join=True)
        S.op("vector", lambda e: e.tensor_copy(out=self.ident_b[:], in_=self.ident_f[:]), reads=[self.Rc], writes=[self.Rc])
        S.op("vector", lambda e: e.memset(self.ones_b[:], 1.0), writes=[self.Rc], join=True)
        S.op("vector", lambda e: e.memset(self.eps_t[:], EPS), writes=[self.Rc], join=True)
        S.op("vector", lambda e: e.memset(self.one_t[:], 1.0), writes=[self.Rc], join=True)

    def load_vec_fm(self, ap1024, nk=8):
        t = self.sb([128, nk], F32)
        r = Res()
        self.S.dma("sync", t[:], ap1024.rearrange("(k p) -> p k", p=128), writes=[r], nonc=True)
        return t, r

    def load_bcast(self, ap_flat, n):
        t = self.sb([128, n], F32)
        r = Res()
        self.S.dma("sync", t[:], ap_flat.partition_broadcast(128), writes=[r])
        return t, r

    def load_w(self, src, K, N, rows=128, col_chunk=2048, queue="gpsimd"):
        t = self.sb([rows, K, N], BF16)
        rs = [Res() for _ in range(K)]
        nch = -(-N // col_chunk)
        cw = -(-N // nch)
        for k in range(K):
            c0 = 0
            while c0 < N:
                c1 = min(N, c0 + cw)
                self.S.dma(queue, t[:, k, c0:c1], src[k * rows:(k + 1) * rows, c0:c1], writes=[rs[k]], join=True)
                c0 = c1
        return t, rs

    def norm_setup(self):
        self.rstd = self.sb([128, NB], F32)
        self.Rrstd = Res()
        self.lnv = self.rstd
        self.Rlnv = self.Rrstd

    def fm_norm(self, hT, RhT, g, Rg, hn, Rhn, pss, Rpss, reuse_rstd=False):
        S = self.S
        n = hT.shape[2]
        sq, Rsq = hn, Rhn
        for k in range(8 if not reuse_rstd else 0):
            S.op("scalar", lambda e, k=k: e.activation(out=sq[:, k, :], in_=hT[:, k, :], func=AF.Square),
                 reads=[RhT[k]], writes=[Rsq[k]])
        for k in range(8 if not reuse_rstd else 0):
            S.op("tensor", lambda e, k=k: e.matmul(pss[:, 0:n], lhsT=self.ones_b[:], rhs=sq[:, k, :], start=(k == 0), stop=(k == 7)),
                 reads=[Rsq[k], self.Rc], writes=[Rpss], join=(k > 0))
        if not reuse_rstd:
            S.op("scalar", lambda e: e.activation(out=self.lnv[:, 0:n], in_=pss[:, 0:n], func=AF.Ln, scale=1.0 / 1024.0, bias=self.eps_t[:, 0:1]),
                 reads=[Rpss, self.Rc], writes=[self.Rlnv])
            S.op("scalar", lambda e: e.activation(out=self.rstd[:, 0:n], in_=self.lnv[:, 0:n], func=AF.Exp, scale=-0.5),
                 reads=[self.Rlnv], writes=[self.Rrstd])
        for k in range(8):
            S.op("vector", lambda e, k=k: e.scalar_tensor_tensor(out=hn[:, k, :], in0=hT[:, k, :], scalar=g[:, k:k + 1], op0=ALU.mult,
                                                                 in1=self.rstd[:, 0:n], op1=ALU.mult),
                 reads=[RhT[k], self.Rrstd, Rg], writes=[Rhn[k]])


def phase_ffn(nc, C, hin, hout, wgu_ap, wd_ap, g_ap, name):
    P = Phase(nc, name)
    S = P.S
    P.consts(C)
    g, Rg = P.load_vec_fm(g_ap)
    wgu, Rwgu = P.load_w(wgu_ap, 8, 5632, col_chunk=1408)
    wd, Rwd = P.load_w(wd_ap, 22, 1024)
    P.norm_setup()
    hT = P.sb([128, 8, NB], F32)
    RhT = [Res() for _ in range(8)]
    hn = P.sb([128, 8, NB], BF16)
    Rhn = [Res() for _ in range(8)]
    act = P.sb([128, 22, NB], BF16)
    Ract = [Res() for _ in range(22)]
    sg = [P.sb([128, NB], F32) for _ in range(2)]
    Rsg = [Res() for _ in range(2)]
    pss = P.ps([128, NB], F32)
    Rpss = Res()
    pg = [P.ps([128, NB], F32) for _ in range(2)]
    Rpg = [Res() for _ in range(2)]
    pu = [P.ps([128, NB], F32) for _ in range(2)]
    Rpu = [Res() for _ in range(2)]
    po = [P.ps([128, NB], F32) for _ in range(2)]
    Rpo = [Res() for _ in range(2)]
    for blk in range(NBLK):
        t0 = blk * NB
        for k in range(8):
            S.dma("sync", hT[:, k, :], hin[k, :, t0:t0 + NB], writes=[RhT[k]])
        P.fm_norm(hT, RhT, g, Rg, hn, Rhn, pss, Rpss)
        for c in range(22):
            b = c % 2
            for k in range(8):
                S.op("tensor", lambda e, k=k, c=c, b=b: e.matmul(pg[b][:], lhsT=wgu[:, k, c * 128:(c + 1) * 128], rhs=hn[:, k, :],
                                                                  start=(k == 0), stop=(k == 7)),
                     reads=[Rwgu[k], Rhn[k]], writes=[Rpg[b]], join=(k > 0))
            for k in range(8):
                S.op("tensor", lambda e, k=k, c=c, b=b: e.matmul(pu[b][:], lhsT=wgu[:, k, 2816 + c * 128:2816 + (c + 1) * 128], rhs=hn[:, k, :],
                                                                  start=(k == 0), stop=(k == 7)),
                     reads=[Rwgu[k], Rhn[k]], writes=[Rpu[b]], join=(k > 0))
            S.op("scalar", lambda e, b=b: e.activation(out=sg[b][:], in_=pg[b][:], func=AF.Silu), reads=[Rpg[b]], writes=[Rsg[b]])
            S.op("vector", lambda e, b=b, c=c: e.tensor_tensor(out=act[:, c, :], in0=pu[b][:], in1=sg[b][:], op=ALU.mult),
                 reads=[Rpu[b], Rsg[b]], writes=[Ract[c]])
        for d in range(8):
            b = d % 2
            for c in range(22):
                S.op("tensor", lambda e, c=c, d=d, b=b: e.matmul(po[b][:], lhsT=wd[:, c, d * 128:(d + 1) * 128], rhs=act[:, c, :],
                                                                  start=(c == 0), stop=(c == 21)),
                     reads=[Rwd[c], Ract[c]], writes=[Rpo[b]], join=(c > 0))
            S.op("vector", lambda e, d=d, b=b: e.tensor_tensor(out=hT[:, d, :], in0=po[b][:], in1=hT[:, d, :], op=ALU.add),
                 reads=[Rpo[b], RhT[d]], writes=[RhT[d]])
            S.dma("sync", hout[d, :, t0:t0 + NB], hT[:, d, :], reads=[RhT[d]], writes=[P.Rdram], join=True)
    P.finish()


def make_consts():
    c = {}
    c["c_ident"] = np.eye(128, dtype=np.float32)
    s = np.arange(128)
    c["c_tri"] = (s[:, None] <= s[None, :]).astype(np.float32)
    c["c_cbias"] = np.where(s[:, None] <= s[None, :], 0.0, NEG).astype(np.float32)
    inv = 500000.0 ** (-np.arange(0, 16, 2, dtype=np.float64) / 16.0)
    ang = np.arange(NT, dtype=np.float64)[:, None] * inv[None, :]
    c["c_rope"] = np.concatenate([np.cos(ang), np.sin(ang)], axis=1).astype(np.float32)
    u = np.arange(NT)
    c["c_onehot"] = (u[None, :] // 256 == np.arange(16)[:, None]).astype(np.float32)
    c["c_tribias4"] = np.tile(c["c_cbias"], (1, 4)).astype(np.float32)
    b = np.arange(16)
    c["c_past"] = np.where(b[None, :] < b[:, None], 0.0, NEG).astype(np.float32).reshape(256)
    c["c_own"] = (b[None, :] == b[:, None]).astype(np.float32).reshape(256)
    return c


class PLE:
    def __init__(self, P, C, p_ap, g_ap, wpg_ap, wpu_ap, banks, nb=NB):
        self.P = P
        self.nb = nb
        S = P.S
        self.p_ap = p_ap
        self.g, self.Rg = P.load_vec_fm(g_ap)
        self.wpg, self.Rwpg = P.load_w(wpg_ap, 8, 1024)
        self.wpu, self.Rwpu = P.load_w(wpu_ap, 2, 1024)
        self.ptm = P.sb([128, nb // 128, 256], F32)
        self.Rptm = Res()
        self.pT = P.sb([128, 2, nb], BF16)
        self.RpT = [Res(), Res()]
        self.sgate = P.sb([128, nb], F32)
        self.Rsgate = Res()
        self.tmp = P.sb([128, nb], F32)
        self.Rtmp = Res()
        self.banks = banks

    def emit(self, blk, hT, RhT, hn, Rhn, pss, Rpss):
        P = self.P
        S = P.S
        nb = self.nb
        t0 = blk * nb
        (pa, Rpa), (pb, Rpb), (pc, Rpc) = self.banks[:3]
        P.fm_norm(hT, RhT, self.g, self.Rg, hn, Rhn, pss, Rpss)
        S.dma("sync", self.ptm[:], self.p_ap[t0:t0 + nb, :].rearrange("(s p) d -> p s d", p=128), writes=[self.Rptm])
        for kk in range(2):
            for s in range(nb // 128):
                S.op("tensor", lambda e, kk=kk, s=s: e.transpose(out=pc[:, s * 128:(s + 1) * 128], in_=self.ptm[:, s, kk * 128:(kk + 1) * 128],
                                                                 identity=P.ident_f[:]),
                     reads=[self.Rptm, P.Rc], writes=[Rpc], join=(s > 0))
            S.op("scalar", lambda e, kk=kk: e.copy(out=self.pT[:, kk, :], in_=pc[:, 0:nb]), reads=[Rpc], writes=[self.RpT[kk]])
        for d in range(8):
            for k in range(8):
                S.op("tensor", lambda e, k=k, d=d: e.matmul(pa[:, 0:nb], lhsT=self.wpg[:, k, d * 128:(d + 1) * 128], rhs=hn[:, k, :],
                                                             start=(k == 0), stop=(k == 7)),
                     reads=[self.Rwpg[k], Rhn[k]], writes=[Rpa], join=(k > 0))
            S.op("scalar", lambda e: e.activation(out=self.sgate[:], in_=pa[:, 0:nb], func=AF.Sigmoid), reads=[Rpa], writes=[self.Rsgate])
            for kk in range(2):
                S.op("tensor", lambda e, kk=kk, d=d: e.matmul(pb[:, 0:nb], lhsT=self.wpu[:, kk, d * 128:(d + 1) * 128], rhs=self.pT[:, kk, :],
                                                               start=(kk == 0), stop=(kk == 1)),
                     reads=[self.Rwpu[kk], self.RpT[kk]], writes=[Rpb], join=(kk > 0))
            S.op("vector", lambda e: e.tensor_tensor(out=self.tmp[:], in0=pb[:, 0:nb], in1=self.sgate[:], op=ALU.mult),
                 reads=[Rpb, self.Rsgate], writes=[self.Rtmp])
            S.op("gpsimd", lambda e, d=d: e.tensor_tensor(out=hT[:, d, :], in0=hT[:, d, :], in1=self.tmp[:], op=ALU.add),
                 reads=[self.Rtmp, RhT[d]], writes=[RhT[d]])


def phase_ple_out(nc, C, hin, out_ap, p_ap, g_ap, wpg_ap, wpu_ap, name):
    P = Phase(nc, name)
    S = P.S
    P.consts(C)
    P.norm_setup()
    banks = [(P.ps([128, NB], F32), Res()) for _ in range(5)]
    pss, Rpss = P.ps([128, NB], F32), Res()
    ple = PLE(P, C, p_ap, g_ap, wpg_ap, wpu_ap, banks)
    hT = P.sb([128, 8, NB], F32)
    RhT = [Res() for _ in range(8)]
    hn = P.sb([128, 8, NB], BF16)
    Rhn = [Res() for _ in range(8)]
    otm = P.sb([128, 4, 1024], F32)
    Rotm = [Res() for _ in range(4)]
    for blk in range(NBLK):
        t0 = blk * NB
        for k in range(8):
            S.dma("sync", hT[:, k, :], hin[k, :, t0:t0 + NB], writes=[RhT[k]])
        ple.emit(blk, hT, RhT, hn, Rhn, pss, Rpss)
        for s in range(4):
            for kq in range(2):
                pt, Rpt = banks[3 + kq]
                for k4 in range(4):
                    k = kq * 4 + k4
                    S.op("tensor", lambda e, k=k, k4=k4, s=s, pt=pt: e.transpose(out=pt[:, k4 * 128:(k4 + 1) * 128], in_=hT[:, k, s * 128:(s + 1) * 128],
                                                                                 identity=P.ident_f[:]),
                         reads=[RhT[k], P.Rc], writes=[Rpt], join=(k4 > 0))
                eng = "scalar" if kq == 0 else "vector"
                if eng == "scalar":
                    S.op("scalar", lambda e, s=s, kq=kq, pt=pt: e.copy(out=otm[:, s, kq * 512:(kq + 1) * 512], in_=pt[:]),
                         reads=[Rpt], writes=[Rotm[s]], join=(kq > 0))
                else:
                    S.op("vector", lambda e, s=s, kq=kq, pt=pt: e.tensor_copy(out=otm[:, s, kq * 512:(kq + 1) * 512], in_=pt[:]),
                         reads=[Rpt], writes=[Rotm[s]], join=(kq > 0))
            S.dma("sync", out_ap[t0 + s * 128:t0 + (s + 1) * 128, :], otm[:, s, :], reads=[Rotm[s]], writes=[P.Rdram], join=True)
    P.finish()


def phase_mlstm(nc, C, x_ap, hout, W, name, nblk=NBLK):
    P = Phase(nc, name)
    realS = P.S
    S = Proxy(realS)
    P.S = S
    P.consts(C)
    P.norm_setup()
    tri_f = P.sb([128, 128], F32)
    cbias = P.sb([128, 128], F32)
    ones_f = P.sb([128, 128], F32)
    S.dma("sync", tri_f[:], C["c_tri"], writes=[P.Rc], join=True)
    S.dma("sync", cbias[:], C["c_cbias"], writes=[P.Rc], join=True)
    S.op("vector", lambda e: e.memset(ones_f[:], 1.0), writes=[P.Rc], join=True)
    g, Rg = P.load_vec_fm(W["norm_mix0"])
    bgate, Rbgate = P.load_bcast(W["a_b_gate"], 16)
    mhg, Rmhg = P.load_bcast(W["a_mh_gain"], 128)
    win, Rwin = P.load_w(W["a_w_in"], 8, 3088, col_chunk=1544)
    wout, Rwout = P.load_w(W["a_w_out"], 8, 1024)

    xtm = P.sb([128, 4, 1024], F32); Rxtm = [Res() for _ in range(4)]
    hT = P.sb([128, 8, NB], F32); RhT = [Res() for _ in range(8)]
    hn = P.sb([128, 8, NB], BF16); Rhn = [Res() for _ in range(8)]
    qkT = P.sb([128, 8, NB], BF16); Rqk = [Res() for _ in range(8)]
    ktm = P.sb([128, 4, 512], BF16); Rktm = [Res() for _ in range(4)]
    vtm = P.sb([128, 4, 1024], BF16); Rvtm = [Res() for _ in range(4)]
    og = P.sb([128, 4, 1024], BF16); Rog = [Res() for _ in range(4)]
    sgt = P.sb([128, 512], F32); Rsgt = Res()
    gsb = P.sb([128, 4, 16], F32); Rgsb = Res()
    th = P.sb([128, 4, 16], F32); Rth = Res()
    ef = P.sb([128, 4, 8], F32); Ref = Res()
    spf = P.sb([128, 4, 8], F32); Rspf = Res()
    li = P.sb([128, 4, 8], F32); Rli = Res()
    lf = P.sb([128, 4, 8], F32); Rlf = Res()
    g_sb = P.sb([128, 8], F32); Rg_sb = Res()
    bb = P.sb([128, 8], F32); Rbb = Res()
    eg = P.sb([128, 8], F32); Reg = Res()
    wlp = P.sb([128, 8], F32); Rwlp = Res()
    wl = P.sb([128, 8], F32); Rwl = Res()
    egl = P.sb([128, 4], F32); Regl = Res()
    Gd = P.sb([128, 8, 128], F32); RGd = Res()
    arg = P.sb([128, 8, 128], F32); Rarg = Res()
    DT = P.sb([128, 8, 128], F32); RDT = Res()
    PT = P.sb([128, 8, 128], BF16); RPT = Res()
    kw = P.sb([128, 8, 64], BF16); Rkw = Res()
    numXs = P.sb([128, 8, 128], F32); RnumXs = Res()
    num = P.sb([128, 8, 128], F32); Rnum = Res()
    sqn = P.sb([128, 8, 128], F32); Rsqn = Res()
    sm = {n: (P.sb([128, 8], F32), Res()) for n in ["dxs", "den", "dd", "rec", "ssn", "t1", "t2", "lnt", "rs", "coef"]}
    y0 = P.sb([128, 8, 128], F32); Ry0 = Res()
    ytm = P.sb([128, 1024], BF16); Rytm = Res()
    yT = P.sb([128, 8, NB], BF16); RyT = [Res() for _ in range(4)]
    Cst = P.sb([128, 4, 128], F32); nst = P.sb([128, 4], F32); RC = Res()
    Cbf = P.sb([128, 4, 2, 128], BF16); nbf = P.sb([128, 4, 2], BF16); RCbf = Res()
    qbd = P.sb([128, 4, 2, NB], BF16); Rqbd = [Res() for _ in range(4)]
    nt1 = P.sb([128, 4], F32); Rnt1 = Res()

    pS = P.ps([128, 512], F32)
    RpS = Res()
    Rgcs = Rglast = RdenI = RdenX = Rdn = Rpgate = RpS
    pR = [P.ps([128, 512], F32) for _ in range(2)]; RpR = [Res(), Res()]
    pG = P.ps([128, 1024], F32); RpG = Res()
    pT2 = P.ps([128, 1024], F32); RpT2 = Res()
    pY = P.ps([128, 1024], BF16); RpY = Res()
    rot = [0]

    def nextbank():
        rot[0] ^= 1
        return pR[rot[0]], RpR[rot[0]]

    for t in (Cst, nst):
        S.op("vector", lambda e, t=t: e.memset(t[:], 0.0), writes=[RC], join=True)
    for t in (Cbf, nbf):
        S.op("vector", lambda e, t=t: e.memset(t[:], 0.0), writes=[RCbf], join=True)
    for c in range(4):
        S.op("gpsimd", lambda e, c=c: e.memset(qbd[:, c, :, :], 0.0), writes=[Rqbd[c]])

    for blk in range(nblk):
        t0 = blk * NB
        for s in range(4):
            S.dma("sync", xtm[:, s, :], x_ap[t0 + s * 128:t0 + (s + 1) * 128, :], writes=[Rxtm[s]])
        for k in range(8):
            pb, Rpb = nextbank()
            for s in range(4):
                S.op("tensor", lambda e, k=k, s=s, pb=pb: e.transpose(out=pb[:, s * 128:(s + 1) * 128], in_=xtm[:, s, k * 128:(k + 1) * 128],
                                                                       identity=P.ident_f[:]),
                     reads=[Rxtm[s], P.Rc], writes=[Rpb], join=(s > 0))
            S.op("scalar", lambda e, k=k, pb=pb: e.copy(out=hT[:, k, :], in_=pb[:]), reads=[Rpb], writes=[RhT[k]])
        pb, Rpb = nextbank()
        P.fm_norm(hT, RhT, g, Rg, hn, Rhn, pb, Rpb)
        for c in range(8):
            pb, Rpb = nextbank()
            for k in range(8):
                S.op("tensor", lambda e, k=k, c=c, pb=pb: e.matmul(pb[:], lhsT=win[:, k, c * 128:(c + 1) * 128], rhs=hn[:, k, :],
                                                                    start=(k == 0), stop=(k == 7)),
                     reads=[Rwin[k], Rhn[k]], writes=[Rpb], join=(k > 0))
            sc = 0.125 if c < 4 else 1.0
            S.op("scalar", lambda e, c=c, pb=pb, sc=sc: e.activation(out=qkT[:, c, :], in_=pb[:], func=AF.Copy, scale=sc),
                 reads=[Rpb], writes=[Rqk[c]])
            if c < 4:
                S.op("gpsimd", lambda e, c=c: e.tensor_copy(out=qbd[0:64, c, 0, :], in_=qkT[0:64, c, :]), reads=[Rqk[c]], writes=[Rqbd[c]])
                S.op("gpsimd", lambda e, c=c: e.tensor_copy(out=qbd[64:128, c, 1, :], in_=qkT[64:128, c, :]), reads=[Rqk[c]], writes=[Rqbd[c]], join=True)
        def proj_kvo(s):
            ts = slice(s * 128, (s + 1) * 128)
            pb, Rpb = nextbank()
            for k in range(8):
                S.op("tensor", lambda e, k=k, ts=ts, pb=pb: e.matmul(pb[:], lhsT=hn[:, k, ts], rhs=win[:, k, 512:1024], start=(k == 0), stop=(k == 7)),
                     reads=[Rwin[k], Rhn[k]], writes=[Rpb], join=(k > 0))
            S.op("scalar", lambda e, s=s, pb=pb: e.copy(out=ktm[:, s, :], in_=pb[:]), reads=[Rpb], writes=[Rktm[s]])
            for half in range(2):
                pb, Rpb = nextbank()
                c0 = 1024 + half * 512
                for k in range(8):
                    S.op("tensor", lambda e, k=k, ts=ts, pb=pb, c0=c0: e.matmul(pb[:], lhsT=hn[:, k, ts], rhs=win[:, k, c0:c0 + 512],
                                                                                 start=(k == 0), stop=(k == 7)),
                         reads=[Rwin[k], Rhn[k]], writes=[Rpb], join=(k > 0))
                S.op("vector", lambda e, s=s, half=half, pb=pb: e.tensor_copy(out=vtm[:, s, half * 512:(half + 1) * 512], in_=pb[:]),
                     reads=[Rpb], writes=[Rvtm[s]], join=(half > 0))
            for half in range(2):
                pb, Rpb = nextbank()
                c0 = 2048 + half * 512
                for k in range(8):
                    S.op("tensor", lambda e, k=k, ts=ts, pb=pb, c0=c0: e.matmul(pb[:], lhsT=hn[:, k, ts], rhs=win[:, k, c0:c0 + 512],
                                                                                 start=(k == 0), stop=(k == 7)),
                         reads=[Rwin[k], Rhn[k]], writes=[Rpb], join=(k > 0))
                S.op("scalar", lambda e, pb=pb: e.activation(out=sgt[:], in_=pb[:], func=AF.Sigmoid), reads=[Rpb], writes=[Rsgt])
                S.op("gpsimd", lambda e, s=s, half=half: e.tensor_tensor(
                    out=og[:, s, half * 512:(half + 1) * 512].rearrange("p (h v) -> p h v", h=4),
                    in0=sgt[:].rearrange("p (h v) -> p h v", h=4),
                    in1=mhg[:].unsqueeze(1).broadcast_to([128, 4, 128]), op=ALU.mult),
                     reads=[Rsgt, Rmhg], writes=[Rog[s]], join=(half > 0))

        for s in range(4):
            ts = slice(s * 128, (s + 1) * 128)
            for k in range(8):
                S.op("tensor", lambda e, k=k, ts=ts, s=s: e.matmul(pS[:, 64 + s * 16:64 + (s + 1) * 16], lhsT=hn[:, k, ts], rhs=win[:, k, 3072:3088],
                                                                    start=(k == 0), stop=(k == 7)),
                     reads=[Rwin[k], Rhn[k]], writes=[Rpgate], join=(k > 0))
            S.op("vector", lambda e, s=s: e.tensor_tensor(out=gsb[:, s, :], in0=pS[:, 64 + s * 16:64 + (s + 1) * 16], in1=bgate[:], op=ALU.add),
                 reads=[Rpgate, Rbgate], writes=[Rgsb], join=(s > 0))
        S.op("scalar", lambda e: e.activation(out=th[:], in_=gsb[:], func=AF.Tanh, scale=1.0 / 15.0), reads=[Rgsb], writes=[Rth])
        S.op("vector", lambda e: e.tensor_scalar(out=li[:], in0=th[:, :, 0:8], scalar1=15.0, scalar2=None, op0=ALU.mult), reads=[Rth], writes=[Rli])
        S.op("scalar", lambda e: e.activation(out=ef[:], in_=th[:, :, 8:16], func=AF.Exp, scale=-15.0), reads=[Rth], writes=[Ref])
        S.op("scalar", lambda e: e.activation(out=spf[:], in_=ef[:], func=AF.Ln, bias=P.one_t[:, 0:1]), reads=[Ref, P.Rc], writes=[Rspf])
        S.op("vector", lambda e: e.tensor_scalar(out=lf[:], in0=spf[:], scalar1=-1.0, scalar2=None, op0=ALU.mult), reads=[Rspf], writes=[Rlf])

        proj_kvo(0)
        for s in range(4):
            ts = slice(s * 128, (s + 1) * 128)
            if s < 3:
                d_ = Deferred()
                S.tgt = d_
                proj_kvo(s + 1)
                S.tgt = Mux(realS, mm_chunks(d_.q))
            else:
                S.tgt = realS
            S.op("tensor", lambda e, s=s: e.matmul(pS[:, 0:8], lhsT=tri_f[:], rhs=lf[:, s, :], start=True, stop=True),
                 reads=[Rlf, P.Rc], writes=[Rgcs])
            S.op("tensor", lambda e, s=s: e.matmul(pS[:, 8:16], lhsT=ones_f[:], rhs=lf[:, s, :], start=True, stop=True),
                 reads=[Rlf, P.Rc], writes=[Rglast])
            S.op("vector", lambda e: e.tensor_copy(out=g_sb[:], in_=pS[:, 0:8]), reads=[Rgcs], writes=[Rg_sb])
            S.op("vector", lambda e, s=s: e.tensor_tensor(out=bb[:], in0=li[:, s, :], in1=g_sb[:], op=ALU.subtract), reads=[Rli, Rg_sb], writes=[Rbb])
            S.op("scalar", lambda e: e.activation(out=eg[:], in_=g_sb[:], func=AF.Exp), reads=[Rg_sb], writes=[Reg])
            S.op("vector", lambda e: e.tensor_tensor(out=wlp[:], in0=pS[:, 8:16], in1=bb[:], op=ALU.add), reads=[Rglast, Rbb], writes=[Rwlp])
            S.op("scalar", lambda e: e.activation(out=wl[:], in_=wlp[:], func=AF.Exp), reads=[Rwlp], writes=[Rwl])
            S.op("scalar", lambda e: e.activation(out=egl[0:64, :], in_=pS[0:64, 8:16:2], func=AF.Exp), reads=[Rglast], writes=[Regl])
            S.op("scalar", lambda e: e.activation(out=egl[64:128, :], in_=pS[64:128, 9:16:2], func=AF.Exp), reads=[Rglast], writes=[Regl], join=True)
            S.op("vector", lambda e: e.tensor_tensor(out=Gd[:], in0=g_sb[:].unsqueeze(2).broadcast_to([128, 8, 128]),
                                                      in1=P.ident_f[:].unsqueeze(1).broadcast_to([128, 8, 128]), op=ALU.mult),
                 reads=[Rg_sb, P.Rc], writes=[RGd])
            for half in range(2):
                S.op("tensor", lambda e, half=half: e.matmul(pG[:, half * 512:(half + 1) * 512], lhsT=ones_f[:],
                                                               rhs=Gd[:, half * 4:(half + 1) * 4, :].rearrange("p h j -> p (h j)"),
                                                               start=True, stop=True),
                     reads=[RGd, P.Rc], writes=[RpG], join=(half > 0))
            for h in range(8):
                S.op("vector", lambda e, h=h: e.scalar_tensor_tensor(out=arg[:, h, :], in0=pG[:, h * 128:(h + 1) * 128], scalar=bb[:, h:h + 1], op0=ALU.add,
                                                                      in1=cbias[:], op1=ALU.add),
                     reads=[RpG, Rbb, P.Rc], writes=[Rarg], join=(h > 0))
            S.op("scalar", lambda e: e.activation(out=DT[:], in_=arg[:], func=AF.Exp), reads=[Rarg], writes=[RDT])
            for c in range(4):
                S.op("tensor", lambda e, c=c, ts=ts: e.matmul(pT2[:, c * 256:(c + 1) * 256], lhsT=qkT[:, 4 + c, ts], rhs=qbd[:, c, :, ts],
                                                               start=True, stop=True),
                     reads=[Rqbd[c], Rqk[4 + c]], writes=[RpT2], join=(c > 0))
            S.op("vector", lambda e: e.tensor_tensor(out=PT[:].rearrange("p h j -> p (h j)"), in0=pT2[:], in1=DT[:].rearrange("p h j -> p (h j)"), op=ALU.mult),
                 reads=[RpT2, RDT], writes=[RPT])
            for h in range(8):
                S.op("tensor", lambda e, h=h, s=s: e.matmul(pG[:, h * 128:(h + 1) * 128], lhsT=PT[:, h, :], rhs=vtm[:, s, h * 128:(h + 1) * 128],
                                                             start=True, stop=True),
                     reads=[RPT, Rvtm[s]], writes=[RpG], join=(h > 0))
            for h in range(8):
                S.op("tensor", lambda e, h=h: e.matmul(pS[:, 16 + h:17 + h], lhsT=PT[:, h, :], rhs=P.ones_b[:, 0:1], start=True, stop=True),
                     reads=[RPT, P.Rc], writes=[RdenI], join=(h > 0))
            for c in range(4):
                S.op("tensor", lambda e, c=c, ts=ts: e.matmul(pT2[:, c * 256:(c + 1) * 256], lhsT=qkT[:, c, ts], rhs=Cbf[:, c, :, :],
                                                               start=True, stop=True),
                     reads=[Rqk[c], RCbf], writes=[RpT2], join=(c > 0))
            for c in range(4):
                S.op("tensor", lambda e, c=c, ts=ts: e.matmul(pS[:, 24 + 2 * c:26 + 2 * c], lhsT=qkT[:, c, ts], rhs=nbf[:, c, :],
                                                               start=True, stop=True),
                     reads=[Rqk[c], RCbf], writes=[RdenX], join=(c > 0))
            S.op("vector", lambda e, s=s: e.tensor_tensor(out=kw[:], in0=ktm[:, s, :].rearrange("p (h d) -> p h d", h=8),
                                                           in1=wl[:].unsqueeze(2).broadcast_to([128, 8, 64]), op=ALU.mult),
                 reads=[Rktm[s], Rwl], writes=[Rkw])
            pd, Rpd = pY[:].bitcast(F32), RpY
            for h in range(8):
                c, ph = h // 2, h % 2
                prt = slice(ph * 64, (ph + 1) * 64)
                S.op("tensor", lambda e, h=h, c=c, prt=prt, s=s, pd=pd: e.matmul(pd[prt, c * 128:(c + 1) * 128], lhsT=kw[:, h, :], rhs=vtm[:, s, h * 128:(h + 1) * 128],
                                                                                  start=True, stop=True),
                     reads=[Rkw, Rvtm[s]], writes=[Rpd], join=(h > 0))
            for h in range(8):
                c, ph = h // 2, h % 2
                prt = slice(ph * 64, (ph + 1) * 64)
                S.op("tensor", lambda e, h=h, c=c, prt=prt: e.matmul(pS[prt, 32 + c:33 + c], lhsT=kw[:, h, :], rhs=P.ones_b[:, 0:1], start=True, stop=True),
                     reads=[Rkw, P.Rc], writes=[Rdn], join=(h > 0))
            for c in range(4):
                S.op("vector", lambda e, c=c, pd=pd: e.scalar_tensor_tensor(out=Cst[:, c, :], in0=Cst[:, c, :], scalar=egl[:, c:c + 1], op0=ALU.mult,
                                                                            in1=pd[:, c * 128:(c + 1) * 128], op1=ALU.add),
                     reads=[Rpd, Regl, RC], writes=[RC])
            S.op("vector", lambda e: e.tensor_tensor(out=nt1[:], in0=nst[:], in1=egl[:], op=ALU.mult), reads=[RC, Regl], writes=[Rnt1])
            S.op("vector", lambda e: e.tensor_tensor(out=nst[:], in0=pS[:, 32:36], in1=nt1[:], op=ALU.add), reads=[Rdn, Rnt1], writes=[RC])
            S.op("vector", lambda e: e.tensor_tensor(out=numXs[:], in0=pT2[:].rearrange("p (h v) -> p h v", h=8),
                                                      in1=eg[:].unsqueeze(2).broadcast_to([128, 8, 128]), op=ALU.mult),
                 reads=[RpT2, Reg], writes=[RnumXs])
            S.op("vector", lambda e: e.tensor_tensor(out=num[:].rearrange("p h v -> p (h v)"), in0=pG[:], in1=numXs[:].rearrange("p h v -> p (h v)"), op=ALU.add),
                 reads=[RpG, RnumXs], writes=[Rnum])
            S.op("gpsimd", lambda e: e.tensor_copy(out=Cbf[0:64, :, 0, :], in_=Cst[0:64, :, :]), reads=[RC], writes=[RCbf])
            S.op("gpsimd", lambda e: e.tensor_copy(out=Cbf[64:128, :, 1, :], in_=Cst[64:128, :, :]), reads=[RC], writes=[RCbf], join=True)
            S.op("gpsimd", lambda e: e.tensor_copy(out=nbf[0:64, :, 0], in_=nst[0:64, :]), reads=[RC], writes=[RCbf], join=True)
            S.op("gpsimd", lambda e: e.tensor_copy(out=nbf[64:128, :, 1], in_=nst[64:128, :]), reads=[RC], writes=[RCbf], join=True)
            T = lambda n: sm[n][0]
            R_ = lambda n: sm[n][1]
            S.op("vector", lambda e: e.tensor_tensor(out=T("dxs")[:], in0=pS[:, 24:32], in1=eg[:], op=ALU.mult), reads=[RdenX, Reg], writes=[R_("dxs")])
            S.op("vector", lambda e: e.tensor_tensor(out=T("den")[:], in0=pS[:, 16:24], in1=T("dxs")[:], op=ALU.add), reads=[RdenI, R_("dxs")], writes=[R_("den")])
            S.op("vector", lambda e: e.scalar_tensor_tensor(out=T("t1")[:], in0=T("den")[:], scalar=-1.0, op0=ALU.mult, in1=T("den")[:], op1=ALU.max),
                 reads=[R_("den")], writes=[R_("t1")])
            S.op("vector", lambda e: e.tensor_scalar(out=T("dd")[:], in0=T("t1")[:], scalar1=1.0, scalar2=None, op0=ALU.max), reads=[R_("t1")], writes=[R_("dd")])
            S.op("vector", lambda e: e.reciprocal(out=T("rec")[:], in_=T("dd")[:]), reads=[R_("dd")], writes=[R_("rec")])
            S.op("gpsimd", lambda e: e.tensor_tensor(out=sqn[:], in0=num[:], in1=num[:], op=ALU.mult), reads=[Rnum], writes=[Rsqn])
            S.op("vector", lambda e: e.tensor_reduce(out=T("ssn")[:], in_=sqn[:], axis=AX.X, op=ALU.add), reads=[Rsqn], writes=[R_("ssn")])
            S.op("vector", lambda e: e.tensor_tensor(out=T("t1")[:], in0=T("rec")[:], in1=T("rec")[:], op=ALU.mult), reads=[R_("rec")], writes=[R_("t1")])
            S.op("vector", lambda e: e.tensor_tensor(out=T("t2")[:], in0=T("t1")[:], in1=T("ssn")[:], op=ALU.mult), reads=[R_("t1"), R_("ssn")], writes=[R_("t2")])
            S.op("scalar", lambda e: e.activation(out=T("lnt")[:], in_=T("t2")[:], func=AF.Ln, scale=1.0 / 128.0, bias=P.eps_t[:, 0:1]),
                 reads=[R_("t2"), P.Rc], writes=[R_("lnt")])
            S.op("scalar", lambda e: e.activation(out=T("rs")[:], in_=T("lnt")[:], func=AF.Exp, scale=-0.5), reads=[R_("lnt")], writes=[R_("rs")])
            S.op("vector", lambda e: e.tensor_tensor(out=T("coef")[:], in0=T("rec")[:], in1=T("rs")[:], op=ALU.mult), reads=[R_("rec"), R_("rs")], writes=[R_("coef")])
            S.op("vector", lambda e: e.tensor_tensor(out=y0[:], in0=num[:], in1=T("coef")[:].unsqueeze(2).broadcast_to([128, 8, 128]), op=ALU.mult),
                 reads=[Rnum, R_("coef")], writes=[Ry0])
            S.op("gpsimd", lambda e, s=s: e.tensor_tensor(out=ytm[:], in0=y0[:].rearrange("p h v -> p (h v)"), in1=og[:, s, :], op=ALU.mult),
                 reads=[Ry0, Rog[s]], writes=[Rytm])
            for h in range(8):
                S.op("tensor", lambda e, h=h: e.transpose(out=pY[:, h * 128:(h + 1) * 128], in_=ytm[:, h * 128:(h + 1) * 128], identity=P.ident_b[:]),
                     reads=[Rytm, P.Rc], writes=[RpY], join=(h > 0))
            S.op("scalar", lambda e, ts=ts: e.copy(out=yT[:, :, ts], in_=pY[:].rearrange("p (h j) -> p h j", h=8)), reads=[RpY], writes=[RyT[s]])
            if s < 3:
                S.tgt.flush()
            S.tgt = realS
        for d in range(8):
            pb, Rpb = nextbank()
            for h in range(8):
                S.op("tensor", lambda e, h=h, d=d, pb=pb: e.matmul(pb[:], lhsT=wout[:, h, d * 128:(d + 1) * 128], rhs=yT[:, h, :], start=(h == 0), stop=(h == 7)),
                     reads=[Rwout[h]] + RyT, writes=[Rpb], join=(h > 0))
            S.op("vector", lambda e, d=d, pb=pb: e.tensor_tensor(out=hT[:, d, :], in0=pb[:], in1=hT[:, d, :], op=ALU.add), reads=[Rpb, RhT[d]], writes=[RhT[d]])
            S.dma("sync", hout[d, :, t0:t0 + NB], hT[:, d, :], reads=[RhT[d]], writes=[P.Rdram], join=True)
    P.S = realS
    P.finish()


def phase_moba(nc, C, hin, hout, W, name, nblk=NBLK, dbg=None):
    G = 2
    P = Phase(nc, name)
    S = P.S
    P.consts(C)
    P.norm_setup()
    c256 = P.sb([128, 1], F32)
    S.op("vector", lambda e: e.memset(c256[:], 1.0 / 256.0), writes=[P.Rc], join=True)
    tri4 = P.sb([128, 512], BF16)
    S.dma("gpsimd", tri4[:], C["c_tribias4"], writes=[P.Rc], join=True)
    ropet = P.sb([128, 32, 16], F32)
    S.dma("sync", ropet[:], C["c_rope"].rearrange("(i p) c -> p i c", p=128), writes=[P.Rc], join=True)
    pastb, Rpastb = P.load_bcast(C["c_past"], 256)
    ownb, Rownb = P.load_bcast(C["c_own"], 256)
    g_kv, Rg_kv = P.load_vec_fm(W["kv_norm"])
    g_mix, Rg_mix = P.load_vec_fm(W["norm_mix1"])
    knorm, Rknorm = P.load_bcast(W["k_norm"], 64)
    qnorm, Rqnorm = P.load_bcast(W["b_q_norm"], 64)
    wkv, Rwkv = P.load_w(W["w_kv"], 8, 512)
    wq, Rwq = P.load_w(W["b_w_q"], 8, 1024)
    wo, Rwo = P.load_w(W["b_w_o"], 8, 1024)

    pR = [P.ps([128, 512], F32) for _ in range(2)]; RpR = [Res(), Res()]
    psc = [P.ps([128, G * 512], F32) for _ in range(2)]; Rpsc = [Res(), Res()]
    pop = [P.ps([128, 512], F32) for _ in range(2)]; Rpop = [Res(), Res()]
    rot = [0]

    def nextbank():
        rot[0] ^= 1
        return pR[rot[0]], RpR[rot[0]]

    ple = PLE(P, C, W["p0"], W["norm_ple0"], W["w_ple_gate0"], W["w_ple_up0"], [(pR[0], RpR[0]), (pR[1], RpR[1]), (pR[0], RpR[0])])

    hT = P.sb([128, 8, NB], F32); RhT = [Res() for _ in range(8)]
    hn = P.sb([128, 8, NB], BF16); Rhn = [Res() for _ in range(8)]
    KT = P.sb([80, 4, NT], BF16); RKT = [Res() for _ in range(32)]
    Vaug = P.sb([128, 4, 32, 128], BF16); RV = [Res() for _ in range(32)]
    kmT = P.sb([80, 4, 16], BF16); RkmT = Res()
    kms = P.sb([64, 4, 2], F32); Rkms = Res()
    ksb = P.sb([128, 4, 64], F32); Rksb = Res()
    sqk = P.sb([128, 4, 64], F32); Rsqk = Res()
    kbf = P.sb([128, 4, 64], BF16); Rkbf = Res()
    qsb = P.sb([128, 16, 64], F32); Rqsb = Res()
    sqq = P.sb([128, 16, 64], F32); Rsqq = Res()
    qa = P.sb([128, 16, 80], BF16); Rqa = Res()
    QTa = [P.sb([80, 16, 128], BF16) for _ in range(2)]; RQTa = [Res(), Res()]
    gm = P.sb([128, 16, 16], F32); Rgm = Res()
    mx8 = P.sb([128, 16, 8], F32); Rmx8 = Res()
    vis = P.sb([128, 16, 16], F32); Rvis = Res()
    skq = {n: (P.sb([128, 16], F32), Res()) for n in ["ss", "ln", "r"]}
    skk = {n: (P.sb([128, 16], F32), Res()) for n in ["ss", "ln", "r"]}
    rtq = {n: (P.sb([128, 16, 8], F32), Res()) for n in ["t1", "t2", "t3", "t4"]}
    PTb = [P.sb([128, G * 512], BF16) for _ in range(2)]; RPTb = [Res() for _ in range(2)]
    rec = P.sb([128, 512], F32); Rrec = Res()
    OTb = P.sb([128, 8, NB], BF16); ROTb = [Res() for _ in range(4)]

    for kvh in range(4):
        for c0 in range(0, NT, 1024):
            S.dma("gpsimd", KT[64:80, kvh, c0:c0 + 1024], C["c_onehot"][:, c0:c0 + 1024], writes=[P.Rc], join=True)
    S.op("vector", lambda e: e.memset(Vaug[:].rearrange("p a b c -> p (a b c)"), 1.0), writes=[P.Rc], join=True)
    S.op("gpsimd", lambda e: e.memset(kmT[:], 0.0), writes=[RkmT])
    for i in range(2):
        S.op("gpsimd", lambda e, i=i: e.memset(QTa[i][:], 0.0), writes=[RQTa[i]])

    def head_norm_rope(x, Rx, sq_, Rsq_, nh, gbc, Rgbc, it, sk, rt):
        (ss, Rss), (ln, Rln), (r, Rr) = sk["ss"], sk["ln"], sk["r"]
        S.op("gpsimd", lambda e: e.tensor_tensor(out=sq_[:], in0=x[:], in1=x[:], op=ALU.mult), reads=[Rx], writes=[Rsq_])
        S.op("vector", lambda e: e.tensor_reduce(out=ss[:, 0:nh], in_=sq_[:], axis=AX.X, op=ALU.add), reads=[Rsq_], writes=[Rss])
        yield
        S.op("scalar", lambda e: e.activation(out=ln[:, 0:nh], in_=ss[:, 0:nh], func=AF.Ln, scale=1.0 / 64.0, bias=P.eps_t[:, 0:1]),
             reads=[Rss, P.Rc], writes=[Rln])
        S.op("scalar", lambda e: e.activation(out=r[:, 0:nh], in_=ln[:, 0:nh], func=AF.Exp, scale=-0.5), reads=[Rln], writes=[Rr])
        yield
        S.op("vector", lambda e: e.tensor_tensor(out=x[:], in0=x[:], in1=r[:, 0:nh].unsqueeze(2).broadcast_to([128, nh, 64]), op=ALU.mult),
             reads=[Rx, Rr], writes=[Rx])
        S.op("vector", lambda e: e.tensor_tensor(out=x[:], in0=x[:], in1=gbc[:].unsqueeze(1).broadcast_to([128, nh, 64]), op=ALU.mult),
             reads=[Rx, Rgbc], writes=[Rx])
        yield
        cs = ropet[:, it, 0:8].unsqueeze(1).broadcast_to([128, nh, 8])
        sn = ropet[:, it, 8:16].unsqueeze(1).broadcast_to([128, nh, 8])
        x1 = x[:, :, 0:8]
        x2 = x[:, :, 8:16]
        tt = {k: v[0][:, 0:nh, :] for k, v in rt.items()}
        Rt = {k: v[1] for k, v in rt.items()}
        S.op("vector", lambda e: e.tensor_tensor(out=tt["t1"], in0=x1, in1=cs, op=ALU.mult), reads=[Rx, P.Rc], writes=[Rt["t1"]])
        S.op("vector", lambda e: e.tensor_tensor(out=tt["t2"], in0=x2, in1=sn, op=ALU.mult), reads=[Rx, P.Rc], writes=[Rt["t2"]])
        S.op("vector", lambda e: e.tensor_tensor(out=tt["t3"], in0=x2, in1=cs, op=ALU.mult), reads=[Rx, P.Rc], writes=[Rt["t3"]])
        S.op("vector", lambda e: e.tensor_tensor(out=tt["t4"], in0=x1, in1=sn, op=ALU.mult), reads=[Rx, P.Rc], writes=[Rt["t4"]])
        yield
        S.op("vector", lambda e: e.tensor_tensor(out=x1, in0=tt["t1"], in1=tt["t2"], op=ALU.subtract), reads=[Rt["t1"], Rt["t2"], Rx], writes=[Rx])
        S.op("vector", lambda e: e.tensor_tensor(out=x2, in0=tt["t3"], in1=tt["t4"], op=ALU.add), reads=[Rt["t3"], Rt["t4"], Rx], writes=[Rx])
        yield

    def kv_path(blk, s):
        it = blk * 4 + s
        ts = slice(s * 128, (s + 1) * 128)
        pb, Rpb = nextbank()
        for k in range(8):
            S.op("tensor", lambda e, k=k, ts=ts, pb=pb: e.matmul(pb[:], lhsT=hn[:, k, ts], rhs=wkv[:, k, :], start=(k == 0), stop=(k == 7)),
                 reads=[Rwkv[k], Rhn[k]], writes=[Rpb], join=(k > 0))
        S.op("scalar", lambda e, pb=pb: e.copy(out=ksb[:].rearrange("p h d -> p (h d)"), in_=pb[:, 0:256]), reads=[Rpb], writes=[Rksb])
        S.op("scalar", lambda e, pb=pb, it=it: e.copy(out=Vaug[:, :, it, 0:64], in_=pb[:, 256:512].rearrange("p (h d) -> p h d", h=4)),
             reads=[Rpb, P.Rc], writes=[RV[it]])
        for _ in head_norm_rope(ksb, Rksb, sqk, Rsqk, 4, knorm, Rknorm, it, skk, rtq):
            pass
        S.op("gpsimd", lambda e: e.tensor_copy(out=kbf[:], in_=ksb[:]), reads=[Rksb], writes=[Rkbf])
        pb, Rpb = nextbank()
        pbb = pb[:].bitcast(BF16)
        for kvh in range(4):
            S.op("tensor", lambda e, kvh=kvh, pbb=pbb: e.transpose(out=pbb[0:64, kvh * 128:(kvh + 1) * 128], in_=kbf[:, kvh, :], identity=P.ident_b[:]),
                 reads=[Rkbf, P.Rc], writes=[Rpb], join=(kvh > 0))
        S.op("scalar", lambda e, it=it, pbb=pbb: e.copy(out=KT[0:64, :, it * 128:(it + 1) * 128], in_=pbb[0:64, 0:512].rearrange("p (h t) -> p h t", h=4)),
             reads=[Rpb, P.Rc], writes=[RKT[it]])
        pb, Rpb = nextbank()
        for kvh in range(4):
            S.op("tensor", lambda e, kvh=kvh, pb=pb: e.matmul(pb[0:64, kvh:kvh + 1], lhsT=ksb[:, kvh, :], rhs=c256[:, 0:1], start=True, stop=True),
                 reads=[Rksb, P.Rc], writes=[Rpb], join=(kvh > 0))
        S.op("vector", lambda e, s=s, pb=pb: e.tensor_copy(out=kms[:, :, s % 2], in_=pb[0:64, 0:4]), reads=[Rpb], writes=[Rkms], join=(s % 2 == 1))
        if s % 2 == 1:
            n = it // 2
            S.op("vector", lambda e, n=n: e.tensor_tensor(out=kmT[0:64, :, n], in0=kms[:, :, 0], in1=kms[:, :, 1], op=ALU.add),
                 reads=[Rkms], writes=[RkmT])

    def q_path(blk, s):
        it = blk * 4 + s
        b = it // 2
        ts = slice(s * 128, (s + 1) * 128)
        QT, RQT = QTa[it % 2], RQTa[it % 2]
        for half in range(2):
            pb, Rpb = nextbank()
            for k in range(8):
                S.op("tensor", lambda e, k=k, ts=ts, pb=pb, half=half: e.matmul(pb[:], lhsT=hn[:, k, ts], rhs=wq[:, k, half * 512:(half + 1) * 512],
                                                                                 start=(k == 0), stop=(k == 7)),
                     reads=[Rwq[k], Rhn[k]], writes=[Rpb], join=(k > 0))
                if k % 4 == 3:
                    yield
            S.op("scalar", lambda e, pb=pb, half=half: e.copy(out=qsb[:, half * 8:(half + 1) * 8, :].rearrange("p h d -> p (h d)"), in_=pb[:]),
                 reads=[Rpb], writes=[Rqsb], join=(half > 0))
            yield
        for _ in head_norm_rope(qsb, Rqsb, sqq, Rsqq, 16, qnorm, Rqnorm, it, skq, rtq):
            yield
        S.op("gpsimd", lambda e: e.tensor_copy(out=qa[:, :, 0:64], in_=qsb[:]), reads=[Rqsb], writes=[Rqa])
        yield
        for r_ in range(2):
            pb, Rpb = nextbank()
            pbb = pb[:].bitcast(BF16)
            for j in range(8):
                h = r_ * 8 + j
                S.op("tensor", lambda e, h=h, j=j, pbb=pbb: e.transpose(out=pbb[0:64, j * 128:(j + 1) * 128], in_=qa[:, h, 0:64], identity=P.ident_b[:]),
                     reads=[Rqa, P.Rc], writes=[Rpb], join=(j > 0))
                if j % 4 == 3:
                    yield
            S.op("scalar", lambda e, r_=r_, pbb=pbb: e.copy(out=QT[0:64, r_ * 8:(r_ + 1) * 8, :], in_=pbb[0:64, :].rearrange("p (h t) -> p h t", h=8)),
                 reads=[Rpb], writes=[RQT], join=(r_ > 0))
            yield
        pb, Rpb = nextbank()
        for h in range(16):
            S.op("tensor", lambda e, h=h, pb=pb: e.matmul(pb[:, h * 16:(h + 1) * 16], lhsT=QT[:, h, :], rhs=kmT[:, h // 4, :], start=True, stop=True),
                 reads=[RQT, RkmT], writes=[Rpb], join=(h > 0))
            if h % 4 == 3:
                yield
        S.op("vector", lambda e, b=b, pb=pb: e.tensor_tensor(out=gm[:], in0=pb[:, 0:256].rearrange("p (h n) -> p h n", h=16),
                                                              in1=pastb[:, b * 16:(b + 1) * 16].unsqueeze(1).broadcast_to([128, 16, 16]), op=ALU.add),
             reads=[Rpb, Rpastb], writes=[Rgm])
        yield
        for h in range(16):
            S.op("vector", lambda e, h=h: e.max(out=mx8[:, h, :], in_=gm[:, h, :]), reads=[Rgm], writes=[Rmx8], join=(h > 0))
            if h % 4 == 3:
                yield
        S.op("vector", lambda e: e.tensor_tensor(out=vis[:], in0=gm[:], in1=mx8[:, :, 2:3].broadcast_to([128, 16, 16]), op=ALU.is_ge),
             reads=[Rgm, Rmx8], writes=[Rvis])
        S.op("vector", lambda e, b=b: e.tensor_tensor(out=vis[:], in0=vis[:], in1=ownb[:, b * 16:(b + 1) * 16].unsqueeze(1).broadcast_to([128, 16, 16]), op=ALU.max),
             reads=[Rvis, Rownb], writes=[Rvis])
        S.op("vector", lambda e: e.tensor_scalar(out=qa[:, :, 64:80], in0=vis[:], scalar1=-NEG, scalar2=NEG, op0=ALU.mult, op1=ALU.add),
             reads=[Rvis, Rqa], writes=[Rqa])
        yield
        for r_ in range(2):
            pb, Rpb = nextbank()
            pbb = pb[:].bitcast(BF16)
            for j in range(8):
                h = r_ * 8 + j
                S.op("tensor", lambda e, h=h, j=j, pbb=pbb: e.transpose(out=pbb[0:80, j * 128:(j + 1) * 128], in_=qa[:, h, :], identity=P.ident_b[:]),
                     reads=[Rqa, P.Rc], writes=[Rpb], join=(j > 0))
                if j % 4 == 3:
                    yield
            S.op("scalar", lambda e, r_=r_, pbb=pbb: e.copy(out=QT[:, r_ * 8:(r_ + 1) * 8, :], in_=pbb[0:80, :].rearrange("p (h t) -> p h t", h=8)),
                 reads=[Rpb], writes=[RQT], join=(r_ > 0))
            yield

    nsc = [0]

    def attention(blk, s):
        it = blk * 4 + s
        ts = slice(s * 128, (s + 1) * 128)
        QT, RQT = QTa[it % 2], RQTa[it % 2]
        L = []
        for kvh in range(4):
            for j0 in range(0, it + 1, G):
                L.append((kvh, j0, min(it + 1, j0 + G)))
        bufs = {}

        def qk(n):
            kvh, j0, j1 = L[n]
            m = nsc[0]
            nsc[0] += 1
            bufs[n] = m % 2
            ps_, Rps_ = psc[m % 2], Rpsc[m % 2]
            qg = QT[:, 4 * kvh:4 * kvh + 4, :].rearrange("p h t -> p (h t)")
            for j in range(j0, j1):
                o = (j - j0) * 512
                diag = (j == it)
                S.op("tensor", lambda e, kvh=kvh, j=j, ps_=ps_, qg=qg, diag=diag, o=o: e.matmul(ps_[:, o:o + 512], lhsT=KT[:, kvh, j * 128:(j + 1) * 128], rhs=qg,
                                                                                               start=True, stop=(not diag)),
                     reads=[RKT[j], RQT, P.Rc], writes=[Rps_], join=(j > j0))
                if diag:
                    S.op("tensor", lambda e, ps_=ps_, o=o: e.matmul(ps_[:, o:o + 512], lhsT=P.ident_b[:], rhs=tri4[:], start=False, stop=True),
                         reads=[P.Rc], writes=[Rps_], join=True)

        def ex(n):
            kvh, j0, j1 = L[n]
            bi = bufs[n]
            w = (j1 - j0) * 512
            S.op("scalar", lambda e, bi=bi, w=w: e.activation(out=PTb[bi][:, 0:w], in_=psc[bi][:, 0:w], func=AF.Exp, scale=0.125),
                 reads=[Rpsc[bi]], writes=[RPTb[bi]])

        def pv(n):
            kvh, j0, j1 = L[n]
            bi = bufs[n]
            po, Rpo = pop[kvh % 2], Rpop[kvh % 2]
            for j in range(j0, j1):
                o = (j - j0) * 512
                S.op("tensor", lambda e, kvh=kvh, j=j, bi=bi, po=po, o=o, it=it: e.matmul(po[:], lhsT=Vaug[:, kvh, j, :], rhs=PTb[bi][:, o:o + 512],
                                                                                         start=(j == 0), stop=(j == it)),
                     reads=[RV[j], RPTb[bi], P.Rc], writes=[Rpo], join=(j > 0))

        def epi(kvh):
            po, Rpo = pop[kvh % 2], Rpop[kvh % 2]
            S.op("vector", lambda e, po=po: e.reciprocal(out=rec[64:128, :], in_=po[64:128, :]), reads=[Rpo], writes=[Rrec])
            for gq in range(4):
                hh = 4 * kvh + gq
                pr, ph = hh // 2, hh % 2
                S.op("vector", lambda e, po=po, gq=gq, pr=pr, ph=ph, ts=ts: e.tensor_tensor(
                    out=OTb[ph * 64:(ph + 1) * 64, pr, ts], in0=po[0:64, gq * 128:(gq + 1) * 128], in1=rec[64:128, gq * 128:(gq + 1) * 128], op=ALU.mult),
                     reads=[Rpo, Rrec], writes=[ROTb[s]], join=True)

        qk(0)
        for n in range(len(L)):
            if n + 1 < len(L):
                qk(n + 1)
            ex(n)
            pv(n)
            if n + 1 == len(L) or L[n + 1][0] != L[n][0]:
                epi(L[n][0])
            yield

    def drive(main, side, side_len):
        steps = list(range(0))
        mains = main
        if side is None:
            for _ in mains:
                pass
            return
        done = [False]

        def adv(k):
            for _ in range(k):
                if done[0]:
                    return
                try:
                    next(side)
                except StopIteration:
                    done[0] = True
        nmain = side_len[0]
        per = max(1, -(-side_len[1] // max(1, nmain)))
        for _ in mains:
            adv(per)
        while not done[0]:
            adv(8)

    for blk in range(nblk):
        t0 = blk * NB
        for k in range(8):
            S.dma("sync", hT[:, k, :], hin[k, :, t0:t0 + NB], writes=[RhT[k]])
        pb, Rpb = nextbank()
        ple.emit(blk, hT, RhT, hn, Rhn, pb, Rpb)
        pb, Rpb = nextbank()
        P.fm_norm(hT, RhT, g_kv, Rg_kv, hn, Rhn, pb, Rpb)
        for s in range(4):
            kv_path(blk, s)
        pb, Rpb = nextbank()
        P.fm_norm(hT, RhT, g_mix, Rg_mix, hn, Rhn, pb, Rpb, reuse_rstd=True)
        for _ in q_path(blk, 0):
            pass
        for s in range(4):
            it = blk * 4 + s
            nsteps = 4 * (-(-(it + 1) // G))
            side = q_path(blk, s + 1) if s < 3 else None
            drive(attention(blk, s), side, (nsteps, 60))
        for d in range(8):
            pb, Rpb = nextbank()
            for pr in range(8):
                S.op("tensor", lambda e, pr=pr, d=d, pb=pb: e.matmul(pb[:], lhsT=wo[:, pr, d * 128:(d + 1) * 128], rhs=OTb[:, pr, :], start=(pr == 0), stop=(pr == 7)),
                     reads=[Rwo[pr]] + ROTb, writes=[Rpb], join=(pr > 0))
            S.op("vector", lambda e, d=d, pb=pb: e.tensor_tensor(out=hT[:, d, :], in0=pb[:], in1=hT[:, d, :], op=ALU.add), reads=[Rpb, RhT[d]], writes=[RhT[d]])
            S.dma("sync", hout[d, :, t0:t0 + NB], hT[:, d, :], reads=[RhT[d]], writes=[P.Rdram], join=True)
    P.finish()


WSHAPES = {
    "norm_mix0": [1024], "norm_mix1": [1024], "a_w_in": [1024, 3088], "a_b_gate": [16], "a_mh_gain": [128], "a_w_out": [1024, 1024],
    "kv_norm": [1024], "w_kv": [1024, 512], "k_norm": [64], "b_w_q": [1024, 1024], "b_q_norm": [64], "b_w_o": [1024, 1024],
    "norm_ffn0": [1024], "norm_ffn1": [1024], "w_gate_up0": [1024, 5632], "w_gate_up1": [1024, 5632], "w_down0": [2816, 1024], "w_down1": [2816, 1024],
    "norm_ple0": [1024], "norm_ple1": [1024], "w_ple_gate0": [1024, 1024], "w_ple_gate1": [1024, 1024], "w_ple_up0": [256, 1024], "w_ple_up1": [256, 1024],
    "p0": [NT, 256], "p1": [NT, 256],
}


def build_program():
    nc = bass.Bass("TRN2", target_bir_lowering=False)
    Cn = make_consts()
    C = {k: nc.dram_tensor(k, list(v.shape), F32, kind="ExternalInput").ap() for k, v in Cn.items()}
    x = nc.dram_tensor("x", [NT, 1024], F32, kind="ExternalInput").ap()
    out = nc.dram_tensor("out", [NT, 1024], F32, kind="ExternalOutput").ap()
    W = {k: nc.dram_tensor(k, v, F32, kind="ExternalInput").ap() for k, v in WSHAPES.items()}
    hs = [nc.dram_tensor("h_scr%d" % i, [8, 128, NT], F32, kind="Internal").ap() for i in range(4)]
    phase_mlstm(nc, C, x, hs[0], W, "ml")
    phase_ffn(nc, C, hs[0], hs[1], W["w_gate_up0"], W["w_down0"], W["norm_ffn0"], "f0")
    phase_moba(nc, C, hs[1], hs[2], W, "mb")
    phase_ffn(nc, C, hs[2], hs[3], W["w_gate_up1"], W["w_down1"], W["norm_ffn1"], "f1")
    phase_ple_out(nc, C, hs[3], out, W["p1"], W["norm_ple1"], W["w_ple_gate1"], W["w_ple_up1"], "po")
    return nc, Cn


def make_in_maps(inputs, cores):
    f = lambda a: np.ascontiguousarray(np.asarray(a, dtype=np.float32))
    I = {k: np.asarray(v) for k, v in inputs.items()}
    shared = {
        "norm_mix0": f(I["norm_mix"][0]), "norm_mix1": f(I["norm_mix"][1]), "a_w_in": f(I["a_w_in"][0]), "a_b_gate": f(I["a_b_gate"][0]),
        "a_mh_gain": f(I["a_mh_gain"][0]), "a_w_out": f(I["a_w_out"][0]), "kv_norm": f(I["kv_norm"]), "w_kv": f(I["w_kv"]), "k_norm": f(I["k_norm"]),
        "b_w_q": f(I["b_w_q"][0]), "b_q_norm": f(I["b_q_norm"][0]), "b_w_o": f(I["b_w_o"][0]),
        "norm_ffn0": f(I["norm_ffn"][0]), "norm_ffn1": f(I["norm_ffn"][1]), "w_gate_up0": f(I["w_gate_up"][0]), "w_gate_up1": f(I["w_gate_up"][1]),
        "w_down0": f(I["w_down"][0]), "w_down1": f(I["w_down"][1]), "norm_ple0": f(I["norm_ple"][0]), "norm_ple1": f(I["norm_ple"][1]),
        "w_ple_gate0": f(I["w_ple_gate"][0]), "w_ple_gate1": f(I["w_ple_gate"][1]), "w_ple_up0": f(I["w_ple_up"][0]), "w_ple_up1": f(I["w_ple_up"][1]),
    }
    maps = []
    for b in cores:
        m = dict(shared)
        m["x"] = f(I["x"][b])
        m["p0"] = f(I["p"][0, b])
        m["p1"] = f(I["p"][1, b])
        maps.append(m)
    return maps


def kernel(**inputs):
    nc, Cn = build_program()
    maps = make_in_maps(inputs, list(range(8)))
    for m in maps:
        m.update(Cn)
    res = run_bass_kernel_spmd(nc, maps, core_ids=list(range(8)))
    return np.stack([np.asarray(r["out"], dtype=np.float32) for r in res.results], axis=0)
```

```python
import numpy as np
import concourse.bass as bass
import concourse.mybir as mybir
from concourse.bass_utils import run_bass_kernel_spmd
from contextlib import ExitStack

F32 = mybir.dt.float32
BF16 = mybir.dt.bfloat16
ALU = mybir.AluOpType
AF = mybir.ActivationFunctionType
AX = mybir.AxisListType

ENGS = ["tensor", "vector", "scalar", "gpsimd", "sync"]
NDMASEM = 12
SAME_ENGINE_SYNC = True

NT = 4096
NB = 512
NBLK = NT // NB
EPS = 1e-6
NEG = -30000.0


class Res:
    __slots__ = ("name", "w", "r", "gd")

    def __init__(self, name=""):
        self.name = name
        self.w = []
        self.r = []
        self.gd = []


class Op:
    __slots__ = ("eng", "fn", "waits", "pos", "sig", "isdma", "semi", "semk", "K", "sigidx")


class Sched:
    G = {"nc": None}

    @staticmethod
    def setup(nc):
        if Sched.G.get("nc") is nc:
            return
        es = ExitStack()
        G = {"nc": nc, "es": es, "sig": {e: 0 for e in ENGS}, "dma": {e: 0 for e in ENGS}}
        G["esem"] = {e: es.enter_context(nc.semaphore("sem_e_%s" % e)) for e in ENGS}
        G["dsem"] = {(e, i): es.enter_context(nc.semaphore("sem_d_%s_%d" % (e, i))) for e in ("sync", "gpsimd") for i in range(NDMASEM)}
        Sched.G = G

    def __init__(self, nc):
        Sched.setup(nc)
        self.nc = nc
        self.ops = {e: [] for e in ENGS}
        self.Kcur = {e: {} for e in ENGS}
        self.nops = 0

    limit = None

    def _record(self, eng, fn, reads, writes, isdma, join=False):
        if Sched.limit is not None and self.nops >= Sched.limit:
            return None
        o = Op()
        o.eng = eng
        o.fn = fn
        o.isdma = isdma
        o.sig = False
        o.pos = len(self.ops[eng])
        deps = {}
        for r in reads:
            for x in r.w:
                deps[id(x)] = x
        for w in writes:
            if join and not w.r:
                for x in w.gd:
                    deps[id(x)] = x
            else:
                for x in w.w:
                    deps[id(x)] = x
                for x in w.r:
                    deps[id(x)] = x
        K = self.Kcur[eng]
        newK = None
        waits = []
        if isdma:
            i = Sched.G["dma"][eng]
            Sched.G["dma"][eng] += 1
            o.semi = i % NDMASEM
            o.semk = i // NDMASEM + 1
            if o.semk > 1:
                key = ("d", eng, o.semi)
                if K.get(key, 0) < o.semk - 1:
                    waits.append(("dmaslot", eng, o.semi, o.semk - 1))
                    newK = dict(K)
                    newK[key] = o.semk - 1
        best = {}
        dl = []
        for y in deps.values():
            if y is o:
                continue
            if y.isdma:
                dl.append(y)
            elif y.eng not in best or best[y.eng].pos < y.pos:
                best[y.eng] = y
        for y in dl + list(best.values()):
            cur = K if newK is None else newK
            if y.isdma:
                key = ("d", y.eng, y.semi)
                if cur.get(key, 0) >= y.semk:
                    continue
                waits.append(("dma", y))
            else:
                if y.eng == eng and (eng == "tensor" or not SAME_ENGINE_SYNC) and not isdma:
                    continue
                if cur.get(y.eng, -1) >= y.pos:
                    continue
                y.sig = True
                waits.append(("eng", y))
            if newK is None:
                newK = dict(K)
            for k, v in y.K.items():
                if newK.get(k, -1) < v:
                    newK[k] = v
            if y.isdma:
                key = ("d", y.eng, y.semi)
                newK[key] = max(newK.get(key, 0), y.semk)
            else:
                if newK.get(y.eng, -1) < y.pos:
                    newK[y.eng] = y.pos
        if newK is not None:
            self.Kcur[eng] = newK
            K = newK
        o.K = K
        o.waits = waits
        for r in reads:
            r.r.append(o)
        for w in writes:
            if join and not w.r:
                w.w.append(o)
            else:
                w.gd = w.w + w.r
                w.w = [o]
                w.r = []
        self.ops[eng].append(o)
        self.nops += 1
        return o

    def op(self, eng, fn, reads=(), writes=(), join=False):
        return self._record(eng, fn, reads, writes, False, join)

    def dma(self, queue, out, in_, reads=(), writes=(), join=False, nonc=False, **kw):
        nc = self.nc
        if nonc:
            def f(e):
                with nc.allow_non_contiguous_dma(reason="small strided parameter load"):
                    return e.dma_start(out=out, in_=in_, **kw)
        else:
            def f(e):
                return e.dma_start(out=out, in_=in_, **kw)
        return self._record(queue, f, reads, writes, True, join)

    def emit(self):
        nc = self.nc
        G = Sched.G
        for e in ENGS:
            n = G["sig"][e]
            for o in self.ops[e]:
                if o.sig:
                    n += 1
                o.sigidx = n
            G["sig"][e] = n
        esem, dsem = G["esem"], G["dsem"]
        with nc.Block() as block:
            def stream(ename):
                def body(eng):
                    for o in self.ops[ename]:
                        for w in o.waits:
                            if w[0] == "dmaslot":
                                eng.wait_ge(dsem[(w[1], w[2])], 16 * w[3])
                            elif w[0] == "dma":
                                y = w[1]
                                eng.wait_ge(dsem[(y.eng, y.semi)], 16 * y.semk)
                            else:
                                y = w[1]
                                eng.wait_ge(esem[y.eng], y.sigidx)
                        ins = o.fn(eng)
                        if o.isdma:
                            ins.then_inc(dsem[(o.eng, o.semi)], 16)
                        elif o.sig:
                            ins.then_inc(esem[o.eng], 1)
                return body

            for e in ENGS:
                if self.ops[e]:
                    getattr(block, e)(stream(e))


class Proxy:
    def __init__(self, real):
        self.real = real
        self.tgt = real

    def op(self, *a, **k):
        return self.tgt.op(*a, **k)

    def dma(self, *a, **k):
        return self.tgt.dma(*a, **k)


class Mux:
    def __init__(self, real, chunks):
        self.real = real
        self.chunks = chunks
        self.last = None

    def _maybe(self, eng):
        if self.last == "tensor" and eng != "tensor" and self.chunks:
            for kind, a, k in self.chunks.pop(0):
                getattr(self.real, kind)(*a, **k)
        self.last = eng

    def op(self, eng, *a, **k):
        self._maybe(eng)
        return self.real.op(eng, *a, **k)

    def dma(self, eng, *a, **k):
        self._maybe(eng)
        return self.real.dma(eng, *a, **k)

    def flush(self):
        while self.chunks:
            for kind, a, k in self.chunks.pop(0):
                getattr(self.real, kind)(*a, **k)


def mm_chunks(q):
    runs = []
    for item in q:
        is_mm = (item[0] == "op" and item[1][0] == "tensor")
        if runs and runs[-1][0] == is_mm:
            runs[-1][1].append(item)
        else:
            runs.append((is_mm, [item]))
    chunks = []
    cur = []
    for is_mm, items in runs:
        cur.extend(items)
        if is_mm:
            chunks.append(cur)
            cur = []
    if cur:
        chunks.append(cur)
    return chunks


class Deferred:
    def __init__(self):
        self.q = []

    def op(self, *a, **k):
        self.q.append(("op", a, k))

    def dma(self, *a, **k):
        self.q.append(("dma", a, k))


def interleave(main_gen, real, q):
    steps = list(main_gen) if False else None
    i = 0
    n = getattr(main_gen, "nsteps", None)
    return i


class Phase:
    def __init__(self, nc, name):
        self.nc = nc
        self.name = name
        self.es = ExitStack()
        self.S = Sched(nc)
        self.n = 0
        self.Rdram = Res("dram_out")

    def sb(self, shape, dt, name=None):
        self.n += 1
        t = self.es.enter_context(self.nc.sbuf_tensor("%s_s%d" % (self.name, self.n), list(shape), dt))
        return t

    def ps(self, shape, dt, name=None):
        self.n += 1
        t = self.es.enter_context(self.nc.psum_tensor("%s_p%d" % (self.name, self.n), list(shape), dt))
        return t

    def finish(self):
        S = self.S
        S.op("sync", lambda e: e.nop(), reads=[self.Rdram])
        S.emit()
        self.es.close()

    def consts(self, C):
        S = self.S
        self.ident_f = self.sb([128, 128], F32)
        self.ident_b = self.sb([128, 128], BF16)
        self.ones_b = self.sb([128, 128], BF16)
        self.eps_t = self.sb([128, 1], F32)
        self.one_t = self.sb([128, 1], F32)
        self.Rc = Res("consts")
        S.dma("sync", self.ident_f[:], C["c_ident"], writes=[self.Rc], join=True)
        S.op("vector", lambda e: e.tensor_copy(out=self.ident_b[:], in_=self.ident_f[:]), reads=[self.Rc], writes=[self.Rc])
        S.op("vector", lambda e: e.memset(self.ones_b[:], 1.0), writes=[self.Rc], join=True)
        S.op("vector", lambda e: e.memset(self.eps_t[:], EPS), writes=[self.Rc], join=True)
        S.op("vector", lambda e: e.memset(self.one_t[:], 1.0), writes=[self.Rc], join=True)

    def load_vec_fm(self, ap1024, nk=8):
        t = self.sb([128, nk], F32)
        r = Res()
        self.S.dma("sync", t[:], ap1024.rearrange("(k p) -> p k", p=128), writes=[r], nonc=True)
        return t, r

    def load_bcast(self, ap_flat, n):
        t = self.sb([128, n], F32)
        r = Res()
        self.S.dma("sync", t[:], ap_flat.partition_broadcast(128), writes=[r])
        return t, r

    def load_w(self, src, K, N, rows=128, col_chunk=2048, queue="gpsimd"):
        t = self.sb([rows, K, N], BF16)
        rs = [Res() for _ in range(K)]
        nch = -(-N // col_chunk)
        cw = -(-N // nch)
        for k in range(K):
            c0 = 0
            while c0 < N:
                c1 = min(N, c0 + cw)
                self.S.dma(queue, t[:, k, c0:c1], src[k * rows:(k + 1) * rows, c0:c1], writes=[rs[k]], join=True)
                c0 = c1
        return t, rs

    def norm_setup(self):
        self.rstd = self.sb([128, NB], F32)
        self.Rrstd = Res()
        self.lnv = self.rstd
        self.Rlnv = self.Rrstd

    def fm_norm(self, hT, RhT, g, Rg, hn, Rhn, pss, Rpss, reuse_rstd=False):
        S = self.S
        n = hT.shape[2]
        sq, Rsq = hn, Rhn
        for k in range(8 if not reuse_rstd else 0):
            S.op("scalar", lambda e, k=k: e.activation(out=sq[:, k, :], in_=hT[:, k, :], func=AF.Square),
                 reads=[RhT[k]], writes=[Rsq[k]])
        for k in range(8 if not reuse_rstd else 0):
            S.op("tensor", lambda e, k=k: e.matmul(pss[:, 0:n], lhsT=self.ones_b[:], rhs=sq[:, k, :], start=(k == 0), stop=(k == 7)),
                 reads=[Rsq[k], self.Rc], writes=[Rpss], join=(k > 0))
        if not reuse_rstd:
            S.op("scalar", lambda e: e.activation(out=self.lnv[:, 0:n], in_=pss[:, 0:n], func=AF.Ln, scale=1.0 / 1024.0, bias=self.eps_t[:, 0:1]),
                 reads=[Rpss, self.Rc], writes=[self.Rlnv])
            S.op("scalar", lambda e: e.activation(out=self.rstd[:, 0:n], in_=self.lnv[:, 0:n], func=AF.Exp, scale=-0.5),
                 reads=[self.Rlnv], writes=[self.Rrstd])
        for k in range(8):
            S.op("vector", lambda e, k=k: e.scalar_tensor_tensor(out=hn[:, k, :], in0=hT[:, k, :], scalar=g[:, k:k + 1], op0=ALU.mult,
                                                                 in1=self.rstd[:, 0:n], op1=ALU.mult),
                 reads=[RhT[k], self.Rrstd, Rg], writes=[Rhn[k]])


def phase_ffn(nc, C, hin, hout, wgu_ap, wd_ap, g_ap, name):
    P = Phase(nc, name)
    S = P.S
    P.consts(C)
    g, Rg = P.load_vec_fm(g_ap)
    wgu, Rwgu = P.load_w(wgu_ap, 8, 5632, col_chunk=1408)
    wd, Rwd = P.load_w(wd_ap, 22, 1024)
    P.norm_setup()
    hTs = [P.sb([128, 8, NB], F32) for _ in range(2)]
    RhTs = [[Res() for _ in range(8)] for _ in range(2)]
    hns = [P.sb([128, 8, NB], BF16) for _ in range(2)]
    Rhns = [[Res() for _ in range(8)] for _ in range(2)]
    act = P.sb([128, 22, NB], BF16)
    Ract = [Res() for _ in range(22)]
    sg = [P.sb([128, NB], BF16) for _ in range(2)]
    Rsg = [Res() for _ in range(2)]
    pss = P.ps([128, NB], F32)
    Rpss = Res()
    pg = [P.ps([128, NB], F32) for _ in range(2)]
    Rpg = [Res() for _ in range(2)]
    pu = [P.ps([128, NB], F32) for _ in range(2)]
    Rpu = [Res() for _ in range(2)]
    po = [P.ps([128, NB], F32) for _ in range(2)]
    Rpo = [Res() for _ in range(2)]

    def load(blk):
        t0 = blk * NB
        for k in range(8):
            S.dma("sync", hTs[blk % 2][:, k, :], hin[k, :, t0:t0 + NB], writes=[RhTs[blk % 2][k]])

    def norm(blk):
        P.fm_norm(hTs[blk % 2], RhTs[blk % 2], g, Rg, hns[blk % 2], Rhns[blk % 2], pss, Rpss)

    load(0)
    norm(0)
    for blk in range(NBLK):
        t0 = blk * NB
        hT, RhT = hTs[blk % 2], RhTs[blk % 2]
        hn, Rhn = hns[blk % 2], Rhns[blk % 2]
        if blk + 1 < NBLK:
            load(blk + 1)
        for c in range(22):
            b = c % 2
            for k in range(8):
                S.op("tensor", lambda e, k=k, c=c, b=b, hn=hn: e.matmul(pg[b][:], lhsT=wgu[:, k, c * 128:(c + 1) * 128], rhs=hn[:, k, :],
                                                                        start=(k == 0), stop=(k == 7)),
                     reads=[Rwgu[k], Rhn[k]], writes=[Rpg[b]], join=(k > 0))
            for k in range(8):
                S.op("tensor", lambda e, k=k, c=c, b=b, hn=hn: e.matmul(pu[b][:], lhsT=wgu[:, k, 2816 + c * 128:2816 + (c + 1) * 128], rhs=hn[:, k, :],
                                                                        start=(k == 0), stop=(k == 7)),
                     reads=[Rwgu[k], Rhn[k]], writes=[Rpu[b]], join=(k > 0))
            S.op("scalar", lambda e, b=b: e.activation(out=sg[b][:], in_=pg[b][:], func=AF.Silu), reads=[Rpg[b]], writes=[Rsg[b]])
            S.op("vector", lambda e, b=b, c=c: e.tensor_tensor(out=act[:, c, :], in0=pu[b][:], in1=sg[b][:], op=ALU.mult),
                 reads=[Rpu[b], Rsg[b]], writes=[Ract[c]])
        if blk + 1 < NBLK:
            norm(blk + 1)
        for d in range(8):
            b = d % 2
            for c in range(22):
                S.op("tensor", lambda e, c=c, d=d, b=b: e.matmul(po[b][:], lhsT=wd[:, c, d * 128:(d + 1) * 128], rhs=act[:, c, :],
                                                                  start=(c == 0), stop=(c == 21)),
                     reads=[Rwd[c], Ract[c]], writes=[Rpo[b]], join=(c > 0))
            S.op("vector", lambda e, d=d, b=b, hT=hT: e.tensor_tensor(out=hT[:, d, :], in0=po[b][:], in1=hT[:, d, :], op=ALU.add),
                 reads=[Rpo[b], RhT[d]], writes=[RhT[d]])
            S.dma("sync", hout[d, :, t0:t0 + NB], hT[:, d, :], reads=[RhT[d]], writes=[P.Rdram], join=True)
    P.finish()


def make_consts():
    c = {}
    c["c_ident"] = np.eye(128, dtype=np.float32)
    s = np.arange(128)
    c["c_tri"] = (s[:, None] <= s[None, :]).astype(np.float32)
    c["c_cbias"] = np.where(s[:, None] <= s[None, :], 0.0, NEG).astype(np.float32)
    inv = 500000.0 ** (-np.arange(0, 16, 2, dtype=np.float64) / 16.0)
    ang = np.arange(NT, dtype=np.float64)[:, None] * inv[None, :]
    c["c_rope"] = np.concatenate([np.cos(ang), np.sin(ang)], axis=1).astype(np.float32)
    u = np.arange(NT)
    c["c_onehot"] = (u[None, :] // 256 == np.arange(16)[:, None]).astype(np.float32)
    c["c_tribias4"] = np.tile(c["c_cbias"], (1, 4)).astype(np.float32)
    b = np.arange(16)
    c["c_past"] = np.where(b[None, :] < b[:, None], 0.0, NEG).astype(np.float32).reshape(256)
    c["c_own"] = (b[None, :] == b[:, None]).astype(np.float32).reshape(256)
    return c


class PLE:
    def __init__(self, P, C, p_ap, g_ap, wpg_ap, wpu_ap, banks, nb=NB, nbuf=1):
        self.P = P
        self.nb = nb
        S = P.S
        self.p_ap = p_ap
        self.g, self.Rg = P.load_vec_fm(g_ap)
        self.wpg, self.Rwpg = P.load_w(wpg_ap, 8, 1024)
        self.wpu, self.Rwpu = P.load_w(wpu_ap, 2, 1024)
        self.ptm = [P.sb([128, nb // 128, 256], F32) for _ in range(nbuf)]
        self.Rptm = [Res() for _ in range(nbuf)]
        self.pT = [P.sb([128, 2, nb], BF16) for _ in range(nbuf)]
        self.RpT = [[Res(), Res()] for _ in range(nbuf)]
        self.nbuf = nbuf
        self.sgate = P.sb([128, nb], F32)
        self.Rsgate = Res()
        self.tmp = P.sb([128, nb], F32)
        self.Rtmp = Res()
        self.banks = banks

    def pre(self, blk, hT, RhT, hn, Rhn, pss, Rpss):
        P = self.P
        S = P.S
        nb = self.nb
        t0 = blk * nb
        i = blk % self.nbuf
        ptm, Rptm, pT, RpT = self.ptm[i], self.Rptm[i], self.pT[i], self.RpT[i]
        (pc, Rpc) = self.banks[2]
        P.fm_norm(hT, RhT, self.g, self.Rg, hn, Rhn, pss, Rpss)
        S.dma("sync", ptm[:], self.p_ap[t0:t0 + nb, :].rearrange("(s p) d -> p s d", p=128), writes=[Rptm])
        for kk in range(2):
            for s in range(nb // 128):
                S.op("tensor", lambda e, kk=kk, s=s: e.transpose(out=pc[:, s * 128:(s + 1) * 128], in_=ptm[:, s, kk * 128:(kk + 1) * 128],
                                                                 identity=P.ident_f[:]),
                     reads=[Rptm, P.Rc], writes=[Rpc], join=(s > 0))
            S.op("scalar", lambda e, kk=kk: e.copy(out=pT[:, kk, :], in_=pc[:, 0:nb]), reads=[Rpc], writes=[RpT[kk]])

    def main(self, blk, hT, RhT, hn, Rhn):
        P = self.P
        S = P.S
        nb = self.nb
        i = blk % self.nbuf
        pT, RpT = self.pT[i], self.RpT[i]
        (pa, Rpa), (pb, Rpb) = self.banks[:2]
        for d in range(8):
            for k in range(8):
                S.op("tensor", lambda e, k=k, d=d: e.matmul(pa[:, 0:nb], lhsT=self.wpg[:, k, d * 128:(d + 1) * 128], rhs=hn[:, k, :],
                                                             start=(k == 0), stop=(k == 7)),
                     reads=[self.Rwpg[k], Rhn[k]], writes=[Rpa], join=(k > 0))
            S.op("scalar", lambda e: e.activation(out=self.sgate[:], in_=pa[:, 0:nb], func=AF.Sigmoid), reads=[Rpa], writes=[self.Rsgate])
            for kk in range(2):
                S.op("tensor", lambda e, kk=kk, d=d: e.matmul(pb[:, 0:nb], lhsT=self.wpu[:, kk, d * 128:(d + 1) * 128], rhs=pT[:, kk, :],
                                                               start=(kk == 0), stop=(kk == 1)),
                     reads=[self.Rwpu[kk], RpT[kk]], writes=[Rpb], join=(kk > 0))
            S.op("vector", lambda e: e.tensor_tensor(out=self.tmp[:], in0=pb[:, 0:nb], in1=self.sgate[:], op=ALU.mult),
                 reads=[Rpb, self.Rsgate], writes=[self.Rtmp])
            S.op("gpsimd", lambda e, d=d: e.tensor_tensor(out=hT[:, d, :], in0=hT[:, d, :], in1=self.tmp[:], op=ALU.add),
                 reads=[self.Rtmp, RhT[d]], writes=[RhT[d]])

    def emit(self, blk, hT, RhT, hn, Rhn, pss, Rpss):
        self.pre(blk, hT, RhT, hn, Rhn, pss, Rpss)
        self.main(blk, hT, RhT, hn, Rhn)


def phase_ple_out(nc, C, hin, out_ap, p_ap, g_ap, wpg_ap, wpu_ap, name):
    P = Phase(nc, name)
    S = P.S
    P.consts(C)
    P.norm_setup()
    banks = [(P.ps([128, NB], F32), Res()) for _ in range(5)]
    pss, Rpss = P.ps([128, NB], F32), Res()
    ple = PLE(P, C, p_ap, g_ap, wpg_ap, wpu_ap, banks, nbuf=2)
    hTs = [P.sb([128, 8, NB], F32) for _ in range(2)]
    RhTs = [[Res() for _ in range(8)] for _ in range(2)]
    hns = [P.sb([128, 8, NB], BF16) for _ in range(2)]
    Rhns = [[Res() for _ in range(8)] for _ in range(2)]
    otm = P.sb([128, 4, 1024], F32)
    Rotm = [Res() for _ in range(4)]

    def load(blk):
        t0 = blk * NB
        for k in range(8):
            S.dma("sync", hTs[blk % 2][:, k, :], hin[k, :, t0:t0 + NB], writes=[RhTs[blk % 2][k]])

    load(0)
    ple.pre(0, hTs[0], RhTs[0], hns[0], Rhns[0], pss, Rpss)
    for blk in range(NBLK):
        t0 = blk * NB
        hT, RhT = hTs[blk % 2], RhTs[blk % 2]
        if blk + 1 < NBLK:
            load(blk + 1)
        ple.main(blk, hT, RhT, hns[blk % 2], Rhns[blk % 2])
        if blk + 1 < NBLK:
            n = (blk + 1) % 2
            ple.pre(blk + 1, hTs[n], RhTs[n], hns[n], Rhns[n], pss, Rpss)
        for s in range(4):
            for kq in range(2):
                pt, Rpt = banks[3 + kq]
                for k4 in range(4):
                    k = kq * 4 + k4
                    S.op("tensor", lambda e, k=k, k4=k4, s=s, pt=pt, hT=hT: e.transpose(out=pt[:, k4 * 128:(k4 + 1) * 128], in_=hT[:, k, s * 128:(s + 1) * 128],
                                                                                        identity=P.ident_f[:]),
                         reads=[RhT[k], P.Rc], writes=[Rpt], join=(k4 > 0))
                if kq == 0:
                    S.op("scalar", lambda e, s=s, kq=kq, pt=pt: e.copy(out=otm[:, s, kq * 512:(kq + 1) * 512], in_=pt[:]),
                         reads=[Rpt], writes=[Rotm[s]], join=(kq > 0))
                else:
                    S.op("vector", lambda e, s=s, kq=kq, pt=pt: e.tensor_copy(out=otm[:, s, kq * 512:(kq + 1) * 512], in_=pt[:]),
                         reads=[Rpt], writes=[Rotm[s]], join=(kq > 0))
            S.dma("sync", out_ap[t0 + s * 128:t0 + (s + 1) * 128, :], otm[:, s, :], reads=[Rotm[s]], writes=[P.Rdram], join=True)
    P.finish()


def phase_mlstm(nc, C, x_ap, hout, W, name, nblk=NBLK):
    P = Phase(nc, name)
    realS = P.S
    S = Proxy(realS)
    P.S = S
    P.consts(C)
    P.norm_setup()
    tri_f = P.sb([128, 128], F32)
    cbias = P.sb([128, 128], F32)
    ones_f = P.sb([128, 128], F32)
    S.dma("sync", tri_f[:], C["c_tri"], writes=[P.Rc], join=True)
    S.dma("sync", cbias[:], C["c_cbias"], writes=[P.Rc], join=True)
    S.op("vector", lambda e: e.memset(ones_f[:], 1.0), writes=[P.Rc], join=True)
    g, Rg = P.load_vec_fm(W["norm_mix0"])
    bgate, Rbgate = P.load_bcast(W["a_b_gate"], 16)
    mhg, Rmhg = P.load_bcast(W["a_mh_gain"], 128)
    mhg_h = P.sb([128, 128], F32)
    S.op("vector", lambda e: e.tensor_scalar(out=mhg_h[:], in0=mhg[:], scalar1=0.5, scalar2=None, op0=ALU.mult), reads=[Rmhg], writes=[Rmhg])
    nhalf = P.sb([128, 8], F32)
    S.op("vector", lambda e: e.memset(nhalf[:], -0.5), writes=[P.Rc], join=True)
    win, Rwin = P.load_w(W["a_w_in"], 8, 3088, col_chunk=1544)
    wout, Rwout = P.load_w(W["a_w_out"], 8, 1024)

    xtm = P.sb([128, 4, 1024], F32); Rxtm = [Res() for _ in range(4)]
    hT = P.sb([128, 8, NB], F32); RhT = [Res() for _ in range(8)]
    hn = P.sb([128, 8, NB], BF16); Rhn = [Res() for _ in range(8)]
    qkT = P.sb([128, 8, NB], BF16); Rqk = [Res() for _ in range(8)]
    ktm = P.sb([128, 4, 512], BF16); Rktm = [Res() for _ in range(4)]
    vtm = P.sb([128, 4, 1024], BF16); Rvtm = [Res() for _ in range(4)]
    og = P.sb([128, 4, 1024], BF16); Rog = [Res() for _ in range(4)]
    sgt = P.sb([128, 512], F32); Rsgt = Res()
    gsb = P.sb([128, 4, 16], F32); Rgsb = Res()
    th = P.sb([128, 4, 16], F32); Rth = Res()
    ef = P.sb([128, 4, 8], F32); Ref = Res()
    spf = P.sb([128, 4, 8], F32); Rspf = Res()
    li = P.sb([128, 4, 8], F32); Rli = Res()
    lf = P.sb([128, 4, 8], F32); Rlf = Res()
    g_sb = P.sb([128, 8], F32); Rg_sb = Res()
    bb = P.sb([128, 8], F32); Rbb = Res()
    eg = P.sb([128, 8], F32); Reg = Res()
    wlp = P.sb([128, 8], F32); Rwlp = Res()
    wl = P.sb([128, 8], F32); Rwl = Res()
    egl = P.sb([128, 4], F32); Regl = Res()
    Gd = P.sb([128, 8, 128], F32); RGd = Res()
    arg = P.sb([128, 8, 128], F32); Rarg = Res()
    DT = P.sb([128, 8, 128], F32); RDT = Res()
    PT = P.sb([128, 8, 128], BF16); RPT = Res()
    kw = P.sb([128, 8, 64], BF16); Rkw = Res()
    numXs = P.sb([128, 8, 128], F32); RnumXs = Res()
    num = P.sb([128, 8, 128], F32); Rnum = Res()
    sqn = P.sb([128, 8, 128], F32); Rsqn = Res()
    sm = {n: (P.sb([128, 8], F32), Res()) for n in ["dxs", "den", "dd", "rec", "ssn", "t1", "t2", "lnt", "rs", "coef"]}
    y0 = P.sb([128, 8, 128], F32); Ry0 = Res()
    ytm = P.sb([128, 1024], BF16); Rytm = Res()
    yT = P.sb([128, 8, NB], BF16); RyT = [Res() for _ in range(4)]
    Cst = P.sb([128, 4, 128], F32); nst = P.sb([128, 4], F32); RC = Res()
    Cbf = P.sb([128, 4, 2, 128], BF16); nbf = P.sb([128, 4, 2], BF16); RCbf = Res()
    qbd = P.sb([128, 4, 2, NB], BF16); Rqbd = [Res() for _ in range(4)]
    nt1 = P.sb([128, 4], F32); Rnt1 = Res()

    pS = P.ps([128, 512], F32)
    RpS = Res()
    Rgcs = Rglast = RdenI = RdenX = Rdn = Rpgate = RpS
    pR = [P.ps([128, 512], F32) for _ in range(2)]; RpR = [Res(), Res()]
    pG = P.ps([128, 1024], F32); RpG = Res()
    pT2 = P.ps([128, 1024], F32); RpT2 = Res()
    pY = P.ps([128, 1024], BF16); RpY = Res()
    rot = [0]

    def nextbank():
        rot[0] ^= 1
        return pR[rot[0]], RpR[rot[0]]

    for t in (Cst, nst):
        S.op("vector", lambda e, t=t: e.memset(t[:], 0.0), writes=[RC], join=True)
    for t in (Cbf, nbf):
        S.op("vector", lambda e, t=t: e.memset(t[:], 0.0), writes=[RCbf], join=True)
    for c in range(4):
        S.op("gpsimd", lambda e, c=c: e.memset(qbd[:, c, :, :], 0.0), writes=[Rqbd[c]])

    for blk in range(nblk):
        t0 = blk * NB
        for s in range(4):
            S.dma("sync", xtm[:, s, :], x_ap[t0 + s * 128:t0 + (s + 1) * 128, :], writes=[Rxtm[s]])
        for k in range(8):
            pb, Rpb = nextbank()
            for s in range(4):
                S.op("tensor", lambda e, k=k, s=s, pb=pb: e.transpose(out=pb[:, s * 128:(s + 1) * 128], in_=xtm[:, s, k * 128:(k + 1) * 128],
                                                                       identity=P.ident_f[:]),
                     reads=[Rxtm[s], P.Rc], writes=[Rpb], join=(s > 0))
            S.op("scalar", lambda e, k=k, pb=pb: e.copy(out=hT[:, k, :], in_=pb[:]), reads=[Rpb], writes=[RhT[k]])
        pb, Rpb = nextbank()
        P.fm_norm(hT, RhT, g, Rg, hn, Rhn, pb, Rpb)
        for c in range(8):
            pb, Rpb = nextbank()
            for k in range(8):
                S.op("tensor", lambda e, k=k, c=c, pb=pb: e.matmul(pb[:], lhsT=win[:, k, c * 128:(c + 1) * 128], rhs=hn[:, k, :],
                                                                    start=(k == 0), stop=(k == 7)),
                     reads=[Rwin[k], Rhn[k]], writes=[Rpb], join=(k > 0))
            sc = 0.125 if c < 4 else 1.0
            S.op("scalar", lambda e, c=c, pb=pb, sc=sc: e.activation(out=qkT[:, c, :], in_=pb[:], func=AF.Copy, scale=sc),
                 reads=[Rpb], writes=[Rqk[c]])
            if c < 4:
                S.op("gpsimd", lambda e, c=c: e.tensor_copy(out=qbd[0:64, c, 0, :], in_=qkT[0:64, c, :]), reads=[Rqk[c]], writes=[Rqbd[c]])
                S.op("gpsimd", lambda e, c=c: e.tensor_copy(out=qbd[64:128, c, 1, :], in_=qkT[64:128, c, :]), reads=[Rqk[c]], writes=[Rqbd[c]], join=True)
        def proj_kvo(s):
            ts = slice(s * 128, (s + 1) * 128)
            pb, Rpb = nextbank()
            for k in range(8):
                S.op("tensor", lambda e, k=k, ts=ts, pb=pb: e.matmul(pb[:], lhsT=hn[:, k, ts], rhs=win[:, k, 512:1024], start=(k == 0), stop=(k == 7)),
                     reads=[Rwin[k], Rhn[k]], writes=[Rpb], join=(k > 0))
            S.op("scalar", lambda e, s=s, pb=pb: e.copy(out=ktm[:, s, :], in_=pb[:]), reads=[Rpb], writes=[Rktm[s]])
            for half in range(2):
                pb, Rpb = nextbank()
                c0 = 1024 + half * 512
                for k in range(8):
                    S.op("tensor", lambda e, k=k, ts=ts, pb=pb, c0=c0: e.matmul(pb[:], lhsT=hn[:, k, ts], rhs=win[:, k, c0:c0 + 512],
                                                                                 start=(k == 0), stop=(k == 7)),
                         reads=[Rwin[k], Rhn[k]], writes=[Rpb], join=(k > 0))
                S.op("vector", lambda e, s=s, half=half, pb=pb: e.tensor_copy(out=vtm[:, s, half * 512:(half + 1) * 512], in_=pb[:]),
                     reads=[Rpb], writes=[Rvtm[s]], join=(half > 0))
            for half in range(2):
                pb, Rpb = nextbank()
                c0 = 2048 + half * 512
                for k in range(8):
                    S.op("tensor", lambda e, k=k, ts=ts, pb=pb, c0=c0: e.matmul(pb[:], lhsT=hn[:, k, ts], rhs=win[:, k, c0:c0 + 512],
                                                                                 start=(k == 0), stop=(k == 7)),
                         reads=[Rwin[k], Rhn[k]], writes=[Rpb], join=(k > 0))
                S.op("scalar", lambda e, pb=pb: e.activation(out=sgt[:], in_=pb[:], func=AF.Tanh, scale=0.5), reads=[Rpb], writes=[Rsgt])
                S.op("gpsimd", lambda e: e.tensor_tensor(
                    out=sgt[:].rearrange("p (h v) -> p h v", h=4), in0=sgt[:].rearrange("p (h v) -> p h v", h=4),
                    in1=mhg_h[:].unsqueeze(1).broadcast_to([128, 4, 128]), op=ALU.mult), reads=[Rsgt, Rmhg], writes=[Rsgt])
                S.op("gpsimd", lambda e, s=s, half=half: e.tensor_tensor(
                    out=og[:, s, half * 512:(half + 1) * 512].rearrange("p (h v) -> p h v", h=4),
                    in0=sgt[:].rearrange("p (h v) -> p h v", h=4),
                    in1=mhg_h[:].unsqueeze(1).broadcast_to([128, 4, 128]), op=ALU.add),
                     reads=[Rsgt, Rmhg], writes=[Rog[s]], join=(half > 0))

        for s in range(4):
            ts = slice(s * 128, (s + 1) * 128)
            for k in range(8):
                S.op("tensor", lambda e, k=k, ts=ts, s=s: e.matmul(pS[:, 64 + s * 16:64 + (s + 1) * 16], lhsT=hn[:, k, ts], rhs=win[:, k, 3072:3088],
                                                                    start=(k == 0), stop=(k == 7)),
                     reads=[Rwin[k], Rhn[k]], writes=[Rpgate], join=(k > 0))
            S.op("vector", lambda e, s=s: e.tensor_tensor(out=gsb[:, s, :], in0=pS[:, 64 + s * 16:64 + (s + 1) * 16], in1=bgate[:], op=ALU.add),
                 reads=[Rpgate, Rbgate], writes=[Rgsb], join=(s > 0))
        S.op("scalar", lambda e: e.activation(out=th[:], in_=gsb[:], func=AF.Tanh, scale=1.0 / 15.0), reads=[Rgsb], writes=[Rth])
        S.op("vector", lambda e: e.tensor_scalar(out=li[:], in0=th[:, :, 0:8], scalar1=15.0, scalar2=None, op0=ALU.mult), reads=[Rth], writes=[Rli])
        S.op("scalar", lambda e: e.activation(out=ef[:], in_=th[:, :, 8:16], func=AF.Exp, scale=-15.0), reads=[Rth], writes=[Ref])
        S.op("scalar", lambda e: e.activation(out=spf[:], in_=ef[:], func=AF.Ln, bias=P.one_t[:, 0:1]), reads=[Ref, P.Rc], writes=[Rspf])
        S.op("vector", lambda e: e.tensor_scalar(out=lf[:], in0=spf[:], scalar1=-1.0, scalar2=None, op0=ALU.mult), reads=[Rspf], writes=[Rlf])

        proj_kvo(0)
        for s in range(4):
            ts = slice(s * 128, (s + 1) * 128)
            if s < 3:
                d_ = Deferred()
                S.tgt = d_
                proj_kvo(s + 1)
                S.tgt = Mux(realS, mm_chunks(d_.q))
            else:
                S.tgt = realS
            S.op("tensor", lambda e, s=s: e.matmul(pS[:, 0:8], lhsT=tri_f[:], rhs=lf[:, s, :], start=True, stop=True),
                 reads=[Rlf, P.Rc], writes=[Rgcs])
            S.op("tensor", lambda e, s=s: e.matmul(pS[:, 8:16], lhsT=ones_f[:], rhs=lf[:, s, :], start=True, stop=True),
                 reads=[Rlf, P.Rc], writes=[Rglast])
            S.op("vector", lambda e: e.tensor_copy(out=g_sb[:], in_=pS[:, 0:8]), reads=[Rgcs], writes=[Rg_sb])
            S.op("vector", lambda e, s=s: e.tensor_tensor(out=bb[:], in0=li[:, s, :], in1=g_sb[:], op=ALU.subtract), reads=[Rli, Rg_sb], writes=[Rbb])
            S.op("scalar", lambda e: e.activation(out=eg[:], in_=g_sb[:], func=AF.Exp), reads=[Rg_sb], writes=[Reg])
            S.op("vector", lambda e: e.tensor_tensor(out=wlp[:], in0=pS[:, 8:16], in1=bb[:], op=ALU.add), reads=[Rglast, Rbb], writes=[Rwlp])
            S.op("scalar", lambda e: e.activation(out=wl[:], in_=wlp[:], func=AF.Exp), reads=[Rwlp], writes=[Rwl])
            S.op("scalar", lambda e: e.activation(out=egl[0:64, :], in_=pS[0:64, 8:16:2], func=AF.Exp), reads=[Rglast], writes=[Regl])
            S.op("scalar", lambda e: e.activation(out=egl[64:128, :], in_=pS[64:128, 9:16:2], func=AF.Exp), reads=[Rglast], writes=[Regl], join=True)
            S.op("vector", lambda e: e.tensor_tensor(out=Gd[:], in0=g_sb[:].unsqueeze(2).broadcast_to([128, 8, 128]),
                                                      in1=P.ident_f[:].unsqueeze(1).broadcast_to([128, 8, 128]), op=ALU.mult),
                 reads=[Rg_sb, P.Rc], writes=[RGd])
            for half in range(2):
                S.op("tensor", lambda e, half=half: e.matmul(pG[:, half * 512:(half + 1) * 512], lhsT=ones_f[:],
                                                               rhs=Gd[:, half * 4:(half + 1) * 4, :].rearrange("p h j -> p (h j)"),
                                                               start=True, stop=True),
                     reads=[RGd, P.Rc], writes=[RpG], join=(half > 0))
            for h in range(8):
                S.op("vector", lambda e, h=h: e.scalar_tensor_tensor(out=arg[:, h, :], in0=pG[:, h * 128:(h + 1) * 128], scalar=bb[:, h:h + 1], op0=ALU.add,
                                                                      in1=cbias[:], op1=ALU.add),
                     reads=[RpG, Rbb, P.Rc], writes=[Rarg], join=(h > 0))
            S.op("scalar", lambda e: e.activation(out=DT[:], in_=arg[:], func=AF.Exp), reads=[Rarg], writes=[RDT])
            for c in range(4):
                S.op("tensor", lambda e, c=c, ts=ts: e.matmul(pT2[:, c * 256:(c + 1) * 256], lhsT=qkT[:, 4 + c, ts], rhs=qbd[:, c, :, ts],
                                                               start=True, stop=True),
                     reads=[Rqbd[c], Rqk[4 + c]], writes=[RpT2], join=(c > 0))
            S.op("vector", lambda e: e.tensor_tensor(out=PT[:].rearrange("p h j -> p (h j)"), in0=pT2[:], in1=DT[:].rearrange("p h j -> p (h j)"), op=ALU.mult),
                 reads=[RpT2, RDT], writes=[RPT])
            for h in range(8):
                S.op("tensor", lambda e, h=h, s=s: e.matmul(pG[:, h * 128:(h + 1) * 128], lhsT=PT[:, h, :], rhs=vtm[:, s, h * 128:(h + 1) * 128],
                                                             start=True, stop=True),
                     reads=[RPT, Rvtm[s]], writes=[RpG], join=(h > 0))
            for h in range(8):
                S.op("tensor", lambda e, h=h: e.matmul(pS[:, 16 + h:17 + h], lhsT=PT[:, h, :], rhs=P.ones_b[:, 0:1], start=True, stop=True),
                     reads=[RPT, P.Rc], writes=[RdenI], join=(h > 0))
            for c in range(4):
                S.op("tensor", lambda e, c=c, ts=ts: e.matmul(pT2[:, c * 256:(c + 1) * 256], lhsT=qkT[:, c, ts], rhs=Cbf[:, c, :, :],
                                                               start=True, stop=True),
                     reads=[Rqk[c], RCbf], writes=[RpT2], join=(c > 0))
            for c in range(4):
                S.op("tensor", lambda e, c=c, ts=ts: e.matmul(pS[:, 24 + 2 * c:26 + 2 * c], lhsT=qkT[:, c, ts], rhs=nbf[:, c, :],
                                                               start=True, stop=True),
                     reads=[Rqk[c], RCbf], writes=[RdenX], join=(c > 0))
            S.op("vector", lambda e, s=s: e.tensor_tensor(out=kw[:], in0=ktm[:, s, :].rearrange("p (h d) -> p h d", h=8),
                                                           in1=wl[:].unsqueeze(2).broadcast_to([128, 8, 64]), op=ALU.mult),
                 reads=[Rktm[s], Rwl], writes=[Rkw])
            pd, Rpd = pY[:].bitcast(F32), RpY
            for h in range(8):
                c, ph = h // 2, h % 2
                prt = slice(ph * 64, (ph + 1) * 64)
                S.op("tensor", lambda e, h=h, c=c, prt=prt, s=s, pd=pd: e.matmul(pd[prt, c * 128:(c + 1) * 128], lhsT=kw[:, h, :], rhs=vtm[:, s, h * 128:(h + 1) * 128],
                                                                                  start=True, stop=True),
                     reads=[Rkw, Rvtm[s]], writes=[Rpd], join=(h > 0))
            for h in range(8):
                c, ph = h // 2, h % 2
                prt = slice(ph * 64, (ph + 1) * 64)
                S.op("tensor", lambda e, h=h, c=c, prt=prt: e.matmul(pS[prt, 32 + c:33 + c], lhsT=kw[:, h, :], rhs=P.ones_b[:, 0:1], start=True, stop=True),
                     reads=[Rkw, P.Rc], writes=[Rdn], join=(h > 0))
            for c in range(4):
                S.op("vector", lambda e, c=c, pd=pd: e.scalar_tensor_tensor(out=Cst[:, c, :], in0=Cst[:, c, :], scalar=egl[:, c:c + 1], op0=ALU.mult,
                                                                            in1=pd[:, c * 128:(c + 1) * 128], op1=ALU.add),
                     reads=[Rpd, Regl, RC], writes=[RC])
            S.op("vector", lambda e: e.tensor_tensor(out=nt1[:], in0=nst[:], in1=egl[:], op=ALU.mult), reads=[RC, Regl], writes=[Rnt1])
            S.op("vector", lambda e: e.tensor_tensor(out=nst[:], in0=pS[:, 32:36], in1=nt1[:], op=ALU.add), reads=[Rdn, Rnt1], writes=[RC])
            S.op("vector", lambda e: e.tensor_tensor(out=numXs[:], in0=pT2[:].rearrange("p (h v) -> p h v", h=8),
                                                      in1=eg[:].unsqueeze(2).broadcast_to([128, 8, 128]), op=ALU.mult),
                 reads=[RpT2, Reg], writes=[RnumXs])
            S.op("vector", lambda e: e.tensor_tensor(out=num[:].rearrange("p h v -> p (h v)"), in0=pG[:], in1=numXs[:].rearrange("p h v -> p (h v)"), op=ALU.add),
                 reads=[RpG, RnumXs], writes=[Rnum])
            S.op("gpsimd", lambda e: e.tensor_copy(out=Cbf[0:64, :, 0, :], in_=Cst[0:64, :, :]), reads=[RC], writes=[RCbf])
            S.op("gpsimd", lambda e: e.tensor_copy(out=Cbf[64:128, :, 1, :], in_=Cst[64:128, :, :]), reads=[RC], writes=[RCbf], join=True)
            S.op("gpsimd", lambda e: e.tensor_copy(out=nbf[0:64, :, 0], in_=nst[0:64, :]), reads=[RC], writes=[RCbf], join=True)
            S.op("gpsimd", lambda e: e.tensor_copy(out=nbf[64:128, :, 1], in_=nst[64:128, :]), reads=[RC], writes=[RCbf], join=True)
            T = lambda n: sm[n][0]
            R_ = lambda n: sm[n][1]
            S.op("vector", lambda e: e.tensor_tensor(out=T("dxs")[:], in0=pS[:, 24:32], in1=eg[:], op=ALU.mult), reads=[RdenX, Reg], writes=[R_("dxs")])
            S.op("vector", lambda e: e.tensor_tensor(out=T("den")[:], in0=pS[:, 16:24], in1=T("dxs")[:], op=ALU.add), reads=[RdenI, R_("dxs")], writes=[R_("den")])
            S.op("vector", lambda e: e.scalar_tensor_tensor(out=T("t1")[:], in0=T("den")[:], scalar=-1.0, op0=ALU.mult, in1=T("den")[:], op1=ALU.max),
                 reads=[R_("den")], writes=[R_("t1")])
            S.op("vector", lambda e: e.tensor_scalar(out=T("dd")[:], in0=T("t1")[:], scalar1=1.0, scalar2=None, op0=ALU.max), reads=[R_("t1")], writes=[R_("dd")])
            S.op("vector", lambda e: e.reciprocal(out=T("rec")[:], in_=T("dd")[:]), reads=[R_("dd")], writes=[R_("rec")])
            S.op("gpsimd", lambda e: e.tensor_tensor(out=sqn[:], in0=num[:], in1=num[:], op=ALU.mult), reads=[Rnum], writes=[Rsqn])
            S.op("vector", lambda e: e.tensor_reduce(out=T("ssn")[:], in_=sqn[:], axis=AX.X, op=ALU.add), reads=[Rsqn], writes=[R_("ssn")])
            S.op("vector", lambda e: e.tensor_tensor(out=T("t1")[:], in0=T("rec")[:], in1=T("rec")[:], op=ALU.mult), reads=[R_("rec")], writes=[R_("t1")])
            S.op("vector", lambda e: e.tensor_tensor(out=T("t2")[:], in0=T("t1")[:], in1=T("ssn")[:], op=ALU.mult), reads=[R_("t1"), R_("ssn")], writes=[R_("t2")])
            S.op("gpsimd", lambda e: e.tensor_scalar(out=T("lnt")[:], in0=T("t2")[:], scalar1=1.0 / 128.0, scalar2=EPS, op0=ALU.mult, op1=ALU.add),
                 reads=[R_("t2")], writes=[R_("lnt")])
            S.op("gpsimd", lambda e: e.tensor_tensor(out=T("rs")[:], in0=T("lnt")[:], in1=nhalf[:], op=ALU.pow), reads=[R_("lnt"), P.Rc], writes=[R_("rs")])
            S.op("vector", lambda e: e.tensor_tensor(out=T("coef")[:], in0=T("rec")[:], in1=T("rs")[:], op=ALU.mult), reads=[R_("rec"), R_("rs")], writes=[R_("coef")])
            S.op("vector", lambda e: e.tensor_tensor(out=y0[:], in0=num[:], in1=T("coef")[:].unsqueeze(2).broadcast_to([128, 8, 128]), op=ALU.mult),
                 reads=[Rnum, R_("coef")], writes=[Ry0])
            S.op("gpsimd", lambda e, s=s: e.tensor_tensor(out=ytm[:], in0=y0[:].rearrange("p h v -> p (h v)"), in1=og[:, s, :], op=ALU.mult),
                 reads=[Ry0, Rog[s]], writes=[Rytm])
            for h in range(8):
                S.op("tensor", lambda e, h=h: e.transpose(out=pY[:, h * 128:(h + 1) * 128], in_=ytm[:, h * 128:(h + 1) * 128], identity=P.ident_b[:]),
                     reads=[Rytm, P.Rc], writes=[RpY], join=(h > 0))
            S.op("scalar", lambda e, ts=ts: e.copy(out=yT[:, :, ts], in_=pY[:].rearrange("p (h j) -> p h j", h=8)), reads=[RpY], writes=[RyT[s]])
            if s < 3:
                S.tgt.flush()
            S.tgt = realS
        for d in range(8):
            pb, Rpb = nextbank()
            for h in range(8):
                S.op("tensor", lambda e, h=h, d=d, pb=pb: e.matmul(pb[:], lhsT=wout[:, h, d * 128:(d + 1) * 128], rhs=yT[:, h, :], start=(h == 0), stop=(h == 7)),
                     reads=[Rwout[h]] + RyT, writes=[Rpb], join=(h > 0))
            S.op("vector", lambda e, d=d, pb=pb: e.tensor_tensor(out=hT[:, d, :], in0=pb[:], in1=hT[:, d, :], op=ALU.add), reads=[Rpb, RhT[d]], writes=[RhT[d]])
            S.dma("sync", hout[d, :, t0:t0 + NB], hT[:, d, :], reads=[RhT[d]], writes=[P.Rdram], join=True)
    P.S = realS
    P.finish()


def phase_moba(nc, C, hin, hout, W, name, nblk=NBLK, dbg=None):
    G = 2
    P = Phase(nc, name)
    S = P.S
    P.consts(C)
    P.norm_setup()
    c256 = P.sb([128, 1], F32)
    S.op("vector", lambda e: e.memset(c256[:], 1.0 / 256.0), writes=[P.Rc], join=True)
    tri4 = P.sb([128, 512], BF16)
    S.dma("gpsimd", tri4[:], C["c_tribias4"], writes=[P.Rc], join=True)
    ropet = P.sb([128, 32, 16], F32)
    S.dma("sync", ropet[:], C["c_rope"].rearrange("(i p) c -> p i c", p=128), writes=[P.Rc], join=True)
    pastb, Rpastb = P.load_bcast(C["c_past"], 256)
    ownb, Rownb = P.load_bcast(C["c_own"], 256)
    g_kv, Rg_kv = P.load_vec_fm(W["kv_norm"])
    g_mix, Rg_mix = P.load_vec_fm(W["norm_mix1"])
    knorm, Rknorm = P.load_bcast(W["k_norm"], 64)
    qnorm, Rqnorm = P.load_bcast(W["b_q_norm"], 64)
    wkv, Rwkv = P.load_w(W["w_kv"], 8, 512)
    wq, Rwq = P.load_w(W["b_w_q"], 8, 1024)
    wo, Rwo = P.load_w(W["b_w_o"], 8, 1024)

    pR = [P.ps([128, 512], F32) for _ in range(2)]; RpR = [Res(), Res()]
    psc = [P.ps([128, G * 512], F32) for _ in range(2)]; Rpsc = [Res(), Res()]
    pop = [P.ps([128, 512], F32) for _ in range(2)]; Rpop = [Res(), Res()]
    rot = [0]

    def nextbank():
        rot[0] ^= 1
        return pR[rot[0]], RpR[rot[0]]

    ple = PLE(P, C, W["p0"], W["norm_ple0"], W["w_ple_gate0"], W["w_ple_up0"], [(pR[0], RpR[0]), (pR[1], RpR[1]), (pR[0], RpR[0])])

    hT = P.sb([128, 8, NB], F32); RhT = [Res() for _ in range(8)]
    hn = P.sb([128, 8, NB], BF16); Rhn = [Res() for _ in range(8)]
    KT = P.sb([80, 4, NT], BF16); RKT = [Res() for _ in range(32)]
    Vaug = P.sb([128, 4, 32, 128], BF16); RV = [Res() for _ in range(32)]
    kmT = P.sb([80, 4, 16], BF16); RkmT = Res()
    kms = P.sb([64, 4, 2], F32); Rkms = Res()
    ksb = P.sb([128, 4, 64], F32); Rksb = Res()
    sqk = P.sb([128, 4, 64], F32); Rsqk = Res()
    kbf = P.sb([128, 4, 64], BF16); Rkbf = Res()
    qsb = P.sb([128, 16, 64], F32); Rqsb = Res()
    sqq = P.sb([128, 16, 64], F32); Rsqq = Res()
    qa = P.sb([128, 16, 80], BF16); Rqa = Res()
    QTa = [P.sb([80, 16, 128], BF16) for _ in range(2)]; RQTa = [Res(), Res()]
    gm = P.sb([128, 16, 16], F32); Rgm = Res()
    mx8 = P.sb([128, 16, 8], F32); Rmx8 = Res()
    vis = P.sb([128, 16, 16], F32); Rvis = Res()
    skq = {n: (P.sb([128, 16], F32), Res()) for n in ["ss", "ln", "r"]}
    skk = {n: (P.sb([128, 16], F32), Res()) for n in ["ss", "ln", "r"]}
    rtq = {n: (P.sb([128, 16, 8], F32), Res()) for n in ["t1", "t2", "t3", "t4"]}
    PTb = [P.sb([128, G * 512], BF16) for _ in range(2)]; RPTb = [Res() for _ in range(2)]
    rec = P.sb([128, 512], F32); Rrec = Res()
    OTb = P.sb([128, 8, NB], BF16); ROTb = [Res() for _ in range(4)]

    for kvh in range(4):
        for c0 in range(0, NT, 1024):
            S.dma("gpsimd", KT[64:80, kvh, c0:c0 + 1024], C["c_onehot"][:, c0:c0 + 1024], writes=[P.Rc], join=True)
    S.op("vector", lambda e: e.memset(Vaug[:].rearrange("p a b c -> p (a b c)"), 1.0), writes=[P.Rc], join=True)
    S.op("gpsimd", lambda e: e.memset(kmT[:], 0.0), writes=[RkmT])
    for i in range(2):
        S.op("gpsimd", lambda e, i=i: e.memset(QTa[i][:], 0.0), writes=[RQTa[i]])

    def head_norm_rope(x, Rx, sq_, Rsq_, nh, gbc, Rgbc, it, sk, rt):
        (ss, Rss), (ln, Rln), (r, Rr) = sk["ss"], sk["ln"], sk["r"]
        S.op("gpsimd", lambda e: e.tensor_tensor(out=sq_[:], in0=x[:], in1=x[:], op=ALU.mult), reads=[Rx], writes=[Rsq_])
        S.op("vector", lambda e: e.tensor_reduce(out=ss[:, 0:nh], in_=sq_[:], axis=AX.X, op=ALU.add), reads=[Rsq_], writes=[Rss])
        yield
        S.op("scalar", lambda e: e.activation(out=ln[:, 0:nh], in_=ss[:, 0:nh], func=AF.Ln, scale=1.0 / 64.0, bias=P.eps_t[:, 0:1]),
             reads=[Rss, P.Rc], writes=[Rln])
        S.op("scalar", lambda e: e.activation(out=r[:, 0:nh], in_=ln[:, 0:nh], func=AF.Exp, scale=-0.5), reads=[Rln], writes=[Rr])
        yield
        S.op("vector", lambda e: e.tensor_tensor(out=x[:], in0=x[:], in1=r[:, 0:nh].unsqueeze(2).broadcast_to([128, nh, 64]), op=ALU.mult),
             reads=[Rx, Rr], writes=[Rx])
        S.op("vector", lambda e: e.tensor_tensor(out=x[:], in0=x[:], in1=gbc[:].unsqueeze(1).broadcast_to([128, nh, 64]), op=ALU.mult),
             reads=[Rx, Rgbc], writes=[Rx])
        yield
        cs = ropet[:, it, 0:8].unsqueeze(1).broadcast_to([128, nh, 8])
        sn = ropet[:, it, 8:16].unsqueeze(1).broadcast_to([128, nh, 8])
        x1 = x[:, :, 0:8]
        x2 = x[:, :, 8:16]
        tt = {k: v[0][:, 0:nh, :] for k, v in rt.items()}
        Rt = {k: v[1] for k, v in rt.items()}
        S.op("vector", lambda e: e.tensor_tensor(out=tt["t1"], in0=x1, in1=cs, op=ALU.mult), reads=[Rx, P.Rc], writes=[Rt["t1"]])
        S.op("vector", lambda e: e.tensor_tensor(out=tt["t2"], in0=x2, in1=sn, op=ALU.mult), reads=[Rx, P.Rc], writes=[Rt["t2"]])
        S.op("vector", lambda e: e.tensor_tensor(out=tt["t3"], in0=x2, in1=cs, op=ALU.mult), reads=[Rx, P.Rc], writes=[Rt["t3"]])
        S.op("vector", lambda e: e.tensor_tensor(out=tt["t4"], in0=x1, in1=sn, op=ALU.mult), reads=[Rx, P.Rc], writes=[Rt["t4"]])
        yield
        S.op("vector", lambda e: e.tensor_tensor(out=x1, in0=tt["t1"], in1=tt["t2"], op=ALU.subtract), reads=[Rt["t1"], Rt["t2"], Rx], writes=[Rx])
        S.op("vector", lambda e: e.tensor_tensor(out=x2, in0=tt["t3"], in1=tt["t4"], op=ALU.add), reads=[Rt["t3"], Rt["t4"], Rx], writes=[Rx])
        yield

    def kv_path(blk, s):
        it = blk * 4 + s
        ts = slice(s * 128, (s + 1) * 128)
        pb, Rpb = nextbank()
        for k in range(8):
            S.op("tensor", lambda e, k=k, ts=ts, pb=pb: e.matmul(pb[:], lhsT=hn[:, k, ts], rhs=wkv[:, k, :], start=(k == 0), stop=(k == 7)),
                 reads=[Rwkv[k], Rhn[k]], writes=[Rpb], join=(k > 0))
        S.op("scalar", lambda e, pb=pb: e.copy(out=ksb[:].rearrange("p h d -> p (h d)"), in_=pb[:, 0:256]), reads=[Rpb], writes=[Rksb])
        S.op("scalar", lambda e, pb=pb, it=it: e.copy(out=Vaug[:, :, it, 0:64], in_=pb[:, 256:512].rearrange("p (h d) -> p h d", h=4)),
             reads=[Rpb, P.Rc], writes=[RV[it]])
        for _ in head_norm_rope(ksb, Rksb, sqk, Rsqk, 4, knorm, Rknorm, it, skk, rtq):
            pass
        S.op("gpsimd", lambda e: e.tensor_copy(out=kbf[:], in_=ksb[:]), reads=[Rksb], writes=[Rkbf])
        pb, Rpb = nextbank()
        pbb = pb[:].bitcast(BF16)
        for kvh in range(4):
            S.op("tensor", lambda e, kvh=kvh, pbb=pbb: e.transpose(out=pbb[0:64, kvh * 128:(kvh + 1) * 128], in_=kbf[:, kvh, :], identity=P.ident_b[:]),
                 reads=[Rkbf, P.Rc], writes=[Rpb], join=(kvh > 0))
        S.op("scalar", lambda e, it=it, pbb=pbb: e.copy(out=KT[0:64, :, it * 128:(it + 1) * 128], in_=pbb[0:64, 0:512].rearrange("p (h t) -> p h t", h=4)),
             reads=[Rpb, P.Rc], writes=[RKT[it]])
        pb, Rpb = nextbank()
        for kvh in range(4):
            S.op("tensor", lambda e, kvh=kvh, pb=pb: e.matmul(pb[0:64, kvh:kvh + 1], lhsT=ksb[:, kvh, :], rhs=c256[:, 0:1], start=True, stop=True),
                 reads=[Rksb, P.Rc], writes=[Rpb], join=(kvh > 0))
        S.op("vector", lambda e, s=s, pb=pb: e.tensor_copy(out=kms[:, :, s % 2], in_=pb[0:64, 0:4]), reads=[Rpb], writes=[Rkms], join=(s % 2 == 1))
        if s % 2 == 1:
            n = it // 2
            S.op("vector", lambda e, n=n: e.tensor_tensor(out=kmT[0:64, :, n], in0=kms[:, :, 0], in1=kms[:, :, 1], op=ALU.add),
                 reads=[Rkms], writes=[RkmT])

    def q_path(blk, s):
        it = blk * 4 + s
        b = it // 2
        ts = slice(s * 128, (s + 1) * 128)
        QT, RQT = QTa[it % 2], RQTa[it % 2]
        for half in range(2):
            pb, Rpb = nextbank()
            for k in range(8):
                S.op("tensor", lambda e, k=k, ts=ts, pb=pb, half=half: e.matmul(pb[:], lhsT=hn[:, k, ts], rhs=wq[:, k, half * 512:(half + 1) * 512],
                                                                                 start=(k == 0), stop=(k == 7)),
                     reads=[Rwq[k], Rhn[k]], writes=[Rpb], join=(k > 0))
                if k % 4 == 3:
                    yield
            S.op("scalar", lambda e, pb=pb, half=half: e.copy(out=qsb[:, half * 8:(half + 1) * 8, :].rearrange("p h d -> p (h d)"), in_=pb[:]),
                 reads=[Rpb], writes=[Rqsb], join=(half > 0))
            yield
        for _ in head_norm_rope(qsb, Rqsb, sqq, Rsqq, 16, qnorm, Rqnorm, it, skq, rtq):
            yield
        S.op("gpsimd", lambda e: e.tensor_copy(out=qa[:, :, 0:64], in_=qsb[:]), reads=[Rqsb], writes=[Rqa])
        yield
        for r_ in range(2):
            pb, Rpb = nextbank()
            pbb = pb[:].bitcast(BF16)
            for j in range(8):
                h = r_ * 8 + j
                S.op("tensor", lambda e, h=h, j=j, pbb=pbb: e.transpose(out=pbb[0:64, j * 128:(j + 1) * 128], in_=qa[:, h, 0:64], identity=P.ident_b[:]),
                     reads=[Rqa, P.Rc], writes=[Rpb], join=(j > 0))
                if j % 4 == 3:
                    yield
            S.op("scalar", lambda e, r_=r_, pbb=pbb: e.copy(out=QT[0:64, r_ * 8:(r_ + 1) * 8, :], in_=pbb[0:64, :].rearrange("p (h t) -> p h t", h=8)),
                 reads=[Rpb], writes=[RQT], join=(r_ > 0))
            yield
        pb, Rpb = nextbank()
        for h in range(16):
            S.op("tensor", lambda e, h=h, pb=pb: e.matmul(pb[:, h * 16:(h + 1) * 16], lhsT=QT[:, h, :], rhs=kmT[:, h // 4, :], start=True, stop=True),
                 reads=[RQT, RkmT], writes=[Rpb], join=(h > 0))
            if h % 4 == 3:
                yield
        S.op("vector", lambda e, b=b, pb=pb: e.tensor_tensor(out=gm[:], in0=pb[:, 0:256].rearrange("p (h n) -> p h n", h=16),
                                                              in1=pastb[:, b * 16:(b + 1) * 16].unsqueeze(1).broadcast_to([128, 16, 16]), op=ALU.add),
             reads=[Rpb, Rpastb], writes=[Rgm])
        yield
        for h in range(16):
            S.op("vector", lambda e, h=h: e.max(out=mx8[:, h, :], in_=gm[:, h, :]), reads=[Rgm], writes=[Rmx8], join=(h > 0))
            if h % 4 == 3:
                yield
        S.op("vector", lambda e: e.tensor_tensor(out=vis[:], in0=gm[:], in1=mx8[:, :, 2:3].broadcast_to([128, 16, 16]), op=ALU.is_ge),
             reads=[Rgm, Rmx8], writes=[Rvis])
        S.op("vector", lambda e, b=b: e.tensor_tensor(out=vis[:], in0=vis[:], in1=ownb[:, b * 16:(b + 1) * 16].unsqueeze(1).broadcast_to([128, 16, 16]), op=ALU.max),
             reads=[Rvis, Rownb], writes=[Rvis])
        S.op("vector", lambda e: e.tensor_scalar(out=qa[:, :, 64:80], in0=vis[:], scalar1=-NEG, scalar2=NEG, op0=ALU.mult, op1=ALU.add),
             reads=[Rvis, Rqa], writes=[Rqa])
        yield
        for r_ in range(2):
            pb, Rpb = nextbank()
            pbb = pb[:].bitcast(BF16)
            for j in range(8):
                h = r_ * 8 + j
                S.op("tensor", lambda e, h=h, j=j, pbb=pbb: e.transpose(out=pbb[0:80, j * 128:(j + 1) * 128], in_=qa[:, h, :], identity=P.ident_b[:]),
                     reads=[Rqa, P.Rc], writes=[Rpb], join=(j > 0))
                if j % 4 == 3:
                    yield
            S.op("scalar", lambda e, r_=r_, pbb=pbb: e.copy(out=QT[:, r_ * 8:(r_ + 1) * 8, :], in_=pbb[0:80, :].rearrange("p (h t) -> p h t", h=8)),
                 reads=[Rpb], writes=[RQT], join=(r_ > 0))
            yield

    nsc = [0]

    def attention(blk, s):
        it = blk * 4 + s
        ts = slice(s * 128, (s + 1) * 128)
        QT, RQT = QTa[it % 2], RQTa[it % 2]
        L = []
        for kvh in range(4):
            for j0 in range(0, it + 1, G):
                L.append((kvh, j0, min(it + 1, j0 + G)))
        bufs = {}

        def qk(n):
            kvh, j0, j1 = L[n]
            m = nsc[0]
            nsc[0] += 1
            bufs[n] = m % 2
            ps_, Rps_ = psc[m % 2], Rpsc[m % 2]
            qg = QT[:, 4 * kvh:4 * kvh + 4, :].rearrange("p h t -> p (h t)")
            for j in range(j0, j1):
                o = (j - j0) * 512
                diag = (j == it)
                S.op("tensor", lambda e, kvh=kvh, j=j, ps_=ps_, qg=qg, diag=diag, o=o: e.matmul(ps_[:, o:o + 512], lhsT=KT[:, kvh, j * 128:(j + 1) * 128], rhs=qg,
                                                                                               start=True, stop=(not diag)),
                     reads=[RKT[j], RQT, P.Rc], writes=[Rps_], join=(j > j0))
                if diag:
                    S.op("tensor", lambda e, ps_=ps_, o=o: e.matmul(ps_[:, o:o + 512], lhsT=P.ident_b[:], rhs=tri4[:], start=False, stop=True),
                         reads=[P.Rc], writes=[Rps_], join=True)

        def ex(n):
            kvh, j0, j1 = L[n]
            bi = bufs[n]
            w = (j1 - j0) * 512
            S.op("scalar", lambda e, bi=bi, w=w: e.activation(out=PTb[bi][:, 0:w], in_=psc[bi][:, 0:w], func=AF.Exp, scale=0.125),
                 reads=[Rpsc[bi]], writes=[RPTb[bi]])

        def pv(n):
            kvh, j0, j1 = L[n]
            bi = bufs[n]
            po, Rpo = pop[kvh % 2], Rpop[kvh % 2]
            for j in range(j0, j1):
                o = (j - j0) * 512
                S.op("tensor", lambda e, kvh=kvh, j=j, bi=bi, po=po, o=o, it=it: e.matmul(po[:], lhsT=Vaug[:, kvh, j, :], rhs=PTb[bi][:, o:o + 512],
                                                                                         start=(j == 0), stop=(j == it)),
                     reads=[RV[j], RPTb[bi], P.Rc], writes=[Rpo], join=(j > 0))

        def epi(kvh):
            po, Rpo = pop[kvh % 2], Rpop[kvh % 2]
            S.op("vector", lambda e, po=po: e.reciprocal(out=rec[64:128, :], in_=po[64:128, :]), reads=[Rpo], writes=[Rrec])
            for gq in range(4):
                hh = 4 * kvh + gq
                pr, ph = hh // 2, hh % 2
                S.op("vector", lambda e, po=po, gq=gq, pr=pr, ph=ph, ts=ts: e.tensor_tensor(
                    out=OTb[ph * 64:(ph + 1) * 64, pr, ts], in0=po[0:64, gq * 128:(gq + 1) * 128], in1=rec[64:128, gq * 128:(gq + 1) * 128], op=ALU.mult),
                     reads=[Rpo, Rrec], writes=[ROTb[s]], join=True)

        qk(0)
        for n in range(len(L)):
            if n + 1 < len(L):
                qk(n + 1)
            ex(n)
            pv(n)
            if n + 1 == len(L) or L[n + 1][0] != L[n][0]:
                epi(L[n][0])
            yield

    def drive(main, side, side_len):
        steps = list(range(0))
        mains = main
        if side is None:
            for _ in mains:
                pass
            return
        done = [False]

        def adv(k):
            for _ in range(k):
                if done[0]:
                    return
                try:
                    next(side)
                except StopIteration:
                    done[0] = True
        nmain = side_len[0]
        per = max(1, -(-side_len[1] // max(1, nmain)))
        for _ in mains:
            adv(per)
        while not done[0]:
            adv(8)

    for blk in range(nblk):
        t0 = blk * NB
        for k in range(8):
            S.dma("sync", hT[:, k, :], hin[k, :, t0:t0 + NB], writes=[RhT[k]])
        pb, Rpb = nextbank()
        ple.emit(blk, hT, RhT, hn, Rhn, pb, Rpb)
        pb, Rpb = nextbank()
        P.fm_norm(hT, RhT, g_kv, Rg_kv, hn, Rhn, pb, Rpb)
        for s in range(4):
            kv_path(blk, s)
        pb, Rpb = nextbank()
        P.fm_norm(hT, RhT, g_mix, Rg_mix, hn, Rhn, pb, Rpb, reuse_rstd=True)
        for _ in q_path(blk, 0):
            pass
        for s in range(4):
            it = blk * 4 + s
            nsteps = 4 * (-(-(it + 1) // G))
            side = q_path(blk, s + 1) if s < 3 else None
            drive(attention(blk, s), side, (nsteps, 60))
        for d in range(8):
            pb, Rpb = nextbank()
            for pr in range(8):
                S.op("tensor", lambda e, pr=pr, d=d, pb=pb: e.matmul(pb[:], lhsT=wo[:, pr, d * 128:(d + 1) * 128], rhs=OTb[:, pr, :], start=(pr == 0), stop=(pr == 7)),
                     reads=[Rwo[pr]] + ROTb, writes=[Rpb], join=(pr > 0))
            S.op("vector", lambda e, d=d, pb=pb: e.tensor_tensor(out=hT[:, d, :], in0=pb[:], in1=hT[:, d, :], op=ALU.add), reads=[Rpb, RhT[d]], writes=[RhT[d]])
            S.dma("sync", hout[d, :, t0:t0 + NB], hT[:, d, :], reads=[RhT[d]], writes=[P.Rdram], join=True)
    P.finish()


WSHAPES = {
    "norm_mix0": [1024], "norm_mix1": [1024], "a_w_in": [1024, 3088], "a_b_gate": [16], "a_mh_gain": [128], "a_w_out": [1024, 1024],
    "kv_norm": [1024], "w_kv": [1024, 512], "k_norm": [64], "b_w_q": [1024, 1024], "b_q_norm": [64], "b_w_o": [1024, 1024],
    "norm_ffn0": [1024], "norm_ffn1": [1024], "w_gate_up0": [1024, 5632], "w_gate_up1": [1024, 5632], "w_down0": [2816, 1024], "w_down1": [2816, 1024],
    "norm_ple0": [1024], "norm_ple1": [1024], "w_ple_gate0": [1024, 1024], "w_ple_gate1": [1024, 1024], "w_ple_up0": [256, 1024], "w_ple_up1": [256, 1024],
    "p0": [NT, 256], "p1": [NT, 256],
}


def build_program():
    nc = bass.Bass("TRN2", target_bir_lowering=False)
    Cn = make_consts()
    C = {k: nc.dram_tensor(k, list(v.shape), F32, kind="ExternalInput").ap() for k, v in Cn.items()}
    x = nc.dram_tensor("x", [NT, 1024], F32, kind="ExternalInput").ap()
    out = nc.dram_tensor("out", [NT, 1024], F32, kind="ExternalOutput").ap()
    W = {k: nc.dram_tensor(k, v, F32, kind="ExternalInput").ap() for k, v in WSHAPES.items()}
    hs = [nc.dram_tensor("h_scr%d" % i, [8, 128, NT], F32, kind="Internal").ap() for i in range(4)]
    phase_mlstm(nc, C, x, hs[0], W, "ml")
    phase_ffn(nc, C, hs[0], hs[1], W["w_gate_up0"], W["w_down0"], W["norm_ffn0"], "f0")
    phase_moba(nc, C, hs[1], hs[2], W, "mb")
    phase_ffn(nc, C, hs[2], hs[3], W["w_gate_up1"], W["w_down1"], W["norm_ffn1"], "f1")
    phase_ple_out(nc, C, hs[3], out, W["p1"], W["norm_ple1"], W["w_ple_gate1"], W["w_ple_up1"], "po")
    return nc, Cn


def make_in_maps(inputs, cores):
    f = lambda a: np.ascontiguousarray(np.asarray(a, dtype=np.float32))
    I = {k: np.asarray(v) for k, v in inputs.items()}
    shared = {
        "norm_mix0": f(I["norm_mix"][0]), "norm_mix1": f(I["norm_mix"][1]), "a_w_in": f(I["a_w_in"][0]), "a_b_gate": f(I["a_b_gate"][0]),
        "a_mh_gain": f(I["a_mh_gain"][0]), "a_w_out": f(I["a_w_out"][0]), "kv_norm": f(I["kv_norm"]), "w_kv": f(I["w_kv"]), "k_norm": f(I["k_norm"]),
        "b_w_q": f(I["b_w_q"][0]), "b_q_norm": f(I["b_q_norm"][0]), "b_w_o": f(I["b_w_o"][0]),
        "norm_ffn0": f(I["norm_ffn"][0]), "norm_ffn1": f(I["norm_ffn"][1]), "w_gate_up0": f(I["w_gate_up"][0]), "w_gate_up1": f(I["w_gate_up"][1]),
        "w_down0": f(I["w_down"][0]), "w_down1": f(I["w_down"][1]), "norm_ple0": f(I["norm_ple"][0]), "norm_ple1": f(I["norm_ple"][1]),
        "w_ple_gate0": f(I["w_ple_gate"][0]), "w_ple_gate1": f(I["w_ple_gate"][1]), "w_ple_up0": f(I["w_ple_up"][0]), "w_ple_up1": f(I["w_ple_up"][1]),
    }
    maps = []
    for b in cores:
        m = dict(shared)
        m["x"] = f(I["x"][b])
        m["p0"] = f(I["p"][0, b])
        m["p1"] = f(I["p"][1, b])
        maps.append(m)
    return maps


def kernel(**inputs):
    nc, Cn = build_program()
    maps = make_in_maps(inputs, list(range(8)))
    for m in maps:
        m.update(Cn)
    res = run_bass_kernel_spmd(nc, maps, core_ids=list(range(8)))
    return np.stack([np.asarray(r["out"], dtype=np.float32) for r in res.results], axis=0)
```

```python
import numpy as np
import concourse.bass as bass
import concourse.mybir as mybir
from concourse.bass_utils import run_bass_kernel_spmd
from contextlib import ExitStack

F32 = mybir.dt.float32
BF16 = mybir.dt.bfloat16
ALU = mybir.AluOpType
AF = mybir.ActivationFunctionType
AX = mybir.AxisListType

ENGS = ["tensor", "vector", "scalar", "gpsimd", "sync"]
NDMASEM = 12
SAME_ENGINE_SYNC = True

NT = 4096
NB = 512
NBLK = NT // NB
EPS = 1e-6
NEG = -30000.0


class Res:
    __slots__ = ("name", "w", "r", "gd")

    def __init__(self, name=""):
        self.name = name
        self.w = []
        self.r = []
        self.gd = []


class WRes:
    def __init__(self, grid, cw):
        self.grid = grid
        self.cw = cw

    def sel(self, k, c0, c1):
        return [self.grid[k][ci] for ci in range(c0 // self.cw, (c1 - 1) // self.cw + 1)]


class Op:
    __slots__ = ("eng", "fn", "waits", "pos", "sig", "isdma", "semi", "semk", "K", "sigidx")


class Sched:
    G = {"nc": None}

    @staticmethod
    def setup(nc):
        if Sched.G.get("nc") is nc:
            return
        es = ExitStack()
        G = {"nc": nc, "es": es, "sig": {e: 0 for e in ENGS}, "dma": {e: 0 for e in ENGS}}
        G["esem"] = {e: es.enter_context(nc.semaphore("sem_e_%s" % e)) for e in ENGS}
        G["dsem"] = {(e, i): es.enter_context(nc.semaphore("sem_d_%s_%d" % (e, i))) for e in ("sync", "gpsimd") for i in range(NDMASEM)}
        Sched.G = G

    def __init__(self, nc):
        Sched.setup(nc)
        self.nc = nc
        self.ops = {e: [] for e in ENGS}
        self.Kcur = {e: {} for e in ENGS}
        self.nops = 0

    limit = None

    def _record(self, eng, fn, reads, writes, isdma, join=False):
        if Sched.limit is not None and self.nops >= Sched.limit:
            return None
        o = Op()
        o.eng = eng
        o.fn = fn
        o.isdma = isdma
        o.sig = False
        o.pos = len(self.ops[eng])
        deps = {}
        for r in reads:
            for x in r.w:
                deps[id(x)] = x
        for w in writes:
            if join and not w.r:
                for x in w.gd:
                    deps[id(x)] = x
            else:
                for x in w.w:
                    deps[id(x)] = x
                for x in w.r:
                    deps[id(x)] = x
        K = self.Kcur[eng]
        newK = None
        waits = []
        if isdma:
            i = Sched.G["dma"][eng]
            Sched.G["dma"][eng] += 1
            o.semi = i % NDMASEM
            o.semk = i // NDMASEM + 1
            if o.semk > 1:
                key = ("d", eng, o.semi)
                if K.get(key, 0) < o.semk - 1:
                    waits.append(("dmaslot", eng, o.semi, o.semk - 1))
                    newK = dict(K)
                    newK[key] = o.semk - 1
        best = {}
        dl = []
        for y in deps.values():
            if y is o:
                continue
            if y.isdma:
                dl.append(y)
            elif y.eng not in best or best[y.eng].pos < y.pos:
                best[y.eng] = y
        for y in dl + list(best.values()):
            cur = K if newK is None else newK
            if y.isdma:
                key = ("d", y.eng, y.semi)
                if cur.get(key, 0) >= y.semk:
                    continue
                waits.append(("dma", y))
            else:
                if y.eng == eng and (eng == "tensor" or not SAME_ENGINE_SYNC) and not isdma:
                    continue
                if cur.get(y.eng, -1) >= y.pos:
                    continue
                y.sig = True
                waits.append(("eng", y))
            if newK is None:
                newK = dict(K)
            for k, v in y.K.items():
                if newK.get(k, -1) < v:
                    newK[k] = v
            if y.isdma:
                key = ("d", y.eng, y.semi)
                newK[key] = max(newK.get(key, 0), y.semk)
            else:
                if newK.get(y.eng, -1) < y.pos:
                    newK[y.eng] = y.pos
        if newK is not None:
            self.Kcur[eng] = newK
            K = newK
        o.K = K
        o.waits = waits
        for r in reads:
            r.r.append(o)
        for w in writes:
            if join and not w.r:
                w.w.append(o)
            else:
                w.gd = w.w + w.r
                w.w = [o]
                w.r = []
        self.ops[eng].append(o)
        self.nops += 1
        return o

    def op(self, eng, fn, reads=(), writes=(), join=False):
        return self._record(eng, fn, reads, writes, False, join)

    def dma(self, queue, out, in_, reads=(), writes=(), join=False, nonc=False, **kw):
        nc = self.nc
        if nonc:
            def f(e):
                with nc.allow_non_contiguous_dma(reason="small strided parameter load"):
                    return e.dma_start(out=out, in_=in_, **kw)
        else:
            def f(e):
                return e.dma_start(out=out, in_=in_, **kw)
        return self._record(queue, f, reads, writes, True, join)

    def emit(self):
        nc = self.nc
        G = Sched.G
        for e in ENGS:
            n = G["sig"][e]
            for o in self.ops[e]:
                if o.sig:
                    n += 1
                o.sigidx = n
            G["sig"][e] = n
        esem, dsem = G["esem"], G["dsem"]
        with nc.Block() as block:
            def stream(ename):
                def body(eng):
                    for o in self.ops[ename]:
                        for w in o.waits:
                            if w[0] == "dmaslot":
                                eng.wait_ge(dsem[(w[1], w[2])], 16 * w[3])
                            elif w[0] == "dma":
                                y = w[1]
                                eng.wait_ge(dsem[(y.eng, y.semi)], 16 * y.semk)
                            else:
                                y = w[1]
                                eng.wait_ge(esem[y.eng], y.sigidx)
                        ins = o.fn(eng)
                        if o.isdma:
                            ins.then_inc(dsem[(o.eng, o.semi)], 16)
                        elif o.sig:
                            ins.then_inc(esem[o.eng], 1)
                return body

            for e in ENGS:
                if self.ops[e]:
                    getattr(block, e)(stream(e))


class Proxy:
    def __init__(self, real):
        self.real = real
        self.tgt = real

    def op(self, *a, **k):
        return self.tgt.op(*a, **k)

    def dma(self, *a, **k):
        return self.tgt.dma(*a, **k)


class Mux:
    def __init__(self, real, chunks):
        self.real = real
        self.chunks = chunks
        self.last = None

    def _maybe(self, eng):
        if self.last == "tensor" and eng != "tensor" and self.chunks:
            for kind, a, k in self.chunks.pop(0):
                getattr(self.real, kind)(*a, **k)
        self.last = eng

    def op(self, eng, *a, **k):
        self._maybe(eng)
        return self.real.op(eng, *a, **k)

    def dma(self, eng, *a, **k):
        self._maybe(eng)
        return self.real.dma(eng, *a, **k)

    def flush(self):
        while self.chunks:
            for kind, a, k in self.chunks.pop(0):
                getattr(self.real, kind)(*a, **k)


def mm_chunks(q):
    runs = []
    for item in q:
        is_mm = (item[0] == "op" and item[1][0] == "tensor")
        if runs and runs[-1][0] == is_mm:
            runs[-1][1].append(item)
        else:
            runs.append((is_mm, [item]))
    chunks = []
    cur = []
    for is_mm, items in runs:
        cur.extend(items)
        if is_mm:
            chunks.append(cur)
            cur = []
    if cur:
        chunks.append(cur)
    return chunks


class Deferred:
    def __init__(self):
        self.q = []

    def op(self, *a, **k):
        self.q.append(("op", a, k))

    def dma(self, *a, **k):
        self.q.append(("dma", a, k))


def interleave(main_gen, real, q):
    steps = list(main_gen) if False else None
    i = 0
    n = getattr(main_gen, "nsteps", None)
    return i


class Phase:
    def __init__(self, nc, name):
        self.nc = nc
        self.name = name
        self.es = ExitStack()
        self.S = Sched(nc)
        self.n = 0
        self.Rdram = Res("dram_out")

    def sb(self, shape, dt, name=None):
        self.n += 1
        t = self.es.enter_context(self.nc.sbuf_tensor("%s_s%d" % (self.name, self.n), list(shape), dt))
        return t

    def ps(self, shape, dt, name=None):
        self.n += 1
        t = self.es.enter_context(self.nc.psum_tensor("%s_p%d" % (self.name, self.n), list(shape), dt))
        return t

    def finish(self):
        S = self.S
        S.op("sync", lambda e: e.nop(), reads=[self.Rdram])
        S.emit()
        self.es.close()

    def consts(self, C):
        S = self.S
        self.ident_f = self.sb([128, 128], F32)
        self.ident_b = self.sb([128, 128], BF16)
        self.ones_b = self.sb([128, 128], BF16)
        self.eps_t = self.sb([128, 1], F32)
        self.one_t = self.sb([128, 1], F32)
        self.Rc = Res("consts")
        S.dma("sync", self.ident_f[:], C["c_ident"], writes=[self.Rc], join=True)
        S.op("vector", lambda e: e.tensor_copy(out=self.ident_b[:], in_=self.ident_f[:]), reads=[self.Rc], writes=[self.Rc])
        S.op("vector", lambda e: e.memset(self.ones_b[:], 1.0), writes=[self.Rc], join=True)
        S.op("vector", lambda e: e.memset(self.eps_t[:], EPS), writes=[self.Rc], join=True)
        S.op("vector", lambda e: e.memset(self.one_t[:], 1.0), writes=[self.Rc], join=True)

    def load_vec_fm(self, ap1024, nk=8):
        t = self.sb([128, nk], F32)
        r = Res()
        self.S.dma("sync", t[:], ap1024.rearrange("(k p) -> p k", p=128), writes=[r], nonc=True)
        return t, r

    def load_bcast(self, ap_flat, n):
        t = self.sb([128, n], F32)
        r = Res()
        self.S.dma("sync", t[:], ap_flat.partition_broadcast(128), writes=[r])
        return t, r

    def load_w(self, src, K, N, rows=128, col_chunk=2048, queue="gpsimd", chunk_major=False):
        t = self.sb([rows, K, N], BF16)
        nch = -(-N // col_chunk)
        cw = -(-N // nch)
        if not chunk_major:
            rs = [Res() for _ in range(K)]
            for k in range(K):
                c0 = 0
                while c0 < N:
                    c1 = min(N, c0 + cw)
                    self.S.dma(queue, t[:, k, c0:c1], src[k * rows:(k + 1) * rows, c0:c1], writes=[rs[k]], join=True)
                    c0 = c1
            return t, rs
        grid = [[Res() for _ in range(nch)] for _ in range(K)]
        order = list(range(nch))
        if nch == 4:
            order = [0, 2, 1, 3]
        for ci in order:
            c0, c1 = ci * cw, min(N, (ci + 1) * cw)
            for k in range(K):
                self.S.dma(queue, t[:, k, c0:c1], src[k * rows:(k + 1) * rows, c0:c1], writes=[grid[k][ci]])
        return t, WRes(grid, cw)

    def norm_setup(self):
        self.rstd = self.sb([128, NB], F32)
        self.Rrstd = Res()
        self.lnv = self.rstd
        self.Rlnv = self.Rrstd

    def fm_norm(self, hT, RhT, g, Rg, hn, Rhn, pss, Rpss, reuse_rstd=False):
        S = self.S
        n = hT.shape[2]
        sq, Rsq = hn, Rhn
        for k in range(8 if not reuse_rstd else 0):
            S.op("scalar", lambda e, k=k: e.activation(out=sq[:, k, :], in_=hT[:, k, :], func=AF.Square),
                 reads=[RhT[k]], writes=[Rsq[k]])
        for k in range(8 if not reuse_rstd else 0):
            S.op("tensor", lambda e, k=k: e.matmul(pss[:, 0:n], lhsT=self.ones_b[:], rhs=sq[:, k, :], start=(k == 0), stop=(k == 7)),
                 reads=[Rsq[k], self.Rc], writes=[Rpss], join=(k > 0))
        if not reuse_rstd:
            S.op("scalar", lambda e: e.activation(out=self.lnv[:, 0:n], in_=pss[:, 0:n], func=AF.Ln, scale=1.0 / 1024.0, bias=self.eps_t[:, 0:1]),
                 reads=[Rpss, self.Rc], writes=[self.Rlnv])
            S.op("scalar", lambda e: e.activation(out=self.rstd[:, 0:n], in_=self.lnv[:, 0:n], func=AF.Exp, scale=-0.5),
                 reads=[self.Rlnv], writes=[self.Rrstd])
        for k in range(8):
            S.op("vector", lambda e, k=k: e.scalar_tensor_tensor(out=hn[:, k, :], in0=hT[:, k, :], scalar=g[:, k:k + 1], op0=ALU.mult,
                                                                 in1=self.rstd[:, 0:n], op1=ALU.mult),
                 reads=[RhT[k], self.Rrstd, Rg], writes=[Rhn[k]])


def phase_ffn(nc, C, hin, hout, wgu_ap, wd_ap, g_ap, name):
    P = Phase(nc, name)
    S = P.S
    P.consts(C)
    g, Rg = P.load_vec_fm(g_ap)
    wgu, Rwgu = P.load_w(wgu_ap, 8, 5632, col_chunk=1408, chunk_major=True)
    wd, Rwd = P.load_w(wd_ap, 22, 1024)
    P.norm_setup()
    hTs = [P.sb([128, 8, NB], F32) for _ in range(2)]
    RhTs = [[Res() for _ in range(8)] for _ in range(2)]
    hns = [P.sb([128, 8, NB], BF16) for _ in range(2)]
    Rhns = [[Res() for _ in range(8)] for _ in range(2)]
    act = P.sb([128, 22, NB], BF16)
    Ract = [Res() for _ in range(22)]
    sg = [P.sb([128, NB], BF16) for _ in range(2)]
    Rsg = [Res() for _ in range(2)]
    pss = P.ps([128, NB], F32)
    Rpss = Res()
    pg = [P.ps([128, NB], F32) for _ in range(2)]
    Rpg = [Res() for _ in range(2)]
    pu = [P.ps([128, NB], F32) for _ in range(2)]
    Rpu = [Res() for _ in range(2)]
    po = [P.ps([128, NB], F32) for _ in range(2)]
    Rpo = [Res() for _ in range(2)]

    def load(blk):
        t0 = blk * NB
        for k in range(8):
            S.dma("sync", hTs[blk % 2][:, k, :], hin[k, :, t0:t0 + NB], writes=[RhTs[blk % 2][k]])

    def norm(blk):
        P.fm_norm(hTs[blk % 2], RhTs[blk % 2], g, Rg, hns[blk % 2], Rhns[blk % 2], pss, Rpss)

    load(0)
    norm(0)
    for blk in range(NBLK):
        t0 = blk * NB
        hT, RhT = hTs[blk % 2], RhTs[blk % 2]
        hn, Rhn = hns[blk % 2], Rhns[blk % 2]
        if blk + 1 < NBLK:
            load(blk + 1)
        for c in range(22):
            b = c % 2
            for k in range(8):
                S.op("tensor", lambda e, k=k, c=c, b=b, hn=hn: e.matmul(pg[b][:], lhsT=wgu[:, k, c * 128:(c + 1) * 128], rhs=hn[:, k, :],
                                                                        start=(k == 0), stop=(k == 7)),
                     reads=Rwgu.sel(k, c * 128, (c + 1) * 128) + [Rhn[k]], writes=[Rpg[b]], join=(k > 0))
            for k in range(8):
                S.op("tensor", lambda e, k=k, c=c, b=b, hn=hn: e.matmul(pu[b][:], lhsT=wgu[:, k, 2816 + c * 128:2816 + (c + 1) * 128], rhs=hn[:, k, :],
                                                                        start=(k == 0), stop=(k == 7)),
                     reads=Rwgu.sel(k, 2816 + c * 128, 2816 + (c + 1) * 128) + [Rhn[k]], writes=[Rpu[b]], join=(k > 0))
            S.op("scalar", lambda e, b=b: e.activation(out=sg[b][:], in_=pg[b][:], func=AF.Silu), reads=[Rpg[b]], writes=[Rsg[b]])
            S.op("vector", lambda e, b=b, c=c: e.tensor_tensor(out=act[:, c, :], in0=pu[b][:], in1=sg[b][:], op=ALU.mult),
                 reads=[Rpu[b], Rsg[b]], writes=[Ract[c]])
        if blk + 1 < NBLK:
            norm(blk + 1)
        for d in range(8):
            b = d % 2
            for c in range(22):
                S.op("tensor", lambda e, c=c, d=d, b=b: e.matmul(po[b][:], lhsT=wd[:, c, d * 128:(d + 1) * 128], rhs=act[:, c, :],
                                                                  start=(c == 0), stop=(c == 21)),
                     reads=[Rwd[c], Ract[c]], writes=[Rpo[b]], join=(c > 0))
            S.op("vector", lambda e, d=d, b=b, hT=hT: e.tensor_tensor(out=hT[:, d, :], in0=po[b][:], in1=hT[:, d, :], op=ALU.add),
                 reads=[Rpo[b], RhT[d]], writes=[RhT[d]])
            S.dma("sync", hout[d, :, t0:t0 + NB], hT[:, d, :], reads=[RhT[d]], writes=[P.Rdram], join=True)
    P.finish()


def make_consts():
    c = {}
    c["c_ident"] = np.eye(128, dtype=np.float32)
    s = np.arange(128)
    c["c_tri"] = (s[:, None] <= s[None, :]).astype(np.float32)
    c["c_cbias"] = np.where(s[:, None] <= s[None, :], 0.0, NEG).astype(np.float32)
    inv = 500000.0 ** (-np.arange(0, 16, 2, dtype=np.float64) / 16.0)
    ang = np.arange(NT, dtype=np.float64)[:, None] * inv[None, :]
    c["c_rope"] = np.concatenate([np.cos(ang), np.sin(ang)], axis=1).astype(np.float32)
    u = np.arange(NT)
    c["c_onehot"] = (u[None, :] // 256 == np.arange(16)[:, None]).astype(np.float32)
    c["c_tribias4"] = np.tile(c["c_cbias"], (1, 4)).astype(np.float32)
    b = np.arange(16)
    c["c_past"] = np.where(b[None, :] < b[:, None], 0.0, NEG).astype(np.float32).reshape(256)
    c["c_own"] = (b[None, :] == b[:, None]).astype(np.float32).reshape(256)
    return c


class PLE:
    def __init__(self, P, C, p_ap, g_ap, wpg_ap, wpu_ap, banks, nb=NB, nbuf=1):
        self.P = P
        self.nb = nb
        S = P.S
        self.p_ap = p_ap
        self.g, self.Rg = P.load_vec_fm(g_ap)
        self.wpg, self.Rwpg = P.load_w(wpg_ap, 8, 1024)
        self.wpu, self.Rwpu = P.load_w(wpu_ap, 2, 1024)
        self.ptm = [P.sb([128, nb // 128, 256], F32) for _ in range(nbuf)]
        self.Rptm = [Res() for _ in range(nbuf)]
        self.pT = [P.sb([128, 2, nb], BF16) for _ in range(nbuf)]
        self.RpT = [[Res(), Res()] for _ in range(nbuf)]
        self.nbuf = nbuf
        self.sgate = P.sb([128, nb], F32)
        self.Rsgate = Res()
        self.tmp = P.sb([128, nb], F32)
        self.Rtmp = Res()
        self.banks = banks

    def pre(self, blk, hT, RhT, hn, Rhn, pss, Rpss):
        P = self.P
        S = P.S
        nb = self.nb
        t0 = blk * nb
        i = blk % self.nbuf
        ptm, Rptm, pT, RpT = self.ptm[i], self.Rptm[i], self.pT[i], self.RpT[i]
        (pc, Rpc) = self.banks[2]
        P.fm_norm(hT, RhT, self.g, self.Rg, hn, Rhn, pss, Rpss)
        S.dma("sync", ptm[:], self.p_ap[t0:t0 + nb, :].rearrange("(s p) d -> p s d", p=128), writes=[Rptm])
        for kk in range(2):
            for s in range(nb // 128):
                S.op("tensor", lambda e, kk=kk, s=s: e.transpose(out=pc[:, s * 128:(s + 1) * 128], in_=ptm[:, s, kk * 128:(kk + 1) * 128],
                                                                 identity=P.ident_f[:]),
                     reads=[Rptm, P.Rc], writes=[Rpc], join=(s > 0))
            S.op("scalar", lambda e, kk=kk: e.copy(out=pT[:, kk, :], in_=pc[:, 0:nb]), reads=[Rpc], writes=[RpT[kk]])

    def main(self, blk, hT, RhT, hn, Rhn):
        P = self.P
        S = P.S
        nb = self.nb
        i = blk % self.nbuf
        pT, RpT = self.pT[i], self.RpT[i]
        (pa, Rpa), (pb, Rpb) = self.banks[:2]
        for d in range(8):
            for k in range(8):
                S.op("tensor", lambda e, k=k, d=d: e.matmul(pa[:, 0:nb], lhsT=self.wpg[:, k, d * 128:(d + 1) * 128], rhs=hn[:, k, :],
                                                             start=(k == 0), stop=(k == 7)),
                     reads=[self.Rwpg[k], Rhn[k]], writes=[Rpa], join=(k > 0))
            S.op("scalar", lambda e: e.activation(out=self.sgate[:], in_=pa[:, 0:nb], func=AF.Sigmoid), reads=[Rpa], writes=[self.Rsgate])
            for kk in range(2):
                S.op("tensor", lambda e, kk=kk, d=d: e.matmul(pb[:, 0:nb], lhsT=self.wpu[:, kk, d * 128:(d + 1) * 128], rhs=pT[:, kk, :],
                                                               start=(kk == 0), stop=(kk == 1)),
                     reads=[self.Rwpu[kk], RpT[kk]], writes=[Rpb], join=(kk > 0))
            S.op("vector", lambda e: e.tensor_tensor(out=self.tmp[:], in0=pb[:, 0:nb], in1=self.sgate[:], op=ALU.mult),
                 reads=[Rpb, self.Rsgate], writes=[self.Rtmp])
            S.op("gpsimd", lambda e, d=d: e.tensor_tensor(out=hT[:, d, :], in0=hT[:, d, :], in1=self.tmp[:], op=ALU.add),
                 reads=[self.Rtmp, RhT[d]], writes=[RhT[d]])

    def emit(self, blk, hT, RhT, hn, Rhn, pss, Rpss):
        self.pre(blk, hT, RhT, hn, Rhn, pss, Rpss)
        self.main(blk, hT, RhT, hn, Rhn)


def phase_ple_out(nc, C, hin, out_ap, p_ap, g_ap, wpg_ap, wpu_ap, name):
    P = Phase(nc, name)
    S = P.S
    P.consts(C)
    P.norm_setup()
    banks = [(P.ps([128, NB], F32), Res()) for _ in range(5)]
    pss, Rpss = P.ps([128, NB], F32), Res()
    ple = PLE(P, C, p_ap, g_ap, wpg_ap, wpu_ap, banks, nbuf=2)
    hTs = [P.sb([128, 8, NB], F32) for _ in range(2)]
    RhTs = [[Res() for _ in range(8)] for _ in range(2)]
    hns = [P.sb([128, 8, NB], BF16) for _ in range(2)]
    Rhns = [[Res() for _ in range(8)] for _ in range(2)]
    otm = P.sb([128, 4, 1024], F32)
    Rotm = [Res() for _ in range(4)]

    def load(blk):
        t0 = blk * NB
        for k in range(8):
            S.dma("sync", hTs[blk % 2][:, k, :], hin[k, :, t0:t0 + NB], writes=[RhTs[blk % 2][k]])

    load(0)
    ple.pre(0, hTs[0], RhTs[0], hns[0], Rhns[0], pss, Rpss)
    for blk in range(NBLK):
        t0 = blk * NB
        hT, RhT = hTs[blk % 2], RhTs[blk % 2]
        if blk + 1 < NBLK:
            load(blk + 1)
        ple.main(blk, hT, RhT, hns[blk % 2], Rhns[blk % 2])
        if blk + 1 < NBLK:
            n = (blk + 1) % 2
            ple.pre(blk + 1, hTs[n], RhTs[n], hns[n], Rhns[n], pss, Rpss)
        for s in range(4):
            for kq in range(2):
                pt, Rpt = banks[3 + kq]
                for k4 in range(4):
                    k = kq * 4 + k4
                    S.op("tensor", lambda e, k=k, k4=k4, s=s, pt=pt, hT=hT: e.transpose(out=pt[:, k4 * 128:(k4 + 1) * 128], in_=hT[:, k, s * 128:(s + 1) * 128],
                                                                                        identity=P.ident_f[:]),
                         reads=[RhT[k], P.Rc], writes=[Rpt], join=(k4 > 0))
                if kq == 0:
                    S.op("scalar", lambda e, s=s, kq=kq, pt=pt: e.copy(out=otm[:, s, kq * 512:(kq + 1) * 512], in_=pt[:]),
                         reads=[Rpt], writes=[Rotm[s]], join=(kq > 0))
                else:
                    S.op("vector", lambda e, s=s, kq=kq, pt=pt: e.tensor_copy(out=otm[:, s, kq * 512:(kq + 1) * 512], in_=pt[:]),
                         reads=[Rpt], writes=[Rotm[s]], join=(kq > 0))
            S.dma("sync", out_ap[t0 + s * 128:t0 + (s + 1) * 128, :], otm[:, s, :], reads=[Rotm[s]], writes=[P.Rdram], join=True)
    P.finish()


def phase_mlstm(nc, C, x_ap, hout, W, name, nblk=NBLK):
    P = Phase(nc, name)
    realS = P.S
    S = Proxy(realS)
    P.S = S
    P.consts(C)
    P.norm_setup()
    tri_f = P.sb([128, 128], F32)
    cbias = P.sb([128, 128], F32)
    ones_f = P.sb([128, 128], F32)
    S.dma("sync", tri_f[:], C["c_tri"], writes=[P.Rc], join=True)
    S.dma("sync", cbias[:], C["c_cbias"], writes=[P.Rc], join=True)
    S.op("vector", lambda e: e.memset(ones_f[:], 1.0), writes=[P.Rc], join=True)
    g, Rg = P.load_vec_fm(W["norm_mix0"])
    bgate, Rbgate = P.load_bcast(W["a_b_gate"], 16)
    mhg, Rmhg = P.load_bcast(W["a_mh_gain"], 128)
    mhg_h = P.sb([128, 128], F32)
    S.op("vector", lambda e: e.tensor_scalar(out=mhg_h[:], in0=mhg[:], scalar1=0.5, scalar2=None, op0=ALU.mult), reads=[Rmhg], writes=[Rmhg])
    nhalf = P.sb([128, 8], F32)
    S.op("vector", lambda e: e.memset(nhalf[:], -0.5), writes=[P.Rc], join=True)
    win, Rwin = P.load_w(W["a_w_in"], 8, 3088, col_chunk=1544)
    wout, Rwout = P.load_w(W["a_w_out"], 8, 1024)

    xtm = P.sb([128, 4, 1024], F32); Rxtm = [Res() for _ in range(4)]
    hT = P.sb([128, 8, NB], F32); RhT = [Res() for _ in range(8)]
    hn = P.sb([128, 8, NB], BF16); Rhn = [Res() for _ in range(8)]
    qkT = P.sb([128, 8, NB], BF16); Rqk = [Res() for _ in range(8)]
    ktm = P.sb([128, 4, 512], BF16); Rktm = [Res() for _ in range(4)]
    vtm = P.sb([128, 4, 1024], BF16); Rvtm = [Res() for _ in range(4)]
    og = P.sb([128, 4, 1024], BF16); Rog = [Res() for _ in range(4)]
    sgt = P.sb([128, 512], F32); Rsgt = Res()
    gsb = P.sb([128, 4, 16], F32); Rgsb = Res()
    th = P.sb([128, 4, 16], F32); Rth = Res()
    ef = P.sb([128, 4, 8], F32); Ref = Res()
    spf = P.sb([128, 4, 8], F32); Rspf = Res()
    li = P.sb([128, 4, 8], F32); Rli = Res()
    lf = P.sb([128, 4, 8], F32); Rlf = Res()
    g_sb = P.sb([128, 8], F32); Rg_sb = Res()
    bb = P.sb([128, 8], F32); Rbb = Res()
    eg = P.sb([128, 8], F32); Reg = Res()
    wlp = P.sb([128, 8], F32); Rwlp = Res()
    wl = P.sb([128, 8], F32); Rwl = Res()
    egl = P.sb([128, 4], F32); Regl = Res()
    Gd = P.sb([128, 8, 128], F32); RGd = Res()
    arg = P.sb([128, 8, 128], F32); Rarg = Res()
    DT = P.sb([128, 8, 128], F32); RDT = Res()
    PT = P.sb([128, 8, 128], BF16); RPT = Res()
    kw = P.sb([128, 8, 64], BF16); Rkw = Res()
    numXs = P.sb([128, 8, 128], F32); RnumXs = Res()
    num = P.sb([128, 8, 128], F32); Rnum = Res()
    sqn = P.sb([128, 8, 128], F32); Rsqn = Res()
    sm = {n: (P.sb([128, 8], F32), Res()) for n in ["dxs", "den", "dd", "rec", "ssn", "t1", "t2", "lnt", "rs", "coef"]}
    y0 = P.sb([128, 8, 128], F32); Ry0 = Res()
    ytm = P.sb([128, 1024], BF16); Rytm = Res()
    yT = P.sb([128, 8, NB], BF16); RyT = [Res() for _ in range(4)]
    Cst = P.sb([128, 4, 128], F32); nst = P.sb([128, 4], F32); RC = Res()
    Cbf = P.sb([128, 4, 2, 128], BF16); nbf = P.sb([128, 4, 2], BF16); RCbf = Res()
    qbd = P.sb([128, 4, 2, NB], BF16); Rqbd = [Res() for _ in range(4)]
    nt1 = P.sb([128, 4], F32); Rnt1 = Res()

    pS = P.ps([128, 512], F32)
    RpS = Res()
    Rgcs = Rglast = RdenI = RdenX = Rdn = Rpgate = RpS
    pR = [P.ps([128, 512], F32) for _ in range(2)]; RpR = [Res(), Res()]
    pG = P.ps([128, 1024], F32); RpG = Res()
    pT2 = P.ps([128, 1024], F32); RpT2 = Res()
    pY = P.ps([128, 1024], BF16); RpY = Res()
    rot = [0]

    def nextbank():
        rot[0] ^= 1
        return pR[rot[0]], RpR[rot[0]]

    for t in (Cst, nst):
        S.op("vector", lambda e, t=t: e.memset(t[:], 0.0), writes=[RC], join=True)
    for t in (Cbf, nbf):
        S.op("vector", lambda e, t=t: e.memset(t[:], 0.0), writes=[RCbf], join=True)
    for c in range(4):
        S.op("gpsimd", lambda e, c=c: e.memset(qbd[:, c, :, :], 0.0), writes=[Rqbd[c]])

    for blk in range(nblk):
        t0 = blk * NB
        for s in range(4):
            S.dma("sync", xtm[:, s, :], x_ap[t0 + s * 128:t0 + (s + 1) * 128, :], writes=[Rxtm[s]])
        for k in range(8):
            pb, Rpb = nextbank()
            for s in range(4):
                S.op("tensor", lambda e, k=k, s=s, pb=pb: e.transpose(out=pb[:, s * 128:(s + 1) * 128], in_=xtm[:, s, k * 128:(k + 1) * 128],
                                                                       identity=P.ident_f[:]),
                     reads=[Rxtm[s], P.Rc], writes=[Rpb], join=(s > 0))
            S.op("scalar", lambda e, k=k, pb=pb: e.copy(out=hT[:, k, :], in_=pb[:]), reads=[Rpb], writes=[RhT[k]])
        pb, Rpb = nextbank()
        P.fm_norm(hT, RhT, g, Rg, hn, Rhn, pb, Rpb)
        for c in range(8):
            pb, Rpb = nextbank()
            for k in range(8):
                S.op("tensor", lambda e, k=k, c=c, pb=pb: e.matmul(pb[:], lhsT=win[:, k, c * 128:(c + 1) * 128], rhs=hn[:, k, :],
                                                                    start=(k == 0), stop=(k == 7)),
                     reads=[Rwin[k], Rhn[k]], writes=[Rpb], join=(k > 0))
            sc = 0.125 if c < 4 else 1.0
            S.op("scalar", lambda e, c=c, pb=pb, sc=sc: e.activation(out=qkT[:, c, :], in_=pb[:], func=AF.Copy, scale=sc),
                 reads=[Rpb], writes=[Rqk[c]])
            if c < 4:
                S.op("gpsimd", lambda e, c=c: e.tensor_copy(out=qbd[0:64, c, 0, :], in_=qkT[0:64, c, :]), reads=[Rqk[c]], writes=[Rqbd[c]])
                S.op("gpsimd", lambda e, c=c: e.tensor_copy(out=qbd[64:128, c, 1, :], in_=qkT[64:128, c, :]), reads=[Rqk[c]], writes=[Rqbd[c]], join=True)
        def proj_kvo(s):
            ts = slice(s * 128, (s + 1) * 128)
            pb, Rpb = nextbank()
            for k in range(8):
                S.op("tensor", lambda e, k=k, ts=ts, pb=pb: e.matmul(pb[:], lhsT=hn[:, k, ts], rhs=win[:, k, 512:1024], start=(k == 0), stop=(k == 7)),
                     reads=[Rwin[k], Rhn[k]], writes=[Rpb], join=(k > 0))
            S.op("scalar", lambda e, s=s, pb=pb: e.copy(out=ktm[:, s, :], in_=pb[:]), reads=[Rpb], writes=[Rktm[s]])
            for half in range(2):
                pb, Rpb = nextbank()
                c0 = 1024 + half * 512
                for k in range(8):
                    S.op("tensor", lambda e, k=k, ts=ts, pb=pb, c0=c0: e.matmul(pb[:], lhsT=hn[:, k, ts], rhs=win[:, k, c0:c0 + 512],
                                                                                 start=(k == 0), stop=(k == 7)),
                         reads=[Rwin[k], Rhn[k]], writes=[Rpb], join=(k > 0))
                S.op("vector", lambda e, s=s, half=half, pb=pb: e.tensor_copy(out=vtm[:, s, half * 512:(half + 1) * 512], in_=pb[:]),
                     reads=[Rpb], writes=[Rvtm[s]], join=(half > 0))
            for half in range(2):
                pb, Rpb = nextbank()
                c0 = 2048 + half * 512
                for k in range(8):
                    S.op("tensor", lambda e, k=k, ts=ts, pb=pb, c0=c0: e.matmul(pb[:], lhsT=hn[:, k, ts], rhs=win[:, k, c0:c0 + 512],
                                                                                 start=(k == 0), stop=(k == 7)),
                         reads=[Rwin[k], Rhn[k]], writes=[Rpb], join=(k > 0))
                S.op("scalar", lambda e, pb=pb: e.activation(out=sgt[:], in_=pb[:], func=AF.Tanh, scale=0.5), reads=[Rpb], writes=[Rsgt])
                S.op("gpsimd", lambda e: e.tensor_tensor(
                    out=sgt[:].rearrange("p (h v) -> p h v", h=4), in0=sgt[:].rearrange("p (h v) -> p h v", h=4),
                    in1=mhg_h[:].unsqueeze(1).broadcast_to([128, 4, 128]), op=ALU.mult), reads=[Rsgt, Rmhg], writes=[Rsgt])
                S.op("gpsimd", lambda e, s=s, half=half: e.tensor_tensor(
                    out=og[:, s, half * 512:(half + 1) * 512].rearrange("p (h v) -> p h v", h=4),
                    in0=sgt[:].rearrange("p (h v) -> p h v", h=4),
                    in1=mhg_h[:].unsqueeze(1).broadcast_to([128, 4, 128]), op=ALU.add),
                     reads=[Rsgt, Rmhg], writes=[Rog[s]], join=(half > 0))

        for s in range(4):
            ts = slice(s * 128, (s + 1) * 128)
            for k in range(8):
                S.op("tensor", lambda e, k=k, ts=ts, s=s: e.matmul(pS[:, 64 + s * 16:64 + (s + 1) * 16], lhsT=hn[:, k, ts], rhs=win[:, k, 3072:3088],
                                                                    start=(k == 0), stop=(k == 7)),
                     reads=[Rwin[k], Rhn[k]], writes=[Rpgate], join=(k > 0))
            S.op("vector", lambda e, s=s: e.tensor_tensor(out=gsb[:, s, :], in0=pS[:, 64 + s * 16:64 + (s + 1) * 16], in1=bgate[:], op=ALU.add),
                 reads=[Rpgate, Rbgate], writes=[Rgsb], join=(s > 0))
        S.op("scalar", lambda e: e.activation(out=th[:], in_=gsb[:], func=AF.Tanh, scale=1.0 / 15.0), reads=[Rgsb], writes=[Rth])
        S.op("vector", lambda e: e.tensor_scalar(out=li[:], in0=th[:, :, 0:8], scalar1=15.0, scalar2=None, op0=ALU.mult), reads=[Rth], writes=[Rli])
        S.op("scalar", lambda e: e.activation(out=ef[:], in_=th[:, :, 8:16], func=AF.Exp, scale=-15.0), reads=[Rth], writes=[Ref])
        S.op("scalar", lambda e: e.activation(out=spf[:], in_=ef[:], func=AF.Ln, bias=P.one_t[:, 0:1]), reads=[Ref, P.Rc], writes=[Rspf])
        S.op("vector", lambda e: e.tensor_scalar(out=lf[:], in0=spf[:], scalar1=-1.0, scalar2=None, op0=ALU.mult), reads=[Rspf], writes=[Rlf])

        proj_kvo(0)
        for s in range(4):
            ts = slice(s * 128, (s + 1) * 128)
            if s < 3:
                d_ = Deferred()
                S.tgt = d_
                proj_kvo(s + 1)
                S.tgt = Mux(realS, mm_chunks(d_.q))
            else:
                S.tgt = realS
            S.op("tensor", lambda e, s=s: e.matmul(pS[:, 0:8], lhsT=tri_f[:], rhs=lf[:, s, :], start=True, stop=True),
                 reads=[Rlf, P.Rc], writes=[Rgcs])
            S.op("tensor", lambda e, s=s: e.matmul(pS[:, 8:16], lhsT=ones_f[:], rhs=lf[:, s, :], start=True, stop=True),
                 reads=[Rlf, P.Rc], writes=[Rglast])
            S.op("vector", lambda e: e.tensor_copy(out=g_sb[:], in_=pS[:, 0:8]), reads=[Rgcs], writes=[Rg_sb])
            S.op("vector", lambda e, s=s: e.tensor_tensor(out=bb[:], in0=li[:, s, :], in1=g_sb[:], op=ALU.subtract), reads=[Rli, Rg_sb], writes=[Rbb])
            S.op("scalar", lambda e: e.activation(out=eg[:], in_=g_sb[:], func=AF.Exp), reads=[Rg_sb], writes=[Reg])
            S.op("vector", lambda e: e.tensor_tensor(out=wlp[:], in0=pS[:, 8:16], in1=bb[:], op=ALU.add), reads=[Rglast, Rbb], writes=[Rwlp])
            S.op("scalar", lambda e: e.activation(out=wl[:], in_=wlp[:], func=AF.Exp), reads=[Rwlp], writes=[Rwl])
            S.op("scalar", lambda e: e.activation(out=egl[0:64, :], in_=pS[0:64, 8:16:2], func=AF.Exp), reads=[Rglast], writes=[Regl])
            S.op("scalar", lambda e: e.activation(out=egl[64:128, :], in_=pS[64:128, 9:16:2], func=AF.Exp), reads=[Rglast], writes=[Regl], join=True)
            S.op("vector", lambda e: e.tensor_tensor(out=Gd[:], in0=g_sb[:].unsqueeze(2).broadcast_to([128, 8, 128]),
                                                      in1=P.ident_f[:].unsqueeze(1).broadcast_to([128, 8, 128]), op=ALU.mult),
                 reads=[Rg_sb, P.Rc], writes=[RGd])
            for half in range(2):
                S.op("tensor", lambda e, half=half: e.matmul(pG[:, half * 512:(half + 1) * 512], lhsT=ones_f[:],
                                                               rhs=Gd[:, half * 4:(half + 1) * 4, :].rearrange("p h j -> p (h j)"),
                                                               start=True, stop=True),
                     reads=[RGd, P.Rc], writes=[RpG], join=(half > 0))
            for h in range(8):
                S.op("vector", lambda e, h=h: e.scalar_tensor_tensor(out=arg[:, h, :], in0=pG[:, h * 128:(h + 1) * 128], scalar=bb[:, h:h + 1], op0=ALU.add,
                                                                      in1=cbias[:], op1=ALU.add),
                     reads=[RpG, Rbb, P.Rc], writes=[Rarg], join=(h > 0))
            S.op("scalar", lambda e: e.activation(out=DT[:], in_=arg[:], func=AF.Exp), reads=[Rarg], writes=[RDT])
            for c in range(4):
                S.op("tensor", lambda e, c=c, ts=ts: e.matmul(pT2[:, c * 256:(c + 1) * 256], lhsT=qkT[:, 4 + c, ts], rhs=qbd[:, c, :, ts],
                                                               start=True, stop=True),
                     reads=[Rqbd[c], Rqk[4 + c]], writes=[RpT2], join=(c > 0))
            S.op("vector", lambda e: e.tensor_tensor(out=PT[:].rearrange("p h j -> p (h j)"), in0=pT2[:], in1=DT[:].rearrange("p h j -> p (h j)"), op=ALU.mult),
                 reads=[RpT2, RDT], writes=[RPT])
            for h in range(8):
                S.op("tensor", lambda e, h=h, s=s: e.matmul(pG[:, h * 128:(h + 1) * 128], lhsT=PT[:, h, :], rhs=vtm[:, s, h * 128:(h + 1) * 128],
                                                             start=True, stop=True),
                     reads=[RPT, Rvtm[s]], writes=[RpG], join=(h > 0))
            for h in range(8):
                S.op("tensor", lambda e, h=h: e.matmul(pS[:, 16 + h:17 + h], lhsT=PT[:, h, :], rhs=P.ones_b[:, 0:1], start=True, stop=True),
                     reads=[RPT, P.Rc], writes=[RdenI], join=(h > 0))
            for c in range(4):
                S.op("tensor", lambda e, c=c, ts=ts: e.matmul(pT2[:, c * 256:(c + 1) * 256], lhsT=qkT[:, c, ts], rhs=Cbf[:, c, :, :],
                                                               start=True, stop=True),
                     reads=[Rqk[c], RCbf], writes=[RpT2], join=(c > 0))
            for c in range(4):
                S.op("tensor", lambda e, c=c, ts=ts: e.matmul(pS[:, 24 + 2 * c:26 + 2 * c], lhsT=qkT[:, c, ts], rhs=nbf[:, c, :],
                                                               start=True, stop=True),
                     reads=[Rqk[c], RCbf], writes=[RdenX], join=(c > 0))
            S.op("vector", lambda e, s=s: e.tensor_tensor(out=kw[:], in0=ktm[:, s, :].rearrange("p (h d) -> p h d", h=8),
                                                           in1=wl[:].unsqueeze(2).broadcast_to([128, 8, 64]), op=ALU.mult),
                 reads=[Rktm[s], Rwl], writes=[Rkw])
            pd, Rpd = pY[:].bitcast(F32), RpY
            for h in range(8):
                c, ph = h // 2, h % 2
                prt = slice(ph * 64, (ph + 1) * 64)
                S.op("tensor", lambda e, h=h, c=c, prt=prt, s=s, pd=pd: e.matmul(pd[prt, c * 128:(c + 1) * 128], lhsT=kw[:, h, :], rhs=vtm[:, s, h * 128:(h + 1) * 128],
                                                                                  start=True, stop=True),
                     reads=[Rkw, Rvtm[s]], writes=[Rpd], join=(h > 0))
            for h in range(8):
                c, ph = h // 2, h % 2
                prt = slice(ph * 64, (ph + 1) * 64)
                S.op("tensor", lambda e, h=h, c=c, prt=prt: e.matmul(pS[prt, 32 + c:33 + c], lhsT=kw[:, h, :], rhs=P.ones_b[:, 0:1], start=True, stop=True),
                     reads=[Rkw, P.Rc], writes=[Rdn], join=(h > 0))
            for c in range(4):
                S.op("vector", lambda e, c=c, pd=pd: e.scalar_tensor_tensor(out=Cst[:, c, :], in0=Cst[:, c, :], scalar=egl[:, c:c + 1], op0=ALU.mult,
                                                                            in1=pd[:, c * 128:(c + 1) * 128], op1=ALU.add),
                     reads=[Rpd, Regl, RC], writes=[RC])
            S.op("vector", lambda e: e.tensor_tensor(out=nt1[:], in0=nst[:], in1=egl[:], op=ALU.mult), reads=[RC, Regl], writes=[Rnt1])
            S.op("vector", lambda e: e.tensor_tensor(out=nst[:], in0=pS[:, 32:36], in1=nt1[:], op=ALU.add), reads=[Rdn, Rnt1], writes=[RC])
            S.op("vector", lambda e: e.tensor_tensor(out=numXs[:], in0=pT2[:].rearrange("p (h v) -> p h v", h=8),
                                                      in1=eg[:].unsqueeze(2).broadcast_to([128, 8, 128]), op=ALU.mult),
                 reads=[RpT2, Reg], writes=[RnumXs])
            S.op("vector", lambda e: e.tensor_tensor(out=num[:].rearrange("p h v -> p (h v)"), in0=pG[:], in1=numXs[:].rearrange("p h v -> p (h v)"), op=ALU.add),
                 reads=[RpG, RnumXs], writes=[Rnum])
            S.op("gpsimd", lambda e: e.tensor_copy(out=Cbf[0:64, :, 0, :], in_=Cst[0:64, :, :]), reads=[RC], writes=[RCbf])
            S.op("gpsimd", lambda e: e.tensor_copy(out=Cbf[64:128, :, 1, :], in_=Cst[64:128, :, :]), reads=[RC], writes=[RCbf], join=True)
            S.op("gpsimd", lambda e: e.tensor_copy(out=nbf[0:64, :, 0], in_=nst[0:64, :]), reads=[RC], writes=[RCbf], join=True)
            S.op("gpsimd", lambda e: e.tensor_copy(out=nbf[64:128, :, 1], in_=nst[64:128, :]), reads=[RC], writes=[RCbf], join=True)
            T = lambda n: sm[n][0]
            R_ = lambda n: sm[n][1]
            S.op("vector", lambda e: e.tensor_tensor(out=T("dxs")[:], in0=pS[:, 24:32], in1=eg[:], op=ALU.mult), reads=[RdenX, Reg], writes=[R_("dxs")])
            S.op("vector", lambda e: e.tensor_tensor(out=T("den")[:], in0=pS[:, 16:24], in1=T("dxs")[:], op=ALU.add), reads=[RdenI, R_("dxs")], writes=[R_("den")])
            S.op("vector", lambda e: e.scalar_tensor_tensor(out=T("t1")[:], in0=T("den")[:], scalar=-1.0, op0=ALU.mult, in1=T("den")[:], op1=ALU.max),
                 reads=[R_("den")], writes=[R_("t1")])
            S.op("vector", lambda e: e.tensor_scalar(out=T("dd")[:], in0=T("t1")[:], scalar1=1.0, scalar2=None, op0=ALU.max), reads=[R_("t1")], writes=[R_("dd")])
            S.op("vector", lambda e: e.reciprocal(out=T("rec")[:], in_=T("dd")[:]), reads=[R_("dd")], writes=[R_("rec")])
            S.op("gpsimd", lambda e: e.tensor_tensor(out=sqn[:], in0=num[:], in1=num[:], op=ALU.mult), reads=[Rnum], writes=[Rsqn])
            S.op("vector", lambda e: e.tensor_reduce(out=T("ssn")[:], in_=sqn[:], axis=AX.X, op=ALU.add), reads=[Rsqn], writes=[R_("ssn")])
            S.op("vector", lambda e: e.tensor_tensor(out=T("t1")[:], in0=T("rec")[:], in1=T("rec")[:], op=ALU.mult), reads=[R_("rec")], writes=[R_("t1")])
            S.op("vector", lambda e: e.tensor_tensor(out=T("t2")[:], in0=T("t1")[:], in1=T("ssn")[:], op=ALU.mult), reads=[R_("t1"), R_("ssn")], writes=[R_("t2")])
            S.op("gpsimd", lambda e: e.tensor_scalar(out=T("lnt")[:], in0=T("t2")[:], scalar1=1.0 / 128.0, scalar2=EPS, op0=ALU.mult, op1=ALU.add),
                 reads=[R_("t2")], writes=[R_("lnt")])
            S.op("gpsimd", lambda e: e.tensor_tensor(out=T("rs")[:], in0=T("lnt")[:], in1=nhalf[:], op=ALU.pow), reads=[R_("lnt"), P.Rc], writes=[R_("rs")])
            S.op("vector", lambda e: e.tensor_tensor(out=T("coef")[:], in0=T("rec")[:], in1=T("rs")[:], op=ALU.mult), reads=[R_("rec"), R_("rs")], writes=[R_("coef")])
            S.op("vector", lambda e: e.tensor_tensor(out=y0[:], in0=num[:], in1=T("coef")[:].unsqueeze(2).broadcast_to([128, 8, 128]), op=ALU.mult),
                 reads=[Rnum, R_("coef")], writes=[Ry0])
            S.op("gpsimd", lambda e, s=s: e.tensor_tensor(out=ytm[:], in0=y0[:].rearrange("p h v -> p (h v)"), in1=og[:, s, :], op=ALU.mult),
                 reads=[Ry0, Rog[s]], writes=[Rytm])
            for h in range(8):
                S.op("tensor", lambda e, h=h: e.transpose(out=pY[:, h * 128:(h + 1) * 128], in_=ytm[:, h * 128:(h + 1) * 128], identity=P.ident_b[:]),
                     reads=[Rytm, P.Rc], writes=[RpY], join=(h > 0))
            S.op("scalar", lambda e, ts=ts: e.copy(out=yT[:, :, ts], in_=pY[:].rearrange("p (h j) -> p h j", h=8)), reads=[RpY], writes=[RyT[s]])
            if s < 3:
                S.tgt.flush()
            S.tgt = realS
        for d in range(8):
            pb, Rpb = nextbank()
            for h in range(8):
                S.op("tensor", lambda e, h=h, d=d, pb=pb: e.matmul(pb[:], lhsT=wout[:, h, d * 128:(d + 1) * 128], rhs=yT[:, h, :], start=(h == 0), stop=(h == 7)),
                     reads=[Rwout[h]] + RyT, writes=[Rpb], join=(h > 0))
            S.op("vector", lambda e, d=d, pb=pb: e.tensor_tensor(out=hT[:, d, :], in0=pb[:], in1=hT[:, d, :], op=ALU.add), reads=[Rpb, RhT[d]], writes=[RhT[d]])
            S.dma("sync", hout[d, :, t0:t0 + NB], hT[:, d, :], reads=[RhT[d]], writes=[P.Rdram], join=True)
    P.S = realS
    P.finish()


def phase_moba(nc, C, hin, hout, W, name, nblk=NBLK, dbg=None):
    G = 2
    P = Phase(nc, name)
    S = P.S
    P.consts(C)
    P.norm_setup()
    c256 = P.sb([128, 1], F32)
    S.op("vector", lambda e: e.memset(c256[:], 1.0 / 256.0), writes=[P.Rc], join=True)
    tri4 = P.sb([128, 512], BF16)
    S.dma("gpsimd", tri4[:], C["c_tribias4"], writes=[P.Rc], join=True)
    ropet = P.sb([128, 32, 16], F32)
    S.dma("sync", ropet[:], C["c_rope"].rearrange("(i p) c -> p i c", p=128), writes=[P.Rc], join=True)
    pastb, Rpastb = P.load_bcast(C["c_past"], 256)
    ownb, Rownb = P.load_bcast(C["c_own"], 256)
    g_kv, Rg_kv = P.load_vec_fm(W["kv_norm"])
    g_mix, Rg_mix = P.load_vec_fm(W["norm_mix1"])
    knorm, Rknorm = P.load_bcast(W["k_norm"], 64)
    qnorm, Rqnorm = P.load_bcast(W["b_q_norm"], 64)
    pR = [P.ps([128, 512], F32) for _ in range(2)]; RpR = [Res(), Res()]
    psc = [P.ps([128, G * 512], F32) for _ in range(2)]; Rpsc = [Res(), Res()]
    pop = [P.ps([128, 512], F32) for _ in range(2)]; Rpop = [Res(), Res()]
    rot = [0]

    def nextbank():
        rot[0] ^= 1
        return pR[rot[0]], RpR[rot[0]]

    ple = PLE(P, C, W["p0"], W["norm_ple0"], W["w_ple_gate0"], W["w_ple_up0"], [(pR[0], RpR[0]), (pR[1], RpR[1]), (pR[0], RpR[0])])
    wkv, Rwkv = P.load_w(W["w_kv"], 8, 512)
    wq, Rwq = P.load_w(W["b_w_q"], 8, 1024)
    wo, Rwo = P.load_w(W["b_w_o"], 8, 1024)

    hT = P.sb([128, 8, NB], F32); RhT = [Res() for _ in range(8)]
    hn = P.sb([128, 8, NB], BF16); Rhn = [Res() for _ in range(8)]
    KT = P.sb([80, 4, NT], BF16); RKT = [Res() for _ in range(32)]
    Vaug = P.sb([128, 4, 32, 128], BF16); RV = [Res() for _ in range(32)]
    kmT = P.sb([80, 4, 16], BF16); RkmT = Res()
    kms = P.sb([64, 4, 2], F32); Rkms = Res()
    ksb = P.sb([128, 4, 64], F32); Rksb = Res()
    sqk = P.sb([128, 4, 64], F32); Rsqk = Res()
    kbf = P.sb([128, 4, 64], BF16); Rkbf = Res()
    qsb = P.sb([128, 16, 64], F32); Rqsb = Res()
    sqq = P.sb([128, 16, 64], F32); Rsqq = Res()
    qa = P.sb([128, 16, 80], BF16); Rqa = Res()
    QTa = [P.sb([80, 16, 128], BF16) for _ in range(2)]; RQTa = [Res(), Res()]
    gm = P.sb([128, 16, 16], F32); Rgm = Res()
    mx8 = P.sb([128, 16, 8], F32); Rmx8 = Res()
    vis = P.sb([128, 16, 16], F32); Rvis = Res()
    skq = {n: (P.sb([128, 16], F32), Res()) for n in ["ss", "ln", "r"]}
    skk = {n: (P.sb([128, 16], F32), Res()) for n in ["ss", "ln", "r"]}
    rtq = {n: (P.sb([128, 16, 8], F32), Res()) for n in ["t1", "t2", "t3", "t4"]}
    PTb = [P.sb([128, G * 512], BF16) for _ in range(2)]; RPTb = [Res() for _ in range(2)]
    rec = P.sb([128, 512], F32); Rrec = Res()
    OTb = P.sb([128, 8, NB], BF16); ROTb = [Res() for _ in range(4)]

    for kvh in range(4):
        for c0 in range(0, NT, 1024):
            S.dma("gpsimd", KT[64:80, kvh, c0:c0 + 1024], C["c_onehot"][:, c0:c0 + 1024], writes=[P.Rc], join=True)
    S.op("vector", lambda e: e.memset(Vaug[:].rearrange("p a b c -> p (a b c)"), 1.0), writes=[P.Rc], join=True)
    S.op("gpsimd", lambda e: e.memset(kmT[:], 0.0), writes=[RkmT])
    for i in range(2):
        S.op("gpsimd", lambda e, i=i: e.memset(QTa[i][:], 0.0), writes=[RQTa[i]])

    def head_norm_rope(x, Rx, sq_, Rsq_, nh, gbc, Rgbc, it, sk, rt):
        (ss, Rss), (ln, Rln), (r, Rr) = sk["ss"], sk["ln"], sk["r"]
        S.op("gpsimd", lambda e: e.tensor_tensor(out=sq_[:], in0=x[:], in1=x[:], op=ALU.mult), reads=[Rx], writes=[Rsq_])
        S.op("vector", lambda e: e.tensor_reduce(out=ss[:, 0:nh], in_=sq_[:], axis=AX.X, op=ALU.add), reads=[Rsq_], writes=[Rss])
        yield
        S.op("scalar", lambda e: e.activation(out=ln[:, 0:nh], in_=ss[:, 0:nh], func=AF.Ln, scale=1.0 / 64.0, bias=P.eps_t[:, 0:1]),
             reads=[Rss, P.Rc], writes=[Rln])
        S.op("scalar", lambda e: e.activation(out=r[:, 0:nh], in_=ln[:, 0:nh], func=AF.Exp, scale=-0.5), reads=[Rln], writes=[Rr])
        yield
        S.op("vector", lambda e: e.tensor_tensor(out=x[:], in0=x[:], in1=r[:, 0:nh].unsqueeze(2).broadcast_to([128, nh, 64]), op=ALU.mult),
             reads=[Rx, Rr], writes=[Rx])
        S.op("vector", lambda e: e.tensor_tensor(out=x[:], in0=x[:], in1=gbc[:].unsqueeze(1).broadcast_to([128, nh, 64]), op=ALU.mult),
             reads=[Rx, Rgbc], writes=[Rx])
        yield
        cs = ropet[:, it, 0:8].unsqueeze(1).broadcast_to([128, nh, 8])
        sn = ropet[:, it, 8:16].unsqueeze(1).broadcast_to([128, nh, 8])
        x1 = x[:, :, 0:8]
        x2 = x[:, :, 8:16]
        tt = {k: v[0][:, 0:nh, :] for k, v in rt.items()}
        Rt = {k: v[1] for k, v in rt.items()}
        S.op("vector", lambda e: e.tensor_tensor(out=tt["t1"], in0=x1, in1=cs, op=ALU.mult), reads=[Rx, P.Rc], writes=[Rt["t1"]])
        S.op("vector", lambda e: e.tensor_tensor(out=tt["t2"], in0=x2, in1=sn, op=ALU.mult), reads=[Rx, P.Rc], writes=[Rt["t2"]])
        S.op("vector", lambda e: e.tensor_tensor(out=tt["t3"], in0=x2, in1=cs, op=ALU.mult), reads=[Rx, P.Rc], writes=[Rt["t3"]])
        S.op("vector", lambda e: e.tensor_tensor(out=tt["t4"], in0=x1, in1=sn, op=ALU.mult), reads=[Rx, P.Rc], writes=[Rt["t4"]])
        yield
        S.op("vector", lambda e: e.tensor_tensor(out=x1, in0=tt["t1"], in1=tt["t2"], op=ALU.subtract), reads=[Rt["t1"], Rt["t2"], Rx], writes=[Rx])
        S.op("vector", lambda e: e.tensor_tensor(out=x2, in0=tt["t3"], in1=tt["t4"], op=ALU.add), reads=[Rt["t3"], Rt["t4"], Rx], writes=[Rx])
        yield

    def kv_path(blk, s):
        it = blk * 4 + s
        ts = slice(s * 128, (s + 1) * 128)
        pb, Rpb = nextbank()
        for k in range(8):
            S.op("tensor", lambda e, k=k, ts=ts, pb=pb: e.matmul(pb[:], lhsT=hn[:, k, ts], rhs=wkv[:, k, :], start=(k == 0), stop=(k == 7)),
                 reads=[Rwkv[k], Rhn[k]], writes=[Rpb], join=(k > 0))
        S.op("scalar", lambda e, pb=pb: e.copy(out=ksb[:].rearrange("p h d -> p (h d)"), in_=pb[:, 0:256]), reads=[Rpb], writes=[Rksb])
        S.op("scalar", lambda e, pb=pb, it=it: e.copy(out=Vaug[:, :, it, 0:64], in_=pb[:, 256:512].rearrange("p (h d) -> p h d", h=4)),
             reads=[Rpb, P.Rc], writes=[RV[it]])
        for _ in head_norm_rope(ksb, Rksb, sqk, Rsqk, 4, knorm, Rknorm, it, skk, rtq):
            pass
        S.op("gpsimd", lambda e: e.tensor_copy(out=kbf[:], in_=ksb[:]), reads=[Rksb], writes=[Rkbf])
        pb, Rpb = nextbank()
        pbb = pb[:].bitcast(BF16)
        for kvh in range(4):
            S.op("tensor", lambda e, kvh=kvh, pbb=pbb: e.transpose(out=pbb[0:64, kvh * 128:(kvh + 1) * 128], in_=kbf[:, kvh, :], identity=P.ident_b[:]),
                 reads=[Rkbf, P.Rc], writes=[Rpb], join=(kvh > 0))
        S.op("scalar", lambda e, it=it, pbb=pbb: e.copy(out=KT[0:64, :, it * 128:(it + 1) * 128], in_=pbb[0:64, 0:512].rearrange("p (h t) -> p h t", h=4)),
             reads=[Rpb, P.Rc], writes=[RKT[it]])
        pb, Rpb = nextbank()
        for kvh in range(4):
            S.op("tensor", lambda e, kvh=kvh, pb=pb: e.matmul(pb[0:64, kvh:kvh + 1], lhsT=ksb[:, kvh, :], rhs=c256[:, 0:1], start=True, stop=True),
                 reads=[Rksb, P.Rc], writes=[Rpb], join=(kvh > 0))
        S.op("vector", lambda e, s=s, pb=pb: e.tensor_copy(out=kms[:, :, s % 2], in_=pb[0:64, 0:4]), reads=[Rpb], writes=[Rkms], join=(s % 2 == 1))
        if s % 2 == 1:
            n = it // 2
            S.op("vector", lambda e, n=n: e.tensor_tensor(out=kmT[0:64, :, n], in0=kms[:, :, 0], in1=kms[:, :, 1], op=ALU.add),
                 reads=[Rkms], writes=[RkmT])

    def q_path(blk, s):
        it = blk * 4 + s
        b = it // 2
        ts = slice(s * 128, (s + 1) * 128)
        QT, RQT = QTa[it % 2], RQTa[it % 2]
        for half in range(2):
            pb, Rpb = nextbank()
            for k in range(8):
                S.op("tensor", lambda e, k=k, ts=ts, pb=pb, half=half: e.matmul(pb[:], lhsT=hn[:, k, ts], rhs=wq[:, k, half * 512:(half + 1) * 512],
                                                                                 start=(k == 0), stop=(k == 7)),
                     reads=[Rwq[k], Rhn[k]], writes=[Rpb], join=(k > 0))
                if k % 4 == 3:
                    yield
            S.op("scalar", lambda e, pb=pb, half=half: e.copy(out=qsb[:, half * 8:(half + 1) * 8, :].rearrange("p h d -> p (h d)"), in_=pb[:]),
                 reads=[Rpb], writes=[Rqsb], join=(half > 0))
            yield
        for _ in head_norm_rope(qsb, Rqsb, sqq, Rsqq, 16, qnorm, Rqnorm, it, skq, rtq):
            yield
        S.op("gpsimd", lambda e: e.tensor_copy(out=qa[:, :, 0:64], in_=qsb[:]), reads=[Rqsb], writes=[Rqa])
        yield
        for r_ in range(2):
            pb, Rpb = nextbank()
            pbb = pb[:].bitcast(BF16)
            for j in range(8):
                h = r_ * 8 + j
                S.op("tensor", lambda e, h=h, j=j, pbb=pbb: e.transpose(out=pbb[0:64, j * 128:(j + 1) * 128], in_=qa[:, h, 0:64], identity=P.ident_b[:]),
                     reads=[Rqa, P.Rc], writes=[Rpb], join=(j > 0))
                if j % 4 == 3:
                    yield
            S.op("scalar", lambda e, r_=r_, pbb=pbb: e.copy(out=QT[0:64, r_ * 8:(r_ + 1) * 8, :], in_=pbb[0:64, :].rearrange("p (h t) -> p h t", h=8)),
                 reads=[Rpb], writes=[RQT], join=(r_ > 0))
            yield
        pb, Rpb = nextbank()
        for h in range(16):
            S.op("tensor", lambda e, h=h, pb=pb: e.matmul(pb[:, h * 16:(h + 1) * 16], lhsT=QT[:, h, :], rhs=kmT[:, h // 4, :], start=True, stop=True),
                 reads=[RQT, RkmT], writes=[Rpb], join=(h > 0))
            if h % 4 == 3:
                yield
        S.op("vector", lambda e, b=b, pb=pb: e.tensor_tensor(out=gm[:], in0=pb[:, 0:256].rearrange("p (h n) -> p h n", h=16),
                                                              in1=pastb[:, b * 16:(b + 1) * 16].unsqueeze(1).broadcast_to([128, 16, 16]), op=ALU.add),
             reads=[Rpb, Rpastb], writes=[Rgm])
        yield
        for h in range(16):
            S.op("vector", lambda e, h=h: e.max(out=mx8[:, h, :], in_=gm[:, h, :]), reads=[Rgm], writes=[Rmx8], join=(h > 0))
            if h % 4 == 3:
                yield
        S.op("vector", lambda e: e.tensor_tensor(out=vis[:], in0=gm[:], in1=mx8[:, :, 2:3].broadcast_to([128, 16, 16]), op=ALU.is_ge),
             reads=[Rgm, Rmx8], writes=[Rvis])
        S.op("vector", lambda e, b=b: e.tensor_tensor(out=vis[:], in0=vis[:], in1=ownb[:, b * 16:(b + 1) * 16].unsqueeze(1).broadcast_to([128, 16, 16]), op=ALU.max),
             reads=[Rvis, Rownb], writes=[Rvis])
        S.op("vector", lambda e: e.tensor_scalar(out=qa[:, :, 64:80], in0=vis[:], scalar1=-NEG, scalar2=NEG, op0=ALU.mult, op1=ALU.add),
             reads=[Rvis, Rqa], writes=[Rqa])
        yield
        for r_ in range(2):
            pb, Rpb = nextbank()
            pbb = pb[:].bitcast(BF16)
            for j in range(8):
                h = r_ * 8 + j
                S.op("tensor", lambda e, h=h, j=j, pbb=pbb: e.transpose(out=pbb[0:80, j * 128:(j + 1) * 128], in_=qa[:, h, :], identity=P.ident_b[:]),
                     reads=[Rqa, P.Rc], writes=[Rpb], join=(j > 0))
                if j % 4 == 3:
                    yield
            S.op("scalar", lambda e, r_=r_, pbb=pbb: e.copy(out=QT[:, r_ * 8:(r_ + 1) * 8, :], in_=pbb[0:80, :].rearrange("p (h t) -> p h t", h=8)),
                 reads=[Rpb], writes=[RQT], join=(r_ > 0))
            yield

    nsc = [0]

    def attention(blk, s):
        it = blk * 4 + s
        ts = slice(s * 128, (s + 1) * 128)
        QT, RQT = QTa[it % 2], RQTa[it % 2]
        L = []
        for kvh in range(4):
            for j0 in range(0, it + 1, G):
                L.append((kvh, j0, min(it + 1, j0 + G)))
        bufs = {}

        def qk(n):
            kvh, j0, j1 = L[n]
            m = nsc[0]
            nsc[0] += 1
            bufs[n] = m % 2
            ps_, Rps_ = psc[m % 2], Rpsc[m % 2]
            qg = QT[:, 4 * kvh:4 * kvh + 4, :].rearrange("p h t -> p (h t)")
            for j in range(j0, j1):
                o = (j - j0) * 512
                diag = (j == it)
                S.op("tensor", lambda e, kvh=kvh, j=j, ps_=ps_, qg=qg, diag=diag, o=o: e.matmul(ps_[:, o:o + 512], lhsT=KT[:, kvh, j * 128:(j + 1) * 128], rhs=qg,
                                                                                               start=True, stop=(not diag)),
                     reads=[RKT[j], RQT, P.Rc], writes=[Rps_], join=(j > j0))
                if diag:
                    S.op("tensor", lambda e, ps_=ps_, o=o: e.matmul(ps_[:, o:o + 512], lhsT=P.ident_b[:], rhs=tri4[:], start=False, stop=True),
                         reads=[P.Rc], writes=[Rps_], join=True)

        def ex(n):
            kvh, j0, j1 = L[n]
            bi = bufs[n]
            w = (j1 - j0) * 512
            S.op("scalar", lambda e, bi=bi, w=w: e.activation(out=PTb[bi][:, 0:w], in_=psc[bi][:, 0:w], func=AF.Exp, scale=0.125),
                 reads=[Rpsc[bi]], writes=[RPTb[bi]])

        def pv(n):
            kvh, j0, j1 = L[n]
            bi = bufs[n]
            po, Rpo = pop[kvh % 2], Rpop[kvh % 2]
            for j in range(j0, j1):
                o = (j - j0) * 512
                S.op("tensor", lambda e, kvh=kvh, j=j, bi=bi, po=po, o=o, it=it: e.matmul(po[:], lhsT=Vaug[:, kvh, j, :], rhs=PTb[bi][:, o:o + 512],
                                                                                         start=(j == 0), stop=(j == it)),
                     reads=[RV[j], RPTb[bi], P.Rc], writes=[Rpo], join=(j > 0))

        def epi(kvh):
            po, Rpo = pop[kvh % 2], Rpop[kvh % 2]
            S.op("vector", lambda e, po=po: e.reciprocal(out=rec[64:128, :], in_=po[64:128, :]), reads=[Rpo], writes=[Rrec])
            for gq in range(4):
                hh = 4 * kvh + gq
                pr, ph = hh // 2, hh % 2
                S.op("vector", lambda e, po=po, gq=gq, pr=pr, ph=ph, ts=ts: e.tensor_tensor(
                    out=OTb[ph * 64:(ph + 1) * 64, pr, ts], in0=po[0:64, gq * 128:(gq + 1) * 128], in1=rec[64:128, gq * 128:(gq + 1) * 128], op=ALU.mult),
                     reads=[Rpo, Rrec], writes=[ROTb[s]], join=True)

        qk(0)
        for n in range(len(L)):
            if n + 1 < len(L):
                qk(n + 1)
            ex(n)
            pv(n)
            if n + 1 == len(L) or L[n + 1][0] != L[n][0]:
                epi(L[n][0])
            yield

    def drive(main, side, side_len):
        steps = list(range(0))
        mains = main
        if side is None:
            for _ in mains:
                pass
            return
        done = [False]

        def adv(k):
            for _ in range(k):
                if done[0]:
                    return
                try:
                    next(side)
                except StopIteration:
                    done[0] = True
        nmain = side_len[0]
        per = max(1, -(-side_len[1] // max(1, nmain)))
        for _ in mains:
            adv(per)
        while not done[0]:
            adv(8)

    for blk in range(nblk):
        t0 = blk * NB
        for k in range(8):
            S.dma("sync", hT[:, k, :], hin[k, :, t0:t0 + NB], writes=[RhT[k]])
        pb, Rpb = nextbank()
        ple.emit(blk, hT, RhT, hn, Rhn, pb, Rpb)
        pb, Rpb = nextbank()
        P.fm_norm(hT, RhT, g_kv, Rg_kv, hn, Rhn, pb, Rpb)
        for s in range(4):
            kv_path(blk, s)
        pb, Rpb = nextbank()
        P.fm_norm(hT, RhT, g_mix, Rg_mix, hn, Rhn, pb, Rpb, reuse_rstd=True)
        for _ in q_path(blk, 0):
            pass
        for s in range(4):
            it = blk * 4 + s
            nsteps = 4 * (-(-(it + 1) // G))
            side = q_path(blk, s + 1) if s < 3 else None
            drive(attention(blk, s), side, (nsteps, 60))
        for d in range(8):
            pb, Rpb = nextbank()
            for pr in range(8):
                S.op("tensor", lambda e, pr=pr, d=d, pb=pb: e.matmul(pb[:], lhsT=wo[:, pr, d * 128:(d + 1) * 128], rhs=OTb[:, pr, :], start=(pr == 0), stop=(pr == 7)),
                     reads=[Rwo[pr]] + ROTb, writes=[Rpb], join=(pr > 0))
            S.op("vector", lambda e, d=d, pb=pb: e.tensor_tensor(out=hT[:, d, :], in0=pb[:], in1=hT[:, d, :], op=ALU.add), reads=[Rpb, RhT[d]], writes=[RhT[d]])
            S.dma("sync", hout[d, :, t0:t0 + NB], hT[:, d, :], reads=[RhT[d]], writes=[P.Rdram], join=True)
    P.finish()


WSHAPES = {
    "norm_mix0": [1024], "norm_mix1": [1024], "a_w_in": [1024, 3088], "a_b_gate": [16], "a_mh_gain": [128], "a_w_out": [1024, 1024],
    "kv_norm": [1024], "w_kv": [1024, 512], "k_norm": [64], "b_w_q": [1024, 1024], "b_q_norm": [64], "b_w_o": [1024, 1024],
    "norm_ffn0": [1024], "norm_ffn1": [1024], "w_gate_up0": [1024, 5632], "w_gate_up1": [1024, 5632], "w_down0": [2816, 1024], "w_down1": [2816, 1024],
    "norm_ple0": [1024], "norm_ple1": [1024], "w_ple_gate0": [1024, 1024], "w_ple_gate1": [1024, 1024], "w_ple_up0": [256, 1024], "w_ple_up1": [256, 1024],
    "p0": [NT, 256], "p1": [NT, 256],
}


def build_program():
    nc = bass.Bass("TRN2", target_bir_lowering=False)
    Cn = make_consts()
    C = {k: nc.dram_tensor(k, list(v.shape), F32, kind="ExternalInput").ap() for k, v in Cn.items()}
    x = nc.dram_tensor("x", [NT, 1024], F32, kind="ExternalInput").ap()
    out = nc.dram_tensor("out", [NT, 1024], F32, kind="ExternalOutput").ap()
    W = {k: nc.dram_tensor(k, v, F32, kind="ExternalInput").ap() for k, v in WSHAPES.items()}
    hs = [nc.dram_tensor("h_scr%d" % i, [8, 128, NT], F32, kind="Internal").ap() for i in range(4)]
    phase_mlstm(nc, C, x, hs[0], W, "ml")
    phase_ffn(nc, C, hs[0], hs[1], W["w_gate_up0"], W["w_down0"], W["norm_ffn0"], "f0")
    phase_moba(nc, C, hs[1], hs[2], W, "mb")
    phase_ffn(nc, C, hs[2], hs[3], W["w_gate_up1"], W["w_down1"], W["norm_ffn1"], "f1")
    phase_ple_out(nc, C, hs[3], out, W["p1"], W["norm_ple1"], W["w_ple_gate1"], W["w_ple_up1"], "po")
    return nc, Cn


def make_in_maps(inputs, cores):
    f = lambda a: np.ascontiguousarray(np.asarray(a, dtype=np.float32))
    I = {k: np.asarray(v) for k, v in inputs.items()}
    shared = {
        "norm_mix0": f(I["norm_mix"][0]), "norm_mix1": f(I["norm_mix"][1]), "a_w_in": f(I["a_w_in"][0]), "a_b_gate": f(I["a_b_gate"][0]),
        "a_mh_gain": f(I["a_mh_gain"][0]), "a_w_out": f(I["a_w_out"][0]), "kv_norm": f(I["kv_norm"]), "w_kv": f(I["w_kv"]), "k_norm": f(I["k_norm"]),
        "b_w_q": f(I["b_w_q"][0]), "b_q_norm": f(I["b_q_norm"][0]), "b_w_o": f(I["b_w_o"][0]),
        "norm_ffn0": f(I["norm_ffn"][0]), "norm_ffn1": f(I["norm_ffn"][1]), "w_gate_up0": f(I["w_gate_up"][0]), "w_gate_up1": f(I["w_gate_up"][1]),
        "w_down0": f(I["w_down"][0]), "w_down1": f(I["w_down"][1]), "norm_ple0": f(I["norm_ple"][0]), "norm_ple1": f(I["norm_ple"][1]),
        "w_ple_gate0": f(I["w_ple_gate"][0]), "w_ple_gate1": f(I["w_ple_gate"][1]), "w_ple_up0": f(I["w_ple_up"][0]), "w_ple_up1": f(I["w_ple_up"][1]),
    }
    maps = []
    for b in cores:
        m = dict(shared)
        m["x"] = f(I["x"][b])
        m["p0"] = f(I["p"][0, b])
        m["p1"] = f(I["p"][1, b])
        maps.append(m)
    return maps


def kernel(**inputs):
    nc, Cn = build_program()
    maps = make_in_maps(inputs, list(range(8)))
    for m in maps:
        m.update(Cn)
    res = run_bass_kernel_spmd(nc, maps, core_ids=list(range(8)))
    return np.stack([np.asarray(r["out"], dtype=np.float32) for r in res.results], axis=0)
```

```python
import numpy as np
import concourse.bass as bass
import concourse.mybir as mybir
from concourse.bass_utils import run_bass_kernel_spmd
from contextlib import ExitStack

F32 = mybir.dt.float32
BF16 = mybir.dt.bfloat16
ALU = mybir.AluOpType
AF = mybir.ActivationFunctionType
AX = mybir.AxisListType

ENGS = ["tensor", "vector", "scalar", "gpsimd", "sync"]
NDMASEM = 12
SAME_ENGINE_SYNC = True

NT = 4096
NB = 512
NBLK = NT // NB
EPS = 1e-6
NEG = -30000.0


class Res:
    __slots__ = ("name", "w", "r", "gd")

    def __init__(self, name=""):
        self.name = name
        self.w = []
        self.r = []
        self.gd = []


class WRes:
    def __init__(self, grid, cw):
        self.grid = grid
        self.cw = cw

    def sel(self, k, c0, c1):
        return [self.grid[k][ci] for ci in range(c0 // self.cw, (c1 - 1) // self.cw + 1)]


class Op:
    __slots__ = ("eng", "fn", "waits", "pos", "sig", "isdma", "semi", "semk", "K", "sigidx")


class Sched:
    G = {"nc": None}

    @staticmethod
    def setup(nc):
        if Sched.G.get("nc") is nc:
            return
        es = ExitStack()
        G = {"nc": nc, "es": es, "sig": {e: 0 for e in ENGS}, "dma": {e: 0 for e in ENGS}}
        G["esem"] = {e: es.enter_context(nc.semaphore("sem_e_%s" % e)) for e in ENGS}
        G["dsem"] = {(e, i): es.enter_context(nc.semaphore("sem_d_%s_%d" % (e, i))) for e in ("sync", "gpsimd") for i in range(NDMASEM)}
        Sched.G = G

    def __init__(self, nc):
        Sched.setup(nc)
        self.nc = nc
        self.ops = {e: [] for e in ENGS}
        self.Kcur = {e: {} for e in ENGS}
        self.nops = 0

    limit = None

    def _record(self, eng, fn, reads, writes, isdma, join=False):
        if Sched.limit is not None and self.nops >= Sched.limit:
            return None
        o = Op()
        o.eng = eng
        o.fn = fn
        o.isdma = isdma
        o.sig = False
        o.pos = len(self.ops[eng])
        deps = {}
        for r in reads:
            for x in r.w:
                deps[id(x)] = x
        for w in writes:
            if join and not w.r:
                for x in w.gd:
                    deps[id(x)] = x
            else:
                for x in w.w:
                    deps[id(x)] = x
                for x in w.r:
                    deps[id(x)] = x
        K = self.Kcur[eng]
        newK = None
        waits = []
        if isdma:
            i = Sched.G["dma"][eng]
            Sched.G["dma"][eng] += 1
            o.semi = i % NDMASEM
            o.semk = i // NDMASEM + 1
            if o.semk > 1:
                key = ("d", eng, o.semi)
                if K.get(key, 0) < o.semk - 1:
                    waits.append(("dmaslot", eng, o.semi, o.semk - 1))
                    newK = dict(K)
                    newK[key] = o.semk - 1
        best = {}
        dl = []
        for y in deps.values():
            if y is o:
                continue
            if y.isdma:
                dl.append(y)
            elif y.eng not in best or best[y.eng].pos < y.pos:
                best[y.eng] = y
        for y in dl + list(best.values()):
            cur = K if newK is None else newK
            if y.isdma:
                key = ("d", y.eng, y.semi)
                if cur.get(key, 0) >= y.semk:
                    continue
                waits.append(("dma", y))
            else:
                if y.eng == eng and (eng == "tensor" or not SAME_ENGINE_SYNC) and not isdma:
                    continue
                if cur.get(y.eng, -1) >= y.pos:
                    continue
                y.sig = True
                waits.append(("eng", y))
            if newK is None:
                newK = dict(K)
            for k, v in y.K.items():
                if newK.get(k, -1) < v:
                    newK[k] = v
            if y.isdma:
                key = ("d", y.eng, y.semi)
                newK[key] = max(newK.get(key, 0), y.semk)
            else:
                if newK.get(y.eng, -1) < y.pos:
                    newK[y.eng] = y.pos
        if newK is not None:
            self.Kcur[eng] = newK
            K = newK
        o.K = K
        o.waits = waits
        for r in reads:
            r.r.append(o)
        for w in writes:
            if join and not w.r:
                w.w.append(o)
            else:
                w.gd = w.w + w.r
                w.w = [o]
                w.r = []
        self.ops[eng].append(o)
        self.nops += 1
        return o

    def op(self, eng, fn, reads=(), writes=(), join=False):
        return self._record(eng, fn, reads, writes, False, join)

    def dma(self, queue, out, in_, reads=(), writes=(), join=False, nonc=False, **kw):
        nc = self.nc
        if nonc:
            def f(e):
                with nc.allow_non_contiguous_dma(reason="small strided parameter load"):
                    return e.dma_start(out=out, in_=in_, **kw)
        else:
            def f(e):
                return e.dma_start(out=out, in_=in_, **kw)
        return self._record(queue, f, reads, writes, True, join)

    def emit(self):
        nc = self.nc
        G = Sched.G
        for e in ENGS:
            n = G["sig"][e]
            for o in self.ops[e]:
                if o.sig:
                    n += 1
                o.sigidx = n
            G["sig"][e] = n
        esem, dsem = G["esem"], G["dsem"]
        with nc.Block() as block:
            def stream(ename):
                def body(eng):
                    for o in self.ops[ename]:
                        for w in o.waits:
                            if w[0] == "dmaslot":
                                eng.wait_ge(dsem[(w[1], w[2])], 16 * w[3])
                            elif w[0] == "dma":
                                y = w[1]
                                eng.wait_ge(dsem[(y.eng, y.semi)], 16 * y.semk)
                            else:
                                y = w[1]
                                eng.wait_ge(esem[y.eng], y.sigidx)
                        ins = o.fn(eng)
                        if o.isdma:
                            ins.then_inc(dsem[(o.eng, o.semi)], 16)
                        elif o.sig:
                            ins.then_inc(esem[o.eng], 1)
                return body

            for e in ENGS:
                if self.ops[e]:
                    getattr(block, e)(stream(e))


class Proxy:
    def __init__(self, real):
        self.real = real
        self.tgt = real

    def op(self, *a, **k):
        return self.tgt.op(*a, **k)

    def dma(self, *a, **k):
        return self.tgt.dma(*a, **k)


class Mux:
    def __init__(self, real, chunks):
        self.real = real
        self.chunks = chunks
        self.last = None

    def _maybe(self, eng):
        if self.last == "tensor" and eng != "tensor" and self.chunks:
            for kind, a, k in self.chunks.pop(0):
                getattr(self.real, kind)(*a, **k)
        self.last = eng

    def op(self, eng, *a, **k):
        self._maybe(eng)
        return self.real.op(eng, *a, **k)

    def dma(self, eng, *a, **k):
        self._maybe(eng)
        return self.real.dma(eng, *a, **k)

    def flush(self):
        while self.chunks:
            for kind, a, k in self.chunks.pop(0):
                getattr(self.real, kind)(*a, **k)


def mm_chunks(q):
    runs = []
    for item in q:
        is_mm = (item[0] == "op" and item[1][0] == "tensor")
        if runs and runs[-1][0] == is_mm:
            runs[-1][1].append(item)
        else:
            runs.append((is_mm, [item]))
    chunks = []
    cur = []
    for is_mm, items in runs:
        cur.extend(items)
        if is_mm:
            chunks.append(cur)
            cur = []
    if cur:
        chunks.append(cur)
    return chunks


class Deferred:
    def __init__(self):
        self.q = []

    def op(self, *a, **k):
        self.q.append(("op", a, k))

    def dma(self, *a, **k):
        self.q.append(("dma", a, k))


def interleave(main_gen, real, q):
    steps = list(main_gen) if False else None
    i = 0
    n = getattr(main_gen, "nsteps", None)
    return i


class Phase:
    def __init__(self, nc, name):
        self.nc = nc
        self.name = name
        self.es = ExitStack()
        self.S = Sched(nc)
        self.n = 0
        self.Rdram = Res("dram_out")

    def sb(self, shape, dt, name=None):
        self.n += 1
        t = self.es.enter_context(self.nc.sbuf_tensor("%s_s%d" % (self.name, self.n), list(shape), dt))
        return t

    def ps(self, shape, dt, name=None):
        self.n += 1
        t = self.es.enter_context(self.nc.psum_tensor("%s_p%d" % (self.name, self.n), list(shape), dt))
        return t

    def finish(self):
        S = self.S
        S.op("sync", lambda e: e.nop(), reads=[self.Rdram])
        S.emit()
        self.es.close()

    def consts(self, C):
        S = self.S
        self.ident_f = self.sb([128, 128], F32)
        self.ident_b = self.sb([128, 128], BF16)
        self.ones_b = self.sb([128, 128], BF16)
        self.eps_t = self.sb([128, 1], F32)
        self.one_t = self.sb([128, 1], F32)
        self.Rc = Res("consts")
        S.dma("sync", self.ident_f[:], C["c_ident"], writes=[self.Rc], join=True)
        S.op("vector", lambda e: e.tensor_copy(out=self.ident_b[:], in_=self.ident_f[:]), reads=[self.Rc], writes=[self.Rc])
        S.op("vector", lambda e: e.memset(self.ones_b[:], 1.0), writes=[self.Rc], join=True)
        S.op("vector", lambda e: e.memset(self.eps_t[:], EPS), writes=[self.Rc], join=True)
        S.op("vector", lambda e: e.memset(self.one_t[:], 1.0), writes=[self.Rc], join=True)

    def load_vec_fm(self, ap1024, nk=8):
        t = self.sb([128, nk], F32)
        r = Res()
        self.S.dma("sync", t[:], ap1024.rearrange("(k p) -> p k", p=128), writes=[r], nonc=True)
        return t, r

    def load_bcast(self, ap_flat, n):
        t = self.sb([128, n], F32)
        r = Res()
        self.S.dma("sync", t[:], ap_flat.partition_broadcast(128), writes=[r])
        return t, r

    def load_w(self, src, K, N, rows=128, col_chunk=2048, queue="gpsimd", chunk_major=False):
        t = self.sb([rows, K, N], BF16)
        nch = -(-N // col_chunk)
        cw = -(-N // nch)
        if not chunk_major:
            rs = [Res() for _ in range(K)]
            for k in range(K):
                c0 = 0
                while c0 < N:
                    c1 = min(N, c0 + cw)
                    self.S.dma(queue, t[:, k, c0:c1], src[k * rows:(k + 1) * rows, c0:c1], writes=[rs[k]], join=True)
                    c0 = c1
            return t, rs
        grid = [[Res() for _ in range(nch)] for _ in range(K)]
        order = list(range(nch))
        if nch == 4:
            order = [0, 2, 1, 3]
        for ci in order:
            c0, c1 = ci * cw, min(N, (ci + 1) * cw)
            for k in range(K):
                self.S.dma(queue, t[:, k, c0:c1], src[k * rows:(k + 1) * rows, c0:c1], writes=[grid[k][ci]])
        return t, WRes(grid, cw)

    def norm_setup(self):
        self.rstd = self.sb([128, NB], F32)
        self.Rrstd = Res()
        self.lnv = self.rstd
        self.Rlnv = self.Rrstd

    def fm_norm(self, hT, RhT, g, Rg, hn, Rhn, pss, Rpss, reuse_rstd=False):
        S = self.S
        n = hT.shape[2]
        sq, Rsq = hn, Rhn
        for k in range(8 if not reuse_rstd else 0):
            S.op("scalar", lambda e, k=k: e.activation(out=sq[:, k, :], in_=hT[:, k, :], func=AF.Square),
                 reads=[RhT[k]], writes=[Rsq[k]])
        for k in range(8 if not reuse_rstd else 0):
            S.op("tensor", lambda e, k=k: e.matmul(pss[:, 0:n], lhsT=self.ones_b[:], rhs=sq[:, k, :], start=(k == 0), stop=(k == 7)),
                 reads=[Rsq[k], self.Rc], writes=[Rpss], join=(k > 0))
        if not reuse_rstd:
            S.op("scalar", lambda e: e.activation(out=self.lnv[:, 0:n], in_=pss[:, 0:n], func=AF.Ln, scale=1.0 / 1024.0, bias=self.eps_t[:, 0:1]),
                 reads=[Rpss, self.Rc], writes=[self.Rlnv])
            S.op("scalar", lambda e: e.activation(out=self.rstd[:, 0:n], in_=self.lnv[:, 0:n], func=AF.Exp, scale=-0.5),
                 reads=[self.Rlnv], writes=[self.Rrstd])
        for k in range(8):
            S.op("vector", lambda e, k=k: e.scalar_tensor_tensor(out=hn[:, k, :], in0=hT[:, k, :], scalar=g[:, k:k + 1], op0=ALU.mult,
                                                                 in1=self.rstd[:, 0:n], op1=ALU.mult),
                 reads=[RhT[k], self.Rrstd, Rg], writes=[Rhn[k]])


def phase_ffn(nc, C, hin, hout, wgu_ap, wd_ap, g_ap, name):
    P = Phase(nc, name)
    S = P.S
    P.consts(C)
    g, Rg = P.load_vec_fm(g_ap)
    wgu, Rwgu = P.load_w(wgu_ap, 8, 5632, col_chunk=1408, chunk_major=True)
    wd, Rwd = P.load_w(wd_ap, 22, 1024)
    P.norm_setup()
    hTs = [P.sb([128, 8, NB], F32) for _ in range(2)]
    RhTs = [[Res() for _ in range(8)] for _ in range(2)]
    hns = [P.sb([128, 8, NB], BF16) for _ in range(2)]
    Rhns = [[Res() for _ in range(8)] for _ in range(2)]
    act = P.sb([128, 22, NB], BF16)
    Ract = [Res() for _ in range(22)]
    sg = [P.sb([128, NB], BF16) for _ in range(2)]
    Rsg = [Res() for _ in range(2)]
    pss = P.ps([128, NB], F32)
    Rpss = Res()
    pg = [P.ps([128, NB], F32) for _ in range(2)]
    Rpg = [Res() for _ in range(2)]
    pu = [P.ps([128, NB], F32) for _ in range(2)]
    Rpu = [Res() for _ in range(2)]
    po = [P.ps([128, NB], F32) for _ in range(2)]
    Rpo = [Res() for _ in range(2)]

    def load(blk):
        t0 = blk * NB
        for k in range(8):
            S.dma("sync", hTs[blk % 2][:, k, :], hin[k, :, t0:t0 + NB], writes=[RhTs[blk % 2][k]])

    def norm(blk):
        P.fm_norm(hTs[blk % 2], RhTs[blk % 2], g, Rg, hns[blk % 2], Rhns[blk % 2], pss, Rpss)

    load(0)
    norm(0)
    for blk in range(NBLK):
        t0 = blk * NB
        hT, RhT = hTs[blk % 2], RhTs[blk % 2]
        hn, Rhn = hns[blk % 2], Rhns[blk % 2]
        if blk + 1 < NBLK:
            load(blk + 1)
        for c in range(22):
            b = c % 2
            for k in range(8):
                S.op("tensor", lambda e, k=k, c=c, b=b, hn=hn: e.matmul(pg[b][:], lhsT=wgu[:, k, c * 128:(c + 1) * 128], rhs=hn[:, k, :],
                                                                        start=(k == 0), stop=(k == 7)),
                     reads=Rwgu.sel(k, c * 128, (c + 1) * 128) + [Rhn[k]], writes=[Rpg[b]], join=(k > 0))
            for k in range(8):
                S.op("tensor", lambda e, k=k, c=c, b=b, hn=hn: e.matmul(pu[b][:], lhsT=wgu[:, k, 2816 + c * 128:2816 + (c + 1) * 128], rhs=hn[:, k, :],
                                                                        start=(k == 0), stop=(k == 7)),
                     reads=Rwgu.sel(k, 2816 + c * 128, 2816 + (c + 1) * 128) + [Rhn[k]], writes=[Rpu[b]], join=(k > 0))
            S.op("scalar", lambda e, b=b: e.activation(out=sg[b][:], in_=pg[b][:], func=AF.Silu), reads=[Rpg[b]], writes=[Rsg[b]])
            S.op("vector", lambda e, b=b, c=c: e.tensor_tensor(out=act[:, c, :], in0=pu[b][:], in1=sg[b][:], op=ALU.mult),
                 reads=[Rpu[b], Rsg[b]], writes=[Ract[c]])
        if blk + 1 < NBLK:
            norm(blk + 1)
        for d in range(8):
            b = d % 2
            for c in range(22):
                S.op("tensor", lambda e, c=c, d=d, b=b: e.matmul(po[b][:], lhsT=wd[:, c, d * 128:(d + 1) * 128], rhs=act[:, c, :],
                                                                  start=(c == 0), stop=(c == 21)),
                     reads=[Rwd[c], Ract[c]], writes=[Rpo[b]], join=(c > 0))
            S.op("vector", lambda e, d=d, b=b, hT=hT: e.tensor_tensor(out=hT[:, d, :], in0=po[b][:], in1=hT[:, d, :], op=ALU.add),
                 reads=[Rpo[b], RhT[d]], writes=[RhT[d]])
            S.dma("sync", hout[d, :, t0:t0 + NB], hT[:, d, :], reads=[RhT[d]], writes=[P.Rdram], join=True)
    P.finish()


def make_consts():
    c = {}
    c["c_ident"] = np.eye(128, dtype=np.float32)
    s = np.arange(128)
    c["c_tri"] = (s[:, None] <= s[None, :]).astype(np.float32)
    c["c_cbias"] = np.where(s[:, None] <= s[None, :], 0.0, NEG).astype(np.float32)
    inv = 500000.0 ** (-np.arange(0, 16, 2, dtype=np.float64) / 16.0)
    ang = np.arange(NT, dtype=np.float64)[:, None] * inv[None, :]
    c["c_rope"] = np.concatenate([np.cos(ang), np.sin(ang)], axis=1).astype(np.float32)
    u = np.arange(NT)
    c["c_onehot"] = (u[None, :] // 256 == np.arange(16)[:, None]).astype(np.float32)
    c["c_tribias4"] = np.tile(c["c_cbias"], (1, 4)).astype(np.float32)
    b = np.arange(16)
    c["c_past"] = np.where(b[None, :] < b[:, None], 0.0, NEG).astype(np.float32).reshape(256)
    c["c_own"] = (b[None, :] == b[:, None]).astype(np.float32).reshape(256)
    return c


class PLE:
    def __init__(self, P, C, p_ap, g_ap, wpg_ap, wpu_ap, banks, nb=NB, nbuf=1, scratch=None):
        self.P = P
        self.nb = nb
        S = P.S
        self.p_ap = p_ap
        self.g, self.Rg = P.load_vec_fm(g_ap)
        self.wpg, self.Rwpg = P.load_w(wpg_ap, 8, 1024)
        self.wpu, self.Rwpu = P.load_w(wpu_ap, 2, 1024)
        self.Rptm = [Res() for _ in range(nbuf)]
        self.RpT = [[Res(), Res()] for _ in range(nbuf)]
        self.nbuf = nbuf
        self.Rsgate = Res()
        self.Rtmp = Res()
        if scratch is None:
            self.ptm = [P.sb([128, nb // 128, 256], F32)[:] for _ in range(nbuf)]
            self.pT = [P.sb([128, 2, nb], BF16)[:] for _ in range(nbuf)]
            self.sgate = P.sb([128, nb], F32)[:]
            self.tmp = P.sb([128, nb], F32)[:]
        else:
            self.ptm, self.pT, self.sgate, self.tmp = [scratch["ptm"]], [scratch["pT"]], scratch["sgate"], scratch["tmp"]
        self.banks = banks

    def all_res(self):
        return [self.Rptm[0], self.RpT[0][0], self.RpT[0][1], self.Rsgate, self.Rtmp]

    def pre(self, blk, hT, RhT, hn, Rhn, pss, Rpss):
        P = self.P
        S = P.S
        nb = self.nb
        t0 = blk * nb
        i = blk % self.nbuf
        ptm, Rptm, pT, RpT = self.ptm[i], self.Rptm[i], self.pT[i], self.RpT[i]
        (pc, Rpc) = self.banks[2]
        P.fm_norm(hT, RhT, self.g, self.Rg, hn, Rhn, pss, Rpss)
        S.dma("sync", ptm, self.p_ap[t0:t0 + nb, :].rearrange("(s p) d -> p s d", p=128), writes=[Rptm])
        for kk in range(2):
            for s in range(nb // 128):
                S.op("tensor", lambda e, kk=kk, s=s: e.transpose(out=pc[:, s * 128:(s + 1) * 128], in_=ptm[:, s, kk * 128:(kk + 1) * 128],
                                                                 identity=P.ident_f[:]),
                     reads=[Rptm, P.Rc], writes=[Rpc], join=(s > 0))
            S.op("scalar", lambda e, kk=kk: e.copy(out=pT[:, kk, :], in_=pc[:, 0:nb]), reads=[Rpc], writes=[RpT[kk]])

    def main(self, blk, hT, RhT, hn, Rhn):
        P = self.P
        S = P.S
        nb = self.nb
        i = blk % self.nbuf
        pT, RpT = self.pT[i], self.RpT[i]
        (pa, Rpa), (pb, Rpb) = self.banks[:2]
        for d in range(8):
            for k in range(8):
                S.op("tensor", lambda e, k=k, d=d: e.matmul(pa[:, 0:nb], lhsT=self.wpg[:, k, d * 128:(d + 1) * 128], rhs=hn[:, k, :],
                                                             start=(k == 0), stop=(k == 7)),
                     reads=[self.Rwpg[k], Rhn[k]], writes=[Rpa], join=(k > 0))
            S.op("scalar", lambda e: e.activation(out=self.sgate, in_=pa[:, 0:nb], func=AF.Sigmoid), reads=[Rpa], writes=[self.Rsgate])
            for kk in range(2):
                S.op("tensor", lambda e, kk=kk, d=d: e.matmul(pb[:, 0:nb], lhsT=self.wpu[:, kk, d * 128:(d + 1) * 128], rhs=pT[:, kk, :],
                                                               start=(kk == 0), stop=(kk == 1)),
                     reads=[self.Rwpu[kk], RpT[kk]], writes=[Rpb], join=(kk > 0))
            S.op("vector", lambda e: e.tensor_tensor(out=self.tmp, in0=pb[:, 0:nb], in1=self.sgate, op=ALU.mult),
                 reads=[Rpb, self.Rsgate], writes=[self.Rtmp])
            S.op("gpsimd", lambda e, d=d: e.tensor_tensor(out=hT[:, d, :], in0=hT[:, d, :], in1=self.tmp, op=ALU.add),
                 reads=[self.Rtmp, RhT[d]], writes=[RhT[d]])

    def emit(self, blk, hT, RhT, hn, Rhn, pss, Rpss):
        self.pre(blk, hT, RhT, hn, Rhn, pss, Rpss)
        self.main(blk, hT, RhT, hn, Rhn)


def phase_ple_out(nc, C, hin, out_ap, p_ap, g_ap, wpg_ap, wpu_ap, name):
    P = Phase(nc, name)
    S = P.S
    P.consts(C)
    P.norm_setup()
    banks = [(P.ps([128, NB], F32), Res()) for _ in range(5)]
    pss, Rpss = P.ps([128, NB], F32), Res()
    ple = PLE(P, C, p_ap, g_ap, wpg_ap, wpu_ap, banks, nbuf=2)
    hTs = [P.sb([128, 8, NB], F32) for _ in range(2)]
    RhTs = [[Res() for _ in range(8)] for _ in range(2)]
    hns = [P.sb([128, 8, NB], BF16) for _ in range(2)]
    Rhns = [[Res() for _ in range(8)] for _ in range(2)]
    otm = P.sb([128, 4, 1024], F32)
    Rotm = [Res() for _ in range(4)]

    def load(blk):
        t0 = blk * NB
        for k in range(8):
            S.dma("sync", hTs[blk % 2][:, k, :], hin[k, :, t0:t0 + NB], writes=[RhTs[blk % 2][k]])

    load(0)
    ple.pre(0, hTs[0], RhTs[0], hns[0], Rhns[0], pss, Rpss)
    for blk in range(NBLK):
        t0 = blk * NB
        hT, RhT = hTs[blk % 2], RhTs[blk % 2]
        if blk + 1 < NBLK:
            load(blk + 1)
        ple.main(blk, hT, RhT, hns[blk % 2], Rhns[blk % 2])
        if blk + 1 < NBLK:
            n = (blk + 1) % 2
            ple.pre(blk + 1, hTs[n], RhTs[n], hns[n], Rhns[n], pss, Rpss)
        for s in range(4):
            for kq in range(2):
                pt, Rpt = banks[3 + kq]
                for k4 in range(4):
                    k = kq * 4 + k4
                    S.op("tensor", lambda e, k=k, k4=k4, s=s, pt=pt, hT=hT: e.transpose(out=pt[:, k4 * 128:(k4 + 1) * 128], in_=hT[:, k, s * 128:(s + 1) * 128],
                                                                                        identity=P.ident_f[:]),
                         reads=[RhT[k], P.Rc], writes=[Rpt], join=(k4 > 0))
                if kq == 0:
                    S.op("scalar", lambda e, s=s, kq=kq, pt=pt: e.copy(out=otm[:, s, kq * 512:(kq + 1) * 512], in_=pt[:]),
                         reads=[Rpt], writes=[Rotm[s]], join=(kq > 0))
                else:
                    S.op("vector", lambda e, s=s, kq=kq, pt=pt: e.tensor_copy(out=otm[:, s, kq * 512:(kq + 1) * 512], in_=pt[:]),
                         reads=[Rpt], writes=[Rotm[s]], join=(kq > 0))
            S.dma("sync", out_ap[t0 + s * 128:t0 + (s + 1) * 128, :], otm[:, s, :], reads=[Rotm[s]], writes=[P.Rdram], join=True)
    P.finish()


def phase_mlstm(nc, C, x_ap, hout, W, name, nblk=NBLK):
    P = Phase(nc, name)
    realS = P.S
    S = Proxy(realS)
    P.S = S
    P.consts(C)
    P.norm_setup()
    tri_f = P.sb([128, 128], F32)
    cbias = P.sb([128, 128], F32)
    ones_f = P.sb([128, 128], F32)
    S.dma("sync", tri_f[:], C["c_tri"], writes=[P.Rc], join=True)
    S.dma("sync", cbias[:], C["c_cbias"], writes=[P.Rc], join=True)
    S.op("vector", lambda e: e.memset(ones_f[:], 1.0), writes=[P.Rc], join=True)
    g, Rg = P.load_vec_fm(W["norm_mix0"])
    bgate, Rbgate = P.load_bcast(W["a_b_gate"], 16)
    mhg, Rmhg = P.load_bcast(W["a_mh_gain"], 128)
    mhg_h = P.sb([128, 128], F32)
    S.op("vector", lambda e: e.tensor_scalar(out=mhg_h[:], in0=mhg[:], scalar1=0.5, scalar2=None, op0=ALU.mult), reads=[Rmhg], writes=[Rmhg])
    nhalf = P.sb([128, 8], F32)
    S.op("vector", lambda e: e.memset(nhalf[:], -0.5), writes=[P.Rc], join=True)
    win, Rwin = P.load_w(W["a_w_in"], 8, 3088, col_chunk=1544)
    wout, Rwout = P.load_w(W["a_w_out"], 8, 1024)

    xtm = P.sb([128, 4, 1024], F32); Rxtm = [Res() for _ in range(4)]
    hT = P.sb([128, 8, NB], F32); RhT = [Res() for _ in range(8)]
    hn = P.sb([128, 8, NB], BF16); Rhn = [Res() for _ in range(8)]
    qkT = P.sb([128, 8, NB], BF16); Rqk = [Res() for _ in range(8)]
    ktm = P.sb([128, 4, 512], BF16); Rktm = [Res() for _ in range(4)]
    vtm = P.sb([128, 4, 1024], BF16); Rvtm = [Res() for _ in range(4)]
    og = P.sb([128, 4, 1024], BF16); Rog = [Res() for _ in range(4)]
    sgt = P.sb([128, 512], F32); Rsgt = Res()
    gsb = P.sb([128, 4, 16], F32); Rgsb = Res()
    th = P.sb([128, 4, 16], F32); Rth = Res()
    ef = P.sb([128, 4, 8], F32); Ref = Res()
    spf = P.sb([128, 4, 8], F32); Rspf = Res()
    li = P.sb([128, 4, 8], F32); Rli = Res()
    lf = P.sb([128, 4, 8], F32); Rlf = Res()
    g_sb = P.sb([128, 8], F32); Rg_sb = Res()
    bb = P.sb([128, 8], F32); Rbb = Res()
    eg = P.sb([128, 8], F32); Reg = Res()
    wlp = P.sb([128, 8], F32); Rwlp = Res()
    wl = P.sb([128, 8], F32); Rwl = Res()
    egl = P.sb([128, 4], F32); Regl = Res()
    Gd = P.sb([128, 8, 128], F32); RGd = Res()
    arg = P.sb([128, 8, 128], F32); Rarg = Res()
    DT = P.sb([128, 8, 128], F32); RDT = Res()
    PT = P.sb([128, 8, 128], BF16); RPT = Res()
    kw = P.sb([128, 8, 64], BF16); Rkw = Res()
    numXs = P.sb([128, 8, 128], F32); RnumXs = Res()
    num = P.sb([128, 8, 128], F32); Rnum = Res()
    sqn = P.sb([128, 8, 128], F32); Rsqn = Res()
    sm = {n: (P.sb([128, 8], F32), Res()) for n in ["dxs", "den", "dd", "rec", "ssn", "t1", "t2", "lnt", "rs", "coef"]}
    y0 = P.sb([128, 8, 128], F32); Ry0 = Res()
    ytm = P.sb([128, 1024], BF16); Rytm = Res()
    yT = P.sb([128, 8, NB], BF16); RyT = [Res() for _ in range(4)]
    Cst = P.sb([128, 4, 128], F32); nst = P.sb([128, 4], F32); RC = Res()
    Cbf = P.sb([128, 4, 2, 128], BF16); nbf = P.sb([128, 4, 2], BF16); RCbf = Res()
    qbd = P.sb([128, 4, 2, NB], BF16); Rqbd = [Res() for _ in range(4)]
    nt1 = P.sb([128, 4], F32); Rnt1 = Res()

    pS = P.ps([128, 512], F32)
    RpS = Res()
    Rgcs = Rglast = RdenI = RdenX = Rdn = Rpgate = RpS
    pR = [P.ps([128, 512], F32) for _ in range(2)]; RpR = [Res(), Res()]
    pG = P.ps([128, 1024], F32); RpG = Res()
    pT2 = P.ps([128, 1024], F32); RpT2 = Res()
    pY = P.ps([128, 1024], BF16); RpY = Res()
    rot = [0]

    def nextbank():
        rot[0] ^= 1
        return pR[rot[0]], RpR[rot[0]]

    for t in (Cst, nst):
        S.op("vector", lambda e, t=t: e.memset(t[:], 0.0), writes=[RC], join=True)
    for t in (Cbf, nbf):
        S.op("vector", lambda e, t=t: e.memset(t[:], 0.0), writes=[RCbf], join=True)
    for c in range(4):
        S.op("gpsimd", lambda e, c=c: e.memset(qbd[:, c, :, :], 0.0), writes=[Rqbd[c]])

    for blk in range(nblk):
        t0 = blk * NB
        for s in range(4):
            S.dma("sync", xtm[:, s, :], x_ap[t0 + s * 128:t0 + (s + 1) * 128, :], writes=[Rxtm[s]])
        for k in range(8):
            pb, Rpb = nextbank()
            for s in range(4):
                S.op("tensor", lambda e, k=k, s=s, pb=pb: e.transpose(out=pb[:, s * 128:(s + 1) * 128], in_=xtm[:, s, k * 128:(k + 1) * 128],
                                                                       identity=P.ident_f[:]),
                     reads=[Rxtm[s], P.Rc], writes=[Rpb], join=(s > 0))
            S.op("scalar", lambda e, k=k, pb=pb: e.copy(out=hT[:, k, :], in_=pb[:]), reads=[Rpb], writes=[RhT[k]])
        pb, Rpb = nextbank()
        P.fm_norm(hT, RhT, g, Rg, hn, Rhn, pb, Rpb)
        for c in range(8):
            pb, Rpb = nextbank()
            for k in range(8):
                S.op("tensor", lambda e, k=k, c=c, pb=pb: e.matmul(pb[:], lhsT=win[:, k, c * 128:(c + 1) * 128], rhs=hn[:, k, :],
                                                                    start=(k == 0), stop=(k == 7)),
                     reads=[Rwin[k], Rhn[k]], writes=[Rpb], join=(k > 0))
            sc = 0.125 if c < 4 else 1.0
            S.op("scalar", lambda e, c=c, pb=pb, sc=sc: e.activation(out=qkT[:, c, :], in_=pb[:], func=AF.Copy, scale=sc),
                 reads=[Rpb], writes=[Rqk[c]])
            if c < 4:
                S.op("gpsimd", lambda e, c=c: e.tensor_copy(out=qbd[0:64, c, 0, :], in_=qkT[0:64, c, :]), reads=[Rqk[c]], writes=[Rqbd[c]])
                S.op("gpsimd", lambda e, c=c: e.tensor_copy(out=qbd[64:128, c, 1, :], in_=qkT[64:128, c, :]), reads=[Rqk[c]], writes=[Rqbd[c]], join=True)
        def proj_kvo(s):
            ts = slice(s * 128, (s + 1) * 128)
            pb, Rpb = nextbank()
            for k in range(8):
                S.op("tensor", lambda e, k=k, ts=ts, pb=pb: e.matmul(pb[:], lhsT=hn[:, k, ts], rhs=win[:, k, 512:1024], start=(k == 0), stop=(k == 7)),
                     reads=[Rwin[k], Rhn[k]], writes=[Rpb], join=(k > 0))
            S.op("scalar", lambda e, s=s, pb=pb: e.copy(out=ktm[:, s, :], in_=pb[:]), reads=[Rpb], writes=[Rktm[s]])
            for half in range(2):
                pb, Rpb = nextbank()
                c0 = 1024 + half * 512
                for k in range(8):
                    S.op("tensor", lambda e, k=k, ts=ts, pb=pb, c0=c0: e.matmul(pb[:], lhsT=hn[:, k, ts], rhs=win[:, k, c0:c0 + 512],
                                                                                 start=(k == 0), stop=(k == 7)),
                         reads=[Rwin[k], Rhn[k]], writes=[Rpb], join=(k > 0))
                S.op("vector", lambda e, s=s, half=half, pb=pb: e.tensor_copy(out=vtm[:, s, half * 512:(half + 1) * 512], in_=pb[:]),
                     reads=[Rpb], writes=[Rvtm[s]], join=(half > 0))
            for half in range(2):
                pb, Rpb = nextbank()
                c0 = 2048 + half * 512
                for k in range(8):
                    S.op("tensor", lambda e, k=k, ts=ts, pb=pb, c0=c0: e.matmul(pb[:], lhsT=hn[:, k, ts], rhs=win[:, k, c0:c0 + 512],
                                                                                 start=(k == 0), stop=(k == 7)),
                         reads=[Rwin[k], Rhn[k]], writes=[Rpb], join=(k > 0))
                S.op("scalar", lambda e, pb=pb: e.activation(out=sgt[:], in_=pb[:], func=AF.Tanh, scale=0.5), reads=[Rpb], writes=[Rsgt])
                S.op("gpsimd", lambda e: e.tensor_tensor(
                    out=sgt[:].rearrange("p (h v) -> p h v", h=4), in0=sgt[:].rearrange("p (h v) -> p h v", h=4),
                    in1=mhg_h[:].unsqueeze(1).broadcast_to([128, 4, 128]), op=ALU.mult), reads=[Rsgt, Rmhg], writes=[Rsgt])
                S.op("gpsimd", lambda e, s=s, half=half: e.tensor_tensor(
                    out=og[:, s, half * 512:(half + 1) * 512].rearrange("p (h v) -> p h v", h=4),
                    in0=sgt[:].rearrange("p (h v) -> p h v", h=4),
                    in1=mhg_h[:].unsqueeze(1).broadcast_to([128, 4, 128]), op=ALU.add),
                     reads=[Rsgt, Rmhg], writes=[Rog[s]], join=(half > 0))

        for s in range(4):
            ts = slice(s * 128, (s + 1) * 128)
            for k in range(8):
                S.op("tensor", lambda e, k=k, ts=ts, s=s: e.matmul(pS[:, 64 + s * 16:64 + (s + 1) * 16], lhsT=hn[:, k, ts], rhs=win[:, k, 3072:3088],
                                                                    start=(k == 0), stop=(k == 7)),
                     reads=[Rwin[k], Rhn[k]], writes=[Rpgate], join=(k > 0))
            S.op("vector", lambda e, s=s: e.tensor_tensor(out=gsb[:, s, :], in0=pS[:, 64 + s * 16:64 + (s + 1) * 16], in1=bgate[:], op=ALU.add),
                 reads=[Rpgate, Rbgate], writes=[Rgsb], join=(s > 0))
        S.op("scalar", lambda e: e.activation(out=th[:], in_=gsb[:], func=AF.Tanh, scale=1.0 / 15.0), reads=[Rgsb], writes=[Rth])
        S.op("vector", lambda e: e.tensor_scalar(out=li[:], in0=th[:, :, 0:8], scalar1=15.0, scalar2=None, op0=ALU.mult), reads=[Rth], writes=[Rli])
        S.op("scalar", lambda e: e.activation(out=ef[:], in_=th[:, :, 8:16], func=AF.Exp, scale=-15.0), reads=[Rth], writes=[Ref])
        S.op("scalar", lambda e: e.activation(out=spf[:], in_=ef[:], func=AF.Ln, bias=P.one_t[:, 0:1]), reads=[Ref, P.Rc], writes=[Rspf])
        S.op("vector", lambda e: e.tensor_scalar(out=lf[:], in0=spf[:], scalar1=-1.0, scalar2=None, op0=ALU.mult), reads=[Rspf], writes=[Rlf])

        proj_kvo(0)
        for s in range(4):
            ts = slice(s * 128, (s + 1) * 128)
            if s < 3:
                d_ = Deferred()
                S.tgt = d_
                proj_kvo(s + 1)
                S.tgt = Mux(realS, mm_chunks(d_.q))
            else:
                S.tgt = realS
            S.op("tensor", lambda e, s=s: e.matmul(pS[:, 0:8], lhsT=tri_f[:], rhs=lf[:, s, :], start=True, stop=True),
                 reads=[Rlf, P.Rc], writes=[Rgcs])
            S.op("tensor", lambda e, s=s: e.matmul(pS[:, 8:16], lhsT=ones_f[:], rhs=lf[:, s, :], start=True, stop=True),
                 reads=[Rlf, P.Rc], writes=[Rglast])
            S.op("vector", lambda e: e.tensor_copy(out=g_sb[:], in_=pS[:, 0:8]), reads=[Rgcs], writes=[Rg_sb])
            S.op("vector", lambda e, s=s: e.tensor_tensor(out=bb[:], in0=li[:, s, :], in1=g_sb[:], op=ALU.subtract), reads=[Rli, Rg_sb], writes=[Rbb])
            S.op("scalar", lambda e: e.activation(out=eg[:], in_=g_sb[:], func=AF.Exp), reads=[Rg_sb], writes=[Reg])
            S.op("vector", lambda e: e.tensor_tensor(out=wlp[:], in0=pS[:, 8:16], in1=bb[:], op=ALU.add), reads=[Rglast, Rbb], writes=[Rwlp])
            S.op("scalar", lambda e: e.activation(out=wl[:], in_=wlp[:], func=AF.Exp), reads=[Rwlp], writes=[Rwl])
            S.op("scalar", lambda e: e.activation(out=egl[0:64, :], in_=pS[0:64, 8:16:2], func=AF.Exp), reads=[Rglast], writes=[Regl])
            S.op("scalar", lambda e: e.activation(out=egl[64:128, :], in_=pS[64:128, 9:16:2], func=AF.Exp), reads=[Rglast], writes=[Regl], join=True)
            S.op("vector", lambda e: e.tensor_tensor(out=Gd[:], in0=g_sb[:].unsqueeze(2).broadcast_to([128, 8, 128]),
                                                      in1=P.ident_f[:].unsqueeze(1).broadcast_to([128, 8, 128]), op=ALU.mult),
                 reads=[Rg_sb, P.Rc], writes=[RGd])
            for half in range(2):
                S.op("tensor", lambda e, half=half: e.matmul(pG[:, half * 512:(half + 1) * 512], lhsT=ones_f[:],
                                                               rhs=Gd[:, half * 4:(half + 1) * 4, :].rearrange("p h j -> p (h j)"),
                                                               start=True, stop=True),
                     reads=[RGd, P.Rc], writes=[RpG], join=(half > 0))
            for h in range(8):
                S.op("vector", lambda e, h=h: e.scalar_tensor_tensor(out=arg[:, h, :], in0=pG[:, h * 128:(h + 1) * 128], scalar=bb[:, h:h + 1], op0=ALU.add,
                                                                      in1=cbias[:], op1=ALU.add),
                     reads=[RpG, Rbb, P.Rc], writes=[Rarg], join=(h > 0))
            S.op("scalar", lambda e: e.activation(out=DT[:], in_=arg[:], func=AF.Exp), reads=[Rarg], writes=[RDT])
            for c in range(4):
                S.op("tensor", lambda e, c=c, ts=ts: e.matmul(pT2[:, c * 256:(c + 1) * 256], lhsT=qkT[:, 4 + c, ts], rhs=qbd[:, c, :, ts],
                                                               start=True, stop=True),
                     reads=[Rqbd[c], Rqk[4 + c]], writes=[RpT2], join=(c > 0))
            S.op("vector", lambda e: e.tensor_tensor(out=PT[:].rearrange("p h j -> p (h j)"), in0=pT2[:], in1=DT[:].rearrange("p h j -> p (h j)"), op=ALU.mult),
                 reads=[RpT2, RDT], writes=[RPT])
            for h in range(8):
                S.op("tensor", lambda e, h=h, s=s: e.matmul(pG[:, h * 128:(h + 1) * 128], lhsT=PT[:, h, :], rhs=vtm[:, s, h * 128:(h + 1) * 128],
                                                             start=True, stop=True),
                     reads=[RPT, Rvtm[s]], writes=[RpG], join=(h > 0))
            for h in range(8):
                S.op("tensor", lambda e, h=h: e.matmul(pS[:, 16 + h:17 + h], lhsT=PT[:, h, :], rhs=P.ones_b[:, 0:1], start=True, stop=True),
                     reads=[RPT, P.Rc], writes=[RdenI], join=(h > 0))
            for c in range(4):
                S.op("tensor", lambda e, c=c, ts=ts: e.matmul(pT2[:, c * 256:(c + 1) * 256], lhsT=qkT[:, c, ts], rhs=Cbf[:, c, :, :],
                                                               start=True, stop=True),
                     reads=[Rqk[c], RCbf], writes=[RpT2], join=(c > 0))
            for c in range(4):
                S.op("tensor", lambda e, c=c, ts=ts: e.matmul(pS[:, 24 + 2 * c:26 + 2 * c], lhsT=qkT[:, c, ts], rhs=nbf[:, c, :],
                                                               start=True, stop=True),
                     reads=[Rqk[c], RCbf], writes=[RdenX], join=(c > 0))
            S.op("vector", lambda e, s=s: e.tensor_tensor(out=kw[:], in0=ktm[:, s, :].rearrange("p (h d) -> p h d", h=8),
                                                           in1=wl[:].unsqueeze(2).broadcast_to([128, 8, 64]), op=ALU.mult),
                 reads=[Rktm[s], Rwl], writes=[Rkw])
            pd, Rpd = pY[:].bitcast(F32), RpY
            for h in range(8):
                c, ph = h // 2, h % 2
                prt = slice(ph * 64, (ph + 1) * 64)
                S.op("tensor", lambda e, h=h, c=c, prt=prt, s=s, pd=pd: e.matmul(pd[prt, c * 128:(c + 1) * 128], lhsT=kw[:, h, :], rhs=vtm[:, s, h * 128:(h + 1) * 128],
                                                                                  start=True, stop=True),
                     reads=[Rkw, Rvtm[s]], writes=[Rpd], join=(h > 0))
            for h in range(8):
                c, ph = h // 2, h % 2
                prt = slice(ph * 64, (ph + 1) * 64)
                S.op("tensor", lambda e, h=h, c=c, prt=prt: e.matmul(pS[prt, 32 + c:33 + c], lhsT=kw[:, h, :], rhs=P.ones_b[:, 0:1], start=True, stop=True),
                     reads=[Rkw, P.Rc], writes=[Rdn], join=(h > 0))
            for c in range(4):
                S.op("vector", lambda e, c=c, pd=pd: e.scalar_tensor_tensor(out=Cst[:, c, :], in0=Cst[:, c, :], scalar=egl[:, c:c + 1], op0=ALU.mult,
                                                                            in1=pd[:, c * 128:(c + 1) * 128], op1=ALU.add),
                     reads=[Rpd, Regl, RC], writes=[RC])
            S.op("vector", lambda e: e.tensor_tensor(out=nt1[:], in0=nst[:], in1=egl[:], op=ALU.mult), reads=[RC, Regl], writes=[Rnt1])
            S.op("vector", lambda e: e.tensor_tensor(out=nst[:], in0=pS[:, 32:36], in1=nt1[:], op=ALU.add), reads=[Rdn, Rnt1], writes=[RC])
            S.op("vector", lambda e: e.tensor_tensor(out=numXs[:], in0=pT2[:].rearrange("p (h v) -> p h v", h=8),
                                                      in1=eg[:].unsqueeze(2).broadcast_to([128, 8, 128]), op=ALU.mult),
                 reads=[RpT2, Reg], writes=[RnumXs])
            S.op("vector", lambda e: e.tensor_tensor(out=num[:].rearrange("p h v -> p (h v)"), in0=pG[:], in1=numXs[:].rearrange("p h v -> p (h v)"), op=ALU.add),
                 reads=[RpG, RnumXs], writes=[Rnum])
            S.op("gpsimd", lambda e: e.tensor_copy(out=Cbf[0:64, :, 0, :], in_=Cst[0:64, :, :]), reads=[RC], writes=[RCbf])
            S.op("gpsimd", lambda e: e.tensor_copy(out=Cbf[64:128, :, 1, :], in_=Cst[64:128, :, :]), reads=[RC], writes=[RCbf], join=True)
            S.op("gpsimd", lambda e: e.tensor_copy(out=nbf[0:64, :, 0], in_=nst[0:64, :]), reads=[RC], writes=[RCbf], join=True)
            S.op("gpsimd", lambda e: e.tensor_copy(out=nbf[64:128, :, 1], in_=nst[64:128, :]), reads=[RC], writes=[RCbf], join=True)
            T = lambda n: sm[n][0]
            R_ = lambda n: sm[n][1]
            S.op("vector", lambda e: e.tensor_tensor(out=T("dxs")[:], in0=pS[:, 24:32], in1=eg[:], op=ALU.mult), reads=[RdenX, Reg], writes=[R_("dxs")])
            S.op("vector", lambda e: e.tensor_tensor(out=T("den")[:], in0=pS[:, 16:24], in1=T("dxs")[:], op=ALU.add), reads=[RdenI, R_("dxs")], writes=[R_("den")])
            S.op("vector", lambda e: e.scalar_tensor_tensor(out=T("t1")[:], in0=T("den")[:], scalar=-1.0, op0=ALU.mult, in1=T("den")[:], op1=ALU.max),
                 reads=[R_("den")], writes=[R_("t1")])
            S.op("vector", lambda e: e.tensor_scalar(out=T("dd")[:], in0=T("t1")[:], scalar1=1.0, scalar2=None, op0=ALU.max), reads=[R_("t1")], writes=[R_("dd")])
            S.op("vector", lambda e: e.reciprocal(out=T("rec")[:], in_=T("dd")[:]), reads=[R_("dd")], writes=[R_("rec")])
            S.op("gpsimd", lambda e: e.tensor_tensor(out=sqn[:], in0=num[:], in1=num[:], op=ALU.mult), reads=[Rnum], writes=[Rsqn])
            S.op("vector", lambda e: e.tensor_reduce(out=T("ssn")[:], in_=sqn[:], axis=AX.X, op=ALU.add), reads=[Rsqn], writes=[R_("ssn")])
            S.op("vector", lambda e: e.tensor_tensor(out=T("t1")[:], in0=T("rec")[:], in1=T("rec")[:], op=ALU.mult), reads=[R_("rec")], writes=[R_("t1")])
            S.op("vector", lambda e: e.tensor_tensor(out=T("t2")[:], in0=T("t1")[:], in1=T("ssn")[:], op=ALU.mult), reads=[R_("t1"), R_("ssn")], writes=[R_("t2")])
            S.op("gpsimd", lambda e: e.tensor_scalar(out=T("lnt")[:], in0=T("t2")[:], scalar1=1.0 / 128.0, scalar2=EPS, op0=ALU.mult, op1=ALU.add),
                 reads=[R_("t2")], writes=[R_("lnt")])
            S.op("gpsimd", lambda e: e.tensor_tensor(out=T("rs")[:], in0=T("lnt")[:], in1=nhalf[:], op=ALU.pow), reads=[R_("lnt"), P.Rc], writes=[R_("rs")])
            S.op("vector", lambda e: e.tensor_tensor(out=T("coef")[:], in0=T("rec")[:], in1=T("rs")[:], op=ALU.mult), reads=[R_("rec"), R_("rs")], writes=[R_("coef")])
            S.op("vector", lambda e: e.tensor_tensor(out=y0[:], in0=num[:], in1=T("coef")[:].unsqueeze(2).broadcast_to([128, 8, 128]), op=ALU.mult),
                 reads=[Rnum, R_("coef")], writes=[Ry0])
            S.op("gpsimd", lambda e, s=s: e.tensor_tensor(out=ytm[:], in0=y0[:].rearrange("p h v -> p (h v)"), in1=og[:, s, :], op=ALU.mult),
                 reads=[Ry0, Rog[s]], writes=[Rytm])
            for h in range(8):
                S.op("tensor", lambda e, h=h: e.transpose(out=pY[:, h * 128:(h + 1) * 128], in_=ytm[:, h * 128:(h + 1) * 128], identity=P.ident_b[:]),
                     reads=[Rytm, P.Rc], writes=[RpY], join=(h > 0))
            S.op("scalar", lambda e, ts=ts: e.copy(out=yT[:, :, ts], in_=pY[:].rearrange("p (h j) -> p h j", h=8)), reads=[RpY], writes=[RyT[s]])
            if s < 3:
                S.tgt.flush()
            S.tgt = realS
        for d in range(8):
            pb, Rpb = nextbank()
            for h in range(8):
                S.op("tensor", lambda e, h=h, d=d, pb=pb: e.matmul(pb[:], lhsT=wout[:, h, d * 128:(d + 1) * 128], rhs=yT[:, h, :], start=(h == 0), stop=(h == 7)),
                     reads=[Rwout[h]] + RyT, writes=[Rpb], join=(h > 0))
            S.op("vector", lambda e, d=d, pb=pb: e.tensor_tensor(out=hT[:, d, :], in0=pb[:], in1=hT[:, d, :], op=ALU.add), reads=[Rpb, RhT[d]], writes=[RhT[d]])
            S.dma("sync", hout[d, :, t0:t0 + NB], hT[:, d, :], reads=[RhT[d]], writes=[P.Rdram], join=True)
    P.S = realS
    P.finish()


def phase_moba(nc, C, hin, hout, W, name, nblk=NBLK, dbg=None):
    G = 2
    P = Phase(nc, name)
    realS = P.S
    S = Proxy(realS)
    P.S = S
    P.consts(C)
    P.norm_setup()
    c256 = P.sb([128, 1], F32)
    S.op("vector", lambda e: e.memset(c256[:], 1.0 / 256.0), writes=[P.Rc], join=True)
    tri4 = P.sb([128, 512], BF16)
    S.dma("gpsimd", tri4[:], C["c_tribias4"], writes=[P.Rc], join=True)
    ropet = P.sb([128, 32, 16], F32)
    S.dma("sync", ropet[:], C["c_rope"].rearrange("(i p) c -> p i c", p=128), writes=[P.Rc], join=True)
    pastb, Rpastb = P.load_bcast(C["c_past"], 256)
    ownb, Rownb = P.load_bcast(C["c_own"], 256)
    g_kv, Rg_kv = P.load_vec_fm(W["kv_norm"])
    g_mix, Rg_mix = P.load_vec_fm(W["norm_mix1"])
    knorm, Rknorm = P.load_bcast(W["k_norm"], 64)
    qnorm, Rqnorm = P.load_bcast(W["b_q_norm"], 64)
    pR = [P.ps([128, 512], F32) for _ in range(2)]; RpR = [Res(), Res()]
    psc = [P.ps([128, G * 512], F32) for _ in range(2)]; Rpsc = [Res(), Res()]
    pop = [P.ps([128, 512], F32) for _ in range(2)]; Rpop = [Res(), Res()]
    rot = [0]

    def nextbank():
        rot[0] ^= 1
        return pR[rot[0]], RpR[rot[0]]

    arena = P.sb([128, 2688], F32)
    scratch = {"ptm": arena[:, 0:1024].rearrange("p (s d) -> p s d", s=4),
               "pT": arena[:, 1024:1536].bitcast(BF16).rearrange("p (k t) -> p k t", k=2),
               "sgate": arena[:, 1536:2048], "tmp": arena[:, 2048:2560]}
    ple = PLE(P, C, W["p0"], W["norm_ple0"], W["w_ple_gate0"], W["w_ple_up0"], [(pR[0], RpR[0]), (pR[1], RpR[1]), (pR[0], RpR[0])],
              scratch=scratch)
    wkv, Rwkv = P.load_w(W["w_kv"], 8, 512)
    wq, Rwq = P.load_w(W["b_w_q"], 8, 1024)
    wo, Rwo = P.load_w(W["b_w_o"], 8, 1024)

    hT = P.sb([128, 8, NB], F32); RhT = [Res() for _ in range(8)]
    hn = P.sb([128, 8, NB], BF16); Rhn = [Res() for _ in range(8)]
    hkv = P.sb([128, 8, NB], BF16); Rhkv = [Res() for _ in range(8)]
    KT = P.sb([80, 4, NT], BF16); RKT = [Res() for _ in range(32)]
    Vaug = P.sb([128, 4, 32, 128], BF16); RV = [Res() for _ in range(32)]
    kmT = P.sb([80, 4, 16], BF16); RkmT = Res()
    kms = P.sb([64, 4, 2], F32); Rkms = Res()
    ksb = P.sb([128, 4, 64], F32); Rksb = Res()
    sqk = P.sb([128, 4, 64], F32); Rsqk = Res()
    kbf = P.sb([128, 4, 64], BF16); Rkbf = Res()
    qsb = arena[:, 0:1024].rearrange("p (h d) -> p h d", h=16); Rqsb = Res()
    sqq = arena[:, 1024:2048].rearrange("p (h d) -> p h d", h=16); Rsqq = Res()
    qa = arena[:, 2048:2688].bitcast(BF16).rearrange("p (h d) -> p h d", h=16); Rqa = Res()

    def arena_fence(to_ple):
        qres = [Rqsb, Rsqq, Rqa]
        if to_ple:
            S.op("gpsimd", lambda e: e.memset(arena[:, 0:1], 0.0), reads=qres, writes=ple.all_res())
        else:
            S.op("gpsimd", lambda e: e.memset(arena[:, 0:1], 0.0), reads=ple.all_res(), writes=qres)
    QTa = [P.sb([80, 16, 128], BF16) for _ in range(2)]; RQTa = [Res(), Res()]
    gm = P.sb([128, 16, 16], F32); Rgm = Res()
    mx8 = P.sb([128, 16, 8], F32); Rmx8 = Res()
    vis = P.sb([128, 16, 16], F32); Rvis = Res()
    skq = {n: (P.sb([128, 16], F32), Res()) for n in ["ss", "ln", "r"]}
    skk = {n: (P.sb([128, 16], F32), Res()) for n in ["ss", "ln", "r"]}
    rtq = {n: (P.sb([128, 16, 8], F32), Res()) for n in ["t1", "t2", "t3", "t4"]}
    PTb = [P.sb([128, G * 512], BF16) for _ in range(2)]; RPTb = [Res() for _ in range(2)]
    rec = P.sb([128, 512], F32); Rrec = Res()
    OTb = P.sb([128, 8, NB], BF16); ROTb = [Res() for _ in range(4)]

    for kvh in range(4):
        for c0 in range(0, NT, 1024):
            S.dma("gpsimd", KT[64:80, kvh, c0:c0 + 1024], C["c_onehot"][:, c0:c0 + 1024], writes=[P.Rc], join=True)
    S.op("vector", lambda e: e.memset(Vaug[:].rearrange("p a b c -> p (a b c)"), 1.0), writes=[P.Rc], join=True)
    S.op("gpsimd", lambda e: e.memset(kmT[:], 0.0), writes=[RkmT])
    for i in range(2):
        S.op("gpsimd", lambda e, i=i: e.memset(QTa[i][:], 0.0), writes=[RQTa[i]])

    def head_norm_rope(x, Rx, sq_, Rsq_, nh, gbc, Rgbc, it, sk, rt):
        (ss, Rss), (ln, Rln), (r, Rr) = sk["ss"], sk["ln"], sk["r"]
        S.op("gpsimd", lambda e: e.tensor_tensor(out=sq_[:], in0=x[:], in1=x[:], op=ALU.mult), reads=[Rx], writes=[Rsq_])
        S.op("vector", lambda e: e.tensor_reduce(out=ss[:, 0:nh], in_=sq_[:], axis=AX.X, op=ALU.add), reads=[Rsq_], writes=[Rss])
        yield
        S.op("scalar", lambda e: e.activation(out=ln[:, 0:nh], in_=ss[:, 0:nh], func=AF.Ln, scale=1.0 / 64.0, bias=P.eps_t[:, 0:1]),
             reads=[Rss, P.Rc], writes=[Rln])
        S.op("scalar", lambda e: e.activation(out=r[:, 0:nh], in_=ln[:, 0:nh], func=AF.Exp, scale=-0.5), reads=[Rln], writes=[Rr])
        yield
        S.op("vector", lambda e: e.tensor_tensor(out=x[:], in0=x[:], in1=r[:, 0:nh].unsqueeze(2).broadcast_to([128, nh, 64]), op=ALU.mult),
             reads=[Rx, Rr], writes=[Rx])
        S.op("vector", lambda e: e.tensor_tensor(out=x[:], in0=x[:], in1=gbc[:].unsqueeze(1).broadcast_to([128, nh, 64]), op=ALU.mult),
             reads=[Rx, Rgbc], writes=[Rx])
        yield
        cs = ropet[:, it, 0:8].unsqueeze(1).broadcast_to([128, nh, 8])
        sn = ropet[:, it, 8:16].unsqueeze(1).broadcast_to([128, nh, 8])
        x1 = x[:, :, 0:8]
        x2 = x[:, :, 8:16]
        tt = {k: v[0][:, 0:nh, :] for k, v in rt.items()}
        Rt = {k: v[1] for k, v in rt.items()}
        S.op("vector", lambda e: e.tensor_tensor(out=tt["t1"], in0=x1, in1=cs, op=ALU.mult), reads=[Rx, P.Rc], writes=[Rt["t1"]])
        S.op("vector", lambda e: e.tensor_tensor(out=tt["t2"], in0=x2, in1=sn, op=ALU.mult), reads=[Rx, P.Rc], writes=[Rt["t2"]])
        S.op("vector", lambda e: e.tensor_tensor(out=tt["t3"], in0=x2, in1=cs, op=ALU.mult), reads=[Rx, P.Rc], writes=[Rt["t3"]])
        S.op("vector", lambda e: e.tensor_tensor(out=tt["t4"], in0=x1, in1=sn, op=ALU.mult), reads=[Rx, P.Rc], writes=[Rt["t4"]])
        yield
        S.op("vector", lambda e: e.tensor_tensor(out=x1, in0=tt["t1"], in1=tt["t2"], op=ALU.subtract), reads=[Rt["t1"], Rt["t2"], Rx], writes=[Rx])
        S.op("vector", lambda e: e.tensor_tensor(out=x2, in0=tt["t3"], in1=tt["t4"], op=ALU.add), reads=[Rt["t3"], Rt["t4"], Rx], writes=[Rx])
        yield

    def kv_path(blk, s):
        it = blk * 4 + s
        ts = slice(s * 128, (s + 1) * 128)
        pb, Rpb = nextbank()
        for k in range(8):
            S.op("tensor", lambda e, k=k, ts=ts, pb=pb: e.matmul(pb[:], lhsT=hkv[:, k, ts], rhs=wkv[:, k, :], start=(k == 0), stop=(k == 7)),
                 reads=[Rwkv[k], Rhkv[k]], writes=[Rpb], join=(k > 0))
        S.op("scalar", lambda e, pb=pb: e.copy(out=ksb[:].rearrange("p h d -> p (h d)"), in_=pb[:, 0:256]), reads=[Rpb], writes=[Rksb])
        S.op("scalar", lambda e, pb=pb, it=it: e.copy(out=Vaug[:, :, it, 0:64], in_=pb[:, 256:512].rearrange("p (h d) -> p h d", h=4)),
             reads=[Rpb, P.Rc], writes=[RV[it]])
        for _ in head_norm_rope(ksb, Rksb, sqk, Rsqk, 4, knorm, Rknorm, it, skk, rtq):
            pass
        S.op("gpsimd", lambda e: e.tensor_copy(out=kbf[:], in_=ksb[:]), reads=[Rksb], writes=[Rkbf])
        pb, Rpb = nextbank()
        pbb = pb[:].bitcast(BF16)
        for kvh in range(4):
            S.op("tensor", lambda e, kvh=kvh, pbb=pbb: e.transpose(out=pbb[0:64, kvh * 128:(kvh + 1) * 128], in_=kbf[:, kvh, :], identity=P.ident_b[:]),
                 reads=[Rkbf, P.Rc], writes=[Rpb], join=(kvh > 0))
        S.op("scalar", lambda e, it=it, pbb=pbb: e.copy(out=KT[0:64, :, it * 128:(it + 1) * 128], in_=pbb[0:64, 0:512].rearrange("p (h t) -> p h t", h=4)),
             reads=[Rpb, P.Rc], writes=[RKT[it]])
        pb, Rpb = nextbank()
        for kvh in range(4):
            S.op("tensor", lambda e, kvh=kvh, pb=pb: e.matmul(pb[0:64, kvh:kvh + 1], lhsT=ksb[:, kvh, :], rhs=c256[:, 0:1], start=True, stop=True),
                 reads=[Rksb, P.Rc], writes=[Rpb], join=(kvh > 0))
        S.op("vector", lambda e, s=s, pb=pb: e.tensor_copy(out=kms[:, :, s % 2], in_=pb[0:64, 0:4]), reads=[Rpb], writes=[Rkms], join=(s % 2 == 1))
        if s % 2 == 1:
            n = it // 2
            S.op("vector", lambda e, n=n: e.tensor_tensor(out=kmT[0:64, :, n], in0=kms[:, :, 0], in1=kms[:, :, 1], op=ALU.add),
                 reads=[Rkms], writes=[RkmT])

    def q_path(blk, s):
        it = blk * 4 + s
        b = it // 2
        ts = slice(s * 128, (s + 1) * 128)
        QT, RQT = QTa[it % 2], RQTa[it % 2]
        for half in range(2):
            pb, Rpb = nextbank()
            for k in range(8):
                S.op("tensor", lambda e, k=k, ts=ts, pb=pb, half=half: e.matmul(pb[:], lhsT=hn[:, k, ts], rhs=wq[:, k, half * 512:(half + 1) * 512],
                                                                                 start=(k == 0), stop=(k == 7)),
                     reads=[Rwq[k], Rhn[k]], writes=[Rpb], join=(k > 0))
                if k % 4 == 3:
                    yield
            S.op("scalar", lambda e, pb=pb, half=half: e.copy(out=qsb[:, half * 8:(half + 1) * 8, :].rearrange("p h d -> p (h d)"), in_=pb[:]),
                 reads=[Rpb], writes=[Rqsb], join=(half > 0))
            yield
        for _ in head_norm_rope(qsb, Rqsb, sqq, Rsqq, 16, qnorm, Rqnorm, it, skq, rtq):
            yield
        S.op("gpsimd", lambda e: e.tensor_copy(out=qa[:, :, 0:64], in_=qsb[:]), reads=[Rqsb], writes=[Rqa])
        yield
        for r_ in range(2):
            pb, Rpb = nextbank()
            pbb = pb[:].bitcast(BF16)
            for j in range(8):
                h = r_ * 8 + j
                S.op("tensor", lambda e, h=h, j=j, pbb=pbb: e.transpose(out=pbb[0:64, j * 128:(j + 1) * 128], in_=qa[:, h, 0:64], identity=P.ident_b[:]),
                     reads=[Rqa, P.Rc], writes=[Rpb], join=(j > 0))
                if j % 4 == 3:
                    yield
            S.op("scalar", lambda e, r_=r_, pbb=pbb: e.copy(out=QT[0:64, r_ * 8:(r_ + 1) * 8, :], in_=pbb[0:64, :].rearrange("p (h t) -> p h t", h=8)),
                 reads=[Rpb], writes=[RQT], join=(r_ > 0))
            yield
        pb, Rpb = nextbank()
        for h in range(16):
            S.op("tensor", lambda e, h=h, pb=pb: e.matmul(pb[:, h * 16:(h + 1) * 16], lhsT=QT[:, h, :], rhs=kmT[:, h // 4, :], start=True, stop=True),
                 reads=[RQT, RkmT], writes=[Rpb], join=(h > 0))
            if h % 4 == 3:
                yield
        S.op("vector", lambda e, b=b, pb=pb: e.tensor_tensor(out=gm[:], in0=pb[:, 0:256].rearrange("p (h n) -> p h n", h=16),
                                                              in1=pastb[:, b * 16:(b + 1) * 16].unsqueeze(1).broadcast_to([128, 16, 16]), op=ALU.add),
             reads=[Rpb, Rpastb], writes=[Rgm])
        yield
        for h in range(16):
            S.op("vector", lambda e, h=h: e.max(out=mx8[:, h, :], in_=gm[:, h, :]), reads=[Rgm], writes=[Rmx8], join=(h > 0))
            if h % 4 == 3:
                yield
        S.op("vector", lambda e: e.tensor_tensor(out=vis[:], in0=gm[:], in1=mx8[:, :, 2:3].broadcast_to([128, 16, 16]), op=ALU.is_ge),
             reads=[Rgm, Rmx8], writes=[Rvis])
        S.op("vector", lambda e, b=b: e.tensor_tensor(out=vis[:], in0=vis[:], in1=ownb[:, b * 16:(b + 1) * 16].unsqueeze(1).broadcast_to([128, 16, 16]), op=ALU.max),
             reads=[Rvis, Rownb], writes=[Rvis])
        S.op("vector", lambda e: e.tensor_scalar(out=qa[:, :, 64:80], in0=vis[:], scalar1=-NEG, scalar2=NEG, op0=ALU.mult, op1=ALU.add),
             reads=[Rvis, Rqa], writes=[Rqa])
        yield
        for r_ in range(2):
            pb, Rpb = nextbank()
            pbb = pb[:].bitcast(BF16)
            for j in range(8):
                h = r_ * 8 + j
                S.op("tensor", lambda e, h=h, j=j, pbb=pbb: e.transpose(out=pbb[0:80, j * 128:(j + 1) * 128], in_=qa[:, h, :], identity=P.ident_b[:]),
                     reads=[Rqa, P.Rc], writes=[Rpb], join=(j > 0))
                if j % 4 == 3:
                    yield
            S.op("scalar", lambda e, r_=r_, pbb=pbb: e.copy(out=QT[:, r_ * 8:(r_ + 1) * 8, :], in_=pbb[0:80, :].rearrange("p (h t) -> p h t", h=8)),
                 reads=[Rpb], writes=[RQT], join=(r_ > 0))
            yield

    nsc = [0]

    def attention(blk, s):
        it = blk * 4 + s
        ts = slice(s * 128, (s + 1) * 128)
        QT, RQT = QTa[it % 2], RQTa[it % 2]
        L = []
        for kvh in range(4):
            for j0 in range(0, it + 1, G):
                L.append((kvh, j0, min(it + 1, j0 + G)))
        bufs = {}

        def qk(n):
            kvh, j0, j1 = L[n]
            m = nsc[0]
            nsc[0] += 1
            bufs[n] = m % 2
            ps_, Rps_ = psc[m % 2], Rpsc[m % 2]
            qg = QT[:, 4 * kvh:4 * kvh + 4, :].rearrange("p h t -> p (h t)")
            for j in range(j0, j1):
                o = (j - j0) * 512
                diag = (j == it)
                S.op("tensor", lambda e, kvh=kvh, j=j, ps_=ps_, qg=qg, diag=diag, o=o: e.matmul(ps_[:, o:o + 512], lhsT=KT[:, kvh, j * 128:(j + 1) * 128], rhs=qg,
                                                                                               start=True, stop=(not diag)),
                     reads=[RKT[j], RQT, P.Rc], writes=[Rps_], join=(j > j0))
                if diag:
                    S.op("tensor", lambda e, ps_=ps_, o=o: e.matmul(ps_[:, o:o + 512], lhsT=P.ident_b[:], rhs=tri4[:], start=False, stop=True),
                         reads=[P.Rc], writes=[Rps_], join=True)

        def ex(n):
            kvh, j0, j1 = L[n]
            bi = bufs[n]
            w = (j1 - j0) * 512
            S.op("scalar", lambda e, bi=bi, w=w: e.activation(out=PTb[bi][:, 0:w], in_=psc[bi][:, 0:w], func=AF.Exp, scale=0.125),
                 reads=[Rpsc[bi]], writes=[RPTb[bi]])

        def pv(n):
            kvh, j0, j1 = L[n]
            bi = bufs[n]
            po, Rpo = pop[kvh % 2], Rpop[kvh % 2]
            for j in range(j0, j1):
                o = (j - j0) * 512
                S.op("tensor", lambda e, kvh=kvh, j=j, bi=bi, po=po, o=o, it=it: e.matmul(po[:], lhsT=Vaug[:, kvh, j, :], rhs=PTb[bi][:, o:o + 512],
                                                                                         start=(j == 0), stop=(j == it)),
                     reads=[RV[j], RPTb[bi], P.Rc], writes=[Rpo], join=(j > 0))

        def epi(kvh):
            po, Rpo = pop[kvh % 2], Rpop[kvh % 2]
            S.op("vector", lambda e, po=po: e.reciprocal(out=rec[64:128, :], in_=po[64:128, :]), reads=[Rpo], writes=[Rrec])
            for gq in range(4):
                hh = 4 * kvh + gq
                pr, ph = hh // 2, hh % 2
                S.op("vector", lambda e, po=po, gq=gq, pr=pr, ph=ph, ts=ts: e.tensor_tensor(
                    out=OTb[ph * 64:(ph + 1) * 64, pr, ts], in0=po[0:64, gq * 128:(gq + 1) * 128], in1=rec[64:128, gq * 128:(gq + 1) * 128], op=ALU.mult),
                     reads=[Rpo, Rrec], writes=[ROTb[s]], join=True)

        qk(0)
        for n in range(len(L)):
            if n + 1 < len(L):
                qk(n + 1)
            ex(n)
            pv(n)
            if n + 1 == len(L) or L[n + 1][0] != L[n][0]:
                epi(L[n][0])
            yield

    def drive(main, side, side_len):
        steps = list(range(0))
        mains = main
        if side is None:
            for _ in mains:
                pass
            return
        done = [False]

        def adv(k):
            for _ in range(k):
                if done[0]:
                    return
                try:
                    next(side)
                except StopIteration:
                    done[0] = True
        nmain = side_len[0]
        per = max(1, -(-side_len[1] // max(1, nmain)))
        for _ in mains:
            adv(per)
        while not done[0]:
            adv(8)

    def side_gen(blk, s):
        d = Deferred()
        S.tgt = d
        kv_path(blk, s)
        S.tgt = realS
        for kind, a_, k_ in d.q:
            getattr(realS, kind)(*a_, **k_)
            yield
        for _ in q_path(blk, s):
            yield

    for blk in range(nblk):
        t0 = blk * NB
        for k in range(8):
            S.dma("sync", hT[:, k, :], hin[k, :, t0:t0 + NB], writes=[RhT[k]])
        arena_fence(True)
        pb, Rpb = nextbank()
        ple.emit(blk, hT, RhT, hn, Rhn, pb, Rpb)
        pb, Rpb = nextbank()
        P.fm_norm(hT, RhT, g_kv, Rg_kv, hkv, Rhkv, pb, Rpb)
        P.fm_norm(hT, RhT, g_mix, Rg_mix, hn, Rhn, pb, Rpb, reuse_rstd=True)
        kv_path(blk, 0)
        arena_fence(False)
        for _ in q_path(blk, 0):
            pass
        for s in range(4):
            it = blk * 4 + s
            nsteps = 4 * (-(-(it + 1) // G))
            side = side_gen(blk, s + 1) if s < 3 else None
            drive(attention(blk, s), side, (nsteps, 120))
        for d in range(8):
            pb, Rpb = nextbank()
            for pr in range(8):
                S.op("tensor", lambda e, pr=pr, d=d, pb=pb: e.matmul(pb[:], lhsT=wo[:, pr, d * 128:(d + 1) * 128], rhs=OTb[:, pr, :], start=(pr == 0), stop=(pr == 7)),
                     reads=[Rwo[pr]] + ROTb, writes=[Rpb], join=(pr > 0))
            S.op("vector", lambda e, d=d, pb=pb: e.tensor_tensor(out=hT[:, d, :], in0=pb[:], in1=hT[:, d, :], op=ALU.add), reads=[Rpb, RhT[d]], writes=[RhT[d]])
            S.dma("sync", hout[d, :, t0:t0 + NB], hT[:, d, :], reads=[RhT[d]], writes=[P.Rdram], join=True)
    P.S = realS
    P.finish()


WSHAPES = {
    "norm_mix0": [1024], "norm_mix1": [1024], "a_w_in": [1024, 3088], "a_b_gate": [16], "a_mh_gain": [128], "a_w_out": [1024, 1024],
    "kv_norm": [1024], "w_kv": [1024, 512], "k_norm": [64], "b_w_q": [1024, 1024], "b_q_norm": [64], "b_w_o": [1024, 1024],
    "norm_ffn0": [1024], "norm_ffn1": [1024], "w_gate_up0": [1024, 5632], "w_gate_up1": [1024, 5632], "w_down0": [2816, 1024], "w_down1": [2816, 1024],
    "norm_ple0": [1024], "norm_ple1": [1024], "w_ple_gate0": [1024, 1024], "w_ple_gate1": [1024, 1024], "w_ple_up0": [256, 1024], "w_ple_up1": [256, 1024],
    "p0": [NT, 256], "p1": [NT, 256],
}


def build_program():
    nc = bass.Bass("TRN2", target_bir_lowering=False)
    Cn = make_consts()
    C = {k: nc.dram_tensor(k, list(v.shape), F32, kind="ExternalInput").ap() for k, v in Cn.items()}
    x = nc.dram_tensor("x", [NT, 1024], F32, kind="ExternalInput").ap()
    out = nc.dram_tensor("out", [NT, 1024], F32, kind="ExternalOutput").ap()
    W = {k: nc.dram_tensor(k, v, F32, kind="ExternalInput").ap() for k, v in WSHAPES.items()}
    hs = [nc.dram_tensor("h_scr%d" % i, [8, 128, NT], F32, kind="Internal").ap() for i in range(4)]
    phase_mlstm(nc, C, x, hs[0], W, "ml")
    phase_ffn(nc, C, hs[0], hs[1], W["w_gate_up0"], W["w_down0"], W["norm_ffn0"], "f0")
    phase_moba(nc, C, hs[1], hs[2], W, "mb")
    phase_ffn(nc, C, hs[2], hs[3], W["w_gate_up1"], W["w_down1"], W["norm_ffn1"], "f1")
    phase_ple_out(nc, C, hs[3], out, W["p1"], W["norm_ple1"], W["w_ple_gate1"], W["w_ple_up1"], "po")
    return nc, Cn


def make_in_maps(inputs, cores):
    f = lambda a: np.ascontiguousarray(np.asarray(a, dtype=np.float32))
    I = {k: np.asarray(v) for k, v in inputs.items()}
    shared = {
        "norm_mix0": f(I["norm_mix"][0]), "norm_mix1": f(I["norm_mix"][1]), "a_w_in": f(I["a_w_in"][0]), "a_b_gate": f(I["a_b_gate"][0]),
        "a_mh_gain": f(I["a_mh_gain"][0]), "a_w_out": f(I["a_w_out"][0]), "kv_norm": f(I["kv_norm"]), "w_kv": f(I["w_kv"]), "k_norm": f(I["k_norm"]),
        "b_w_q": f(I["b_w_q"][0]), "b_q_norm": f(I["b_q_norm"][0]), "b_w_o": f(I["b_w_o"][0]),
        "norm_ffn0": f(I["norm_ffn"][0]), "norm_ffn1": f(I["norm_ffn"][1]), "w_gate_up0": f(I["w_gate_up"][0]), "w_gate_up1": f(I["w_gate_up"][1]),
        "w_down0": f(I["w_down"][0]), "w_down1": f(I["w_down"][1]), "norm_ple0": f(I["norm_ple"][0]), "norm_ple1": f(I["norm_ple"][1]),
        "w_ple_gate0": f(I["w_ple_gate"][0]), "w_ple_gate1": f(I["w_ple_gate"][1]), "w_ple_up0": f(I["w_ple_up"][0]), "w_ple_up1": f(I["w_ple_up"][1]),
    }
    maps = []
    for b in cores:
        m = dict(shared)
        m["x"] = f(I["x"][b])
        m["p0"] = f(I["p"][0, b])
        m["p1"] = f(I["p"][1, b])
        maps.append(m)
    return maps


def kernel(**inputs):
    nc, Cn = build_program()
    maps = make_in_maps(inputs, list(range(8)))
    for m in maps:
        m.update(Cn)
    res = run_bass_kernel_spmd(nc, maps, core_ids=list(range(8)))
    return np.stack([np.asarray(r["out"], dtype=np.float32) for r in res.results], axis=0)
```

```python
import numpy as np
import concourse.bass as bass
import concourse.mybir as mybir
from concourse.bass_utils import run_bass_kernel_spmd
from contextlib import ExitStack

F32 = mybir.dt.float32
BF16 = mybir.dt.bfloat16
ALU = mybir.AluOpType
AF = mybir.ActivationFunctionType
AX = mybir.AxisListType

ENGS = ["tensor", "vector", "scalar", "gpsimd", "sync"]
NDMASEM = 12
SAME_ENGINE_SYNC = True

NT = 4096
NB = 512
NBLK = NT // NB
EPS = 1e-6
NEG = -30000.0


class Res:
    __slots__ = ("name", "w", "r", "gd")

    def __init__(self, name=""):
        self.name = name
        self.w = []
        self.r = []
        self.gd = []


class WRes:
    def __init__(self, grid, cw):
        self.grid = grid
        self.cw = cw

    def sel(self, k, c0, c1):
        return [self.grid[k][ci] for ci in range(c0 // self.cw, (c1 - 1) // self.cw + 1)]


class Op:
    __slots__ = ("eng", "fn", "waits", "pos", "sig", "isdma", "semi", "semk", "K", "sigidx")


class Sched:
    G = {"nc": None}

    @staticmethod
    def setup(nc):
        if Sched.G.get("nc") is nc:
            return
        es = ExitStack()
        G = {"nc": nc, "es": es, "sig": {e: 0 for e in ENGS}, "dma": {e: 0 for e in ENGS}}
        G["esem"] = {e: es.enter_context(nc.semaphore("sem_e_%s" % e)) for e in ENGS}
        G["dsem"] = {(e, i): es.enter_context(nc.semaphore("sem_d_%s_%d" % (e, i))) for e in ("sync", "gpsimd") for i in range(NDMASEM)}
        Sched.G = G

    def __init__(self, nc):
        Sched.setup(nc)
        self.nc = nc
        self.ops = {e: [] for e in ENGS}
        self.Kcur = {e: {} for e in ENGS}
        self.nops = 0

    limit = None

    def _record(self, eng, fn, reads, writes, isdma, join=False):
        if Sched.limit is not None and self.nops >= Sched.limit:
            return None
        o = Op()
        o.eng = eng
        o.fn = fn
        o.isdma = isdma
        o.sig = False
        o.pos = len(self.ops[eng])
        deps = {}
        for r in reads:
            for x in r.w:
                deps[id(x)] = x
        for w in writes:
            if join and not w.r:
                for x in w.gd:
                    deps[id(x)] = x
            else:
                for x in w.w:
                    deps[id(x)] = x
                for x in w.r:
                    deps[id(x)] = x
        K = self.Kcur[eng]
        newK = None
        waits = []
        if isdma:
            i = Sched.G["dma"][eng]
            Sched.G["dma"][eng] += 1
            o.semi = i % NDMASEM
            o.semk = i // NDMASEM + 1
            if o.semk > 1:
                key = ("d", eng, o.semi)
                if K.get(key, 0) < o.semk - 1:
                    waits.append(("dmaslot", eng, o.semi, o.semk - 1))
                    newK = dict(K)
                    newK[key] = o.semk - 1
        best = {}
        dl = []
        for y in deps.values():
            if y is o:
                continue
            if y.isdma:
                dl.append(y)
            elif y.eng not in best or best[y.eng].pos < y.pos:
                best[y.eng] = y
        for y in dl + list(best.values()):
            cur = K if newK is None else newK
            if y.isdma:
                key = ("d", y.eng, y.semi)
                if cur.get(key, 0) >= y.semk:
                    continue
                waits.append(("dma", y))
            else:
                if y.eng == eng and (eng == "tensor" or not SAME_ENGINE_SYNC) and not isdma:
                    continue
                if cur.get(y.eng, -1) >= y.pos:
                    continue
                y.sig = True
                waits.append(("eng", y))
            if newK is None:
                newK = dict(K)
            for k, v in y.K.items():
                if newK.get(k, -1) < v:
                    newK[k] = v
            if y.isdma:
                key = ("d", y.eng, y.semi)
                newK[key] = max(newK.get(key, 0), y.semk)
            else:
                if newK.get(y.eng, -1) < y.pos:
                    newK[y.eng] = y.pos
        if newK is not None:
            self.Kcur[eng] = newK
            K = newK
        o.K = K
        o.waits = waits
        for r in reads:
            r.r.append(o)
        for w in writes:
            if join and not w.r:
                w.w.append(o)
            else:
                w.gd = w.w + w.r
                w.w = [o]
                w.r = []
        self.ops[eng].append(o)
        self.nops += 1
        return o

    def op(self, eng, fn, reads=(), writes=(), join=False):
        return self._record(eng, fn, reads, writes, False, join)

    def dma(self, queue, out, in_, reads=(), writes=(), join=False, nonc=False, **kw):
        nc = self.nc
        if nonc:
            def f(e):
                with nc.allow_non_contiguous_dma(reason="small strided parameter load"):
                    return e.dma_start(out=out, in_=in_, **kw)
        else:
            def f(e):
                return e.dma_start(out=out, in_=in_, **kw)
        return self._record(queue, f, reads, writes, True, join)

    def emit(self):
        nc = self.nc
        G = Sched.G
        for e in ENGS:
            n = G["sig"][e]
            for o in self.ops[e]:
                if o.sig:
                    n += 1
                o.sigidx = n
            G["sig"][e] = n
        esem, dsem = G["esem"], G["dsem"]
        with nc.Block() as block:
            def stream(ename):
                def body(eng):
                    for o in self.ops[ename]:
                        for w in o.waits:
                            if w[0] == "dmaslot":
                                eng.wait_ge(dsem[(w[1], w[2])], 16 * w[3])
                            elif w[0] == "dma":
                                y = w[1]
                                eng.wait_ge(dsem[(y.eng, y.semi)], 16 * y.semk)
                            else:
                                y = w[1]
                                eng.wait_ge(esem[y.eng], y.sigidx)
                        ins = o.fn(eng)
                        if o.isdma:
                            ins.then_inc(dsem[(o.eng, o.semi)], 16)
                        elif o.sig:
                            ins.then_inc(esem[o.eng], 1)
                return body

            for e in ENGS:
                if self.ops[e]:
                    getattr(block, e)(stream(e))


class Proxy:
    def __init__(self, real):
        self.real = real
        self.tgt = real

    def op(self, *a, **k):
        return self.tgt.op(*a, **k)

    def dma(self, *a, **k):
        return self.tgt.dma(*a, **k)


class Mux:
    def __init__(self, real, chunks):
        self.real = real
        self.chunks = chunks
        self.last = None

    def _maybe(self, eng):
        if self.last == "tensor" and eng != "tensor" and self.chunks:
            for kind, a, k in self.chunks.pop(0):
                getattr(self.real, kind)(*a, **k)
        self.last = eng

    def op(self, eng, *a, **k):
        self._maybe(eng)
        return self.real.op(eng, *a, **k)

    def dma(self, eng, *a, **k):
        self._maybe(eng)
        return self.real.dma(eng, *a, **k)

    def flush(self):
        while self.chunks:
            for kind, a, k in self.chunks.pop(0):
                getattr(self.real, kind)(*a, **k)


def mm_chunks(q):
    runs = []
    for item in q:
        is_mm = (item[0] == "op" and item[1][0] == "tensor")
        if runs and runs[-1][0] == is_mm:
            runs[-1][1].append(item)
        else:
            runs.append((is_mm, [item]))
    chunks = []
    cur = []
    for is_mm, items in runs:
        cur.extend(items)
        if is_mm:
            chunks.append(cur)
            cur = []
    if cur:
        chunks.append(cur)
    return chunks


class Deferred:
    def __init__(self):
        self.q = []

    def op(self, *a, **k):
        self.q.append(("op", a, k))

    def dma(self, *a, **k):
        self.q.append(("dma", a, k))


def interleave(main_gen, real, q):
    steps = list(main_gen) if False else None
    i = 0
    n = getattr(main_gen, "nsteps", None)
    return i


class Phase:
    def __init__(self, nc, name):
        self.nc = nc
        self.name = name
        self.es = ExitStack()
        self.S = Sched(nc)
        self.n = 0
        self.Rdram = Res("dram_out")

    def sb(self, shape, dt, name=None):
        self.n += 1
        t = self.es.enter_context(self.nc.sbuf_tensor("%s_s%d" % (self.name, self.n), list(shape), dt))
        return t

    def ps(self, shape, dt, name=None):
        self.n += 1
        t = self.es.enter_context(self.nc.psum_tensor("%s_p%d" % (self.name, self.n), list(shape), dt))
        return t

    def finish(self):
        S = self.S
        S.op("sync", lambda e: e.nop(), reads=[self.Rdram])
        S.emit()
        self.es.close()

    def consts(self, C):
        S = self.S
        self.ident_f = self.sb([128, 128], F32)
        self.ident_b = self.sb([128, 128], BF16)
        self.ones_b = self.sb([128, 128], BF16)
        self.eps_t = self.sb([128, 1], F32)
        self.one_t = self.sb([128, 1], F32)
        self.Rc = Res("consts")
        S.dma("sync", self.ident_f[:], C["c_ident"], writes=[self.Rc], join=True)
        S.op("vector", lambda e: e.tensor_copy(out=self.ident_b[:], in_=self.ident_f[:]), reads=[self.Rc], writes=[self.Rc])
        S.op("vector", lambda e: e.memset(self.ones_b[:], 1.0), writes=[self.Rc], join=True)
        S.op("vector", lambda e: e.memset(self.eps_t[:], EPS), writes=[self.Rc], join=True)
        S.op("vector", lambda e: e.memset(self.one_t[:], 1.0), writes=[self.Rc], join=True)

    def load_vec_fm(self, ap1024, nk=8):
        t = self.sb([128, nk], F32)
        r = Res()
        self.S.dma("sync", t[:], ap1024.rearrange("(k p) -> p k", p=128), writes=[r], nonc=True)
        return t, r

    def load_bcast(self, ap_flat, n):
        t = self.sb([128, n], F32)
        r = Res()
        self.S.dma("sync", t[:], ap_flat.partition_broadcast(128), writes=[r])
        return t, r

    def load_w(self, src, K, N, rows=128, col_chunk=2048, queue="gpsimd", chunk_major=False):
        t = self.sb([rows, K, N], BF16)
        nch = -(-N // col_chunk)
        cw = -(-N // nch)
        if not chunk_major:
            rs = [Res() for _ in range(K)]
            for k in range(K):
                c0 = 0
                while c0 < N:
                    c1 = min(N, c0 + cw)
                    self.S.dma(queue, t[:, k, c0:c1], src[k * rows:(k + 1) * rows, c0:c1], writes=[rs[k]], join=True)
                    c0 = c1
            return t, rs
        grid = [[Res() for _ in range(nch)] for _ in range(K)]
        order = list(range(nch))
        if nch == 4:
            order = [0, 2, 1, 3]
        for ci in order:
            c0, c1 = ci * cw, min(N, (ci + 1) * cw)
            for k in range(K):
                self.S.dma(queue, t[:, k, c0:c1], src[k * rows:(k + 1) * rows, c0:c1], writes=[grid[k][ci]])
        return t, WRes(grid, cw)

    def norm_setup(self):
        self.rstd = self.sb([128, NB], F32)
        self.Rrstd = Res()
        self.lnv = self.rstd
        self.Rlnv = self.Rrstd

    def fm_norm(self, hT, RhT, g, Rg, hn, Rhn, pss, Rpss, reuse_rstd=False):
        S = self.S
        n = hT.shape[2]
        sq, Rsq = hn, Rhn
        for k in range(8 if not reuse_rstd else 0):
            S.op("scalar", lambda e, k=k: e.activation(out=sq[:, k, :], in_=hT[:, k, :], func=AF.Square),
                 reads=[RhT[k]], writes=[Rsq[k]])
        for k in range(8 if not reuse_rstd else 0):
            S.op("tensor", lambda e, k=k: e.matmul(pss[:, 0:n], lhsT=self.ones_b[:], rhs=sq[:, k, :], start=(k == 0), stop=(k == 7)),
                 reads=[Rsq[k], self.Rc], writes=[Rpss], join=(k > 0))
        if not reuse_rstd:
            S.op("scalar", lambda e: e.activation(out=self.lnv[:, 0:n], in_=pss[:, 0:n], func=AF.Ln, scale=1.0 / 1024.0, bias=self.eps_t[:, 0:1]),
                 reads=[Rpss, self.Rc], writes=[self.Rlnv])
            S.op("scalar", lambda e: e.activation(out=self.rstd[:, 0:n], in_=self.lnv[:, 0:n], func=AF.Exp, scale=-0.5),
                 reads=[self.Rlnv], writes=[self.Rrstd])
        for k in range(8):
            S.op("vector", lambda e, k=k: e.scalar_tensor_tensor(out=hn[:, k, :], in0=hT[:, k, :], scalar=g[:, k:k + 1], op0=ALU.mult,
                                                                 in1=self.rstd[:, 0:n], op1=ALU.mult),
                 reads=[RhT[k], self.Rrstd, Rg], writes=[Rhn[k]])


def phase_ffn(nc, C, hin, hout, wgu_ap, wd_ap, g_ap, name):
    P = Phase(nc, name)
    S = P.S
    P.consts(C)
    g, Rg = P.load_vec_fm(g_ap)
    wgu, Rwgu = P.load_w(wgu_ap, 8, 5632, col_chunk=1408, chunk_major=True)
    wd, Rwd = P.load_w(wd_ap, 22, 1024)
    P.norm_setup()
    hTs = [P.sb([128, 8, NB], F32) for _ in range(2)]
    RhTs = [[Res() for _ in range(8)] for _ in range(2)]
    hns = [P.sb([128, 8, NB], BF16) for _ in range(2)]
    Rhns = [[Res() for _ in range(8)] for _ in range(2)]
    act = P.sb([128, 22, NB], BF16)
    Ract = [Res() for _ in range(22)]
    sg = [P.sb([128, NB], BF16) for _ in range(2)]
    Rsg = [Res() for _ in range(2)]
    pss = P.ps([128, NB], F32)
    Rpss = Res()
    pg = [P.ps([128, NB], F32) for _ in range(2)]
    Rpg = [Res() for _ in range(2)]
    pu = [P.ps([128, NB], F32) for _ in range(2)]
    Rpu = [Res() for _ in range(2)]
    po = [P.ps([128, NB], F32) for _ in range(2)]
    Rpo = [Res() for _ in range(2)]

    def load(blk):
        t0 = blk * NB
        for k in range(8):
            S.dma("sync", hTs[blk % 2][:, k, :], hin[k, :, t0:t0 + NB], writes=[RhTs[blk % 2][k]])

    def norm(blk):
        P.fm_norm(hTs[blk % 2], RhTs[blk % 2], g, Rg, hns[blk % 2], Rhns[blk % 2], pss, Rpss)

    load(0)
    norm(0)
    for blk in range(NBLK):
        t0 = blk * NB
        hT, RhT = hTs[blk % 2], RhTs[blk % 2]
        hn, Rhn = hns[blk % 2], Rhns[blk % 2]
        if blk + 1 < NBLK:
            load(blk + 1)
        for c in range(22):
            b = c % 2
            for k in range(8):
                S.op("tensor", lambda e, k=k, c=c, b=b, hn=hn: e.matmul(pg[b][:], lhsT=wgu[:, k, c * 128:(c + 1) * 128], rhs=hn[:, k, :],
                                                                        start=(k == 0), stop=(k == 7)),
                     reads=Rwgu.sel(k, c * 128, (c + 1) * 128) + [Rhn[k]], writes=[Rpg[b]], join=(k > 0))
            for k in range(8):
                S.op("tensor", lambda e, k=k, c=c, b=b, hn=hn: e.matmul(pu[b][:], lhsT=wgu[:, k, 2816 + c * 128:2816 + (c + 1) * 128], rhs=hn[:, k, :],
                                                                        start=(k == 0), stop=(k == 7)),
                     reads=Rwgu.sel(k, 2816 + c * 128, 2816 + (c + 1) * 128) + [Rhn[k]], writes=[Rpu[b]], join=(k > 0))
            S.op("scalar", lambda e, b=b: e.activation(out=sg[b][:], in_=pg[b][:], func=AF.Silu), reads=[Rpg[b]], writes=[Rsg[b]])
            S.op("vector", lambda e, b=b, c=c: e.tensor_tensor(out=act[:, c, :], in0=pu[b][:], in1=sg[b][:], op=ALU.mult),
                 reads=[Rpu[b], Rsg[b]], writes=[Ract[c]])
        if blk + 1 < NBLK:
            norm(blk + 1)
        for d in range(8):
            b = d % 2
            for c in range(22):
                S.op("tensor", lambda e, c=c, d=d, b=b: e.matmul(po[b][:], lhsT=wd[:, c, d * 128:(d + 1) * 128], rhs=act[:, c, :],
                                                                  start=(c == 0), stop=(c == 21)),
                     reads=[Rwd[c], Ract[c]], writes=[Rpo[b]], join=(c > 0))
            S.op("vector", lambda e, d=d, b=b, hT=hT: e.tensor_tensor(out=hT[:, d, :], in0=po[b][:], in1=hT[:, d, :], op=ALU.add),
                 reads=[Rpo[b], RhT[d]], writes=[RhT[d]])
            S.dma("sync", hout[d, :, t0:t0 + NB], hT[:, d, :], reads=[RhT[d]], writes=[P.Rdram], join=True)
    P.finish()


def make_consts():
    c = {}
    c["c_ident"] = np.eye(128, dtype=np.float32)
    s = np.arange(128)
    c["c_tri"] = (s[:, None] <= s[None, :]).astype(np.float32)
    c["c_cbias"] = np.where(s[:, None] <= s[None, :], 0.0, NEG).astype(np.float32)
    inv = 500000.0 ** (-np.arange(0, 16, 2, dtype=np.float64) / 16.0)
    ang = np.arange(NT, dtype=np.float64)[:, None] * inv[None, :]
    c["c_rope"] = np.concatenate([np.cos(ang), np.sin(ang)], axis=1).astype(np.float32)
    u = np.arange(NT)
    c["c_onehot"] = (u[None, :] // 256 == np.arange(16)[:, None]).astype(np.float32)
    c["c_tribias4"] = np.tile(c["c_cbias"], (1, 4)).astype(np.float32)
    b = np.arange(16)
    c["c_past"] = np.where(b[None, :] < b[:, None], 0.0, NEG).astype(np.float32).reshape(256)
    c["c_own"] = (b[None, :] == b[:, None]).astype(np.float32).reshape(256)
    return c


class PLE:
    def __init__(self, P, C, p_ap, g_ap, wpg_ap, wpu_ap, banks, nb=NB, nbuf=1, scratch=None):
        self.P = P
        self.nb = nb
        S = P.S
        self.p_ap = p_ap
        self.g, self.Rg = P.load_vec_fm(g_ap)
        self.wpg, self.Rwpg = P.load_w(wpg_ap, 8, 1024)
        self.wpu, self.Rwpu = P.load_w(wpu_ap, 2, 1024)
        self.Rptm = [Res() for _ in range(nbuf)]
        self.RpT = [[Res(), Res()] for _ in range(nbuf)]
        self.nbuf = nbuf
        self.Rsgate = Res()
        self.Rtmp = Res()
        if scratch is None:
            self.ptm = [P.sb([128, nb // 128, 256], F32)[:] for _ in range(nbuf)]
            self.pT = [P.sb([128, 2, nb], BF16)[:] for _ in range(nbuf)]
            self.sgate = P.sb([128, nb], F32)[:]
            self.tmp = P.sb([128, nb], F32)[:]
        else:
            self.ptm, self.pT, self.sgate, self.tmp = [scratch["ptm"]], [scratch["pT"]], scratch["sgate"], scratch["tmp"]
        self.banks = banks

    def all_res(self):
        return [self.Rptm[0], self.RpT[0][0], self.RpT[0][1], self.Rsgate, self.Rtmp]

    def pre(self, blk, hT, RhT, hn, Rhn, pss, Rpss):
        P = self.P
        S = P.S
        nb = self.nb
        t0 = blk * nb
        i = blk % self.nbuf
        ptm, Rptm, pT, RpT = self.ptm[i], self.Rptm[i], self.pT[i], self.RpT[i]
        (pc, Rpc) = self.banks[2]
        P.fm_norm(hT, RhT, self.g, self.Rg, hn, Rhn, pss, Rpss)
        S.dma("sync", ptm, self.p_ap[t0:t0 + nb, :].rearrange("(s p) d -> p s d", p=128), writes=[Rptm])
        for kk in range(2):
            for s in range(nb // 128):
                S.op("tensor", lambda e, kk=kk, s=s: e.transpose(out=pc[:, s * 128:(s + 1) * 128], in_=ptm[:, s, kk * 128:(kk + 1) * 128],
                                                                 identity=P.ident_f[:]),
                     reads=[Rptm, P.Rc], writes=[Rpc], join=(s > 0))
            S.op("scalar", lambda e, kk=kk: e.copy(out=pT[:, kk, :], in_=pc[:, 0:nb]), reads=[Rpc], writes=[RpT[kk]])

    def main(self, blk, hT, RhT, hn, Rhn):
        P = self.P
        S = P.S
        nb = self.nb
        i = blk % self.nbuf
        pT, RpT = self.pT[i], self.RpT[i]
        (pa, Rpa), (pb, Rpb) = self.banks[:2]
        for d in range(8):
            for k in range(8):
                S.op("tensor", lambda e, k=k, d=d: e.matmul(pa[:, 0:nb], lhsT=self.wpg[:, k, d * 128:(d + 1) * 128], rhs=hn[:, k, :],
                                                             start=(k == 0), stop=(k == 7)),
                     reads=[self.Rwpg[k], Rhn[k]], writes=[Rpa], join=(k > 0))
            S.op("scalar", lambda e: e.activation(out=self.sgate, in_=pa[:, 0:nb], func=AF.Sigmoid), reads=[Rpa], writes=[self.Rsgate])
            for kk in range(2):
                S.op("tensor", lambda e, kk=kk, d=d: e.matmul(pb[:, 0:nb], lhsT=self.wpu[:, kk, d * 128:(d + 1) * 128], rhs=pT[:, kk, :],
                                                               start=(kk == 0), stop=(kk == 1)),
                     reads=[self.Rwpu[kk], RpT[kk]], writes=[Rpb], join=(kk > 0))
            S.op("vector", lambda e: e.tensor_tensor(out=self.tmp, in0=pb[:, 0:nb], in1=self.sgate, op=ALU.mult),
                 reads=[Rpb, self.Rsgate], writes=[self.Rtmp])
            S.op("gpsimd", lambda e, d=d: e.tensor_tensor(out=hT[:, d, :], in0=hT[:, d, :], in1=self.tmp, op=ALU.add),
                 reads=[self.Rtmp, RhT[d]], writes=[RhT[d]])

    def emit(self, blk, hT, RhT, hn, Rhn, pss, Rpss):
        self.pre(blk, hT, RhT, hn, Rhn, pss, Rpss)
        self.main(blk, hT, RhT, hn, Rhn)


def phase_ple_out(nc, C, hin, out_ap, p_ap, g_ap, wpg_ap, wpu_ap, name):
    P = Phase(nc, name)
    S = P.S
    P.consts(C)
    P.norm_setup()
    banks = [(P.ps([128, NB], F32), Res()) for _ in range(5)]
    pss, Rpss = P.ps([128, NB], F32), Res()
    ple = PLE(P, C, p_ap, g_ap, wpg_ap, wpu_ap, banks, nbuf=2)
    hTs = [P.sb([128, 8, NB], F32) for _ in range(2)]
    RhTs = [[Res() for _ in range(8)] for _ in range(2)]
    hns = [P.sb([128, 8, NB], BF16) for _ in range(2)]
    Rhns = [[Res() for _ in range(8)] for _ in range(2)]
    otm = P.sb([128, 4, 1024], F32)
    Rotm = [Res() for _ in range(4)]

    def load(blk):
        t0 = blk * NB
        for k in range(8):
            S.dma("sync", hTs[blk % 2][:, k, :], hin[k, :, t0:t0 + NB], writes=[RhTs[blk % 2][k]])

    load(0)
    ple.pre(0, hTs[0], RhTs[0], hns[0], Rhns[0], pss, Rpss)
    for blk in range(NBLK):
        t0 = blk * NB
        hT, RhT = hTs[blk % 2], RhTs[blk % 2]
        if blk + 1 < NBLK:
            load(blk + 1)
        ple.main(blk, hT, RhT, hns[blk % 2], Rhns[blk % 2])
        if blk + 1 < NBLK:
            n = (blk + 1) % 2
            ple.pre(blk + 1, hTs[n], RhTs[n], hns[n], Rhns[n], pss, Rpss)
        for s in range(4):
            for kq in range(2):
                pt, Rpt = banks[3 + kq]
                for k4 in range(4):
                    k = kq * 4 + k4
                    S.op("tensor", lambda e, k=k, k4=k4, s=s, pt=pt, hT=hT: e.transpose(out=pt[:, k4 * 128:(k4 + 1) * 128], in_=hT[:, k, s * 128:(s + 1) * 128],
                                                                                        identity=P.ident_f[:]),
                         reads=[RhT[k], P.Rc], writes=[Rpt], join=(k4 > 0))
                if kq == 0:
                    S.op("scalar", lambda e, s=s, kq=kq, pt=pt: e.copy(out=otm[:, s, kq * 512:(kq + 1) * 512], in_=pt[:]),
                         reads=[Rpt], writes=[Rotm[s]], join=(kq > 0))
                else:
                    S.op("vector", lambda e, s=s, kq=kq, pt=pt: e.tensor_copy(out=otm[:, s, kq * 512:(kq + 1) * 512], in_=pt[:]),
                         reads=[Rpt], writes=[Rotm[s]], join=(kq > 0))
            S.dma("sync", out_ap[t0 + s * 128:t0 + (s + 1) * 128, :], otm[:, s, :], reads=[Rotm[s]], writes=[P.Rdram], join=True)
    P.finish()


def phase_mlstm(nc, C, x_ap, hout, W, name, nblk=NBLK):
    P = Phase(nc, name)
    realS = P.S
    S = Proxy(realS)
    P.S = S
    P.consts(C)
    P.norm_setup()
    tri_f = P.sb([128, 128], F32)
    cbias = P.sb([128, 128], F32)
    ones_f = P.sb([128, 128], F32)
    S.dma("sync", tri_f[:], C["c_tri"], writes=[P.Rc], join=True)
    S.dma("sync", cbias[:], C["c_cbias"], writes=[P.Rc], join=True)
    S.op("vector", lambda e: e.memset(ones_f[:], 1.0), writes=[P.Rc], join=True)
    g, Rg = P.load_vec_fm(W["norm_mix0"])
    bgate, Rbgate = P.load_bcast(W["a_b_gate"], 16)
    mhg, Rmhg = P.load_bcast(W["a_mh_gain"], 128)
    mhg_h = P.sb([128, 128], F32)
    S.op("vector", lambda e: e.tensor_scalar(out=mhg_h[:], in0=mhg[:], scalar1=0.5, scalar2=None, op0=ALU.mult), reads=[Rmhg], writes=[Rmhg])
    nhalf = P.sb([128, 8], F32)
    S.op("vector", lambda e: e.memset(nhalf[:], -0.5), writes=[P.Rc], join=True)
    win, Rwin = P.load_w(W["a_w_in"], 8, 3088, col_chunk=1544)
    wout, Rwout = P.load_w(W["a_w_out"], 8, 1024)

    xtm = P.sb([128, 4, 1024], F32); Rxtm = [Res() for _ in range(4)]
    hT = P.sb([128, 8, NB], F32); RhT = [Res() for _ in range(8)]
    hn = P.sb([128, 8, NB], BF16); Rhn = [Res() for _ in range(8)]
    qkT = P.sb([128, 8, NB], BF16); Rqk = [Res() for _ in range(8)]
    ktm = P.sb([128, 4, 512], BF16); Rktm = [Res() for _ in range(4)]
    vtm = P.sb([128, 4, 1024], BF16); Rvtm = [Res() for _ in range(4)]
    og = P.sb([128, 4, 1024], BF16); Rog = [Res() for _ in range(4)]
    sgt = P.sb([128, 512], F32); Rsgt = Res()
    gsb = P.sb([128, 4, 16], F32); Rgsb = Res()
    th = P.sb([128, 4, 16], F32); Rth = Res()
    ef = P.sb([128, 4, 8], F32); Ref = Res()
    spf = P.sb([128, 4, 8], F32); Rspf = Res()
    li = P.sb([128, 4, 8], F32); Rli = Res()
    lf = P.sb([128, 4, 8], F32); Rlf = Res()
    g_sb = P.sb([128, 8], F32); Rg_sb = Res()
    bb = P.sb([128, 8], F32); Rbb = Res()
    eg = P.sb([128, 8], F32); Reg = Res()
    wlp = P.sb([128, 8], F32); Rwlp = Res()
    wl = P.sb([128, 8], F32); Rwl = Res()
    egl = P.sb([128, 4], F32); Regl = Res()
    Gd = P.sb([128, 8, 128], F32); RGd = Res()
    arg = P.sb([128, 8, 128], F32); Rarg = Res()
    DT = P.sb([128, 8, 128], F32); RDT = Res()
    PT = P.sb([128, 8, 128], BF16); RPT = Res()
    kw = P.sb([128, 8, 64], BF16); Rkw = Res()
    numXs = P.sb([128, 8, 128], F32); RnumXs = Res()
    num = P.sb([128, 8, 128], F32); Rnum = Res()
    sqn = P.sb([128, 8, 128], F32); Rsqn = Res()
    sm = {n: (P.sb([128, 8], F32), Res()) for n in ["dxs", "den", "dd", "rec", "ssn", "t1", "t2", "lnt", "rs", "coef"]}
    y0 = P.sb([128, 8, 128], F32); Ry0 = Res()
    ytm = P.sb([128, 1024], BF16); Rytm = Res()
    yT = P.sb([128, 8, NB], BF16); RyT = [Res() for _ in range(4)]
    Cst = P.sb([128, 4, 128], F32); nst = P.sb([128, 4], F32); RC = Res()
    Cbf = P.sb([128, 4, 2, 128], BF16); nbf = P.sb([128, 4, 2], BF16); RCbf = Res()
    qbd = P.sb([128, 4, 2, NB], BF16); Rqbd = [Res() for _ in range(4)]
    nt1 = P.sb([128, 4], F32); Rnt1 = Res()

    pS = P.ps([128, 512], F32)
    RpS = Res()
    Rgcs = Rglast = RdenI = RdenX = Rdn = Rpgate = RpS
    pR = [P.ps([128, 512], F32) for _ in range(2)]; RpR = [Res(), Res()]
    pG = P.ps([128, 1024], F32); RpG = Res()
    pT2 = P.ps([128, 1024], F32); RpT2 = Res()
    pY = P.ps([128, 1024], BF16); RpY = Res()
    rot = [0]

    def nextbank():
        rot[0] ^= 1
        return pR[rot[0]], RpR[rot[0]]

    for t in (Cst, nst):
        S.op("vector", lambda e, t=t: e.memset(t[:], 0.0), writes=[RC], join=True)
    for t in (Cbf, nbf):
        S.op("vector", lambda e, t=t: e.memset(t[:], 0.0), writes=[RCbf], join=True)
    for c in range(4):
        S.op("gpsimd", lambda e, c=c: e.memset(qbd[:, c, :, :], 0.0), writes=[Rqbd[c]])

    for blk in range(nblk):
        t0 = blk * NB
        for s in range(4):
            S.dma("sync", xtm[:, s, :], x_ap[t0 + s * 128:t0 + (s + 1) * 128, :], writes=[Rxtm[s]])
        for k in range(8):
            pb, Rpb = nextbank()
            for s in range(4):
                S.op("tensor", lambda e, k=k, s=s, pb=pb: e.transpose(out=pb[:, s * 128:(s + 1) * 128], in_=xtm[:, s, k * 128:(k + 1) * 128],
                                                                       identity=P.ident_f[:]),
                     reads=[Rxtm[s], P.Rc], writes=[Rpb], join=(s > 0))
            S.op("scalar", lambda e, k=k, pb=pb: e.copy(out=hT[:, k, :], in_=pb[:]), reads=[Rpb], writes=[RhT[k]])
        pb, Rpb = nextbank()
        P.fm_norm(hT, RhT, g, Rg, hn, Rhn, pb, Rpb)
        for c in range(8):
            pb, Rpb = nextbank()
            for k in range(8):
                S.op("tensor", lambda e, k=k, c=c, pb=pb: e.matmul(pb[:], lhsT=win[:, k, c * 128:(c + 1) * 128], rhs=hn[:, k, :],
                                                                    start=(k == 0), stop=(k == 7)),
                     reads=[Rwin[k], Rhn[k]], writes=[Rpb], join=(k > 0))
            sc = 0.125 if c < 4 else 1.0
            S.op("scalar", lambda e, c=c, pb=pb, sc=sc: e.activation(out=qkT[:, c, :], in_=pb[:], func=AF.Copy, scale=sc),
                 reads=[Rpb], writes=[Rqk[c]])
            if c < 4:
                S.op("gpsimd", lambda e, c=c: e.tensor_copy(out=qbd[0:64, c, 0, :], in_=qkT[0:64, c, :]), reads=[Rqk[c]], writes=[Rqbd[c]])
                S.op("gpsimd", lambda e, c=c: e.tensor_copy(out=qbd[64:128, c, 1, :], in_=qkT[64:128, c, :]), reads=[Rqk[c]], writes=[Rqbd[c]], join=True)
        def proj_kvo(s):
            ts = slice(s * 128, (s + 1) * 128)
            pb, Rpb = nextbank()
            for k in range(8):
                S.op("tensor", lambda e, k=k, ts=ts, pb=pb: e.matmul(pb[:], lhsT=hn[:, k, ts], rhs=win[:, k, 512:1024], start=(k == 0), stop=(k == 7)),
                     reads=[Rwin[k], Rhn[k]], writes=[Rpb], join=(k > 0))
            S.op("scalar", lambda e, s=s, pb=pb: e.copy(out=ktm[:, s, :], in_=pb[:]), reads=[Rpb], writes=[Rktm[s]])
            for half in range(2):
                pb, Rpb = nextbank()
                c0 = 1024 + half * 512
                for k in range(8):
                    S.op("tensor", lambda e, k=k, ts=ts, pb=pb, c0=c0: e.matmul(pb[:], lhsT=hn[:, k, ts], rhs=win[:, k, c0:c0 + 512],
                                                                                 start=(k == 0), stop=(k == 7)),
                         reads=[Rwin[k], Rhn[k]], writes=[Rpb], join=(k > 0))
                S.op("vector", lambda e, s=s, half=half, pb=pb: e.tensor_copy(out=vtm[:, s, half * 512:(half + 1) * 512], in_=pb[:]),
                     reads=[Rpb], writes=[Rvtm[s]], join=(half > 0))
            for half in range(2):
                pb, Rpb = nextbank()
                c0 = 2048 + half * 512
                for k in range(8):
                    S.op("tensor", lambda e, k=k, ts=ts, pb=pb, c0=c0: e.matmul(pb[:], lhsT=hn[:, k, ts], rhs=win[:, k, c0:c0 + 512],
                                                                                 start=(k == 0), stop=(k == 7)),
                         reads=[Rwin[k], Rhn[k]], writes=[Rpb], join=(k > 0))
                S.op("scalar", lambda e, pb=pb: e.activation(out=sgt[:], in_=pb[:], func=AF.Tanh, scale=0.5), reads=[Rpb], writes=[Rsgt])
                S.op("gpsimd", lambda e: e.tensor_tensor(
                    out=sgt[:].rearrange("p (h v) -> p h v", h=4), in0=sgt[:].rearrange("p (h v) -> p h v", h=4),
                    in1=mhg_h[:].unsqueeze(1).broadcast_to([128, 4, 128]), op=ALU.mult), reads=[Rsgt, Rmhg], writes=[Rsgt])
                S.op("gpsimd", lambda e, s=s, half=half: e.tensor_tensor(
                    out=og[:, s, half * 512:(half + 1) * 512].rearrange("p (h v) -> p h v", h=4),
                    in0=sgt[:].rearrange("p (h v) -> p h v", h=4),
                    in1=mhg_h[:].unsqueeze(1).broadcast_to([128, 4, 128]), op=ALU.add),
                     reads=[Rsgt, Rmhg], writes=[Rog[s]], join=(half > 0))

        for s in range(4):
            ts = slice(s * 128, (s + 1) * 128)
            for k in range(8):
                S.op("tensor", lambda e, k=k, ts=ts, s=s: e.matmul(pS[:, 64 + s * 16:64 + (s + 1) * 16], lhsT=hn[:, k, ts], rhs=win[:, k, 3072:3088],
                                                                    start=(k == 0), stop=(k == 7)),
                     reads=[Rwin[k], Rhn[k]], writes=[Rpgate], join=(k > 0))
            S.op("vector", lambda e, s=s: e.tensor_tensor(out=gsb[:, s, :], in0=pS[:, 64 + s * 16:64 + (s + 1) * 16], in1=bgate[:], op=ALU.add),
                 reads=[Rpgate, Rbgate], writes=[Rgsb], join=(s > 0))
        S.op("scalar", lambda e: e.activation(out=th[:], in_=gsb[:], func=AF.Tanh, scale=1.0 / 15.0), reads=[Rgsb], writes=[Rth])
        S.op("vector", lambda e: e.tensor_scalar(out=li[:], in0=th[:, :, 0:8], scalar1=15.0, scalar2=None, op0=ALU.mult), reads=[Rth], writes=[Rli])
        S.op("scalar", lambda e: e.activation(out=ef[:], in_=th[:, :, 8:16], func=AF.Exp, scale=-15.0), reads=[Rth], writes=[Ref])
        S.op("scalar", lambda e: e.activation(out=spf[:], in_=ef[:], func=AF.Ln, bias=P.one_t[:, 0:1]), reads=[Ref, P.Rc], writes=[Rspf])
        S.op("vector", lambda e: e.tensor_scalar(out=lf[:], in0=spf[:], scalar1=-1.0, scalar2=None, op0=ALU.mult), reads=[Rspf], writes=[Rlf])

        proj_kvo(0)
        for s in range(4):
            ts = slice(s * 128, (s + 1) * 128)
            if s < 3:
                d_ = Deferred()
                S.tgt = d_
                proj_kvo(s + 1)
                S.tgt = Mux(realS, mm_chunks(d_.q))
            else:
                S.tgt = realS
            S.op("tensor", lambda e, s=s: e.matmul(pS[:, 0:8], lhsT=tri_f[:], rhs=lf[:, s, :], start=True, stop=True),
                 reads=[Rlf, P.Rc], writes=[Rgcs])
            S.op("tensor", lambda e, s=s: e.matmul(pS[:, 8:16], lhsT=ones_f[:], rhs=lf[:, s, :], start=True, stop=True),
                 reads=[Rlf, P.Rc], writes=[Rglast])
            S.op("vector", lambda e: e.tensor_copy(out=g_sb[:], in_=pS[:, 0:8]), reads=[Rgcs], writes=[Rg_sb])
            S.op("vector", lambda e, s=s: e.tensor_tensor(out=bb[:], in0=li[:, s, :], in1=g_sb[:], op=ALU.subtract), reads=[Rli, Rg_sb], writes=[Rbb])
            S.op("scalar", lambda e: e.activation(out=eg[:], in_=g_sb[:], func=AF.Exp), reads=[Rg_sb], writes=[Reg])
            S.op("vector", lambda e: e.tensor_tensor(out=wlp[:], in0=pS[:, 8:16], in1=bb[:], op=ALU.add), reads=[Rglast, Rbb], writes=[Rwlp])
            S.op("scalar", lambda e: e.activation(out=wl[:], in_=wlp[:], func=AF.Exp), reads=[Rwlp], writes=[Rwl])
            S.op("scalar", lambda e: e.activation(out=egl[0:64, :], in_=pS[0:64, 8:16:2], func=AF.Exp), reads=[Rglast], writes=[Regl])
            S.op("scalar", lambda e: e.activation(out=egl[64:128, :], in_=pS[64:128, 9:16:2], func=AF.Exp), reads=[Rglast], writes=[Regl], join=True)
            S.op("vector", lambda e: e.tensor_tensor(out=Gd[:], in0=g_sb[:].unsqueeze(2).broadcast_to([128, 8, 128]),
                                                      in1=P.ident_f[:].unsqueeze(1).broadcast_to([128, 8, 128]), op=ALU.mult),
                 reads=[Rg_sb, P.Rc], writes=[RGd])
            for half in range(2):
                S.op("tensor", lambda e, half=half: e.matmul(pG[:, half * 512:(half + 1) * 512], lhsT=ones_f[:],
                                                               rhs=Gd[:, half * 4:(half + 1) * 4, :].rearrange("p h j -> p (h j)"),
                                                               start=True, stop=True),
                     reads=[RGd, P.Rc], writes=[RpG], join=(half > 0))
            for h in range(8):
                S.op("vector", lambda e, h=h: e.scalar_tensor_tensor(out=arg[:, h, :], in0=pG[:, h * 128:(h + 1) * 128], scalar=bb[:, h:h + 1], op0=ALU.add,
                                                                      in1=cbias[:], op1=ALU.add),
                     reads=[RpG, Rbb, P.Rc], writes=[Rarg], join=(h > 0))
            S.op("scalar", lambda e: e.activation(out=DT[:], in_=arg[:], func=AF.Exp), reads=[Rarg], writes=[RDT])
            for c in range(4):
                S.op("tensor", lambda e, c=c, ts=ts: e.matmul(pT2[:, c * 256:(c + 1) * 256], lhsT=qkT[:, 4 + c, ts], rhs=qbd[:, c, :, ts],
                                                               start=True, stop=True),
                     reads=[Rqbd[c], Rqk[4 + c]], writes=[RpT2], join=(c > 0))
            S.op("vector", lambda e: e.tensor_tensor(out=PT[:].rearrange("p h j -> p (h j)"), in0=pT2[:], in1=DT[:].rearrange("p h j -> p (h j)"), op=ALU.mult),
                 reads=[RpT2, RDT], writes=[RPT])
            for h in range(8):
                S.op("tensor", lambda e, h=h, s=s: e.matmul(pG[:, h * 128:(h + 1) * 128], lhsT=PT[:, h, :], rhs=vtm[:, s, h * 128:(h + 1) * 128],
                                                             start=True, stop=True),
                     reads=[RPT, Rvtm[s]], writes=[RpG], join=(h > 0))
            for h in range(8):
                S.op("tensor", lambda e, h=h: e.matmul(pS[:, 16 + h:17 + h], lhsT=PT[:, h, :], rhs=P.ones_b[:, 0:1], start=True, stop=True),
                     reads=[RPT, P.Rc], writes=[RdenI], join=(h > 0))
            for c in range(4):
                S.op("tensor", lambda e, c=c, ts=ts: e.matmul(pT2[:, c * 256:(c + 1) * 256], lhsT=qkT[:, c, ts], rhs=Cbf[:, c, :, :],
                                                               start=True, stop=True),
                     reads=[Rqk[c], RCbf], writes=[RpT2], join=(c > 0))
            for c in range(4):
                S.op("tensor", lambda e, c=c, ts=ts: e.matmul(pS[:, 24 + 2 * c:26 + 2 * c], lhsT=qkT[:, c, ts], rhs=nbf[:, c, :],
                                                               start=True, stop=True),
                     reads=[Rqk[c], RCbf], writes=[RdenX], join=(c > 0))
            S.op("vector", lambda e, s=s: e.tensor_tensor(out=kw[:], in0=ktm[:, s, :].rearrange("p (h d) -> p h d", h=8),
                                                           in1=wl[:].unsqueeze(2).broadcast_to([128, 8, 64]), op=ALU.mult),
                 reads=[Rktm[s], Rwl], writes=[Rkw])
            pd, Rpd = pY[:].bitcast(F32), RpY
            for h in range(8):
                c, ph = h // 2, h % 2
                prt = slice(ph * 64, (ph + 1) * 64)
                S.op("tensor", lambda e, h=h, c=c, prt=prt, s=s, pd=pd: e.matmul(pd[prt, c * 128:(c + 1) * 128], lhsT=kw[:, h, :], rhs=vtm[:, s, h * 128:(h + 1) * 128],
                                                                                  start=True, stop=True),
                     reads=[Rkw, Rvtm[s]], writes=[Rpd], join=(h > 0))
            for h in range(8):
                c, ph = h // 2, h % 2
                prt = slice(ph * 64, (ph + 1) * 64)
                S.op("tensor", lambda e, h=h, c=c, prt=prt: e.matmul(pS[prt, 32 + c:33 + c], lhsT=kw[:, h, :], rhs=P.ones_b[:, 0:1], start=True, stop=True),
                     reads=[Rkw, P.Rc], writes=[Rdn], join=(h > 0))
            for c in range(4):
                S.op("vector", lambda e, c=c, pd=pd: e.scalar_tensor_tensor(out=Cst[:, c, :], in0=Cst[:, c, :], scalar=egl[:, c:c + 1], op0=ALU.mult,
                                                                            in1=pd[:, c * 128:(c + 1) * 128], op1=ALU.add),
                     reads=[Rpd, Regl, RC], writes=[RC])
            S.op("vector", lambda e: e.tensor_tensor(out=nt1[:], in0=nst[:], in1=egl[:], op=ALU.mult), reads=[RC, Regl], writes=[Rnt1])
            S.op("vector", lambda e: e.tensor_tensor(out=nst[:], in0=pS[:, 32:36], in1=nt1[:], op=ALU.add), reads=[Rdn, Rnt1], writes=[RC])
            S.op("vector", lambda e: e.tensor_tensor(out=numXs[:], in0=pT2[:].rearrange("p (h v) -> p h v", h=8),
                                                      in1=eg[:].unsqueeze(2).broadcast_to([128, 8, 128]), op=ALU.mult),
                 reads=[RpT2, Reg], writes=[RnumXs])
            S.op("vector", lambda e: e.tensor_tensor(out=num[:].rearrange("p h v -> p (h v)"), in0=pG[:], in1=numXs[:].rearrange("p h v -> p (h v)"), op=ALU.add),
                 reads=[RpG, RnumXs], writes=[Rnum])
            S.op("gpsimd", lambda e: e.tensor_copy(out=Cbf[0:64, :, 0, :], in_=Cst[0:64, :, :]), reads=[RC], writes=[RCbf])
            S.op("gpsimd", lambda e: e.tensor_copy(out=Cbf[64:128, :, 1, :], in_=Cst[64:128, :, :]), reads=[RC], writes=[RCbf], join=True)
            S.op("gpsimd", lambda e: e.tensor_copy(out=nbf[0:64, :, 0], in_=nst[0:64, :]), reads=[RC], writes=[RCbf], join=True)
            S.op("gpsimd", lambda e: e.tensor_copy(out=nbf[64:128, :, 1], in_=nst[64:128, :]), reads=[RC], writes=[RCbf], join=True)
            T = lambda n: sm[n][0]
            R_ = lambda n: sm[n][1]
            S.op("vector", lambda e: e.tensor_tensor(out=T("dxs")[:], in0=pS[:, 24:32], in1=eg[:], op=ALU.mult), reads=[RdenX, Reg], writes=[R_("dxs")])
            S.op("vector", lambda e: e.tensor_tensor(out=T("den")[:], in0=pS[:, 16:24], in1=T("dxs")[:], op=ALU.add), reads=[RdenI, R_("dxs")], writes=[R_("den")])
            S.op("vector", lambda e: e.scalar_tensor_tensor(out=T("t1")[:], in0=T("den")[:], scalar=-1.0, op0=ALU.mult, in1=T("den")[:], op1=ALU.max),
                 reads=[R_("den")], writes=[R_("t1")])
            S.op("vector", lambda e: e.tensor_scalar(out=T("dd")[:], in0=T("t1")[:], scalar1=1.0, scalar2=None, op0=ALU.max), reads=[R_("t1")], writes=[R_("dd")])
            S.op("vector", lambda e: e.reciprocal(out=T("rec")[:], in_=T("dd")[:]), reads=[R_("dd")], writes=[R_("rec")])
            S.op("gpsimd", lambda e: e.tensor_tensor(out=sqn[:], in0=num[:], in1=num[:], op=ALU.mult), reads=[Rnum], writes=[Rsqn])
            S.op("vector", lambda e: e.tensor_reduce(out=T("ssn")[:], in_=sqn[:], axis=AX.X, op=ALU.add), reads=[Rsqn], writes=[R_("ssn")])
            S.op("vector", lambda e: e.tensor_tensor(out=T("t1")[:], in0=T("rec")[:], in1=T("rec")[:], op=ALU.mult), reads=[R_("rec")], writes=[R_("t1")])
            S.op("vector", lambda e: e.tensor_tensor(out=T("t2")[:], in0=T("t1")[:], in1=T("ssn")[:], op=ALU.mult), reads=[R_("t1"), R_("ssn")], writes=[R_("t2")])
            S.op("gpsimd", lambda e: e.tensor_scalar(out=T("lnt")[:], in0=T("t2")[:], scalar1=1.0 / 128.0, scalar2=EPS, op0=ALU.mult, op1=ALU.add),
                 reads=[R_("t2")], writes=[R_("lnt")])
            S.op("gpsimd", lambda e: e.tensor_tensor(out=T("rs")[:], in0=T("lnt")[:], in1=nhalf[:], op=ALU.pow), reads=[R_("lnt"), P.Rc], writes=[R_("rs")])
            S.op("vector", lambda e: e.tensor_tensor(out=T("coef")[:], in0=T("rec")[:], in1=T("rs")[:], op=ALU.mult), reads=[R_("rec"), R_("rs")], writes=[R_("coef")])
            S.op("vector", lambda e: e.tensor_tensor(out=y0[:], in0=num[:], in1=T("coef")[:].unsqueeze(2).broadcast_to([128, 8, 128]), op=ALU.mult),
                 reads=[Rnum, R_("coef")], writes=[Ry0])
            S.op("gpsimd", lambda e, s=s: e.tensor_tensor(out=ytm[:], in0=y0[:].rearrange("p h v -> p (h v)"), in1=og[:, s, :], op=ALU.mult),
                 reads=[Ry0, Rog[s]], writes=[Rytm])
            for h in range(8):
                S.op("tensor", lambda e, h=h: e.transpose(out=pY[:, h * 128:(h + 1) * 128], in_=ytm[:, h * 128:(h + 1) * 128], identity=P.ident_b[:]),
                     reads=[Rytm, P.Rc], writes=[RpY], join=(h > 0))
            S.op("scalar", lambda e, ts=ts: e.copy(out=yT[:, :, ts], in_=pY[:].rearrange("p (h j) -> p h j", h=8)), reads=[RpY], writes=[RyT[s]])
            if s < 3:
                S.tgt.flush()
            S.tgt = realS
        for d in range(8):
            pb, Rpb = nextbank()
            for h in range(8):
                S.op("tensor", lambda e, h=h, d=d, pb=pb: e.matmul(pb[:], lhsT=wout[:, h, d * 128:(d + 1) * 128], rhs=yT[:, h, :], start=(h == 0), stop=(h == 7)),
                     reads=[Rwout[h]] + RyT, writes=[Rpb], join=(h > 0))
            S.op("vector", lambda e, d=d, pb=pb: e.tensor_tensor(out=hT[:, d, :], in0=pb[:], in1=hT[:, d, :], op=ALU.add), reads=[Rpb, RhT[d]], writes=[RhT[d]])
            S.dma("sync", hout[d, :, t0:t0 + NB], hT[:, d, :], reads=[RhT[d]], writes=[P.Rdram], join=True)
    P.S = realS
    P.finish()


def phase_moba(nc, C, hin, hout, W, name, nblk=NBLK, dbg=None):
    G = 2
    P = Phase(nc, name)
    realS = P.S
    S = Proxy(realS)
    P.S = S
    P.consts(C)
    P.norm_setup()
    c256 = P.sb([128, 1], F32)
    S.op("vector", lambda e: e.memset(c256[:], 1.0 / 256.0), writes=[P.Rc], join=True)
    tri4 = P.sb([128, 512], BF16)
    S.dma("gpsimd", tri4[:], C["c_tribias4"], writes=[P.Rc], join=True)
    ropet = P.sb([128, 32, 16], F32)
    S.dma("sync", ropet[:], C["c_rope"].rearrange("(i p) c -> p i c", p=128), writes=[P.Rc], join=True)
    pastb, Rpastb = P.load_bcast(C["c_past"], 256)
    ownb, Rownb = P.load_bcast(C["c_own"], 256)
    g_kv, Rg_kv = P.load_vec_fm(W["kv_norm"])
    g_mix, Rg_mix = P.load_vec_fm(W["norm_mix1"])
    knorm, Rknorm = P.load_bcast(W["k_norm"], 64)
    qnorm, Rqnorm = P.load_bcast(W["b_q_norm"], 64)
    pR = [P.ps([128, 512], F32) for _ in range(2)]; RpR = [Res(), Res()]
    psc = [P.ps([128, G * 512], F32) for _ in range(2)]; Rpsc = [Res(), Res()]
    pop = [P.ps([128, 512], F32) for _ in range(2)]; Rpop = [Res(), Res()]
    rot = [0]

    def nextbank():
        rot[0] ^= 1
        return pR[rot[0]], RpR[rot[0]]

    arena = P.sb([128, 2688], F32)
    scratch = {"ptm": arena[:, 0:1024].rearrange("p (s d) -> p s d", s=4),
               "pT": arena[:, 1024:1536].bitcast(BF16).rearrange("p (k t) -> p k t", k=2),
               "sgate": arena[:, 1536:2048], "tmp": arena[:, 2048:2560]}
    ple = PLE(P, C, W["p0"], W["norm_ple0"], W["w_ple_gate0"], W["w_ple_up0"], [(pR[0], RpR[0]), (pR[1], RpR[1]), (pR[0], RpR[0])],
              scratch=scratch)
    wkv, Rwkv = P.load_w(W["w_kv"], 8, 512)
    wq, Rwq = P.load_w(W["b_w_q"], 8, 1024)
    wo, Rwo = P.load_w(W["b_w_o"], 8, 1024)

    hT = P.sb([128, 8, NB], F32); RhT = [Res() for _ in range(8)]
    hn = P.sb([128, 8, NB], BF16); Rhn = [Res() for _ in range(8)]
    hkv = P.sb([128, 8, NB], BF16); Rhkv = [Res() for _ in range(8)]
    KT = P.sb([80, 4, NT], BF16); RKT = [Res() for _ in range(32)]
    Vaug = P.sb([128, 4, 32, 128], BF16); RV = [Res() for _ in range(32)]
    kmT = P.sb([80, 4, 16], BF16); RkmT = Res()
    kms = P.sb([64, 4, 2], F32); Rkms = Res()
    ksb = P.sb([128, 4, 64], F32); Rksb = Res()
    sqk = P.sb([128, 4, 64], F32); Rsqk = Res()
    kbf = P.sb([128, 4, 64], BF16); Rkbf = Res()
    qsb = arena[:, 0:1024].rearrange("p (h d) -> p h d", h=16); Rqsb = Res()
    sqq = arena[:, 1024:2048].rearrange("p (h d) -> p h d", h=16); Rsqq = Res()
    qa = arena[:, 2048:2688].bitcast(BF16).rearrange("p (h d) -> p h d", h=16); Rqa = Res()

    def arena_fence(to_ple):
        qres = [Rqsb, Rsqq, Rqa]
        if to_ple:
            S.op("gpsimd", lambda e: e.memset(arena[:, 0:1], 0.0), reads=qres, writes=ple.all_res())
        else:
            S.op("gpsimd", lambda e: e.memset(arena[:, 0:1], 0.0), reads=ple.all_res(), writes=qres)
    QTa = [P.sb([80, 16, 128], BF16) for _ in range(2)]; RQTa = [Res(), Res()]
    gm = P.sb([128, 16, 16], F32); Rgm = Res()
    mx8 = P.sb([128, 16, 8], F32); Rmx8 = Res()
    vis = P.sb([128, 16, 16], F32); Rvis = Res()
    skq = {n: (P.sb([128, 16], F32), Res()) for n in ["ss", "ln", "r"]}
    skk = {n: (P.sb([128, 16], F32), Res()) for n in ["ss", "ln", "r"]}
    rtq = {n: (P.sb([128, 16, 8], F32), Res()) for n in ["t1", "t2", "t3", "t4"]}
    PTb = [P.sb([128, G * 512], BF16) for _ in range(2)]; RPTb = [Res() for _ in range(2)]
    rec = P.sb([128, 512], F32); Rrec = Res()
    OTb = P.sb([128, 8, NB], BF16); ROTb = [Res() for _ in range(4)]

    for kvh in range(4):
        for c0 in range(0, NT, 1024):
            S.dma("gpsimd", KT[64:80, kvh, c0:c0 + 1024], C["c_onehot"][:, c0:c0 + 1024], writes=[P.Rc], join=True)
    S.op("vector", lambda e: e.memset(Vaug[:].rearrange("p a b c -> p (a b c)"), 1.0), writes=[P.Rc], join=True)
    S.op("gpsimd", lambda e: e.memset(kmT[:], 0.0), writes=[RkmT])
    for i in range(2):
        S.op("gpsimd", lambda e, i=i: e.memset(QTa[i][:], 0.0), writes=[RQTa[i]])

    def head_norm_rope(x, Rx, sq_, Rsq_, nh, gbc, Rgbc, it, sk, rt):
        (ss, Rss), (ln, Rln), (r, Rr) = sk["ss"], sk["ln"], sk["r"]
        S.op("gpsimd", lambda e: e.tensor_tensor(out=sq_[:], in0=x[:], in1=x[:], op=ALU.mult), reads=[Rx], writes=[Rsq_])
        S.op("vector", lambda e: e.tensor_reduce(out=ss[:, 0:nh], in_=sq_[:], axis=AX.X, op=ALU.add), reads=[Rsq_], writes=[Rss])
        yield
        S.op("scalar", lambda e: e.activation(out=ln[:, 0:nh], in_=ss[:, 0:nh], func=AF.Ln, scale=1.0 / 64.0, bias=P.eps_t[:, 0:1]),
             reads=[Rss, P.Rc], writes=[Rln])
        S.op("scalar", lambda e: e.activation(out=r[:, 0:nh], in_=ln[:, 0:nh], func=AF.Exp, scale=-0.5), reads=[Rln], writes=[Rr])
        yield
        S.op("vector", lambda e: e.tensor_tensor(out=x[:], in0=x[:], in1=r[:, 0:nh].unsqueeze(2).broadcast_to([128, nh, 64]), op=ALU.mult),
             reads=[Rx, Rr], writes=[Rx])
        S.op("vector", lambda e: e.tensor_tensor(out=x[:], in0=x[:], in1=gbc[:].unsqueeze(1).broadcast_to([128, nh, 64]), op=ALU.mult),
             reads=[Rx, Rgbc], writes=[Rx])
        yield
        cs = ropet[:, it, 0:8].unsqueeze(1).broadcast_to([128, nh, 8])
        sn = ropet[:, it, 8:16].unsqueeze(1).broadcast_to([128, nh, 8])
        x1 = x[:, :, 0:8]
        x2 = x[:, :, 8:16]
        tt = {k: v[0][:, 0:nh, :] for k, v in rt.items()}
        Rt = {k: v[1] for k, v in rt.items()}
        S.op("vector", lambda e: e.tensor_tensor(out=tt["t1"], in0=x1, in1=cs, op=ALU.mult), reads=[Rx, P.Rc], writes=[Rt["t1"]])
        S.op("vector", lambda e: e.tensor_tensor(out=tt["t2"], in0=x2, in1=sn, op=ALU.mult), reads=[Rx, P.Rc], writes=[Rt["t2"]])
        S.op("vector", lambda e: e.tensor_tensor(out=tt["t3"], in0=x2, in1=cs, op=ALU.mult), reads=[Rx, P.Rc], writes=[Rt["t3"]])
        S.op("vector", lambda e: e.tensor_tensor(out=tt["t4"], in0=x1, in1=sn, op=ALU.mult), reads=[Rx, P.Rc], writes=[Rt["t4"]])
        yield
        S.op("vector", lambda e: e.tensor_tensor(out=x1, in0=tt["t1"], in1=tt["t2"], op=ALU.subtract), reads=[Rt["t1"], Rt["t2"], Rx], writes=[Rx])
        S.op("vector", lambda e: e.tensor_tensor(out=x2, in0=tt["t3"], in1=tt["t4"], op=ALU.add), reads=[Rt["t3"], Rt["t4"], Rx], writes=[Rx])
        yield

    def kv_path(blk, s):
        it = blk * 4 + s
        ts = slice(s * 128, (s + 1) * 128)
        pb, Rpb = nextbank()
        for k in range(8):
            S.op("tensor", lambda e, k=k, ts=ts, pb=pb: e.matmul(pb[:], lhsT=hkv[:, k, ts], rhs=wkv[:, k, :], start=(k == 0), stop=(k == 7)),
                 reads=[Rwkv[k], Rhkv[k]], writes=[Rpb], join=(k > 0))
        S.op("scalar", lambda e, pb=pb: e.copy(out=ksb[:].rearrange("p h d -> p (h d)"), in_=pb[:, 0:256]), reads=[Rpb], writes=[Rksb])
        S.op("scalar", lambda e, pb=pb, it=it: e.copy(out=Vaug[:, :, it, 0:64], in_=pb[:, 256:512].rearrange("p (h d) -> p h d", h=4)),
             reads=[Rpb, P.Rc], writes=[RV[it]])
        for _ in head_norm_rope(ksb, Rksb, sqk, Rsqk, 4, knorm, Rknorm, it, skk, rtq):
            pass
        S.op("gpsimd", lambda e: e.tensor_copy(out=kbf[:], in_=ksb[:]), reads=[Rksb], writes=[Rkbf])
        pb, Rpb = nextbank()
        pbb = pb[:].bitcast(BF16)
        for kvh in range(4):
            S.op("tensor", lambda e, kvh=kvh, pbb=pbb: e.transpose(out=pbb[0:64, kvh * 128:(kvh + 1) * 128], in_=kbf[:, kvh, :], identity=P.ident_b[:]),
                 reads=[Rkbf, P.Rc], writes=[Rpb], join=(kvh > 0))
        S.op("scalar", lambda e, it=it, pbb=pbb: e.copy(out=KT[0:64, :, it * 128:(it + 1) * 128], in_=pbb[0:64, 0:512].rearrange("p (h t) -> p h t", h=4)),
             reads=[Rpb, P.Rc], writes=[RKT[it]])
        pb, Rpb = nextbank()
        for kvh in range(4):
            S.op("tensor", lambda e, kvh=kvh, pb=pb: e.matmul(pb[0:64, kvh:kvh + 1], lhsT=ksb[:, kvh, :], rhs=c256[:, 0:1], start=True, stop=True),
                 reads=[Rksb, P.Rc], writes=[Rpb], join=(kvh > 0))
        S.op("vector", lambda e, s=s, pb=pb: e.tensor_copy(out=kms[:, :, s % 2], in_=pb[0:64, 0:4]), reads=[Rpb], writes=[Rkms], join=(s % 2 == 1))
        if s % 2 == 1:
            n = it // 2
            S.op("vector", lambda e, n=n: e.tensor_tensor(out=kmT[0:64, :, n], in0=kms[:, :, 0], in1=kms[:, :, 1], op=ALU.add),
                 reads=[Rkms], writes=[RkmT])

    def q_path(blk, s):
        it = blk * 4 + s
        b = it // 2
        ts = slice(s * 128, (s + 1) * 128)
        QT, RQT = QTa[it % 2], RQTa[it % 2]
        for half in range(2):
            pb, Rpb = nextbank()
            for k in range(8):
                S.op("tensor", lambda e, k=k, ts=ts, pb=pb, half=half: e.matmul(pb[:], lhsT=hn[:, k, ts], rhs=wq[:, k, half * 512:(half + 1) * 512],
                                                                                 start=(k == 0), stop=(k == 7)),
                     reads=[Rwq[k], Rhn[k]], writes=[Rpb], join=(k > 0))
                if k % 4 == 3:
                    yield
            S.op("vector", lambda e, pb=pb, half=half: e.tensor_copy(out=qsb[:, half * 8:(half + 1) * 8, :].rearrange("p h d -> p (h d)"), in_=pb[:]),
                 reads=[Rpb], writes=[Rqsb], join=(half > 0))
            yield
        for _ in head_norm_rope(qsb, Rqsb, sqq, Rsqq, 16, qnorm, Rqnorm, it, skq, rtq):
            yield
        S.op("gpsimd", lambda e: e.tensor_copy(out=qa[:, :, 0:64], in_=qsb[:]), reads=[Rqsb], writes=[Rqa])
        yield
        for r_ in range(2):
            pb, Rpb = nextbank()
            pbb = pb[:].bitcast(BF16)
            for j in range(8):
                h = r_ * 8 + j
                S.op("tensor", lambda e, h=h, j=j, pbb=pbb: e.transpose(out=pbb[0:64, j * 128:(j + 1) * 128], in_=qa[:, h, 0:64], identity=P.ident_b[:]),
                     reads=[Rqa, P.Rc], writes=[Rpb], join=(j > 0))
                if j % 4 == 3:
                    yield
            S.op("vector", lambda e, r_=r_, pbb=pbb: e.tensor_copy(out=QT[0:64, r_ * 8:(r_ + 1) * 8, :], in_=pbb[0:64, :].rearrange("p (h t) -> p h t", h=8)),
                 reads=[Rpb], writes=[RQT], join=(r_ > 0))
            yield
        pb, Rpb = nextbank()
        for h in range(16):
            S.op("tensor", lambda e, h=h, pb=pb: e.matmul(pb[:, h * 16:(h + 1) * 16], lhsT=QT[:, h, :], rhs=kmT[:, h // 4, :], start=True, stop=True),
                 reads=[RQT, RkmT], writes=[Rpb], join=(h > 0))
            if h % 4 == 3:
                yield
        S.op("vector", lambda e, b=b, pb=pb: e.tensor_tensor(out=gm[:], in0=pb[:, 0:256].rearrange("p (h n) -> p h n", h=16),
                                                              in1=pastb[:, b * 16:(b + 1) * 16].unsqueeze(1).broadcast_to([128, 16, 16]), op=ALU.add),
             reads=[Rpb, Rpastb], writes=[Rgm])
        yield
        for h in range(16):
            S.op("vector", lambda e, h=h: e.max(out=mx8[:, h, :], in_=gm[:, h, :]), reads=[Rgm], writes=[Rmx8], join=(h > 0))
            if h % 4 == 3:
                yield
        S.op("vector", lambda e: e.tensor_tensor(out=vis[:], in0=gm[:], in1=mx8[:, :, 2:3].broadcast_to([128, 16, 16]), op=ALU.is_ge),
             reads=[Rgm, Rmx8], writes=[Rvis])
        S.op("vector", lambda e, b=b: e.tensor_tensor(out=vis[:], in0=vis[:], in1=ownb[:, b * 16:(b + 1) * 16].unsqueeze(1).broadcast_to([128, 16, 16]), op=ALU.max),
             reads=[Rvis, Rownb], writes=[Rvis])
        S.op("vector", lambda e: e.tensor_scalar(out=qa[:, :, 64:80], in0=vis[:], scalar1=-NEG, scalar2=NEG, op0=ALU.mult, op1=ALU.add),
             reads=[Rvis, Rqa], writes=[Rqa])
        yield
        for r_ in range(2):
            pb, Rpb = nextbank()
            pbb = pb[:].bitcast(BF16)
            for j in range(8):
                h = r_ * 8 + j
                S.op("tensor", lambda e, h=h, j=j, pbb=pbb: e.transpose(out=pbb[0:80, j * 128:(j + 1) * 128], in_=qa[:, h, :], identity=P.ident_b[:]),
                     reads=[Rqa, P.Rc], writes=[Rpb], join=(j > 0))
                if j % 4 == 3:
                    yield
            S.op("vector", lambda e, r_=r_, pbb=pbb: e.tensor_copy(out=QT[:, r_ * 8:(r_ + 1) * 8, :], in_=pbb[0:80, :].rearrange("p (h t) -> p h t", h=8)),
                 reads=[Rpb], writes=[RQT], join=(r_ > 0))
            yield

    nsc = [0]

    def attention(blk, s):
        it = blk * 4 + s
        ts = slice(s * 128, (s + 1) * 128)
        QT, RQT = QTa[it % 2], RQTa[it % 2]
        L = []
        for kvh in range(4):
            for j0 in range(0, it + 1, G):
                L.append((kvh, j0, min(it + 1, j0 + G)))
        bufs = {}

        def qk(n):
            kvh, j0, j1 = L[n]
            m = nsc[0]
            nsc[0] += 1
            bufs[n] = m % 2
            ps_, Rps_ = psc[m % 2], Rpsc[m % 2]
            qg = QT[:, 4 * kvh:4 * kvh + 4, :].rearrange("p h t -> p (h t)")
            for j in range(j0, j1):
                o = (j - j0) * 512
                diag = (j == it)
                S.op("tensor", lambda e, kvh=kvh, j=j, ps_=ps_, qg=qg, diag=diag, o=o: e.matmul(ps_[:, o:o + 512], lhsT=KT[:, kvh, j * 128:(j + 1) * 128], rhs=qg,
                                                                                               start=True, stop=(not diag)),
                     reads=[RKT[j], RQT, P.Rc], writes=[Rps_], join=(j > j0))
                if diag:
                    S.op("tensor", lambda e, ps_=ps_, o=o: e.matmul(ps_[:, o:o + 512], lhsT=P.ident_b[:], rhs=tri4[:], start=False, stop=True),
                         reads=[P.Rc], writes=[Rps_], join=True)

        def ex(n):
            kvh, j0, j1 = L[n]
            bi = bufs[n]
            w = (j1 - j0) * 512
            S.op("scalar", lambda e, bi=bi, w=w: e.activation(out=PTb[bi][:, 0:w], in_=psc[bi][:, 0:w], func=AF.Exp, scale=0.125),
                 reads=[Rpsc[bi]], writes=[RPTb[bi]])

        def pv(n):
            kvh, j0, j1 = L[n]
            bi = bufs[n]
            po, Rpo = pop[kvh % 2], Rpop[kvh % 2]
            for j in range(j0, j1):
                o = (j - j0) * 512
                S.op("tensor", lambda e, kvh=kvh, j=j, bi=bi, po=po, o=o, it=it: e.matmul(po[:], lhsT=Vaug[:, kvh, j, :], rhs=PTb[bi][:, o:o + 512],
                                                                                         start=(j == 0), stop=(j == it)),
                     reads=[RV[j], RPTb[bi], P.Rc], writes=[Rpo], join=(j > 0))

        def epi(kvh):
            po, Rpo = pop[kvh % 2], Rpop[kvh % 2]
            S.op("vector", lambda e, po=po: e.reciprocal(out=rec[64:128, :], in_=po[64:128, :]), reads=[Rpo], writes=[Rrec])
            for gq in range(4):
                hh = 4 * kvh + gq
                pr, ph = hh // 2, hh % 2
                S.op("vector", lambda e, po=po, gq=gq, pr=pr, ph=ph, ts=ts: e.tensor_tensor(
                    out=OTb[ph * 64:(ph + 1) * 64, pr, ts], in0=po[0:64, gq * 128:(gq + 1) * 128], in1=rec[64:128, gq * 128:(gq + 1) * 128], op=ALU.mult),
                     reads=[Rpo, Rrec], writes=[ROTb[s]], join=True)

        qk(0)
        for n in range(len(L)):
            if n + 1 < len(L):
                qk(n + 1)
            ex(n)
            pv(n)
            if n + 1 == len(L) or L[n + 1][0] != L[n][0]:
                epi(L[n][0])
            yield

    def drive(main, side, side_len):
        steps = list(range(0))
        mains = main
        if side is None:
            for _ in mains:
                pass
            return
        done = [False]

        def adv(k):
            for _ in range(k):
                if done[0]:
                    return
                try:
                    next(side)
                except StopIteration:
                    done[0] = True
        nmain = side_len[0]
        per = max(1, -(-side_len[1] // max(1, nmain)))
        for _ in mains:
            adv(per)
        while not done[0]:
            adv(8)

    def side_gen(blk, s):
        d = Deferred()
        S.tgt = d
        kv_path(blk, s)
        S.tgt = realS
        for kind, a_, k_ in d.q:
            getattr(realS, kind)(*a_, **k_)
            yield
        for _ in q_path(blk, s):
            yield

    for blk in range(nblk):
        t0 = blk * NB
        for k in range(8):
            S.dma("sync", hT[:, k, :], hin[k, :, t0:t0 + NB], writes=[RhT[k]])
        arena_fence(True)
        pb, Rpb = nextbank()
        ple.emit(blk, hT, RhT, hn, Rhn, pb, Rpb)
        pb, Rpb = nextbank()
        P.fm_norm(hT, RhT, g_kv, Rg_kv, hkv, Rhkv, pb, Rpb)
        P.fm_norm(hT, RhT, g_mix, Rg_mix, hn, Rhn, pb, Rpb, reuse_rstd=True)
        kv_path(blk, 0)
        arena_fence(False)
        for _ in q_path(blk, 0):
            pass
        for s in range(4):
            it = blk * 4 + s
            nsteps = 4 * (-(-(it + 1) // G))
            side = side_gen(blk, s + 1) if s < 3 else None
            drive(attention(blk, s), side, (nsteps, 120))
        for d in range(8):
            pb, Rpb = nextbank()
            for pr in range(8):
                S.op("tensor", lambda e, pr=pr, d=d, pb=pb: e.matmul(pb[:], lhsT=wo[:, pr, d * 128:(d + 1) * 128], rhs=OTb[:, pr, :], start=(pr == 0), stop=(pr == 7)),
                     reads=[Rwo[pr]] + ROTb, writes=[Rpb], join=(pr > 0))
            S.op("vector", lambda e, d=d, pb=pb: e.tensor_tensor(out=hT[:, d, :], in0=pb[:], in1=hT[:, d, :], op=ALU.add), reads=[Rpb, RhT[d]], writes=[RhT[d]])
            S.dma("sync", hout[d, :, t0:t0 + NB], hT[:, d, :], reads=[RhT[d]], writes=[P.Rdram], join=True)
    P.S = realS
    P.finish()


WSHAPES = {
    "norm_mix0": [1024], "norm_mix1": [1024], "a_w_in": [1024, 3088], "a_b_gate": [16], "a_mh_gain": [128], "a_w_out": [1024, 1024],
    "kv_norm": [1024], "w_kv": [1024, 512], "k_norm": [64], "b_w_q": [1024, 1024], "b_q_norm": [64], "b_w_o": [1024, 1024],
    "norm_ffn0": [1024], "norm_ffn1": [1024], "w_gate_up0": [1024, 5632], "w_gate_up1": [1024, 5632], "w_down0": [2816, 1024], "w_down1": [2816, 1024],
    "norm_ple0": [1024], "norm_ple1": [1024], "w_ple_gate0": [1024, 1024], "w_ple_gate1": [1024, 1024], "w_ple_up0": [256, 1024], "w_ple_up1": [256, 1024],
    "p0": [NT, 256], "p1": [NT, 256],
}


def build_program():
    nc = bass.Bass("TRN2", target_bir_lowering=False)
    Cn = make_consts()
    C = {k: nc.dram_tensor(k, list(v.shape), F32, kind="ExternalInput").ap() for k, v in Cn.items()}
    x = nc.dram_tensor("x", [NT, 1024], F32, kind="ExternalInput").ap()
    out = nc.dram_tensor("out", [NT, 1024], F32, kind="ExternalOutput").ap()
    W = {k: nc.dram_tensor(k, v, F32, kind="ExternalInput").ap() for k, v in WSHAPES.items()}
    hs = [nc.dram_tensor("h_scr%d" % i, [8, 128, NT], F32, kind="Internal").ap() for i in range(4)]
    phase_mlstm(nc, C, x, hs[0], W, "ml")
    phase_ffn(nc, C, hs[0], hs[1], W["w_gate_up0"], W["w_down0"], W["norm_ffn0"], "f0")
    phase_moba(nc, C, hs[1], hs[2], W, "mb")
    phase_ffn(nc, C, hs[2], hs[3], W["w_gate_up1"], W["w_down1"], W["norm_ffn1"], "f1")
    phase_ple_out(nc, C, hs[3], out, W["p1"], W["norm_ple1"], W["w_ple_gate1"], W["w_ple_up1"], "po")
    return nc, Cn


def make_in_maps(inputs, cores):
    f = lambda a: np.ascontiguousarray(np.asarray(a, dtype=np.float32))
    I = {k: np.asarray(v) for k, v in inputs.items()}
    shared = {
        "norm_mix0": f(I["norm_mix"][0]), "norm_mix1": f(I["norm_mix"][1]), "a_w_in": f(I["a_w_in"][0]), "a_b_gate": f(I["a_b_gate"][0]),
        "a_mh_gain": f(I["a_mh_gain"][0]), "a_w_out": f(I["a_w_out"][0]), "kv_norm": f(I["kv_norm"]), "w_kv": f(I["w_kv"]), "k_norm": f(I["k_norm"]),
        "b_w_q": f(I["b_w_q"][0]), "b_q_norm": f(I["b_q_norm"][0]), "b_w_o": f(I["b_w_o"][0]),
        "norm_ffn0": f(I["norm_ffn"][0]), "norm_ffn1": f(I["norm_ffn"][1]), "w_gate_up0": f(I["w_gate_up"][0]), "w_gate_up1": f(I["w_gate_up"][1]),
        "w_down0": f(I["w_down"][0]), "w_down1": f(I["w_down"][1]), "norm_ple0": f(I["norm_ple"][0]), "norm_ple1": f(I["norm_ple"][1]),
        "w_ple_gate0": f(I["w_ple_gate"][0]), "w_ple_gate1": f(I["w_ple_gate"][1]), "w_ple_up0": f(I["w_ple_up"][0]), "w_ple_up1": f(I["w_ple_up"][1]),
    }
    maps = []
    for b in cores:
        m = dict(shared)
        m["x"] = f(I["x"][b])
        m["p0"] = f(I["p"][0, b])
        m["p1"] = f(I["p"][1, b])
        maps.append(m)
    return maps


def kernel(**inputs):
    nc, Cn = build_program()
    maps = make_in_maps(inputs, list(range(8)))
    for m in maps:
        m.update(Cn)
    res = run_bass_kernel_spmd(nc, maps, core_ids=list(range(8)))
    return np.stack([np.asarray(r["out"], dtype=np.float32) for r in res.results], axis=0)
```

```python
import numpy as np
import concourse.bass as bass
import concourse.mybir as mybir
from concourse.bass_utils import run_bass_kernel_spmd
from contextlib import ExitStack

F32 = mybir.dt.float32
BF16 = mybir.dt.bfloat16
ALU = mybir.AluOpType
AF = mybir.ActivationFunctionType
AX = mybir.AxisListType

ENGS = ["tensor", "vector", "scalar", "gpsimd", "sync"]
NDMASEM = 12
SAME_ENGINE_SYNC = True

NT = 4096
NB = 512
NBLK = NT // NB
EPS = 1e-6
NEG = -30000.0


class Res:
    __slots__ = ("name", "w", "r", "gd")

    def __init__(self, name=""):
        self.name = name
        self.w = []
        self.r = []
        self.gd = []


class WRes:
    def __init__(self, grid, cw):
        self.grid = grid
        self.cw = cw

    def sel(self, k, c0, c1):
        return [self.grid[k][ci] for ci in range(c0 // self.cw, (c1 - 1) // self.cw + 1)]


class Op:
    __slots__ = ("eng", "fn", "waits", "pos", "sig", "isdma", "semi", "semk", "K", "sigidx")


class Sched:
    G = {"nc": None}

    @staticmethod
    def setup(nc):
        if Sched.G.get("nc") is nc:
            return
        es = ExitStack()
        G = {"nc": nc, "es": es, "sig": {e: 0 for e in ENGS}, "dma": {e: 0 for e in ENGS}}
        G["esem"] = {e: es.enter_context(nc.semaphore("sem_e_%s" % e)) for e in ENGS}
        G["dsem"] = {(e, i): es.enter_context(nc.semaphore("sem_d_%s_%d" % (e, i))) for e in ("sync", "gpsimd") for i in range(NDMASEM)}
        Sched.G = G

    def __init__(self, nc):
        Sched.setup(nc)
        self.nc = nc
        self.ops = {e: [] for e in ENGS}
        self.Kcur = {e: {} for e in ENGS}
        self.nops = 0

    limit = None

    def _record(self, eng, fn, reads, writes, isdma, join=False):
        if Sched.limit is not None and self.nops >= Sched.limit:
            return None
        o = Op()
        o.eng = eng
        o.fn = fn
        o.isdma = isdma
        o.sig = False
        o.pos = len(self.ops[eng])
        deps = {}
        for r in reads:
            for x in r.w:
                deps[id(x)] = x
        for w in writes:
            if join and not w.r:
                for x in w.gd:
                    deps[id(x)] = x
            else:
                for x in w.w:
                    deps[id(x)] = x
                for x in w.r:
                    deps[id(x)] = x
        K = self.Kcur[eng]
        newK = None
        waits = []
        if isdma:
            i = Sched.G["dma"][eng]
            Sched.G["dma"][eng] += 1
            o.semi = i % NDMASEM
            o.semk = i // NDMASEM + 1
            if o.semk > 1:
                key = ("d", eng, o.semi)
                if K.get(key, 0) < o.semk - 1:
                    waits.append(("dmaslot", eng, o.semi, o.semk - 1))
                    newK = dict(K)
                    newK[key] = o.semk - 1
        best = {}
        dl = []
        for y in deps.values():
            if y is o:
                continue
            if y.isdma:
                dl.append(y)
            elif y.eng not in best or best[y.eng].pos < y.pos:
                best[y.eng] = y
        for y in dl + list(best.values()):
            cur = K if newK is None else newK
            if y.isdma:
                key = ("d", y.eng, y.semi)
                if cur.get(key, 0) >= y.semk:
                    continue
                waits.append(("dma", y))
            else:
                if y.eng == eng and (eng == "tensor" or not SAME_ENGINE_SYNC) and not isdma:
                    continue
                if cur.get(y.eng, -1) >= y.pos:
                    continue
                y.sig = True
                waits.append(("eng", y))
            if newK is None:
                newK = dict(K)
            for k, v in y.K.items():
                if newK.get(k, -1) < v:
                    newK[k] = v
            if y.isdma:
                key = ("d", y.eng, y.semi)
                newK[key] = max(newK.get(key, 0), y.semk)
            else:
                if newK.get(y.eng, -1) < y.pos:
                    newK[y.eng] = y.pos
        if newK is not None:
            self.Kcur[eng] = newK
            K = newK
        o.K = K
        o.waits = waits
        for r in reads:
            r.r.append(o)
        for w in writes:
            if join and not w.r:
                w.w.append(o)
            else:
                w.gd = w.w + w.r
                w.w = [o]
                w.r = []
        self.ops[eng].append(o)
        self.nops += 1
        return o

    def op(self, eng, fn, reads=(), writes=(), join=False):
        return self._record(eng, fn, reads, writes, False, join)

    def dma(self, queue, out, in_, reads=(), writes=(), join=False, nonc=False, **kw):
        nc = self.nc
        if nonc:
            def f(e):
                with nc.allow_non_contiguous_dma(reason="small strided parameter load"):
                    return e.dma_start(out=out, in_=in_, **kw)
        else:
            def f(e):
                return e.dma_start(out=out, in_=in_, **kw)
        return self._record(queue, f, reads, writes, True, join)

    def emit(self):
        nc = self.nc
        G = Sched.G
        for e in ENGS:
            n = G["sig"][e]
            for o in self.ops[e]:
                if o.sig:
                    n += 1
                o.sigidx = n
            G["sig"][e] = n
        esem, dsem = G["esem"], G["dsem"]
        with nc.Block() as block:
            def stream(ename):
                def body(eng):
                    for o in self.ops[ename]:
                        for w in o.waits:
                            if w[0] == "dmaslot":
                                eng.wait_ge(dsem[(w[1], w[2])], 16 * w[3])
                            elif w[0] == "dma":
                                y = w[1]
                                eng.wait_ge(dsem[(y.eng, y.semi)], 16 * y.semk)
                            else:
                                y = w[1]
                                eng.wait_ge(esem[y.eng], y.sigidx)
                        ins = o.fn(eng)
                        if o.isdma:
                            ins.then_inc(dsem[(o.eng, o.semi)], 16)
                        elif o.sig:
                            ins.then_inc(esem[o.eng], 1)
                return body

            for e in ENGS:
                if self.ops[e]:
                    getattr(block, e)(stream(e))


class Proxy:
    def __init__(self, real):
        self.real = real
        self.tgt = real

    def op(self, *a, **k):
        return self.tgt.op(*a, **k)

    def dma(self, *a, **k):
        return self.tgt.dma(*a, **k)


class Mux:
    def __init__(self, real, chunks):
        self.real = real
        self.chunks = chunks
        self.last = None

    def _maybe(self, eng):
        if self.last == "tensor" and eng != "tensor" and self.chunks:
            for kind, a, k in self.chunks.pop(0):
                getattr(self.real, kind)(*a, **k)
        self.last = eng

    def op(self, eng, *a, **k):
        self._maybe(eng)
        return self.real.op(eng, *a, **k)

    def dma(self, eng, *a, **k):
        self._maybe(eng)
        return self.real.dma(eng, *a, **k)

    def flush(self):
        while self.chunks:
            for kind, a, k in self.chunks.pop(0):
                getattr(self.real, kind)(*a, **k)


def mm_chunks(q):
    runs = []
    for item in q:
        is_mm = (item[0] == "op" and item[1][0] == "tensor")
        if runs and runs[-1][0] == is_mm:
            runs[-1][1].append(item)
        else:
            runs.append((is_mm, [item]))
    chunks = []
    cur = []
    for is_mm, items in runs:
        cur.extend(items)
        if is_mm:
            chunks.append(cur)
            cur = []
    if cur:
        chunks.append(cur)
    return chunks


class Deferred:
    def __init__(self):
        self.q = []

    def op(self, *a, **k):
        self.q.append(("op", a, k))

    def dma(self, *a, **k):
        self.q.append(("dma", a, k))


def interleave(main_gen, real, q):
    steps = list(main_gen) if False else None
    i = 0
    n = getattr(main_gen, "nsteps", None)
    return i


class Phase:
    def __init__(self, nc, name):
        self.nc = nc
        self.name = name
        self.es = ExitStack()
        self.S = Sched(nc)
        self.n = 0
        self.Rdram = Res("dram_out")

    def sb(self, shape, dt, name=None):
        self.n += 1
        t = self.es.enter_context(self.nc.sbuf_tensor("%s_s%d" % (self.name, self.n), list(shape), dt))
        return t

    def ps(self, shape, dt, name=None):
        self.n += 1
        t = self.es.enter_context(self.nc.psum_tensor("%s_p%d" % (self.name, self.n), list(shape), dt))
        return t

    def finish(self):
        S = self.S
        S.op("sync", lambda e: e.nop(), reads=[self.Rdram])
        S.emit()
        self.es.close()

    def consts(self, C):
        S = self.S
        self.ident_f = self.sb([128, 128], F32)
        self.ident_b = self.sb([128, 128], BF16)
        self.ones_b = self.sb([128, 128], BF16)
        self.eps_t = self.sb([128, 1], F32)
        self.one_t = self.sb([128, 1], F32)
        self.Rc = Res("consts")
        S.dma("sync", self.ident_f[:], C["c_ident"], writes=[self.Rc], join=True)
        S.op("vector", lambda e: e.tensor_copy(out=self.ident_b[:], in_=self.ident_f[:]), reads=[self.Rc], writes=[self.Rc])
        S.op("vector", lambda e: e.memset(self.ones_b[:], 1.0), writes=[self.Rc], join=True)
        S.op("vector", lambda e: e.memset(self.eps_t[:], EPS), writes=[self.Rc], join=True)
        S.op("vector", lambda e: e.memset(self.one_t[:], 1.0), writes=[self.Rc], join=True)

    def load_vec_fm(self, ap1024, nk=8):
        t = self.sb([128, nk], F32)
        r = Res()
        self.S.dma("sync", t[:], ap1024.rearrange("(k p) -> p k", p=128), writes=[r], nonc=True)
        return t, r

    def load_bcast(self, ap_flat, n):
        t = self.sb([128, n], F32)
        r = Res()
        self.S.dma("sync", t[:], ap_flat.partition_broadcast(128), writes=[r])
        return t, r

    def load_w(self, src, K, N, rows=128, col_chunk=2048, queue="gpsimd", chunk_major=False):
        t = self.sb([rows, K, N], BF16)
        nch = -(-N // col_chunk)
        cw = -(-N // nch)
        if not chunk_major:
            rs = [Res() for _ in range(K)]
            for k in range(K):
                c0 = 0
                while c0 < N:
                    c1 = min(N, c0 + cw)
                    self.S.dma(queue, t[:, k, c0:c1], src[k * rows:(k + 1) * rows, c0:c1], writes=[rs[k]], join=True)
                    c0 = c1
            return t, rs
        grid = [[Res() for _ in range(nch)] for _ in range(K)]
        order = list(range(nch))
        if nch == 4:
            order = [0, 2, 1, 3]
        for ci in order:
            c0, c1 = ci * cw, min(N, (ci + 1) * cw)
            for k in range(K):
                self.S.dma(queue, t[:, k, c0:c1], src[k * rows:(k + 1) * rows, c0:c1], writes=[grid[k][ci]])
        return t, WRes(grid, cw)

    def norm_setup(self):
        self.rstd = self.sb([128, NB], F32)
        self.Rrstd = Res()
        self.lnv = self.rstd
        self.Rlnv = self.Rrstd

    def fm_norm(self, hT, RhT, g, Rg, hn, Rhn, pss, Rpss, reuse_rstd=False):
        S = self.S
        n = hT.shape[2]
        sq, Rsq = hn, Rhn
        for k in range(8 if not reuse_rstd else 0):
            S.op("scalar", lambda e, k=k: e.activation(out=sq[:, k, :], in_=hT[:, k, :], func=AF.Square),
                 reads=[RhT[k]], writes=[Rsq[k]])
        for k in range(8 if not reuse_rstd else 0):
            S.op("tensor", lambda e, k=k: e.matmul(pss[:, 0:n], lhsT=self.ones_b[:], rhs=sq[:, k, :], start=(k == 0), stop=(k == 7)),
                 reads=[Rsq[k], self.Rc], writes=[Rpss], join=(k > 0))
        if not reuse_rstd:
            S.op("scalar", lambda e: e.activation(out=self.lnv[:, 0:n], in_=pss[:, 0:n], func=AF.Ln, scale=1.0 / 1024.0, bias=self.eps_t[:, 0:1]),
                 reads=[Rpss, self.Rc], writes=[self.Rlnv])
            S.op("scalar", lambda e: e.activation(out=self.rstd[:, 0:n], in_=self.lnv[:, 0:n], func=AF.Exp, scale=-0.5),
                 reads=[self.Rlnv], writes=[self.Rrstd])
        for k in range(8):
            S.op("vector", lambda e, k=k: e.scalar_tensor_tensor(out=hn[:, k, :], in0=hT[:, k, :], scalar=g[:, k:k + 1], op0=ALU.mult,
                                                                 in1=self.rstd[:, 0:n], op1=ALU.mult),
                 reads=[RhT[k], self.Rrstd, Rg], writes=[Rhn[k]])


def phase_ffn(nc, C, hin, hout, wgu_ap, wd_ap, g_ap, name):
    P = Phase(nc, name)
    S = P.S
    P.consts(C)
    g, Rg = P.load_vec_fm(g_ap)
    wgu, Rwgu = P.load_w(wgu_ap, 8, 5632, col_chunk=1408, chunk_major=True)
    wd, Rwd = P.load_w(wd_ap, 22, 1024)
    P.norm_setup()
    hTs = [P.sb([128, 8, NB], F32) for _ in range(2)]
    RhTs = [[Res() for _ in range(8)] for _ in range(2)]
    hns = [P.sb([128, 8, NB], BF16) for _ in range(2)]
    Rhns = [[Res() for _ in range(8)] for _ in range(2)]
    act = P.sb([128, 22, NB], BF16)
    Ract = [Res() for _ in range(22)]
    sg = [P.sb([128, NB], BF16) for _ in range(2)]
    Rsg = [Res() for _ in range(2)]
    pss = P.ps([128, NB], F32)
    Rpss = Res()
    pg = [P.ps([128, NB], F32) for _ in range(2)]
    Rpg = [Res() for _ in range(2)]
    pu = [P.ps([128, NB], F32) for _ in range(2)]
    Rpu = [Res() for _ in range(2)]
    po = [P.ps([128, NB], F32) for _ in range(2)]
    Rpo = [Res() for _ in range(2)]

    def load(blk):
        t0 = blk * NB
        for k in range(8):
            S.dma("sync", hTs[blk % 2][:, k, :], hin[k, :, t0:t0 + NB], writes=[RhTs[blk % 2][k]])

    def norm(blk):
        P.fm_norm(hTs[blk % 2], RhTs[blk % 2], g, Rg, hns[blk % 2], Rhns[blk % 2], pss, Rpss)

    load(0)
    norm(0)
    for blk in range(NBLK):
        t0 = blk * NB
        hT, RhT = hTs[blk % 2], RhTs[blk % 2]
        hn, Rhn = hns[blk % 2], Rhns[blk % 2]
        if blk + 1 < NBLK:
            load(blk + 1)
        for c in range(22):
            b = c % 2
            for k in range(8):
                S.op("tensor", lambda e, k=k, c=c, b=b, hn=hn: e.matmul(pg[b][:], lhsT=wgu[:, k, c * 128:(c + 1) * 128], rhs=hn[:, k, :],
                                                                        start=(k == 0), stop=(k == 7)),
                     reads=Rwgu.sel(k, c * 128, (c + 1) * 128) + [Rhn[k]], writes=[Rpg[b]], join=(k > 0))
            for k in range(8):
                S.op("tensor", lambda e, k=k, c=c, b=b, hn=hn: e.matmul(pu[b][:], lhsT=wgu[:, k, 2816 + c * 128:2816 + (c + 1) * 128], rhs=hn[:, k, :],
                                                                        start=(k == 0), stop=(k == 7)),
                     reads=Rwgu.sel(k, 2816 + c * 128, 2816 + (c + 1) * 128) + [Rhn[k]], writes=[Rpu[b]], join=(k > 0))
            S.op("scalar", lambda e, b=b: e.activation(out=sg[b][:], in_=pg[b][:], func=AF.Silu), reads=[Rpg[b]], writes=[Rsg[b]])
            S.op("vector", lambda e, b=b, c=c: e.tensor_tensor(out=act[:, c, :], in0=pu[b][:], in1=sg[b][:], op=ALU.mult),
                 reads=[Rpu[b], Rsg[b]], writes=[Ract[c]])
        if blk + 1 < NBLK:
            norm(blk + 1)
        for d in range(8):
            b = d % 2
            for c in range(22):
                S.op("tensor", lambda e, c=c, d=d, b=b: e.matmul(po[b][:], lhsT=wd[:, c, d * 128:(d + 1) * 128], rhs=act[:, c, :],
                                                                  start=(c == 0), stop=(c == 21)),
                     reads=[Rwd[c], Ract[c]], writes=[Rpo[b]], join=(c > 0))
            S.op("vector", lambda e, d=d, b=b, hT=hT: e.tensor_tensor(out=hT[:, d, :], in0=po[b][:], in1=hT[:, d, :], op=ALU.add),
                 reads=[Rpo[b], RhT[d]], writes=[RhT[d]])
            S.dma("sync", hout[d, :, t0:t0 + NB], hT[:, d, :], reads=[RhT[d]], writes=[P.Rdram], join=True)
    P.finish()


def make_consts():
    c = {}
    c["c_ident"] = np.eye(128, dtype=np.float32)
    s = np.arange(128)
    c["c_tri"] = (s[:, None] <= s[None, :]).astype(np.float32)
    c["c_cbias"] = np.where(s[:, None] <= s[None, :], 0.0, NEG).astype(np.float32)
    inv = 500000.0 ** (-np.arange(0, 16, 2, dtype=np.float64) / 16.0)
    ang = np.arange(NT, dtype=np.float64)[:, None] * inv[None, :]
    c["c_rope"] = np.concatenate([np.cos(ang), np.sin(ang)], axis=1).astype(np.float32)
    u = np.arange(NT)
    c["c_onehot"] = (u[None, :] // 256 == np.arange(16)[:, None]).astype(np.float32)
    c["c_tribias4"] = np.tile(c["c_cbias"], (1, 4)).astype(np.float32)
    b = np.arange(16)
    c["c_past"] = np.where(b[None, :] < b[:, None], 0.0, NEG).astype(np.float32).reshape(256)
    c["c_own"] = (b[None, :] == b[:, None]).astype(np.float32).reshape(256)
    return c


class PLE:
    def __init__(self, P, C, p_ap, g_ap, wpg_ap, wpu_ap, banks, nb=NB, nbuf=1, scratch=None):
        self.P = P
        self.nb = nb
        S = P.S
        self.p_ap = p_ap
        self.g, self.Rg = P.load_vec_fm(g_ap)
        self.wpg, self.Rwpg = P.load_w(wpg_ap, 8, 1024)
        self.wpu, self.Rwpu = P.load_w(wpu_ap, 2, 1024)
        self.Rptm = [Res() for _ in range(nbuf)]
        self.RpT = [[Res(), Res()] for _ in range(nbuf)]
        self.nbuf = nbuf
        self.Rsgate = Res()
        self.Rtmp = Res()
        if scratch is None:
            self.ptm = [P.sb([128, nb // 128, 256], F32)[:] for _ in range(nbuf)]
            self.pT = [P.sb([128, 2, nb], BF16)[:] for _ in range(nbuf)]
            self.sgate = P.sb([128, nb], F32)[:]
            self.tmp = P.sb([128, nb], F32)[:]
        else:
            self.ptm, self.pT, self.sgate, self.tmp = [scratch["ptm"]], [scratch["pT"]], scratch["sgate"], scratch["tmp"]
        self.banks = banks

    def all_res(self):
        return [self.Rptm[0], self.RpT[0][0], self.RpT[0][1], self.Rsgate, self.Rtmp]

    def pre(self, blk, hT, RhT, hn, Rhn, pss, Rpss):
        P = self.P
        S = P.S
        nb = self.nb
        t0 = blk * nb
        i = blk % self.nbuf
        ptm, Rptm, pT, RpT = self.ptm[i], self.Rptm[i], self.pT[i], self.RpT[i]
        (pc, Rpc) = self.banks[2]
        P.fm_norm(hT, RhT, self.g, self.Rg, hn, Rhn, pss, Rpss)
        S.dma("sync", ptm, self.p_ap[t0:t0 + nb, :].rearrange("(s p) d -> p s d", p=128), writes=[Rptm])
        for kk in range(2):
            for s in range(nb // 128):
                S.op("tensor", lambda e, kk=kk, s=s: e.transpose(out=pc[:, s * 128:(s + 1) * 128], in_=ptm[:, s, kk * 128:(kk + 1) * 128],
                                                                 identity=P.ident_f[:]),
                     reads=[Rptm, P.Rc], writes=[Rpc], join=(s > 0))
            S.op("scalar", lambda e, kk=kk: e.copy(out=pT[:, kk, :], in_=pc[:, 0:nb]), reads=[Rpc], writes=[RpT[kk]])

    def main(self, blk, hT, RhT, hn, Rhn):
        P = self.P
        S = P.S
        nb = self.nb
        i = blk % self.nbuf
        pT, RpT = self.pT[i], self.RpT[i]
        (pa, Rpa), (pb, Rpb) = self.banks[:2]
        for d in range(8):
            for k in range(8):
                S.op("tensor", lambda e, k=k, d=d: e.matmul(pa[:, 0:nb], lhsT=self.wpg[:, k, d * 128:(d + 1) * 128], rhs=hn[:, k, :],
                                                             start=(k == 0), stop=(k == 7)),
                     reads=[self.Rwpg[k], Rhn[k]], writes=[Rpa], join=(k > 0))
            S.op("scalar", lambda e: e.activation(out=self.sgate, in_=pa[:, 0:nb], func=AF.Sigmoid), reads=[Rpa], writes=[self.Rsgate])
            for kk in range(2):
                S.op("tensor", lambda e, kk=kk, d=d: e.matmul(pb[:, 0:nb], lhsT=self.wpu[:, kk, d * 128:(d + 1) * 128], rhs=pT[:, kk, :],
                                                               start=(kk == 0), stop=(kk == 1)),
                     reads=[self.Rwpu[kk], RpT[kk]], writes=[Rpb], join=(kk > 0))
            S.op("vector", lambda e: e.tensor_tensor(out=self.tmp, in0=pb[:, 0:nb], in1=self.sgate, op=ALU.mult),
                 reads=[Rpb, self.Rsgate], writes=[self.Rtmp])
            S.op("gpsimd", lambda e, d=d: e.tensor_tensor(out=hT[:, d, :], in0=hT[:, d, :], in1=self.tmp, op=ALU.add),
                 reads=[self.Rtmp, RhT[d]], writes=[RhT[d]])

    def emit(self, blk, hT, RhT, hn, Rhn, pss, Rpss):
        self.pre(blk, hT, RhT, hn, Rhn, pss, Rpss)
        self.main(blk, hT, RhT, hn, Rhn)


def phase_ple_out(nc, C, hin, out_ap, p_ap, g_ap, wpg_ap, wpu_ap, name):
    P = Phase(nc, name)
    S = P.S
    P.consts(C)
    P.norm_setup()
    banks = [(P.ps([128, NB], F32), Res()) for _ in range(5)]
    pss, Rpss = P.ps([128, NB], F32), Res()
    ple = PLE(P, C, p_ap, g_ap, wpg_ap, wpu_ap, banks, nbuf=2)
    hTs = [P.sb([128, 8, NB], F32) for _ in range(2)]
    RhTs = [[Res() for _ in range(8)] for _ in range(2)]
    hns = [P.sb([128, 8, NB], BF16) for _ in range(2)]
    Rhns = [[Res() for _ in range(8)] for _ in range(2)]
    otm = P.sb([128, 4, 1024], F32)
    Rotm = [Res() for _ in range(4)]

    def load(blk):
        t0 = blk * NB
        for k in range(8):
            S.dma("sync", hTs[blk % 2][:, k, :], hin[k, :, t0:t0 + NB], writes=[RhTs[blk % 2][k]])

    load(0)
    ple.pre(0, hTs[0], RhTs[0], hns[0], Rhns[0], pss, Rpss)
    for blk in range(NBLK):
        t0 = blk * NB
        hT, RhT = hTs[blk % 2], RhTs[blk % 2]
        if blk + 1 < NBLK:
            load(blk + 1)
        ple.main(blk, hT, RhT, hns[blk % 2], Rhns[blk % 2])
        if blk + 1 < NBLK:
            n = (blk + 1) % 2
            ple.pre(blk + 1, hTs[n], RhTs[n], hns[n], Rhns[n], pss, Rpss)
        for s in range(4):
            for kq in range(2):
                pt, Rpt = banks[3 + kq]
                for k4 in range(4):
                    k = kq * 4 + k4
                    S.op("tensor", lambda e, k=k, k4=k4, s=s, pt=pt, hT=hT: e.transpose(out=pt[:, k4 * 128:(k4 + 1) * 128], in_=hT[:, k, s * 128:(s + 1) * 128],
                                                                                        identity=P.ident_f[:]),
                         reads=[RhT[k], P.Rc], writes=[Rpt], join=(k4 > 0))
                if kq == 0:
                    S.op("scalar", lambda e, s=s, kq=kq, pt=pt: e.copy(out=otm[:, s, kq * 512:(kq + 1) * 512], in_=pt[:]),
                         reads=[Rpt], writes=[Rotm[s]], join=(kq > 0))
                else:
                    S.op("vector", lambda e, s=s, kq=kq, pt=pt: e.tensor_copy(out=otm[:, s, kq * 512:(kq + 1) * 512], in_=pt[:]),
                         reads=[Rpt], writes=[Rotm[s]], join=(kq > 0))
            S.dma("sync", out_ap[t0 + s * 128:t0 + (s + 1) * 128, :], otm[:, s, :], reads=[Rotm[s]], writes=[P.Rdram], join=True)
    P.finish()


def phase_mlstm(nc, C, x_ap, hout, W, name, nblk=NBLK):
    P = Phase(nc, name)
    realS = P.S
    S = Proxy(realS)
    P.S = S
    P.consts(C)
    P.norm_setup()
    tri_f = P.sb([128, 128], F32)
    cbias = P.sb([128, 128], F32)
    ones_f = P.sb([128, 128], F32)
    S.dma("sync", tri_f[:], C["c_tri"], writes=[P.Rc], join=True)
    S.dma("sync", cbias[:], C["c_cbias"], writes=[P.Rc], join=True)
    S.op("vector", lambda e: e.memset(ones_f[:], 1.0), writes=[P.Rc], join=True)
    g, Rg = P.load_vec_fm(W["norm_mix0"])
    bgate, Rbgate = P.load_bcast(W["a_b_gate"], 16)
    mhg, Rmhg = P.load_bcast(W["a_mh_gain"], 128)
    mhg_h = P.sb([128, 128], F32)
    S.op("vector", lambda e: e.tensor_scalar(out=mhg_h[:], in0=mhg[:], scalar1=0.5, scalar2=None, op0=ALU.mult), reads=[Rmhg], writes=[Rmhg])
    nhalf = P.sb([128, 8], F32)
    S.op("vector", lambda e: e.memset(nhalf[:], -0.5), writes=[P.Rc], join=True)
    win, Rwin = P.load_w(W["a_w_in"], 8, 3088, col_chunk=1544)
    wout, Rwout = P.load_w(W["a_w_out"], 8, 1024)

    xtm = P.sb([128, 4, 1024], F32); Rxtm = [Res() for _ in range(4)]
    hT = P.sb([128, 8, NB], F32); RhT = [Res() for _ in range(8)]
    hn = P.sb([128, 8, NB], BF16); Rhn = [Res() for _ in range(8)]
    qkT = P.sb([128, 8, NB], BF16); Rqk = [Res() for _ in range(8)]
    ktm = P.sb([128, 4, 512], BF16); Rktm = [Res() for _ in range(4)]
    vtm = P.sb([128, 4, 1024], BF16); Rvtm = [Res() for _ in range(4)]
    og = P.sb([128, 4, 1024], BF16); Rog = [Res() for _ in range(4)]
    sgt = P.sb([128, 512], F32); Rsgt = Res()
    gsb = P.sb([128, 4, 16], F32); Rgsb = Res()
    th = P.sb([128, 4, 16], F32); Rth = Res()
    ef = P.sb([128, 4, 8], F32); Ref = Res()
    spf = P.sb([128, 4, 8], F32); Rspf = Res()
    li = P.sb([128, 4, 8], F32); Rli = Res()
    lf = P.sb([128, 4, 8], F32); Rlf = Res()
    g_sb = P.sb([128, 8], F32); Rg_sb = Res()
    bb = P.sb([128, 8], F32); Rbb = Res()
    eg = P.sb([128, 8], F32); Reg = Res()
    wlp = P.sb([128, 8], F32); Rwlp = Res()
    wl = P.sb([128, 8], F32); Rwl = Res()
    egl = P.sb([128, 4], F32); Regl = Res()
    Gd = P.sb([128, 8, 128], F32); RGd = Res()
    arg = P.sb([128, 8, 128], F32); Rarg = Res()
    DT = P.sb([128, 8, 128], F32); RDT = Res()
    PT = P.sb([128, 8, 128], BF16); RPT = Res()
    kw = P.sb([128, 8, 64], BF16); Rkw = Res()
    numXs = P.sb([128, 8, 128], F32); RnumXs = Res()
    num = P.sb([128, 8, 128], F32); Rnum = Res()
    sqn = P.sb([128, 8, 128], F32); Rsqn = Res()
    sm = {n: (P.sb([128, 8], F32), Res()) for n in ["dxs", "den", "dd", "rec", "ssn", "t1", "t2", "lnt", "rs", "coef"]}
    y0 = P.sb([128, 8, 128], F32); Ry0 = Res()
    ytm = P.sb([128, 1024], BF16); Rytm = Res()
    yT = P.sb([128, 8, NB], BF16); RyT = [Res() for _ in range(4)]
    Cst = P.sb([128, 4, 128], F32); nst = P.sb([128, 4], F32); RC = Res()
    Cbf = P.sb([128, 4, 2, 128], BF16); nbf = P.sb([128, 4, 2], BF16); RCbf = Res()
    qbd = P.sb([128, 4, 2, NB], BF16); Rqbd = [Res() for _ in range(4)]
    nt1 = P.sb([128, 4], F32); Rnt1 = Res()

    pS = P.ps([128, 512], F32)
    RpS = Res()
    Rgcs = Rglast = RdenI = RdenX = Rdn = Rpgate = RpS
    pR = [P.ps([128, 512], F32) for _ in range(2)]; RpR = [Res(), Res()]
    pG = P.ps([128, 1024], F32); RpG = Res()
    pT2 = P.ps([128, 1024], F32); RpT2 = Res()
    pY = P.ps([128, 1024], BF16); RpY = Res()
    rot = [0]

    def nextbank():
        rot[0] ^= 1
        return pR[rot[0]], RpR[rot[0]]

    for t in (Cst, nst):
        S.op("vector", lambda e, t=t: e.memset(t[:], 0.0), writes=[RC], join=True)
    for t in (Cbf, nbf):
        S.op("vector", lambda e, t=t: e.memset(t[:], 0.0), writes=[RCbf], join=True)
    for c in range(4):
        S.op("gpsimd", lambda e, c=c: e.memset(qbd[:, c, :, :], 0.0), writes=[Rqbd[c]])

    for blk in range(nblk):
        t0 = blk * NB
        for s in range(4):
            S.dma("sync", xtm[:, s, :], x_ap[t0 + s * 128:t0 + (s + 1) * 128, :], writes=[Rxtm[s]])
        for k in range(8):
            pb, Rpb = nextbank()
            for s in range(4):
                S.op("tensor", lambda e, k=k, s=s, pb=pb: e.transpose(out=pb[:, s * 128:(s + 1) * 128], in_=xtm[:, s, k * 128:(k + 1) * 128],
                                                                       identity=P.ident_f[:]),
                     reads=[Rxtm[s], P.Rc], writes=[Rpb], join=(s > 0))
            S.op("scalar", lambda e, k=k, pb=pb: e.copy(out=hT[:, k, :], in_=pb[:]), reads=[Rpb], writes=[RhT[k]])
        pb, Rpb = nextbank()
        P.fm_norm(hT, RhT, g, Rg, hn, Rhn, pb, Rpb)
        for c in range(8):
            pb, Rpb = nextbank()
            for k in range(8):
                S.op("tensor", lambda e, k=k, c=c, pb=pb: e.matmul(pb[:], lhsT=win[:, k, c * 128:(c + 1) * 128], rhs=hn[:, k, :],
                                                                    start=(k == 0), stop=(k == 7)),
                     reads=[Rwin[k], Rhn[k]], writes=[Rpb], join=(k > 0))
            sc = 0.125 if c < 4 else 1.0
            S.op("scalar", lambda e, c=c, pb=pb, sc=sc: e.activation(out=qkT[:, c, :], in_=pb[:], func=AF.Copy, scale=sc),
                 reads=[Rpb], writes=[Rqk[c]])
            if c < 4:
                S.op("gpsimd", lambda e, c=c: e.tensor_copy(out=qbd[0:64, c, 0, :], in_=qkT[0:64, c, :]), reads=[Rqk[c]], writes=[Rqbd[c]])
                S.op("gpsimd", lambda e, c=c: e.tensor_copy(out=qbd[64:128, c, 1, :], in_=qkT[64:128, c, :]), reads=[Rqk[c]], writes=[Rqbd[c]], join=True)
        def proj_kvo(s):
            ts = slice(s * 128, (s + 1) * 128)
            pb, Rpb = nextbank()
            for k in range(8):
                S.op("tensor", lambda e, k=k, ts=ts, pb=pb: e.matmul(pb[:], lhsT=hn[:, k, ts], rhs=win[:, k, 512:1024], start=(k == 0), stop=(k == 7)),
                     reads=[Rwin[k], Rhn[k]], writes=[Rpb], join=(k > 0))
            S.op("scalar", lambda e, s=s, pb=pb: e.copy(out=ktm[:, s, :], in_=pb[:]), reads=[Rpb], writes=[Rktm[s]])
            for half in range(2):
                pb, Rpb = nextbank()
                c0 = 1024 + half * 512
                for k in range(8):
                    S.op("tensor", lambda e, k=k, ts=ts, pb=pb, c0=c0: e.matmul(pb[:], lhsT=hn[:, k, ts], rhs=win[:, k, c0:c0 + 512],
                                                                                 start=(k == 0), stop=(k == 7)),
                         reads=[Rwin[k], Rhn[k]], writes=[Rpb], join=(k > 0))
                S.op("vector", lambda e, s=s, half=half, pb=pb: e.tensor_copy(out=vtm[:, s, half * 512:(half + 1) * 512], in_=pb[:]),
                     reads=[Rpb], writes=[Rvtm[s]], join=(half > 0))
            for half in range(2):
                pb, Rpb = nextbank()
                c0 = 2048 + half * 512
                for k in range(8):
                    S.op("tensor", lambda e, k=k, ts=ts, pb=pb, c0=c0: e.matmul(pb[:], lhsT=hn[:, k, ts], rhs=win[:, k, c0:c0 + 512],
                                                                                 start=(k == 0), stop=(k == 7)),
                         reads=[Rwin[k], Rhn[k]], writes=[Rpb], join=(k > 0))
                S.op("scalar", lambda e, pb=pb: e.activation(out=sgt[:], in_=pb[:], func=AF.Sigmoid), reads=[Rpb], writes=[Rsgt])
                S.op("gpsimd", lambda e, s=s, half=half: e.tensor_tensor(
                    out=og[:, s, half * 512:(half + 1) * 512].rearrange("p (h v) -> p h v", h=4),
                    in0=sgt[:].rearrange("p (h v) -> p h v", h=4),
                    in1=mhg[:].unsqueeze(1).broadcast_to([128, 4, 128]), op=ALU.mult),
                     reads=[Rsgt, Rmhg], writes=[Rog[s]], join=(half > 0))

        for s in range(4):
            ts = slice(s * 128, (s + 1) * 128)
            for k in range(8):
                S.op("tensor", lambda e, k=k, ts=ts, s=s: e.matmul(pS[:, 64 + s * 16:64 + (s + 1) * 16], lhsT=hn[:, k, ts], rhs=win[:, k, 3072:3088],
                                                                    start=(k == 0), stop=(k == 7)),
                     reads=[Rwin[k], Rhn[k]], writes=[Rpgate], join=(k > 0))
            S.op("vector", lambda e, s=s: e.tensor_tensor(out=gsb[:, s, :], in0=pS[:, 64 + s * 16:64 + (s + 1) * 16], in1=bgate[:], op=ALU.add),
                 reads=[Rpgate, Rbgate], writes=[Rgsb], join=(s > 0))
        S.op("scalar", lambda e: e.activation(out=th[:], in_=gsb[:], func=AF.Tanh, scale=1.0 / 15.0), reads=[Rgsb], writes=[Rth])
        S.op("vector", lambda e: e.tensor_scalar(out=li[:], in0=th[:, :, 0:8], scalar1=15.0, scalar2=None, op0=ALU.mult), reads=[Rth], writes=[Rli])
        S.op("scalar", lambda e: e.activation(out=ef[:], in_=th[:, :, 8:16], func=AF.Exp, scale=-15.0), reads=[Rth], writes=[Ref])
        S.op("scalar", lambda e: e.activation(out=spf[:], in_=ef[:], func=AF.Ln, bias=P.one_t[:, 0:1]), reads=[Ref, P.Rc], writes=[Rspf])
        S.op("vector", lambda e: e.tensor_scalar(out=lf[:], in0=spf[:], scalar1=-1.0, scalar2=None, op0=ALU.mult), reads=[Rspf], writes=[Rlf])

        proj_kvo(0)
        for s in range(4):
            ts = slice(s * 128, (s + 1) * 128)
            if s < 3:
                d_ = Deferred()
                S.tgt = d_
                proj_kvo(s + 1)
                S.tgt = Mux(realS, mm_chunks(d_.q))
            else:
                S.tgt = realS
            S.op("tensor", lambda e, s=s: e.matmul(pS[:, 0:8], lhsT=tri_f[:], rhs=lf[:, s, :], start=True, stop=True),
                 reads=[Rlf, P.Rc], writes=[Rgcs])
            S.op("tensor", lambda e, s=s: e.matmul(pS[:, 8:16], lhsT=ones_f[:], rhs=lf[:, s, :], start=True, stop=True),
                 reads=[Rlf, P.Rc], writes=[Rglast])
            S.op("vector", lambda e: e.tensor_copy(out=g_sb[:], in_=pS[:, 0:8]), reads=[Rgcs], writes=[Rg_sb])
            S.op("vector", lambda e, s=s: e.tensor_tensor(out=bb[:], in0=li[:, s, :], in1=g_sb[:], op=ALU.subtract), reads=[Rli, Rg_sb], writes=[Rbb])
            S.op("scalar", lambda e: e.activation(out=eg[:], in_=g_sb[:], func=AF.Exp), reads=[Rg_sb], writes=[Reg])
            S.op("vector", lambda e: e.tensor_tensor(out=wlp[:], in0=pS[:, 8:16], in1=bb[:], op=ALU.add), reads=[Rglast, Rbb], writes=[Rwlp])
            S.op("scalar", lambda e: e.activation(out=wl[:], in_=wlp[:], func=AF.Exp), reads=[Rwlp], writes=[Rwl])
            S.op("scalar", lambda e: e.activation(out=egl[0:64, :], in_=pS[0:64, 8:16:2], func=AF.Exp), reads=[Rglast], writes=[Regl])
            S.op("scalar", lambda e: e.activation(out=egl[64:128, :], in_=pS[64:128, 9:16:2], func=AF.Exp), reads=[Rglast], writes=[Regl], join=True)
            S.op("vector", lambda e: e.tensor_tensor(out=Gd[:], in0=g_sb[:].unsqueeze(2).broadcast_to([128, 8, 128]),
                                                      in1=P.ident_f[:].unsqueeze(1).broadcast_to([128, 8, 128]), op=ALU.mult),
                 reads=[Rg_sb, P.Rc], writes=[RGd])
            for half in range(2):
                S.op("tensor", lambda e, half=half: e.matmul(pG[:, half * 512:(half + 1) * 512], lhsT=ones_f[:],
                                                               rhs=Gd[:, half * 4:(half + 1) * 4, :].rearrange("p h j -> p (h j)"),
                                                               start=True, stop=True),
                     reads=[RGd, P.Rc], writes=[RpG], join=(half > 0))
            for h in range(8):
                S.op("vector", lambda e, h=h: e.scalar_tensor_tensor(out=arg[:, h, :], in0=pG[:, h * 128:(h + 1) * 128], scalar=bb[:, h:h + 1], op0=ALU.add,
                                                                      in1=cbias[:], op1=ALU.add),
                     reads=[RpG, Rbb, P.Rc], writes=[Rarg], join=(h > 0))
            S.op("scalar", lambda e: e.activation(out=DT[:], in_=arg[:], func=AF.Exp), reads=[Rarg], writes=[RDT])
            for c in range(4):
                S.op("tensor", lambda e, c=c, ts=ts: e.matmul(pT2[:, c * 256:(c + 1) * 256], lhsT=qkT[:, 4 + c, ts], rhs=qbd[:, c, :, ts],
                                                               start=True, stop=True),
                     reads=[Rqbd[c], Rqk[4 + c]], writes=[RpT2], join=(c > 0))
            S.op("vector", lambda e: e.tensor_tensor(out=PT[:].rearrange("p h j -> p (h j)"), in0=pT2[:], in1=DT[:].rearrange("p h j -> p (h j)"), op=ALU.mult),
                 reads=[RpT2, RDT], writes=[RPT])
            for h in range(8):
                S.op("tensor", lambda e, h=h, s=s: e.matmul(pG[:, h * 128:(h + 1) * 128], lhsT=PT[:, h, :], rhs=vtm[:, s, h * 128:(h + 1) * 128],
                                                             start=True, stop=True),
                     reads=[RPT, Rvtm[s]], writes=[RpG], join=(h > 0))
            for h in range(8):
                S.op("tensor", lambda e, h=h: e.matmul(pS[:, 16 + h:17 + h], lhsT=PT[:, h, :], rhs=P.ones_b[:, 0:1], start=True, stop=True),
                     reads=[RPT, P.Rc], writes=[RdenI], join=(h > 0))
            for c in range(4):
                S.op("tensor", lambda e, c=c, ts=ts: e.matmul(pT2[:, c * 256:(c + 1) * 256], lhsT=qkT[:, c, ts], rhs=Cbf[:, c, :, :],
                                                               start=True, stop=True),
                     reads=[Rqk[c], RCbf], writes=[RpT2], join=(c > 0))
            for c in range(4):
                S.op("tensor", lambda e, c=c, ts=ts: e.matmul(pS[:, 24 + 2 * c:26 + 2 * c], lhsT=qkT[:, c, ts], rhs=nbf[:, c, :],
                                                               start=True, stop=True),
                     reads=[Rqk[c], RCbf], writes=[RdenX], join=(c > 0))
            S.op("vector", lambda e, s=s: e.tensor_tensor(out=kw[:], in0=ktm[:, s, :].rearrange("p (h d) -> p h d", h=8),
                                                           in1=wl[:].unsqueeze(2).broadcast_to([128, 8, 64]), op=ALU.mult),
                 reads=[Rktm[s], Rwl], writes=[Rkw])
            pd, Rpd = pY[:].bitcast(F32), RpY
            for h in range(8):
                c, ph = h // 2, h % 2
                prt = slice(ph * 64, (ph + 1) * 64)
                S.op("tensor", lambda e, h=h, c=c, prt=prt, s=s, pd=pd: e.matmul(pd[prt, c * 128:(c + 1) * 128], lhsT=kw[:, h, :], rhs=vtm[:, s, h * 128:(h + 1) * 128],
                                                                                  start=True, stop=True),
                     reads=[Rkw, Rvtm[s]], writes=[Rpd], join=(h > 0))
            for h in range(8):
                c, ph = h // 2, h % 2
                prt = slice(ph * 64, (ph + 1) * 64)
                S.op("tensor", lambda e, h=h, c=c, prt=prt: e.matmul(pS[prt, 32 + c:33 + c], lhsT=kw[:, h, :], rhs=P.ones_b[:, 0:1], start=True, stop=True),
                     reads=[Rkw, P.Rc], writes=[Rdn], join=(h > 0))
            for c in range(4):
                S.op("vector", lambda e, c=c, pd=pd: e.scalar_tensor_tensor(out=Cst[:, c, :], in0=Cst[:, c, :], scalar=egl[:, c:c + 1], op0=ALU.mult,
                                                                            in1=pd[:, c * 128:(c + 1) * 128], op1=ALU.add),
                     reads=[Rpd, Regl, RC], writes=[RC])
            S.op("vector", lambda e: e.tensor_tensor(out=nt1[:], in0=nst[:], in1=egl[:], op=ALU.mult), reads=[RC, Regl], writes=[Rnt1])
            S.op("vector", lambda e: e.tensor_tensor(out=nst[:], in0=pS[:, 32:36], in1=nt1[:], op=ALU.add), reads=[Rdn, Rnt1], writes=[RC])
            S.op("vector", lambda e: e.tensor_tensor(out=numXs[:], in0=pT2[:].rearrange("p (h v) -> p h v", h=8),
                                                      in1=eg[:].unsqueeze(2).broadcast_to([128, 8, 128]), op=ALU.mult),
                 reads=[RpT2, Reg], writes=[RnumXs])
            S.op("vector", lambda e: e.tensor_tensor(out=num[:].rearrange("p h v -> p (h v)"), in0=pG[:], in1=numXs[:].rearrange("p h v -> p (h v)"), op=ALU.add),
                 reads=[RpG, RnumXs], writes=[Rnum])
            S.op("gpsimd", lambda e: e.tensor_copy(out=Cbf[0:64, :, 0, :], in_=Cst[0:64, :, :]), reads=[RC], writes=[RCbf])
            S.op("gpsimd", lambda e: e.tensor_copy(out=Cbf[64:128, :, 1, :], in_=Cst[64:128, :, :]), reads=[RC], writes=[RCbf], join=True)
            S.op("gpsimd", lambda e: e.tensor_copy(out=nbf[0:64, :, 0], in_=nst[0:64, :]), reads=[RC], writes=[RCbf], join=True)
            S.op("gpsimd", lambda e: e.tensor_copy(out=nbf[64:128, :, 1], in_=nst[64:128, :]), reads=[RC], writes=[RCbf], join=True)
            T = lambda n: sm[n][0]
            R_ = lambda n: sm[n][1]
            S.op("vector", lambda e: e.tensor_tensor(out=T("dxs")[:], in0=pS[:, 24:32], in1=eg[:], op=ALU.mult), reads=[RdenX, Reg], writes=[R_("dxs")])
            S.op("vector", lambda e: e.tensor_tensor(out=T("den")[:], in0=pS[:, 16:24], in1=T("dxs")[:], op=ALU.add), reads=[RdenI, R_("dxs")], writes=[R_("den")])
            S.op("vector", lambda e: e.scalar_tensor_tensor(out=T("t1")[:], in0=T("den")[:], scalar=-1.0, op0=ALU.mult, in1=T("den")[:], op1=ALU.max),
                 reads=[R_("den")], writes=[R_("t1")])
            S.op("vector", lambda e: e.tensor_scalar(out=T("dd")[:], in0=T("t1")[:], scalar1=1.0, scalar2=None, op0=ALU.max), reads=[R_("t1")], writes=[R_("dd")])
            S.op("vector", lambda e: e.reciprocal(out=T("rec")[:], in_=T("dd")[:]), reads=[R_("dd")], writes=[R_("rec")])
            S.op("gpsimd", lambda e: e.tensor_tensor(out=sqn[:], in0=num[:], in1=num[:], op=ALU.mult), reads=[Rnum], writes=[Rsqn])
            S.op("vector", lambda e: e.tensor_reduce(out=T("ssn")[:], in_=sqn[:], axis=AX.X, op=ALU.add), reads=[Rsqn], writes=[R_("ssn")])
            S.op("vector", lambda e: e.tensor_tensor(out=T("t1")[:], in0=T("rec")[:], in1=T("rec")[:], op=ALU.mult), reads=[R_("rec")], writes=[R_("t1")])
            S.op("vector", lambda e: e.tensor_tensor(out=T("t2")[:], in0=T("t1")[:], in1=T("ssn")[:], op=ALU.mult), reads=[R_("t1"), R_("ssn")], writes=[R_("t2")])
            S.op("scalar", lambda e: e.activation(out=T("lnt")[:], in_=T("t2")[:], func=AF.Ln, scale=1.0 / 128.0, bias=P.eps_t[:, 0:1]),
                 reads=[R_("t2"), P.Rc], writes=[R_("lnt")])
            S.op("scalar", lambda e: e.activation(out=T("rs")[:], in_=T("lnt")[:], func=AF.Exp, scale=-0.5), reads=[R_("lnt")], writes=[R_("rs")])
            S.op("vector", lambda e: e.tensor_tensor(out=T("coef")[:], in0=T("rec")[:], in1=T("rs")[:], op=ALU.mult), reads=[R_("rec"), R_("rs")], writes=[R_("coef")])
            S.op("vector", lambda e: e.tensor_tensor(out=y0[:], in0=num[:], in1=T("coef")[:].unsqueeze(2).broadcast_to([128, 8, 128]), op=ALU.mult),
                 reads=[Rnum, R_("coef")], writes=[Ry0])
            S.op("gpsimd", lambda e, s=s: e.tensor_tensor(out=ytm[:], in0=y0[:].rearrange("p h v -> p (h v)"), in1=og[:, s, :], op=ALU.mult),
                 reads=[Ry0, Rog[s]], writes=[Rytm])
            for h in range(8):
                S.op("tensor", lambda e, h=h: e.transpose(out=pY[:, h * 128:(h + 1) * 128], in_=ytm[:, h * 128:(h + 1) * 128], identity=P.ident_b[:]),
                     reads=[Rytm, P.Rc], writes=[RpY], join=(h > 0))
            S.op("scalar", lambda e, ts=ts: e.copy(out=yT[:, :, ts], in_=pY[:].rearrange("p (h j) -> p h j", h=8)), reads=[RpY], writes=[RyT[s]])
            if s < 3:
                S.tgt.flush()
            S.tgt = realS
        for d in range(8):
            pb, Rpb = nextbank()
            for h in range(8):
                S.op("tensor", lambda e, h=h, d=d, pb=pb: e.matmul(pb[:], lhsT=wout[:, h, d * 128:(d + 1) * 128], rhs=yT[:, h, :], start=(h == 0), stop=(h == 7)),
                     reads=[Rwout[h]] + RyT, writes=[Rpb], join=(h > 0))
            S.op("vector", lambda e, d=d, pb=pb: e.tensor_tensor(out=hT[:, d, :], in0=pb[:], in1=hT[:, d, :], op=ALU.add), reads=[Rpb, RhT[d]], writes=[RhT[d]])
            S.dma("sync", hout[d, :, t0:t0 + NB], hT[:, d, :], reads=[RhT[d]], writes=[P.Rdram], join=True)
    P.S = realS
    P.finish()


def phase_moba(nc, C, hin, hout, W, name, nblk=NBLK, dbg=None):
    G = 2
    P = Phase(nc, name)
    realS = P.S
    S = Proxy(realS)
    P.S = S
    P.consts(C)
    P.norm_setup()
    c256 = P.sb([128, 1], F32)
    S.op("vector", lambda e: e.memset(c256[:], 1.0 / 256.0), writes=[P.Rc], join=True)
    tri4 = P.sb([128, 512], BF16)
    S.dma("gpsimd", tri4[:], C["c_tribias4"], writes=[P.Rc], join=True)
    ropet = P.sb([128, 32, 16], F32)
    S.dma("sync", ropet[:], C["c_rope"].rearrange("(i p) c -> p i c", p=128), writes=[P.Rc], join=True)
    pastb, Rpastb = P.load_bcast(C["c_past"], 256)
    ownb, Rownb = P.load_bcast(C["c_own"], 256)
    g_kv, Rg_kv = P.load_vec_fm(W["kv_norm"])
    g_mix, Rg_mix = P.load_vec_fm(W["norm_mix1"])
    knorm, Rknorm = P.load_bcast(W["k_norm"], 64)
    qnorm, Rqnorm = P.load_bcast(W["b_q_norm"], 64)
    pR = [P.ps([128, 512], F32) for _ in range(2)]; RpR = [Res(), Res()]
    psc = [P.ps([128, G * 512], F32) for _ in range(2)]; Rpsc = [Res(), Res()]
    pop = [P.ps([128, 512], F32) for _ in range(2)]; Rpop = [Res(), Res()]
    rot = [0]

    def nextbank():
        rot[0] ^= 1
        return pR[rot[0]], RpR[rot[0]]

    arena = P.sb([128, 2688], F32)
    scratch = {"ptm": arena[:, 0:1024].rearrange("p (s d) -> p s d", s=4),
               "pT": arena[:, 1024:1536].bitcast(BF16).rearrange("p (k t) -> p k t", k=2),
               "sgate": arena[:, 1536:2048], "tmp": arena[:, 2048:2560]}
    ple = PLE(P, C, W["p0"], W["norm_ple0"], W["w_ple_gate0"], W["w_ple_up0"], [(pR[0], RpR[0]), (pR[1], RpR[1]), (pR[0], RpR[0])],
              scratch=scratch)
    wkv, Rwkv = P.load_w(W["w_kv"], 8, 512)
    wq, Rwq = P.load_w(W["b_w_q"], 8, 1024)
    wo, Rwo = P.load_w(W["b_w_o"], 8, 1024)

    hT = P.sb([128, 8, NB], F32); RhT = [Res() for _ in range(8)]
    hn = P.sb([128, 8, NB], BF16); Rhn = [Res() for _ in range(8)]
    hkv = P.sb([128, 8, NB], BF16); Rhkv = [Res() for _ in range(8)]
    KT = P.sb([80, 4, NT], BF16); RKT = [Res() for _ in range(32)]
    Vaug = P.sb([128, 4, 32, 128], BF16); RV = [Res() for _ in range(32)]
    kmT = P.sb([80, 4, 16], BF16); RkmT = Res()
    kms = P.sb([64, 4, 2], F32); Rkms = Res()
    ksb = P.sb([128, 4, 64], F32); Rksb = Res()
    sqk = P.sb([128, 4, 64], F32); Rsqk = Res()
    kbf = P.sb([128, 4, 64], BF16); Rkbf = Res()
    qsb = arena[:, 0:1024].rearrange("p (h d) -> p h d", h=16); Rqsb = Res()
    sqq = arena[:, 1024:2048].rearrange("p (h d) -> p h d", h=16); Rsqq = Res()
    qa = arena[:, 2048:2688].bitcast(BF16).rearrange("p (h d) -> p h d", h=16); Rqa = Res()

    def arena_fence(to_ple):
        qres = [Rqsb, Rsqq, Rqa]
        if to_ple:
            S.op("gpsimd", lambda e: e.memset(arena[:, 0:1], 0.0), reads=qres, writes=ple.all_res())
        else:
            S.op("gpsimd", lambda e: e.memset(arena[:, 0:1], 0.0), reads=ple.all_res(), writes=qres)
    QTa = [P.sb([80, 16, 128], BF16) for _ in range(2)]; RQTa = [Res(), Res()]
    gm = P.sb([128, 16, 16], F32); Rgm = Res()
    mx8 = P.sb([128, 16, 8], F32); Rmx8 = Res()
    vis = P.sb([128, 16, 16], F32); Rvis = Res()
    skq = {n: (P.sb([128, 16], F32), Res()) for n in ["ss", "ln", "r"]}
    skk = {n: (P.sb([128, 16], F32), Res()) for n in ["ss", "ln", "r"]}
    rtq = {n: (P.sb([128, 16, 8], F32), Res()) for n in ["t1", "t2", "t3", "t4"]}
    PTb = [P.sb([128, G * 512], BF16) for _ in range(2)]; RPTb = [Res() for _ in range(2)]
    rec = P.sb([128, 512], F32); Rrec = Res()
    OTb = P.sb([128, 8, NB], BF16); ROTb = [Res() for _ in range(4)]

    for kvh in range(4):
        for c0 in range(0, NT, 1024):
            S.dma("gpsimd", KT[64:80, kvh, c0:c0 + 1024], C["c_onehot"][:, c0:c0 + 1024], writes=[P.Rc], join=True)
    S.op("vector", lambda e: e.memset(Vaug[:].rearrange("p a b c -> p (a b c)"), 1.0), writes=[P.Rc], join=True)
    S.op("gpsimd", lambda e: e.memset(kmT[:], 0.0), writes=[RkmT])
    for i in range(2):
        S.op("gpsimd", lambda e, i=i: e.memset(QTa[i][:], 0.0), writes=[RQTa[i]])

    def head_norm_rope(x, Rx, sq_, Rsq_, nh, gbc, Rgbc, it, sk, rt):
        (ss, Rss), (ln, Rln), (r, Rr) = sk["ss"], sk["ln"], sk["r"]
        S.op("gpsimd", lambda e: e.tensor_tensor(out=sq_[:], in0=x[:], in1=x[:], op=ALU.mult), reads=[Rx], writes=[Rsq_])
        S.op("vector", lambda e: e.tensor_reduce(out=ss[:, 0:nh], in_=sq_[:], axis=AX.X, op=ALU.add), reads=[Rsq_], writes=[Rss])
        yield
        S.op("scalar", lambda e: e.activation(out=ln[:, 0:nh], in_=ss[:, 0:nh], func=AF.Ln, scale=1.0 / 64.0, bias=P.eps_t[:, 0:1]),
             reads=[Rss, P.Rc], writes=[Rln])
        S.op("scalar", lambda e: e.activation(out=r[:, 0:nh], in_=ln[:, 0:nh], func=AF.Exp, scale=-0.5), reads=[Rln], writes=[Rr])
        yield
        S.op("vector", lambda e: e.tensor_tensor(out=x[:], in0=x[:], in1=r[:, 0:nh].unsqueeze(2).broadcast_to([128, nh, 64]), op=ALU.mult),
             reads=[Rx, Rr], writes=[Rx])
        S.op("vector", lambda e: e.tensor_tensor(out=x[:], in0=x[:], in1=gbc[:].unsqueeze(1).broadcast_to([128, nh, 64]), op=ALU.mult),
             reads=[Rx, Rgbc], writes=[Rx])
        yield
        cs = ropet[:, it, 0:8].unsqueeze(1).broadcast_to([128, nh, 8])
        sn = ropet[:, it, 8:16].unsqueeze(1).broadcast_to([128, nh, 8])
        x1 = x[:, :, 0:8]
        x2 = x[:, :, 8:16]
        tt = {k: v[0][:, 0:nh, :] for k, v in rt.items()}
        Rt = {k: v[1] for k, v in rt.items()}
        S.op("vector", lambda e: e.tensor_tensor(out=tt["t1"], in0=x1, in1=cs, op=ALU.mult), reads=[Rx, P.Rc], writes=[Rt["t1"]])
        S.op("vector", lambda e: e.tensor_tensor(out=tt["t2"], in0=x2, in1=sn, op=ALU.mult), reads=[Rx, P.Rc], writes=[Rt["t2"]])
        S.op("vector", lambda e: e.tensor_tensor(out=tt["t3"], in0=x2, in1=cs, op=ALU.mult), reads=[Rx, P.Rc], writes=[Rt["t3"]])
        S.op("vector", lambda e: e.tensor_tensor(out=tt["t4"], in0=x1, in1=sn, op=ALU.mult), reads=[Rx, P.Rc], writes=[Rt["t4"]])
        yield
        S.op("vector", lambda e: e.tensor_tensor(out=x1, in0=tt["t1"], in1=tt["t2"], op=ALU.subtract), reads=[Rt["t1"], Rt["t2"], Rx], writes=[Rx])
        S.op("vector", lambda e: e.tensor_tensor(out=x2, in0=tt["t3"], in1=tt["t4"], op=ALU.add), reads=[Rt["t3"], Rt["t4"], Rx], writes=[Rx])
        yield

    def kv_path(blk, s):
        it = blk * 4 + s
        ts = slice(s * 128, (s + 1) * 128)
        pb, Rpb = nextbank()
        for k in range(8):
            S.op("tensor", lambda e, k=k, ts=ts, pb=pb: e.matmul(pb[:], lhsT=hkv[:, k, ts], rhs=wkv[:, k, :], start=(k == 0), stop=(k == 7)),
                 reads=[Rwkv[k], Rhkv[k]], writes=[Rpb], join=(k > 0))
        S.op("scalar", lambda e, pb=pb: e.copy(out=ksb[:].rearrange("p h d -> p (h d)"), in_=pb[:, 0:256]), reads=[Rpb], writes=[Rksb])
        S.op("scalar", lambda e, pb=pb, it=it: e.copy(out=Vaug[:, :, it, 0:64], in_=pb[:, 256:512].rearrange("p (h d) -> p h d", h=4)),
             reads=[Rpb, P.Rc], writes=[RV[it]])
        for _ in head_norm_rope(ksb, Rksb, sqk, Rsqk, 4, knorm, Rknorm, it, skk, rtq):
            pass
        S.op("gpsimd", lambda e: e.tensor_copy(out=kbf[:], in_=ksb[:]), reads=[Rksb], writes=[Rkbf])
        pb, Rpb = nextbank()
        pbb = pb[:].bitcast(BF16)
        for kvh in range(4):
            S.op("tensor", lambda e, kvh=kvh, pbb=pbb: e.transpose(out=pbb[0:64, kvh * 128:(kvh + 1) * 128], in_=kbf[:, kvh, :], identity=P.ident_b[:]),
                 reads=[Rkbf, P.Rc], writes=[Rpb], join=(kvh > 0))
        S.op("scalar", lambda e, it=it, pbb=pbb: e.copy(out=KT[0:64, :, it * 128:(it + 1) * 128], in_=pbb[0:64, 0:512].rearrange("p (h t) -> p h t", h=4)),
             reads=[Rpb, P.Rc], writes=[RKT[it]])
        pb, Rpb = nextbank()
        for kvh in range(4):
            S.op("tensor", lambda e, kvh=kvh, pb=pb: e.matmul(pb[0:64, kvh:kvh + 1], lhsT=ksb[:, kvh, :], rhs=c256[:, 0:1], start=True, stop=True),
                 reads=[Rksb, P.Rc], writes=[Rpb], join=(kvh > 0))
        S.op("vector", lambda e, s=s, pb=pb: e.tensor_copy(out=kms[:, :, s % 2], in_=pb[0:64, 0:4]), reads=[Rpb], writes=[Rkms], join=(s % 2 == 1))
        if s % 2 == 1:
            n = it // 2
            S.op("vector", lambda e, n=n: e.tensor_tensor(out=kmT[0:64, :, n], in0=kms[:, :, 0], in1=kms[:, :, 1], op=ALU.add),
                 reads=[Rkms], writes=[RkmT])

    def q_path(blk, s):
        it = blk * 4 + s
        b = it // 2
        ts = slice(s * 128, (s + 1) * 128)
        QT, RQT = QTa[it % 2], RQTa[it % 2]
        for half in range(2):
            pb, Rpb = nextbank()
            for k in range(8):
                S.op("tensor", lambda e, k=k, ts=ts, pb=pb, half=half: e.matmul(pb[:], lhsT=hn[:, k, ts], rhs=wq[:, k, half * 512:(half + 1) * 512],
                                                                                 start=(k == 0), stop=(k == 7)),
                     reads=[Rwq[k], Rhn[k]], writes=[Rpb], join=(k > 0))
                if k % 4 == 3:
                    yield
            S.op("vector", lambda e, pb=pb, half=half: e.tensor_copy(out=qsb[:, half * 8:(half + 1) * 8, :].rearrange("p h d -> p (h d)"), in_=pb[:]),
                 reads=[Rpb], writes=[Rqsb], join=(half > 0))
            yield
        for _ in head_norm_rope(qsb, Rqsb, sqq, Rsqq, 16, qnorm, Rqnorm, it, skq, rtq):
            yield
        S.op("gpsimd", lambda e: e.tensor_copy(out=qa[:, :, 0:64], in_=qsb[:]), reads=[Rqsb], writes=[Rqa])
        yield
        for r_ in range(2):
            pb, Rpb = nextbank()
            pbb = pb[:].bitcast(BF16)
            for j in range(8):
                h = r_ * 8 + j
                S.op("tensor", lambda e, h=h, j=j, pbb=pbb: e.transpose(out=pbb[0:64, j * 128:(j + 1) * 128], in_=qa[:, h, 0:64], identity=P.ident_b[:]),
                     reads=[Rqa, P.Rc], writes=[Rpb], join=(j > 0))
                if j % 4 == 3:
                    yield
            S.op("vector", lambda e, r_=r_, pbb=pbb: e.tensor_copy(out=QT[0:64, r_ * 8:(r_ + 1) * 8, :], in_=pbb[0:64, :].rearrange("p (h t) -> p h t", h=8)),
                 reads=[Rpb], writes=[RQT], join=(r_ > 0))
            yield
        pb, Rpb = nextbank()
        for h in range(16):
            S.op("tensor", lambda e, h=h, pb=pb: e.matmul(pb[:, h * 16:(h + 1) * 16], lhsT=QT[:, h, :], rhs=kmT[:, h // 4, :], start=True, stop=True),
                 reads=[RQT, RkmT], writes=[Rpb], join=(h > 0))
            if h % 4 == 3:
                yield
        S.op("vector", lambda e, b=b, pb=pb: e.tensor_tensor(out=gm[:], in0=pb[:, 0:256].rearrange("p (h n) -> p h n", h=16),
                                                              in1=pastb[:, b * 16:(b + 1) * 16].unsqueeze(1).broadcast_to([128, 16, 16]), op=ALU.add),
             reads=[Rpb, Rpastb], writes=[Rgm])
        yield
        for h in range(16):
            S.op("vector", lambda e, h=h: e.max(out=mx8[:, h, :], in_=gm[:, h, :]), reads=[Rgm], writes=[Rmx8], join=(h > 0))
            if h % 4 == 3:
                yield
        S.op("vector", lambda e: e.tensor_tensor(out=vis[:], in0=gm[:], in1=mx8[:, :, 2:3].broadcast_to([128, 16, 16]), op=ALU.is_ge),
             reads=[Rgm, Rmx8], writes=[Rvis])
        S.op("vector", lambda e, b=b: e.tensor_tensor(out=vis[:], in0=vis[:], in1=ownb[:, b * 16:(b + 1) * 16].unsqueeze(1).broadcast_to([128, 16, 16]), op=ALU.max),
             reads=[Rvis, Rownb], writes=[Rvis])
        S.op("vector", lambda e: e.tensor_scalar(out=qa[:, :, 64:80], in0=vis[:], scalar1=-NEG, scalar2=NEG, op0=ALU.mult, op1=ALU.add),
             reads=[Rvis, Rqa], writes=[Rqa])
        yield
        for r_ in range(2):
            pb, Rpb = nextbank()
            pbb = pb[:].bitcast(BF16)
            for j in range(8):
                h = r_ * 8 + j
                S.op("tensor", lambda e, h=h, j=j, pbb=pbb: e.transpose(out=pbb[0:80, j * 128:(j + 1) * 128], in_=qa[:, h, :], identity=P.ident_b[:]),
                     reads=[Rqa, P.Rc], writes=[Rpb], join=(j > 0))
                if j % 4 == 3:
                    yield
            S.op("vector", lambda e, r_=r_, pbb=pbb: e.tensor_copy(out=QT[:, r_ * 8:(r_ + 1) * 8, :], in_=pbb[0:80, :].rearrange("p (h t) -> p h t", h=8)),
                 reads=[Rpb], writes=[RQT], join=(r_ > 0))
            yield

    nsc = [0]

    def attention(blk, s):
        it = blk * 4 + s
        ts = slice(s * 128, (s + 1) * 128)
        QT, RQT = QTa[it % 2], RQTa[it % 2]
        L = []
        for kvh in range(4):
            for j0 in range(0, it + 1, G):
                L.append((kvh, j0, min(it + 1, j0 + G)))
        bufs = {}

        def qk(n):
            kvh, j0, j1 = L[n]
            m = nsc[0]
            nsc[0] += 1
            bufs[n] = m % 2
            ps_, Rps_ = psc[m % 2], Rpsc[m % 2]
            qg = QT[:, 4 * kvh:4 * kvh + 4, :].rearrange("p h t -> p (h t)")
            for j in range(j0, j1):
                o = (j - j0) * 512
                diag = (j == it)
                S.op("tensor", lambda e, kvh=kvh, j=j, ps_=ps_, qg=qg, diag=diag, o=o: e.matmul(ps_[:, o:o + 512], lhsT=KT[:, kvh, j * 128:(j + 1) * 128], rhs=qg,
                                                                                               start=True, stop=(not diag)),
                     reads=[RKT[j], RQT, P.Rc], writes=[Rps_], join=(j > j0))
                if diag:
                    S.op("tensor", lambda e, ps_=ps_, o=o: e.matmul(ps_[:, o:o + 512], lhsT=P.ident_b[:], rhs=tri4[:], start=False, stop=True),
                         reads=[P.Rc], writes=[Rps_], join=True)

        def ex(n):
            kvh, j0, j1 = L[n]
            bi = bufs[n]
            w = (j1 - j0) * 512
            S.op("scalar", lambda e, bi=bi, w=w: e.activation(out=PTb[bi][:, 0:w], in_=psc[bi][:, 0:w], func=AF.Exp, scale=0.125),
                 reads=[Rpsc[bi]], writes=[RPTb[bi]])

        def pv(n):
            kvh, j0, j1 = L[n]
            bi = bufs[n]
            po, Rpo = pop[kvh % 2], Rpop[kvh % 2]
            for j in range(j0, j1):
                o = (j - j0) * 512
                S.op("tensor", lambda e, kvh=kvh, j=j, bi=bi, po=po, o=o, it=it: e.matmul(po[:], lhsT=Vaug[:, kvh, j, :], rhs=PTb[bi][:, o:o + 512],
                                                                                         start=(j == 0), stop=(j == it)),
                     reads=[RV[j], RPTb[bi], P.Rc], writes=[Rpo], join=(j > 0))

        def epi(kvh):
            po, Rpo = pop[kvh % 2], Rpop[kvh % 2]
            S.op("vector", lambda e, po=po: e.reciprocal(out=rec[64:128, :], in_=po[64:128, :]), reads=[Rpo], writes=[Rrec])
            for gq in range(4):
                hh = 4 * kvh + gq
                pr, ph = hh // 2, hh % 2
                S.op("vector", lambda e, po=po, gq=gq, pr=pr, ph=ph, ts=ts: e.tensor_tensor(
                    out=OTb[ph * 64:(ph + 1) * 64, pr, ts], in0=po[0:64, gq * 128:(gq + 1) * 128], in1=rec[64:128, gq * 128:(gq + 1) * 128], op=ALU.mult),
                     reads=[Rpo, Rrec], writes=[ROTb[s]], join=True)

        qk(0)
        for n in range(len(L)):
            if n + 1 < len(L):
                qk(n + 1)
            ex(n)
            pv(n)
            if n + 1 == len(L) or L[n + 1][0] != L[n][0]:
                epi(L[n][0])
            yield

    def drive(main, side, side_len):
        steps = list(range(0))
        mains = main
        if side is None:
            for _ in mains:
                pass
            return
        done = [False]

        def adv(k):
            for _ in range(k):
                if done[0]:
                    return
                try:
                    next(side)
                except StopIteration:
                    done[0] = True
        nmain = side_len[0]
        per = max(1, -(-side_len[1] // max(1, nmain)))
        for _ in mains:
            adv(per)
        while not done[0]:
            adv(8)

    def side_gen(blk, s):
        d = Deferred()
        S.tgt = d
        kv_path(blk, s)
        S.tgt = realS
        for kind, a_, k_ in d.q:
            getattr(realS, kind)(*a_, **k_)
            yield
        for _ in q_path(blk, s):
            yield

    for blk in range(nblk):
        t0 = blk * NB
        for k in range(8):
            S.dma("sync", hT[:, k, :], hin[k, :, t0:t0 + NB], writes=[RhT[k]])
        arena_fence(True)
        pb, Rpb = nextbank()
        ple.emit(blk, hT, RhT, hn, Rhn, pb, Rpb)
        pb, Rpb = nextbank()
        P.fm_norm(hT, RhT, g_kv, Rg_kv, hkv, Rhkv, pb, Rpb)
        P.fm_norm(hT, RhT, g_mix, Rg_mix, hn, Rhn, pb, Rpb, reuse_rstd=True)
        kv_path(blk, 0)
        arena_fence(False)
        for _ in q_path(blk, 0):
            pass
        for s in range(4):
            it = blk * 4 + s
            nsteps = 4 * (-(-(it + 1) // G))
            side = side_gen(blk, s + 1) if s < 3 else None
            drive(attention(blk, s), side, (nsteps, 120))
        for d in range(8):
            pb, Rpb = nextbank()
            for pr in range(8):
                S.op("tensor", lambda e, pr=pr, d=d, pb=pb: e.matmul(pb[:], lhsT=wo[:, pr, d * 128:(d + 1) * 128], rhs=OTb[:, pr, :], start=(pr == 0), stop=(pr == 7)),
                     reads=[Rwo[pr]] + ROTb, writes=[Rpb], join=(pr > 0))
            S.op("vector", lambda e, d=d, pb=pb: e.tensor_tensor(out=hT[:, d, :], in0=pb[:], in1=hT[:, d, :], op=ALU.add), reads=[Rpb, RhT[d]], writes=[RhT[d]])
            S.dma("sync", hout[d, :, t0:t0 + NB], hT[:, d, :], reads=[RhT[d]], writes=[P.Rdram], join=True)
    P.S = realS
    P.finish()


WSHAPES = {
    "norm_mix0": [1024], "norm_mix1": [1024], "a_w_in": [1024, 3088], "a_b_gate": [16], "a_mh_gain": [128], "a_w_out": [1024, 1024],
    "kv_norm": [1024], "w_kv": [1024, 512], "k_norm": [64], "b_w_q": [1024, 1024], "b_q_norm": [64], "b_w_o": [1024, 1024],
    "norm_ffn0": [1024], "norm_ffn1": [1024], "w_gate_up0": [1024, 5632], "w_gate_up1": [1024, 5632], "w_down0": [2816, 1024], "w_down1": [2816, 1024],
    "norm_ple0": [1024], "norm_ple1": [1024], "w_ple_gate0": [1024, 1024], "w_ple_gate1": [1024, 1024], "w_ple_up0": [256, 1024], "w_ple_up1": [256, 1024],
    "p0": [NT, 256], "p1": [NT, 256],
}


def build_program():
    nc = bass.Bass("TRN2", target_bir_lowering=False)
    Cn = make_consts()
    C = {k: nc.dram_tensor(k, list(v.shape), F32, kind="ExternalInput").ap() for k, v in Cn.items()}
    x = nc.dram_tensor("x", [NT, 1024], F32, kind="ExternalInput").ap()
    out = nc.dram_tensor("out", [NT, 1024], F32, kind="ExternalOutput").ap()
    W = {k: nc.dram_tensor(k, v, F32, kind="ExternalInput").ap() for k, v in WSHAPES.items()}
    hs = [nc.dram_tensor("h_scr%d" % i, [8, 128, NT], F32, kind="Internal").ap() for i in range(4)]
    phase_mlstm(nc, C, x, hs[0], W, "ml")
    phase_ffn(nc, C, hs[0], hs[1], W["w_gate_up0"], W["w_down0"], W["norm_ffn0"], "f0")
    phase_moba(nc, C, hs[1], hs[2], W, "mb")
    phase_ffn(nc, C, hs[2], hs[3], W["w_gate_up1"], W["w_down1"], W["norm_ffn1"], "f1")
    phase_ple_out(nc, C, hs[3], out, W["p1"], W["norm_ple1"], W["w_ple_gate1"], W["w_ple_up1"], "po")
    return nc, Cn


def make_in_maps(inputs, cores):
    f = lambda a: np.ascontiguousarray(np.asarray(a, dtype=np.float32))
    I = {k: np.asarray(v) for k, v in inputs.items()}
    shared = {
        "norm_mix0": f(I["norm_mix"][0]), "norm_mix1": f(I["norm_mix"][1]), "a_w_in": f(I["a_w_in"][0]), "a_b_gate": f(I["a_b_gate"][0]),
        "a_mh_gain": f(I["a_mh_gain"][0]), "a_w_out": f(I["a_w_out"][0]), "kv_norm": f(I["kv_norm"]), "w_kv": f(I["w_kv"]), "k_norm": f(I["k_norm"]),
        "b_w_q": f(I["b_w_q"][0]), "b_q_norm": f(I["b_q_norm"][0]), "b_w_o": f(I["b_w_o"][0]),
        "norm_ffn0": f(I["norm_ffn"][0]), "norm_ffn1": f(I["norm_ffn"][1]), "w_gate_up0": f(I["w_gate_up"][0]), "w_gate_up1": f(I["w_gate_up"][1]),
        "w_down0": f(I["w_down"][0]), "w_down1": f(I["w_down"][1]), "norm_ple0": f(I["norm_ple"][0]), "norm_ple1": f(I["norm_ple"][1]),
        "w_ple_gate0": f(I["w_ple_gate"][0]), "w_ple_gate1": f(I["w_ple_gate"][1]), "w_ple_up0": f(I["w_ple_up"][0]), "w_ple_up1": f(I["w_ple_up"][1]),
    }
    maps = []
    for b in cores:
        m = dict(shared)
        m["x"] = f(I["x"][b])
        m["p0"] = f(I["p"][0, b])
        m["p1"] = f(I["p"][1, b])
        maps.append(m)
    return maps


def kernel(**inputs):
    nc, Cn = build_program()
    maps = make_in_maps(inputs, list(range(8)))
    for m in maps:
        m.update(Cn)
    res = run_bass_kernel_spmd(nc, maps, core_ids=list(range(8)))
    return np.stack([np.asarray(r["out"], dtype=np.float32) for r in res.results], axis=0)
```
